# Optimizing a Trainium2 kernel written in Bass

```python
import jax, jax.numpy as jnp
from jax import lax
import numpy as np

D_MODEL = 1024
BATCH = 8
SEQ = 4096
DEPTH = 1

A_HEADS = 8
A_HEAD_DIM = 64
A_W = A_HEADS * A_HEAD_DIM
IDX_HEADS = 8
IDX_DIM = 64
IDX_TOPK_MAX = 256
M_HEADS = 8
M_Q_RANK = 384
M_KV_RANK = 256
M_NOPE = 64
M_ROPE = 32
M_QK = M_NOPE + M_ROPE
M_V = 64
M_W = M_HEADS * M_V
P_HEADS = 8
P_NKEYS = 128
P_NEXP = P_NKEYS * P_NKEYS
P_DKEY = 256
P_TOPK = 16

ROPE_THETA = 10000.0
EPS = 1e-6
Q_BLOCK = 128
TOK_BLOCK = 128

IN_SPLITS = (A_W, A_W, A_W,
             IDX_HEADS * IDX_DIM, IDX_DIM, IDX_HEADS,
             M_Q_RANK, M_KV_RANK, M_ROPE,
             D_MODEL, D_MODEL)
IN_WIDTH = sum(IN_SPLITS)

kernel_name = "hybrid_dsa_mla_peer_adaln"


def rms_norm(x, g):
    xf = x.astype(jnp.float32)
    y = xf * lax.rsqrt(jnp.mean(xf * xf, axis=-1, keepdims=True) + EPS)
    return (y * g.astype(jnp.float32)).astype(x.dtype)


def rope(x, pos):
    d = x.shape[-1]
    half = d // 2
    inv = ROPE_THETA ** (-jnp.arange(half, dtype=jnp.float32) / half)
    ang = pos.astype(jnp.float32)[..., None] * inv
    ang = ang.reshape(ang.shape[:2] + (1,) * (x.ndim - 3) + (half,))
    cos, sin = jnp.cos(ang), jnp.sin(ang)
    xf = x.astype(jnp.float32)
    x1, x2 = xf[..., :half], xf[..., half:]
    return jnp.concatenate([x1 * cos - x2 * sin, x2 * cos + x1 * sin], axis=-1).astype(x.dtype)


def to_blocks(a, nb):
    return a.reshape((a.shape[0], nb, Q_BLOCK) + a.shape[2:]).swapaxes(0, 1)


def dsa_sparse_attention(q, k, v, qi, ki, wi):
    B, S, H, Dh = q.shape
    n_sel = min(IDX_TOPK_MAX, S // 4)
    nb = S // Q_BLOCK
    key_pos = jnp.arange(S)
    scale = Dh ** -0.5

    def block(args):
        qb, qib, wib, start = args
        t = start + jnp.arange(Q_BLOCK)
        causal = key_pos[None, :] <= t[:, None]
        dots = jnp.einsum('bqhd,bsd->bqhs', qib, ki, preferred_element_type=jnp.float32)
        score = jnp.einsum('bqhs,bqh->bqs', jax.nn.relu(dots), wib.astype(jnp.float32))
        score = jnp.where(causal[None], score, -jnp.inf)
        _, idx = lax.top_k(score, n_sel)
        valid = idx <= t[None, :, None]
        k_sel = jax.vmap(lambda kb, ib: kb[ib])(k, idx)
        v_sel = jax.vmap(lambda vb, ib: vb[ib])(v, idx)
        logits = jnp.einsum('bqhd,bqnhd->bhqn', qb, k_sel, preferred_element_type=jnp.float32) * scale
        logits = jnp.where(valid[:, None], logits, -jnp.inf)
        p = jax.nn.softmax(logits, axis=-1)
        return jnp.einsum('bhqn,bqnhd->bqhd', p.astype(v.dtype), v_sel)

    starts = jnp.arange(nb) * Q_BLOCK
    out = lax.map(block, (to_blocks(q, nb), to_blocks(qi, nb), to_blocks(wi, nb), starts))
    return out.swapaxes(0, 1).reshape(B, S, H, Dh)


def causal_block_attention(q, k, v):
    B, S, H, Dq = q.shape
    nb = S // Q_BLOCK
    key_pos = jnp.arange(S)
    scale = Dq ** -0.5

    def block(args):
        qb, start = args
        t = start + jnp.arange(Q_BLOCK)
        logits = jnp.einsum('bqhd,bshd->bhqs', qb, k, preferred_element_type=jnp.float32) * scale
        logits = jnp.where((key_pos[None, :] <= t[:, None])[None, None], logits, -jnp.inf)
        p = jax.nn.softmax(logits, axis=-1)
        return jnp.einsum('bhqs,bshd->bqhd', p.astype(v.dtype), v)

    starts = jnp.arange(nb) * Q_BLOCK
    out = lax.map(block, (to_blocks(q, nb), starts))
    return out.swapaxes(0, 1).reshape(B, S, H, v.shape[-1])


def peer(h, w_q, subkeys, u, v):
    B, S, D = h.shape
    q = (h @ w_q).reshape(B, S, P_HEADS, 2, P_DKEY // 2)
    s = jnp.einsum('bshpd,hpnd->bshpn', q, subkeys, preferred_element_type=jnp.float32)
    top_s, top_i = lax.top_k(s, P_TOPK)
    cand = (top_s[..., 0, :, None] + top_s[..., 1, None, :]).reshape(B, S, P_HEADS, P_TOPK * P_TOPK)
    cand_idx = (top_i[..., 0, :, None] * P_NKEYS + top_i[..., 1, None, :]).reshape(B, S, P_HEADS, P_TOPK * P_TOPK)
    best_s, best_pos = lax.top_k(cand, P_TOPK)
    expert = jnp.take_along_axis(cand_idx, best_pos, axis=-1)
    gate = jax.nn.softmax(best_s, axis=-1)
    T = B * S
    nb = T // TOK_BLOCK
    hb = h.reshape(nb, TOK_BLOCK, D)
    eb = expert.reshape(nb, TOK_BLOCK, P_HEADS * P_TOPK)
    gb = gate.reshape(nb, TOK_BLOCK, P_HEADS * P_TOPK)

    def block(args):
        ht, et, gt = args
        act = jax.nn.gelu(jnp.einsum('td,ted->te', ht, u[et], preferred_element_type=jnp.float32))
        coef = (gt * act).astype(v.dtype)
        return jnp.einsum('te,ted->td', coef, v[et])

    return lax.map(block, (hb, eb, gb)).reshape(B, S, D)


def hybrid_layer(x, c_act, pos, g_norm1, g_norm2, w_ada, b_ada, w_in, g_a_q, g_a_k, g_idx_k,
                 g_mq_a, w_mq_up, g_mkv_a, w_mkv_up, g_m_q, g_m_k, w_o_a, w_o_m, w_out,
                 w_peer_q, peer_subkeys, peer_u, peer_v):
    B, S, D = x.shape
    mod = (c_act @ w_ada + b_ada)[:, None, :]
    sh1, sc1, gt1, sh2, sc2, gt2 = jnp.split(mod, 6, axis=-1)

    h = rms_norm(x, g_norm1) * (1 + sc1) + sh1
    offs = [int(o) for o in np.cumsum(IN_SPLITS)[:-1]]
    qa, ka, va, qi, ki, wi, cq, ckv, kpe, ga, gm = jnp.split(h @ w_in, offs, axis=-1)

    qa = rope(rms_norm(qa.reshape(B, S, A_HEADS, A_HEAD_DIM), g_a_q), pos)
    ka = rope(rms_norm(ka.reshape(B, S, A_HEADS, A_HEAD_DIM), g_a_k), pos)
    va = va.reshape(B, S, A_HEADS, A_HEAD_DIM)
    qi = rope(qi.reshape(B, S, IDX_HEADS, IDX_DIM), pos)
    ki = rope(rms_norm(ki, g_idx_k), pos)
    wi = wi * (IDX_HEADS * IDX_DIM) ** -0.5
    ya = dsa_sparse_attention(qa, ka, va, qi, ki, wi).reshape(B, S, A_W) @ w_o_a

    qm = (rms_norm(cq, g_mq_a) @ w_mq_up).reshape(B, S, M_HEADS, M_QK)
    kv = (rms_norm(ckv, g_mkv_a) @ w_mkv_up).reshape(B, S, M_HEADS, M_NOPE + M_V)
    k_nope, vm = kv[..., :M_NOPE], kv[..., M_NOPE:]
    km = jnp.concatenate([k_nope, jnp.broadcast_to(kpe[:, :, None, :], (B, S, M_HEADS, M_ROPE))], axis=-1)
    qm = rms_norm(qm, g_m_q)
    km = rms_norm(km, g_m_k)
    qm = jnp.concatenate([qm[..., :M_NOPE], rope(qm[..., M_NOPE:], pos)], axis=-1)
    km = jnp.concatenate([km[..., :M_NOPE], rope(km[..., M_NOPE:], pos)], axis=-1)
    ym = causal_block_attention(qm, km, vm).reshape(B, S, M_W) @ w_o_m

    mixed = jax.nn.sigmoid(ga) * ya + jax.nn.sigmoid(gm) * ym
    x = x + gt1 * (mixed @ w_out)

    h2 = rms_norm(x, g_norm2) * (1 + sc2) + sh2
    x = x + gt2 * peer(h2, w_peer_q, peer_subkeys, peer_u, peer_v)
    return x


def setup_inputs(seed: int = 0) -> dict:
    key = jax.random.key(seed)
    ks = jax.random.split(key, 24)

    def nrm(k, shape, scale):
        return jax.random.normal(k, (DEPTH,) + shape, jnp.float32) * scale

    def gain(k, n):
        return 1.0 + 0.02 * jax.random.normal(k, (DEPTH, n), jnp.float32)

    x = jax.random.normal(ks[0], (BATCH, SEQ, D_MODEL), jnp.float32)
    c = jax.random.normal(ks[1], (BATCH, D_MODEL), jnp.float32)
    positions = jnp.broadcast_to(jnp.arange(SEQ, dtype=jnp.int32)[None, :], (BATCH, SEQ))
    return {
        "x": x,
        "c": c,
        "positions": positions,
        "g_norm1": gain(ks[2], D_MODEL),
        "g_norm2": gain(ks[3], D_MODEL),
        "w_ada": nrm(ks[4], (D_MODEL, 6 * D_MODEL), 0.5 * D_MODEL ** -0.5),
        "b_ada": nrm(ks[5], (6 * D_MODEL,), 0.02),
        "w_in": nrm(ks[6], (D_MODEL, IN_WIDTH), D_MODEL ** -0.5),
        "g_a_q": gain(ks[7], A_HEAD_DIM),
        "g_a_k": gain(ks[8], A_HEAD_DIM),
        "g_idx_k": gain(ks[9], IDX_DIM),
        "g_mq_a": gain(ks[10], M_Q_RANK),
        "w_mq_up": nrm(ks[11], (M_Q_RANK, M_HEADS * M_QK), M_Q_RANK ** -0.5),
        "g_mkv_a": gain(ks[12], M_KV_RANK),
        "w_mkv_up": nrm(ks[13], (M_KV_RANK, M_HEADS * (M_NOPE + M_V)), M_KV_RANK ** -0.5),
        "g_m_q": gain(ks[14], M_QK),
        "g_m_k": gain(ks[15], M_QK),
        "w_o_a": nrm(ks[16], (A_W, D_MODEL), A_W ** -0.5),
        "w_o_m": nrm(ks[17], (M_W, D_MODEL), M_W ** -0.5),
        "w_out": nrm(ks[18], (D_MODEL, D_MODEL), D_MODEL ** -0.5),
        "w_peer_q": nrm(ks[19], (D_MODEL, P_HEADS * P_DKEY), D_MODEL ** -0.5),
        "peer_subkeys": nrm(ks[20], (P_HEADS, 2, P_NKEYS, P_DKEY // 2), (P_DKEY // 2) ** -0.5),
        "peer_u": nrm(ks[21], (P_NEXP, D_MODEL), D_MODEL ** -0.5),
        "peer_v": nrm(ks[22], (P_NEXP, D_MODEL), P_HEADS ** -0.5),
    }


def reference(x, c, positions, g_norm1, g_norm2, w_ada, b_ada, w_in, g_a_q, g_a_k, g_idx_k,
              g_mq_a, w_mq_up, g_mkv_a, w_mkv_up, g_m_q, g_m_k, w_o_a, w_o_m, w_out,
              w_peer_q, peer_subkeys, peer_u, peer_v):
    c_act = jax.nn.silu(c)
    for i in range(DEPTH):
        x = hybrid_layer(x, c_act, positions, g_norm1[i], g_norm2[i], w_ada[i], b_ada[i], w_in[i],
                         g_a_q[i], g_a_k[i], g_idx_k[i], g_mq_a[i], w_mq_up[i], g_mkv_a[i],
                         w_mkv_up[i], g_m_q[i], g_m_k[i], w_o_a[i], w_o_m[i], w_out[i],
                         w_peer_q[i], peer_subkeys[i], peer_u[i], peer_v[i])
    return x
```

```python
import contextlib
import numpy as np
import ml_dtypes
import concourse.bass as bass
import concourse.mybir as mybir
from concourse.bass_utils import run_bass_kernel_spmd

F32 = mybir.dt.float32
BF16 = mybir.dt.bfloat16
I32 = mybir.dt.int32
U32 = mybir.dt.uint32
AF = mybir.ActivationFunctionType
ALU = mybir.AluOpType
AX = mybir.AxisListType

S = 4096
D = 1024
NT = 32
NG = 8
EPS = 1e-6
NEG = -30000.0
NITER = 18
NSLOT = 4
TWO_PI = float(2 * np.pi)
NO_SELF_WAIT = False


class Prog:
    ENG = ('pe', 'act', 'dve', 'pool', 'sp')

    def __init__(self):
        self.ops = {e: [] for e in self.ENG}
        self.cnt = {e: 0 for e in self.ENG}
        self.known = {e: {} for e in self.ENG}
        self.res = {}
        self.dma_cnt = {}

    def _deps(self, eng, reads, writes):
        need = {}

        def add(sk, v):
            if sk == ('eng', 'pe') and eng == 'pe':
                return
            if NO_SELF_WAIT and sk == ('eng', eng):
                return
            if v > need.get(sk, 0):
                need[sk] = v
        for k in reads:
            st = self.res.get(k)
            if st and st[0] is not None:
                add(*st[0])
        for k in writes:
            st = self.res.get(k)
            if st:
                if st[0] is not None:
                    add(*st[0])
                for sk, v in st[1].items():
                    add(sk, v)
        waits = []
        kn = self.known[eng]
        for sk, v in need.items():
            if kn.get(sk, 0) >= v:
                continue
            kn[sk] = v
            waits.append((sk, v))
        return waits

    def _commit(self, tok, reads, writes):
        sk, v = tok
        for k in reads:
            st = self.res.setdefault(k, [None, {}])
            if v > st[1].get(sk, 0):
                st[1][sk] = v
        for k in writes:
            self.res[k] = [tok, {}]

    def op(self, eng, fn, r=(), w=()):
        waits = self._deps(eng, r, w)
        self.cnt[eng] += 1
        tok = (('eng', eng), self.cnt[eng])
        self.ops[eng].append((waits, fn, ('eng', eng), 1))
        self._commit(tok, r, w)

    def dma(self, eng, fn, r=(), w=(), sem=None):
        waits = self._deps(eng, r, w)
        c = self.dma_cnt.get(sem, 0) + 16
        self.dma_cnt[sem] = c
        tok = (('dma', sem), c)
        self.ops[eng].append((waits, fn, ('dma', sem), 16))
        self._commit(tok, r, w)

    def barrier(self):
        allk = [(('eng', e), self.cnt[e]) for e in self.ENG if self.cnt[e] > 0]
        allk += [(('dma', k), c) for k, c in self.dma_cnt.items()]
        for e in self.ENG:
            waits = []
            kn = self.known[e]
            for sk, v in allk:
                if sk == ('eng', e):
                    continue
                if kn.get(sk, 0) >= v:
                    continue
                kn[sk] = v
                waits.append((sk, v))
            if waits:
                self.ops[e].append((waits, None, None, 0))

    def emit(self, nc):
        self.barrier()
        with contextlib.ExitStack() as st:
            sems = {}
            for e in self.ENG:
                sems[('eng', e)] = st.enter_context(nc.semaphore("s_" + e))
            for i, k in enumerate(self.dma_cnt):
                sems[('dma', k)] = st.enter_context(nc.semaphore("d%d" % i))
            block = st.enter_context(nc.Block())

            def run(e, name):
                for (waits, fn, sk, inc) in self.ops[name]:
                    for (wk, v) in waits:
                        e.wait_ge(sems[wk], v)
                    if fn is not None:
                        fn(e).then_inc(sems[sk], inc)

            @block.tensor
            def _(e):
                run(e, 'pe')

            @block.scalar
            def _(e):
                run(e, 'act')

            @block.vector
            def _(e):
                run(e, 'dve')

            @block.gpsimd
            def _(e):
                run(e, 'pool')

            @block.sync
            def _(e):
                run(e, 'sp')


def build_nc(debug=False, phases=4, nt1=NT, skip=(), nslot=NSLOT, gcols=D):
    nc = bass.Bass("TRN2", target_bir_lowering=False)
    P = Prog()

    def din(name, shape, dt=F32):
        return nc.dram_tensor(name, list(shape), dt, kind="ExternalInput").ap()

    def dscr(name, shape, dt=BF16):
        return nc.dram_tensor(name, list(shape), dt, kind="ExternalOutput" if debug else "Internal").ap()

    x_d = din("x", [S, D])
    ccol_d = din("c_col", [128, 8])
    pos_d = din("pos_t", [128, NT], I32)
    g1_d = din("g_norm1", [1, D]); g2_d = din("g_norm2", [1, D])
    wada_d = din("w_ada", [D, 6 * D]); bada_d = din("b_ada", [1, 6 * D])
    win_d = din("w_in", [D, 4840])
    gaq_d = din("g_a_q", [1, 64]); gak_d = din("g_a_k", [1, 64]); gik_d = din("g_idx_k", [1, 64])
    gmqa_d = din("g_mq_a", [1, 384]); wmq_d = din("w_mq_up", [384, 768])
    gmkva_d = din("g_mkv_a", [1, 256]); wmkv_d = din("w_mkv_up", [256, 1024])
    gmq_d = din("g_m_q", [1, 96]); gmk_d = din("g_m_k", [1, 96])
    woa_d = din("w_o_a", [512, D]); wom_d = din("w_o_m", [512, D]); wout_d = din("w_out", [D, D])
    wpq_d = din("w_peer_q", [D, 2048]); subk_d = din("peer_subkeys", [16, 128, 128])
    pu_d = din("peer_u", [16384, D]); pv_d = din("peer_v", [16384, D])
    identb_d = din("identb", [128, 128], BF16)
    trib_d = din("trib", [128, 128], BF16)
    inv64_d = din("inv64", [128, 32]); inv32_d = din("inv32", [128, 16])
    iota16_d = din("iota16", [128, 16])
    out_d = nc.dram_tensor("out", [S, D], F32, kind="ExternalOutput").ap()

    qaT_d = dscr("qaT_s", [4, 128, S]); kaT_d = dscr("kaT_s", [4, 128, S]); qiT_d = dscr("qiT_s", [4, 128, S])
    kiT_d = dscr("kiT_s", [128, S])
    va_d = dscr("va_s", [S, 520]); vm_d = dscr("vm_s", [S, 520])
    qmT_d = dscr("qmT_s", [8, 96, S]); kmT_d = dscr("kmT_s", [8, 96, S])
    gS_d = dscr("gS_s", [16, 128, S])
    atA_d = dscr("atA_s", [8, 64, S]); atM_d = dscr("atM_s", [8, 64, S])
    mod_d = dscr("mod_s", [4, 128, D], F32)
    uv_d = nc.dram_tensor("uv_s", [16384, 2048], BF16, kind="Internal").ap()

    top = contextlib.ExitStack()

    uid = [0]

    def sbt(st, name, shape, dt=F32):
        uid[0] += 1
        return st.enter_context(nc.sbuf_tensor("sb%d_%s" % (uid[0], name), list(shape), dt))

    def pst(st, name, shape, dt=F32):
        uid[0] += 1
        return st.enter_context(nc.psum_tensor("ps%d_%s" % (uid[0], name), list(shape), dt))

    def tt(out, in0, in1, op, r, w, eng='dve'):
        P.op(eng, lambda e: e.tensor_tensor(out=out, in0=in0, in1=in1, op=op), r, w)

    def ts(out, in0, s1, s2, op0, op1, r, w, eng='dve', accum_out=None):
        if op1 is None:
            P.op(eng, lambda e: e.tensor_scalar(out=out, in0=in0, scalar1=s1, scalar2=None, op0=op0), r, w)
        elif accum_out is None:
            P.op(eng, lambda e: e.tensor_scalar(out=out, in0=in0, scalar1=s1, scalar2=s2, op0=op0, op1=op1), r, w)
        else:
            P.op(eng, lambda e: e.tensor_scalar(out=out, in0=in0, scalar1=s1, scalar2=s2, op0=op0, op1=op1, accum_out=accum_out), r, w)

    def stt(out, in0, scalar, in1, op0, op1, r, w, accum_out=None):
        if accum_out is None:
            P.op('dve', lambda e: e.scalar_tensor_tensor(out=out, in0=in0, scalar=scalar, in1=in1, op0=op0, op1=op1), r, w)
        else:
            P.op('dve', lambda e: e.scalar_tensor_tensor(out=out, in0=in0, scalar=scalar, in1=in1, op0=op0, op1=op1, accum_out=accum_out), r, w)

    def act(out, in_, func, r, w, scale=None, bias=None, accum_out=None):
        kw = {}
        if scale is not None:
            kw['scale'] = scale
        if bias is not None:
            kw['bias'] = bias
        if accum_out is not None:
            kw['accum_out'] = accum_out
        P.op('act', lambda e: e.activation(out=out, in_=in_, func=func, **kw), r, w)

    def cp(out, in_, r, w, eng='dve'):
        if eng == 'act':
            act(out, in_, AF.Copy, r, w)
        else:
            P.op(eng, lambda e: e.tensor_copy(out=out, in_=in_), r, w)

    def red(out, in_, op, r, w):
        P.op('dve', lambda e: e.tensor_reduce(out=out, in_=in_, axis=AX.X, op=op), r, w)

    def rcp(out, in_, r, w):
        P.op('dve', lambda e: e.reciprocal(out=out, in_=in_), r, w)

    def mm(out, lhsT, rhs, start, stop, r, w):
        P.op('pe', lambda e: e.matmul(out=out, lhsT=lhsT, rhs=rhs, start=start, stop=stop), r, w)

    def tr(out, in_, ident, r, w):
        P.op('pe', lambda e: e.transpose(out=out, in_=in_, identity=ident), r, w)

    def dma(out, in_, r, w, sem, eng='sp'):
        P.dma(eng, lambda e: e.dma_start(out=out, in_=in_), r, w, sem)

    def memset(ap, val, w, eng='dve'):
        P.op(eng, lambda e: e.memset(ap, val), (), w)

    def bc1(ap, shape):
        return ap.unsqueeze(1).to_broadcast(shape)

    def bc2(ap, shape):
        return ap.unsqueeze(2).to_broadcast(shape)

    identb = sbt(top, "identb", [128, 128], BF16)
    trib = sbt(top, "trib", [128, 128], BF16)
    wi_all = sbt(top, "wi_all", [128, NT, 8])
    st01 = contextlib.ExitStack()
    A1 = sbt(st01, "A1", [128, D]); B1 = sbt(st01, "B1", [128, D])
    dma(identb[:], identb_d, [], ['identb'], 'identb')
    dma(trib[:], trib_d, [], ['trib'], 'trib')

    with contextlib.ExitStack() as st:
        ccol = sbt(st, "ccol", [128, 8]); cact = sbt(st, "cact", [128, 8])
        cb = sbt(st, "cb", [128, 8, 128])
        g1b = sbt(st, "g1b", [128, D]); g2b = sbt(st, "g2b", [128, D])
        G1 = sbt(st, "G1", [128, D]); A2 = sbt(st, "A2", [128, D]); B2 = sbt(st, "B2", [128, D]); G2 = sbt(st, "G2", [128, D])
        wa = [sbt(st, "wa%d" % i, [128, 8, 512]) for i in range(2)]
        bb = [sbt(st, "bb%d" % i, [128, 512]) for i in range(2)]
        tmp0 = sbt(st, "tmp0", [128, 512])
        pm0 = [pst(st, "pm0_%d" % i, [128, 512]) for i in range(2)]
        dma(ccol[:], ccol_d, [], ['ccol'], 'ccol')
        dma(g1b[:], g1_d.to_broadcast([128, D]), [], ['g1b'], 'g1b')
        dma(g2b[:], g2_d.to_broadcast([128, D]), [], ['g2b'], 'g2b')
        act(cact[:], ccol[:], AF.Silu, ['ccol'], ['cact'])
        cp(cb[:], cact[:].unsqueeze(2).to_broadcast([128, 8, 128]), ['cact'], ['cb'])
        wada_v = wada_d.rearrange("(k p) n -> p k n", p=128)
        dests = [B1, A1, G1, B2, A2, G2]
        dnames = ['B1', 'A1', 'G1', 'B2', 'A2', 'G2']
        for n in range(12):
            sl = n % 2
            dma(wa[sl][:], wada_v[:, :, n * 512:(n + 1) * 512], [], ['wa%d' % sl], 'wa%d' % sl)
            dma(bb[sl][:], bada_d[:, n * 512:(n + 1) * 512].to_broadcast([128, 512]), [], ['bb%d' % sl], 'bb%d' % sl)
            for k in range(8):
                mm(pm0[sl][:], cb[:, k, :], wa[sl][:, k, :], k == 0, k == 7, ['cb', 'wa%d' % sl], ['pm0_%d' % sl])
            which = n // 2
            dst = dests[which][:, (n % 2) * 512:(n % 2 + 1) * 512]
            dn = dnames[which]
            if which in (1, 4):
                gsrc = (g1b if which == 1 else g2b)[:, (n % 2) * 512:(n % 2 + 1) * 512]
                tt(tmp0[:], pm0[sl][:], bb[sl][:], ALU.add, ['pm0_%d' % sl, 'bb%d' % sl], ['tmp0'])
                stt(dst, tmp0[:], 1.0, gsrc, ALU.add, ALU.mult, ['tmp0', 'g1b', 'g2b'], [dn])
            else:
                tt(dst, pm0[sl][:], bb[sl][:], ALU.add, ['pm0_%d' % sl, 'bb%d' % sl], [dn])
        for qi_, (tn_, nm_) in enumerate([(G1, 'G1'), (A2, 'A2'), (B2, 'B2'), (G2, 'G2')]):
            dma(mod_d[qi_], tn_[:], [nm_], ['mod_d'], 'mod' + nm_)
        P.barrier()

    if phases == 0:
        P.emit(nc)
        st01.close()
        top.close()
        return nc

    with contextlib.ExitStack() as st:
        winb = sbt(st, "winb", [128, 8, 4840], BF16)
        wmqb = sbt(st, "wmqb", [128, 3, 768], BF16)
        wmkvb = sbt(st, "wmkvb", [128, 2, 1024], BF16)
        gaq = sbt(st, "gaq", [128, 64]); gak = sbt(st, "gak", [128, 64]); gik = sbt(st, "gik", [128, 64])
        gmqa = sbt(st, "gmqa", [128, 384]); gmkva = sbt(st, "gmkva", [128, 256])
        gmq = sbt(st, "gmq", [128, 96]); gmk = sbt(st, "gmk", [128, 96])
        cos64 = sbt(st, "cos64", [128, NT, 32]); sin64 = sbt(st, "sin64", [128, NT, 32])
        cos32 = sbt(st, "cos32", [128, NT, 16]); sin32 = sbt(st, "sin32", [128, NT, 16])
        for tns, src, nm in [(gaq, gaq_d, 'gaq'), (gak, gak_d, 'gak'), (gik, gik_d, 'gik'), (gmqa, gmqa_d, 'gmqa'),
                             (gmkva, gmkva_d, 'gmkva'), (gmq, gmq_d, 'gmq'), (gmk, gmk_d, 'gmk')]:
            dma(tns[:], src.to_broadcast(list(tns[:].shape)), [], [nm], nm)
        with contextlib.ExitStack() as st2:
            stage = [sbt(st2, "stage%d" % i, [128, 4096]) for i in range(2)]
            win_v = win_d.rearrange("(k p) n -> p k n", p=128)
            ci = 0
            for c0 in range(0, 4840, 512):
                c1 = min(c0 + 512, 4840)
                wdt = c1 - c0
                sl = ci % 2
                sv = stage[sl][:, 0:8 * wdt].rearrange("p (k n) -> p k n", k=8)
                dma(sv, win_v[:, :, c0:c1], [], ['stage%d' % sl], 'stage%d' % sl)
                cp(winb[:, :, c0:c1], sv, ['stage%d' % sl], ['winb'], eng=('pool' if ci % 2 == 0 else 'act'))
                ci += 1
            sv = stage[ci % 2][:, 0:3 * 768].rearrange("p (k n) -> p k n", k=3)
            dma(sv, wmq_d.rearrange("(k p) n -> p k n", p=128), [], ['stage%d' % (ci % 2)], 'stage%d' % (ci % 2))
            cp(wmqb[:], sv, ['stage%d' % (ci % 2)], ['wmqb'], eng='pool')
            ci += 1
            sv = stage[ci % 2][:, 0:2 * 1024].rearrange("p (k n) -> p k n", k=2)
            dma(sv, wmkv_d.rearrange("(k p) n -> p k n", p=128), [], ['stage%d' % (ci % 2)], 'stage%d' % (ci % 2))
            cp(wmkvb[:], sv, ['stage%d' % (ci % 2)], ['wmkvb'], eng='pool')
            posi = sbt(st2, "posi", [128, NT], I32); posf = sbt(st2, "posf", [128, NT])
            inv64 = sbt(st2, "inv64", [128, 32]); inv32 = sbt(st2, "inv32", [128, 16])
            rt_a = sbt(st2, "rt_a", [128, NT, 32]); rt_k = sbt(st2, "rt_k", [128, NT, 32])
            rt_i = sbt(st2, "rt_i", [128, NT, 32], I32); rt_y = sbt(st2, "rt_y", [128, NT, 32])
            dma(posi[:], pos_d, [], ['posi'], 'posi')
            dma(inv64[:], inv64_d, [], ['inv64'], 'inv64')
            dma(inv32[:], inv32_d, [], ['inv32'], 'inv32')
            cp(posf[:], posi[:], ['posi'], ['posf'])
            for (inv, hf, cs, sn, nm) in [(inv64, 32, cos64, sin64, '64'), (inv32, 16, cos32, sin32, '32')]:
                a = rt_a[:, :, 0:hf]; kk = rt_k[:, :, 0:hf]; ii = rt_i[:, :, 0:hf]; y = rt_y[:, :, 0:hf]
                shp = [128, NT, hf]
                tt(a, bc2(posf[:], shp), bc1(inv[:], shp), ALU.mult, ['posf', 'inv' + nm], ['rt_a'])
                ts(kk, a, float(1.0 / TWO_PI), None, ALU.mult, None, ['rt_a'], ['rt_k'])
                cp(ii, kk, ['rt_k'], ['rt_i'])
                cp(kk, ii, ['rt_i'], ['rt_k'])
                stt(a, kk, -TWO_PI, a, ALU.mult, ALU.add, ['rt_k', 'rt_a'], ['rt_a'])
                ts(kk, a, float(np.pi / 2), float(np.pi), ALU.add, ALU.is_gt, ['rt_a'], ['rt_k'])
                stt(y, kk, -TWO_PI, a, ALU.mult, ALU.add, ['rt_k', 'rt_a'], ['rt_y'])
                ts(y, y, float(np.pi / 2), None, ALU.add, None, ['rt_y'], ['rt_y'])
                ts(y, y, float(np.pi), float(-np.pi), ALU.min, ALU.max, ['rt_y'], ['rt_y'])
                ts(a, a, float(np.pi), float(-np.pi), ALU.min, ALU.max, ['rt_a'], ['rt_a'])
                act(sn[:], a, AF.Sin, ['rt_a'], ['sin' + nm])
                act(cs[:], y, AF.Sin, ['rt_y'], ['cos' + nm])
            P.barrier()

        xt = [sbt(st, "xt%d" % i, [128, D]) for i in range(2)]
        junkb = sbt(st, "junkb", [128, D], BF16)
        ssq = sbt(st, "ssq", [128, 1]); rstd = sbt(st, "rstd", [128, 1])
        htmp = sbt(st, "htmp", [128, D]); hb = sbt(st, "hb", [128, D], BF16)
        hT = [sbt(st, "hT%d" % i, [128, 8, 128], BF16) for i in range(2)]
        proj = sbt(st, "proj", [128, 2792])
        sq = sbt(st, "sq", [128, 1024]); nrm = sbt(st, "nrm", [128, 1024])
        s8 = sbt(st, "s8", [128, 8]); r8 = sbt(st, "r8", [128, 8])
        rp = [sbt(st, "rp%d" % i, [128, 8, 32]) for i in range(4)]
        tokb = sbt(st, "tokb", [128, 768], BF16)
        kib = sbt(st, "kib", [128, 128], BF16)
        cqT = sbt(st, "cqT", [128, 3, 128], BF16); ckvT = sbt(st, "ckvT", [128, 2, 128], BF16)
        qmf = sbt(st, "qmf", [128, 768]); kvf = sbt(st, "kvf", [128, 8, 128]); kmpre = sbt(st, "kmpre", [128, 8, 96])
        vaug = sbt(st, "vaug", [128, 8, 65], BF16); vmaug = sbt(st, "vmaug", [128, 8, 65], BF16)
        qaT_t = sbt(st, "qaT_t", [128, 4, 128], BF16); kaT_t = sbt(st, "kaT_t", [128, 4, 128], BF16)
        qiT_t = sbt(st, "qiT_t", [128, 4, 128], BF16); kiT_t = sbt(st, "kiT_t", [128, 128], BF16)
        qmT_t = sbt(st, "qmT_t", [128, 8, 128], BF16); kmT_t = sbt(st, "kmT_t", [128, 8, 128], BF16)
        gS_t = sbt(st, "gS_t", [128, 16, 128], BF16)
        pT = pst(st, "pT", [128, 8, 128], BF16)
        pT2 = pst(st, "pT2", [128, 8, 128], BF16)
        pproj = [pst(st, "pproj%d" % i, [128, 512]) for i in range(2)]
        pgate = pst(st, "pgate", [128, 512])
        pm = pst(st, "pm", [128, 1024])
        memset(vaug[:], 1.0, ['vaug'], eng='pool')
        memset(vmaug[:], 1.0, ['vmaug'], eng='pool')

        def rms_heads(src3, H, Dh, gain, dst3, rs, ws, gname):
            shp = [128, H, Dh]
            sqv = sq[:, 0:H * Dh].rearrange("p (h d) -> p h d", h=H)
            tt(sqv, src3, src3, ALU.mult, rs, ['sq'])
            red(s8[:, 0:H], sqv, ALU.add, ['sq'], ['s8'])
            ts(s8[:, 0:H], s8[:, 0:H], float(1.0 / Dh), float(EPS), ALU.mult, ALU.add, ['s8'], ['s8'])
            act(s8[:, 0:H], s8[:, 0:H], AF.Sqrt, ['s8'], ['s8'])
            rcp(r8[:, 0:H], s8[:, 0:H], ['s8'], ['r8'])
            nv = nrm[:, 0:H * Dh].rearrange("p (h d) -> p h d", h=H)
            tt(nv, src3, bc2(r8[:, 0:H], shp), ALU.mult, rs + ['r8'], ['nrm'])
            tt(dst3, nv, bc1(gain[:], shp), ALU.mult, ['nrm', gname], ws)

        def rope(src3, H, hf, cosv, sinv, dst3, rs, ws, cname):
            shp = [128, H, hf]
            x1 = src3[:, :, 0:hf]; x2 = src3[:, :, hf:2 * hf]
            cb_ = bc1(cosv, shp); sb_ = bc1(sinv, shp)
            t = [rp[i][:, 0:H, 0:hf] for i in range(4)]
            tt(t[0], x1, cb_, ALU.mult, rs + ['cos' + cname], ['rp0'])
            tt(t[1], x2, sb_, ALU.mult, rs + ['sin' + cname], ['rp1'], eng='pool')
            tt(dst3[:, :, 0:hf], t[0], t[1], ALU.subtract, ['rp0', 'rp1'], ws)
            tt(t[2], x2, cb_, ALU.mult, rs + ['cos' + cname], ['rp2'])
            tt(t[3], x1, sb_, ALU.mult, rs + ['sin' + cname], ['rp3'], eng='pool')
            tt(dst3[:, :, hf:2 * hf], t[2], t[3], ALU.add, ['rp2', 'rp3'], ws)

        for i in range(nt1):
            hs = i % 2
            hTn = 'hT%d' % hs
            xs = i % 2
            xn = 'xt%d' % xs
            tok = slice(i * 128, (i + 1) * 128)
            dma(xt[xs][:], x_d[tok, :], [], [xn], xn)
            act(junkb[:], xt[xs][:], AF.Square, [xn], ['junkb', 'ssq'], accum_out=ssq[:])
            ts(ssq[:], ssq[:], float(1.0 / D), float(EPS), ALU.mult, ALU.add, ['ssq'], ['ssq'])
            act(ssq[:], ssq[:], AF.Sqrt, ['ssq'], ['ssq'])
            rcp(rstd[:], ssq[:], ['ssq'], ['rstd'])
            stt(htmp[:], xt[xs][:], rstd[:, 0:1], A1[:], ALU.mult, ALU.mult, [xn, 'rstd', 'A1'], ['htmp'])
            tt(hb[:], htmp[:], B1[:], ALU.add, ['htmp', 'B1'], ['hb'], eng='pool')
            for k in range(8):
                tr(pT[:, k, :], hb[:, k * 128:(k + 1) * 128], identb[:], ['hb', 'identb'], ['pT'])
            cp(hT[hs][:], pT[:], ['pT'], [hTn], eng='act')
            for cc in range(6):
                c0 = cc * 512
                c1 = min(c0 + 512, 2792)
                pp = cc % 2
                for k in range(8):
                    mm(pproj[pp][:, 0:c1 - c0], hT[hs][:, k, :], winb[:, k, c0:c1], k == 0, k == 7,
                       [hTn, 'winb'], ['pproj%d' % pp])
                cp(proj[:, c0:c1], pproj[pp][:, 0:c1 - c0], ['pproj%d' % pp], ['proj'], eng='act')
            for fc in range(16):
                c0 = 2792 + fc * 128
                for k in range(8):
                    mm(pgate[:, (fc % 4) * 128:(fc % 4 + 1) * 128], winb[:, k, c0:c0 + 128], hT[hs][:, k, :], k == 0, k == 7,
                       [hTn, 'winb'], ['pgate'])
                if fc % 4 == 3:
                    act(gS_t[:, fc - 3:fc + 1, :].rearrange("p c t -> p (c t)"), pgate[:], AF.Sigmoid, ['pgate'], ['gS_t'])
            dma(gS_d[:, :, tok].rearrange("c p t -> p c t"), gS_t[:], ['gS_t'], ['gS_d'], 'gS_t')
            cs64 = cos64[:, i, :]; sn64 = sin64[:, i, :]; cs32 = cos32[:, i, :]; sn32 = sin32[:, i, :]
            for (c0, gn, gt_, dstT, dn, dd_) in [(0, 'gaq', gaq, qaT_t, 'qaT_t', qaT_d), (512, 'gak', gak, kaT_t, 'kaT_t', kaT_d)]:
                src3 = proj[:, c0:c0 + 512].rearrange("p (h d) -> p h d", h=8)
                n3 = sq[:, 0:512].rearrange("p (h d) -> p h d", h=8)
                rms_heads(src3, 8, 64, gt_, n3, ['proj'], ['sq'], gn)
                rope(n3, 8, 32, cs64, sn64, tokb[:, 0:512].rearrange("p (h d) -> p h d", h=8), ['sq'], ['tokb'], '64')
                for k in range(4):
                    tr(pT2[:, k, :], tokb[:, k * 128:(k + 1) * 128], identb[:], ['tokb', 'identb'], ['pT2'])
                cp(dstT[:], pT2[:, 0:4, :], ['pT2'], [dn])
                dma(dd_[:, :, tok].rearrange("c p t -> p c t"), dstT[:], [dn], [dn + '_d'], dn)
            rope(proj[:, 1536:2048].rearrange("p (h d) -> p h d", h=8), 8, 32, cs64, sn64,
                 tokb[:, 0:512].rearrange("p (h d) -> p h d", h=8), ['proj'], ['tokb'], '64')
            for k in range(4):
                tr(pT2[:, k, :], tokb[:, k * 128:(k + 1) * 128], identb[:], ['tokb', 'identb'], ['pT2'])
            cp(qiT_t[:], pT2[:, 0:4, :], ['pT2'], ['qiT_t'])
            dma(qiT_d[:, :, tok].rearrange("c p t -> p c t"), qiT_t[:], ['qiT_t'], ['qiT_t_d'], 'qiT_t')
            n3 = sq[:, 0:64].rearrange("p (h d) -> p h d", h=1)
            rms_heads(proj[:, 2048:2112].rearrange("p (h d) -> p h d", h=1), 1, 64, gik, n3, ['proj'], ['sq'], 'gik')
            rope(n3, 1, 32, cs64, sn64, kib[:, 0:64].rearrange("p (h d) -> p h d", h=1), ['sq'], ['kib'], '64')
            cp(kib[:, 64:128], kib[:, 0:64], ['kib'], ['kib'])
            tr(pT2[:, 0, :], kib[:], identb[:], ['kib', 'identb'], ['pT2'])
            cp(kiT_t[:], pT2[:, 0, :], ['pT2'], ['kiT_t'])
            dma(kiT_d[:, tok], kiT_t[:], ['kiT_t'], ['kiT_t_d'], 'kiT_t')
            ts(wi_all[:, i, :], proj[:, 2112:2120], float(512 ** -0.5), None, ALU.mult, None, ['proj'], ['wi_all'])
            cp(vaug[:, :, 0:64], proj[:, 1024:1536].rearrange("p (h d) -> p h d", h=8), ['proj'], ['vaug'], eng='pool')
            dma(va_d[tok, :], vaug[:].rearrange("p h d -> p (h d)"), ['vaug'], ['va_d'], 'vaug')
            rms_heads(proj[:, 2120:2504].rearrange("p (h d) -> p h d", h=1), 1, 384, gmqa,
                      tokb[:, 0:384].rearrange("p (h d) -> p h d", h=1), ['proj'], ['tokb'], 'gmqa')
            for k in range(3):
                tr(pT2[:, k, :], tokb[:, k * 128:(k + 1) * 128], identb[:], ['tokb', 'identb'], ['pT2'])
            cp(cqT[:], pT2[:, 0:3, :], ['pT2'], ['cqT'])
            for (c0, c1) in [(0, 512), (512, 768)]:
                for k in range(3):
                    mm(pm[:, c0:c1], cqT[:, k, :], wmqb[:, k, c0:c1], k == 0, k == 2, ['cqT', 'wmqb'], ['pm'])
            cp(qmf[:], pm[:, 0:768], ['pm'], ['qmf'], eng='act')
            q3 = qmf[:].rearrange("p (h d) -> p h d", h=8)
            n96 = sq[:, 0:768].rearrange("p (h d) -> p h d", h=8)
            rms_heads(q3, 8, 96, gmq, n96, ['qmf'], ['sq'], 'gmq')
            tb96 = tokb[:, 0:768].rearrange("p (h d) -> p h d", h=8)
            cp(tb96[:, :, 0:64], n96[:, :, 0:64], ['sq'], ['tokb'], eng='pool')
            rope(n96[:, :, 64:96], 8, 16, cs32, sn32, tb96[:, :, 64:96], ['sq'], ['tokb'], '32')
            for h in range(8):
                tr(pT2[0:96, h, :], tokb[:, h * 96:(h + 1) * 96], identb[:], ['tokb', 'identb'], ['pT2'])
            cp(qmT_t[0:96, :, :], pT2[0:96, :, :], ['pT2'], ['qmT_t'])
            dma(qmT_d[:, :, tok].rearrange("h d t -> d h t"), qmT_t[0:96, :, :], ['qmT_t'], ['qmT_t_d'], 'qmT_t')
            rms_heads(proj[:, 2504:2760].rearrange("p (h d) -> p h d", h=1), 1, 256, gmkva,
                      tokb[:, 0:256].rearrange("p (h d) -> p h d", h=1), ['proj'], ['tokb'], 'gmkva')
            for k in range(2):
                tr(pT2[:, k, :], tokb[:, k * 128:(k + 1) * 128], identb[:], ['tokb', 'identb'], ['pT2'])
            cp(ckvT[:], pT2[:, 0:2, :], ['pT2'], ['ckvT'])
            for (c0, c1) in [(0, 512), (512, 1024)]:
                for k in range(2):
                    mm(pm[:, c0:c1], ckvT[:, k, :], wmkvb[:, k, c0:c1], k == 0, k == 1, ['ckvT', 'wmkvb'], ['pm'])
            cp(kvf[:].rearrange("p h d -> p (h d)"), pm[:], ['pm'], ['kvf'], eng='act')
            cp(kmpre[:, :, 0:64], kvf[:, :, 0:64], ['kvf'], ['kmpre'], eng='pool')
            cp(kmpre[:, :, 64:96], bc1(proj[:, 2760:2792], [128, 8, 32]), ['proj'], ['kmpre'], eng='pool')
            cp(vmaug[:, :, 0:64], kvf[:, :, 64:128], ['kvf'], ['vmaug'], eng='pool')
            dma(vm_d[tok, :], vmaug[:].rearrange("p h d -> p (h d)"), ['vmaug'], ['vm_d'], 'vmaug')
            rms_heads(kmpre[:], 8, 96, gmk, n96, ['kmpre'], ['sq'], 'gmk')
            cp(tb96[:, :, 0:64], n96[:, :, 0:64], ['sq'], ['tokb'], eng='pool')
            rope(n96[:, :, 64:96], 8, 16, cs32, sn32, tb96[:, :, 64:96], ['sq'], ['tokb'], '32')
            for h in range(8):
                tr(pT2[0:96, h, :], tokb[:, h * 96:(h + 1) * 96], identb[:], ['tokb', 'identb'], ['pT2'])
            cp(kmT_t[0:96, :, :], pT2[0:96, :, :], ['pT2'], ['kmT_t'])
            dma(kmT_d[:, :, tok].rearrange("h d t -> d h t"), kmT_t[0:96, :, :], ['kmT_t'], ['kmT_t_d'], 'kmT_t')
        P.barrier()
    st01.close()

    def attention_phase(tag, dsa):
        with contextlib.ExitStack() as st:
            ones64 = sbt(st, "ones64" + tag, [128, 64])
            memset(ones64[:], 1.0, ['ones64'])
            if dsa:
                kT = sbt(st, "kaT", [128, 4, S], BF16)
                kiT = sbt(st, "kiT", [128, S], BF16)
                for c in range(4):
                    dma(kT[:, c, :], kaT_d[c], ['kaT_t_d'], ['kT%d' % c], 'kT%d' % c)
                dma(kiT[:], kiT_d, ['kiT_t_d'], ['kiT'], 'kiT')
                v_src = va_d
                qT_g = sbt(st, "qaTg", [128, 4, 512], BF16)
                qiTg = [sbt(st, "qiTg%d" % i, [128, 4, 512], BF16) for i in range(2)]
                score = sbt(st, "score", [128, S])
                junk = sbt(st, "junkc", [128, S], BF16)
                bias_g = [sbt(st, "bias_g%d" % i, [128, 4, S], BF16) for i in range(2)]
                dg = sbt(st, "dg", [128, 8, 128], BF16)
                rbuf = [sbt(st, "rbuf%d" % i, [128, 512], BF16) for i in range(2)]
                lo = sbt(st, "lo", [128, 1]); hi = sbt(st, "hi", [128, 1]); mid = sbt(st, "mid", [128, 1])
                wv = sbt(st, "wv", [128, NITER + 1]); cnt = sbt(st, "cnt", [128, 1]); stp = sbt(st, "stp", [128, 1])
                pd = [pst(st, "pd%d" % i, [128, 512]) for i in range(2)]
                psc = pst(st, "psc", [128, 512])
                scale = 64 ** -0.5
                Kd = 64
                out_d = atA_d
            else:
                kT = sbt(st, "kmT", [128, 8, S], BF16)
                for h in range(8):
                    dma(kT[0:96, h, :], kmT_d[h], ['kmT_t_d'], ['kT%d' % h], 'kT%d' % h)
                v_src = vm_d
                qT_g = sbt(st, "qmTg", [128, 8, 512], BF16)
                scale = 96 ** -0.5
                Kd = 96
                out_d = atM_d
            vv = sbt(st, "vv" + tag, [128, NT, 520], BF16)
            v_v = v_src.rearrange("(t p) c -> p t c", p=128)
            for q in range(4):
                dma(vv[:, q * 8:(q + 1) * 8, :], v_v[:, q * 8:(q + 1) * 8, :], ['va_d', 'vm_d'], ['vv%d' % q], 'vv%d' % q)
            vres = ['vv%d' % q for q in range(4)]
            ptb = [sbt(st, "ptb%d%s" % (i, tag), [128, 512], BF16) for i in range(2)]
            rz = sbt(st, "rz" + tag, [128, 512]); of = sbt(st, "of" + tag, [128, 512])
            yb = [sbt(st, "yb%d%s" % (i, tag), [128, 512], BF16) for i in range(2)]
            pS = [pst(st, "pS%d%s" % (i, tag), [128, 512]) for i in range(2)]
            pO = [pst(st, "pO%d%s" % (i, tag), [128, 512]) for i in range(2)]
            pB = pst(st, "pB" + tag, [128, 512])
            kres = ['kT%d' % c for c in range(8)]
            ctr = 0
            def idx_block(g, b):
                bg = bias_g[g % 2]
                bgn = 'bias_g%d' % (g % 2)
                jq = 4 * g + b
                Sp = (jq + 1) * 128
                shp = [128, 8, 128]
                tt(dg[:], bc1(identb[:], shp), bc2(wi_all[:, jq, :], shp), ALU.mult, ['identb', 'wi_all'], ['dg'])
                items_i = [(c, h) for c in range(g + 1) for h in range(8)]
                qn = 'qiTg%d' % (g % 2)
                qi_ = qiTg[g % 2]

                def idx_dots(k_):
                    c, h = items_i[k_]
                    wc = 512 if c < g else (b + 1) * 128
                    k0 = c * 512
                    hp = h % 2; hc = h // 2
                    prt = slice(hp * 64, hp * 64 + 64)
                    dd = k_ % 2
                    mm(pd[dd][:, 0:wc], qi_[prt, hc, b * 128:(b + 1) * 128], kiT[prt, k0:k0 + wc], True, True,
                       [qn, 'kiT'], ['pd%d' % dd])
                    act(rbuf[dd][:, 0:wc], pd[dd][:, 0:wc], AF.Relu, ['pd%d' % dd], ['rbuf%d' % dd])

                def idx_acc(k_):
                    c, h = items_i[k_]
                    wc = 512 if c < g else (b + 1) * 128
                    k0 = c * 512
                    dd = k_ % 2
                    mm(psc[:, 0:wc], dg[:, h, :], rbuf[dd][:, 0:wc], h == 0, h == 7, ['dg', 'rbuf%d' % dd], ['psc'])
                    if h == 7:
                        if c < g:
                            cp(score[:, k0:k0 + 512], psc[:], ['psc'], ['score'])
                        else:
                            if b > 0:
                                cp(score[:, k0:k0 + b * 128], psc[:, 0:b * 128], ['psc'], ['score'])
                            tt(score[:, jq * 128:(jq + 1) * 128], psc[:, b * 128:(b + 1) * 128], trib[:], ALU.add,
                               ['psc', 'trib'], ['score'])
                idx_dots(0)
                for k_ in range(len(items_i)):
                    if k_ + 1 < len(items_i):
                        idx_dots(k_ + 1)
                    idx_acc(k_)
                if jq >= 2:
                    red(hi[:], score[:, 0:Sp], ALU.max, ['score'], ['hi'])
                    red(lo[:], score[:, 0:jq * 128], ALU.min, ['score'], ['lo'])
                    tt(stp[:], hi[:], lo[:], ALU.subtract, ['hi', 'lo'], ['stp'])
                    for k in range(NITER + 1):
                        ts(wv[:, k:k + 1], stp[:], float(2.0 ** -(k + 1)), None, ALU.mult, None, ['stp'], ['wv'])
                    tt(mid[:], lo[:], wv[:, 0:1], ALU.add, ['lo', 'wv'], ['mid'])
                    for k in range(NITER):
                        ts(junk[:, 0:Sp], score[:, 0:Sp], mid[:, 0:1], 0.0, ALU.is_ge, ALU.add, ['score', 'mid'],
                           ['junk', 'cnt'], accum_out=cnt[:])
                        ts(stp[:], cnt[:], 255.5, wv[:, k:k + 1], ALU.is_ge, ALU.mult, ['cnt', 'wv'], ['stp'])
                        tt(lo[:], lo[:], stp[:], ALU.add, ['lo', 'stp'], ['lo'])
                        tt(mid[:], lo[:], wv[:, k + 1:k + 2], ALU.add, ['lo', 'wv'], ['mid'])
                else:
                    memset(lo[:], -10000.0, ['lo'])
                ts(bg[:, b, 0:Sp], score[:, 0:Sp], lo[:, 0:1], NEG, ALU.is_lt, ALU.mult, ['score', 'lo'], [bgn])

            def att_part(g, heads):
                gsl = slice(g * 512, (g + 1) * 512)
                nsc = 4 * g + 4
                items_a = [(h, sc) for h in heads for sc in range(nsc)]
                if dsa:
                    bg = bias_g[g % 2]
                    bgn = 'bias_g%d' % (g % 2)

                def att_S(k_):
                    h, sc = items_a[k_]
                    r_ = sc - 4 * g
                    qlo = max(r_, 0) * 128
                    ss = k_ % 2
                    ssl = slice(sc * 128, (sc + 1) * 128)
                    if dsa:
                        hp = h % 2; hc = h // 2
                        prt = slice(hp * 64, hp * 64 + 64)
                        mm(pS[ss][:, qlo:512], kT[prt, hc, ssl], qT_g[prt, hc, qlo:512], True, False,
                           ['kT%d' % hc, 'qT_g'], ['pS%d' % ss])
                        b0 = max(r_, 0)
                        for b in range(b0, 4):
                            mm(pS[ss][:, b * 128:(b + 1) * 128], bg[:, b, ssl], identb[:], False, b == 3,
                               [bgn, 'identb'], ['pS%d' % ss])
                    else:
                        last = r_ < 0
                        mm(pS[ss][:, qlo:512], kT[0:96, h, ssl], qT_g[0:96, h, qlo:512], True, last,
                           ['kT%d' % h, 'qT_g'], ['pS%d' % ss])
                        if r_ >= 0:
                            mm(pS[ss][:, qlo:qlo + 128], trib[:], identb[:], False, True, ['trib', 'identb'], ['pS%d' % ss])
                    act(ptb[ss][:, qlo:512], pS[ss][:, qlo:512], AF.Exp, ['pS%d' % ss], ['ptb%d' % ss], scale=float(scale))

                def att_PV(k_):
                    h, sc = items_a[k_]
                    r_ = sc - 4 * g
                    qlo = max(r_, 0) * 128
                    ss = k_ % 2
                    po = h % 2
                    pon = 'pO%d' % po
                    mm(pO[po][0:65, qlo:512], vv[:, sc, h * 65:(h + 1) * 65], ptb[ss][:, qlo:512], sc == 0, sc == nsc - 1,
                       ['ptb%d' % ss] + vres, [pon])
                    if sc == nsc - 1:
                        rcp(rz[64:65, :], pO[po][64:65, :], [pon], ['rz'])
                        mm(pB[0:64, :], ones64[64:65, 0:64], rz[64:65, :], True, True, ['ones64', 'rz'], ['pB'])
                        cp(of[0:64, :], pO[po][0:64, :], [pon], ['of'], eng='act')
                        ybn = 'yb%d' % po
                        tt(yb[po][0:64, :], of[0:64, :], pB[0:64, :], ALU.mult, ['of', 'pB'], [ybn])
                        dma(out_d[h, :, gsl], yb[po][0:64, :], [ybn], ['at_d' + tag], ybn + tag)
                att_S(0)
                for k_ in range(len(items_a)):
                    if k_ + 1 < len(items_a):
                        att_S(k_ + 1)
                    att_PV(k_)

            def load_q(g):
                gsl = slice(g * 512, (g + 1) * 512)
                if dsa:
                    dma(qT_g[:], qaT_d[:, :, gsl].rearrange("c p t -> p c t"), ['qaT_t_d'], ['qT_g'], 'qT_g')
                else:
                    dma(qT_g[0:96, :, :], qmT_d[:, :, gsl].rearrange("h d t -> d h t"), ['qmT_t_d'], ['qT_g'], 'qT_g')

            def load_qi(g):
                gsl = slice(g * 512, (g + 1) * 512)
                dma(qiTg[g % 2][:], qiT_d[:, :, gsl].rearrange("c p t -> p c t"), ['qiT_t_d'], ['qiTg%d' % (g % 2)], 'qiTg%d' % (g % 2))

            if dsa:
                load_qi(0)
                for b in range(4):
                    idx_block(0, b)
                for g in range(NG):
                    load_q(g)
                    if g + 1 < NG:
                        load_qi(g + 1)
                    for part in range(4):
                        if g + 1 < NG:
                            idx_block(g + 1, part)
                        att_part(g, [2 * part, 2 * part + 1])
            else:
                stgc = [sbt(st, "stgc%d" % i, [128, 4096]) for i in range(2)]
                cvb = [sbt(st, "cvb%d" % i, [128, 4, D], BF16) for i in range(3)]
                uv_v = uv_d.rearrange("(p r) d -> p r d", p=128)
                conv = [(tsrc, col, rc) for (tsrc, col) in [(pu_d, 0), (pv_d, D)] for rc in range(32)]

                def conv_iter(it):
                    tsrc, col, rc = conv[it]
                    t_v = tsrc.rearrange("(p r) d -> p r d", p=128)
                    sl = it % 2
                    cs_ = it % 3
                    sv = stgc[sl][:].rearrange("p (r d) -> p r d", r=4)
                    dma(sv, t_v[:, rc * 4:(rc + 1) * 4, :], [], ['stgc%d' % sl], 'stgc%d' % sl)
                    cp(cvb[cs_][:], sv, ['stgc%d' % sl], ['cvb%d' % cs_], eng=('pool', 'dve')[it % 2])
                    dma(uv_v[:, rc * 4:(rc + 1) * 4, col:col + D], cvb[cs_][:], ['cvb%d' % cs_], ['uv_d%d' % cs_], 'cvb%d' % cs_)
                for g in range(NG):
                    load_q(g)
                    for it in range(g * 8, g * 8 + 8):
                        conv_iter(it)
                    att_part(g, list(range(8)))
            P.barrier()

    if phases >= 2:
        attention_phase("A", True)
    if phases >= 3:
        attention_phase("M", False)

    if phases >= 4:
        with contextlib.ExitStack() as st:
            woab = sbt(st, "woab", [128, 4, D], BF16); womb = sbt(st, "womb", [128, 4, D], BF16)
            woutb = sbt(st, "woutb", [128, 8, D], BF16); wpqb = sbt(st, "wpqb", [128, 8, 2048], BF16)
            subkT = sbt(st, "subkT", [128, 16, 128], BF16)
            iota16 = sbt(st, "iota16", [128, 16])
            G1 = sbt(st, "G1p", [128, D]); A2 = sbt(st, "A2p", [128, D]); B2 = sbt(st, "B2p", [128, D]); G2 = sbt(st, "G2p", [128, D])
            mixT = sbt(st, "mixT", [128, 8, 512], BF16)
            dma(iota16[:], iota16_d, [], ['iota16'], 'iota16')
            for qi_, (tn_, nm_) in enumerate([(G1, 'G1'), (A2, 'A2'), (B2, 'B2'), (G2, 'G2')]):
                dma(tn_[:], mod_d[qi_], ['mod_d'], [nm_], 'ld' + nm_)
            pxo = pst(st, "pxo", [128, 1024])
            pbank = [pxo[:, 0:512], pxo[:, 512:1024]]
            PX = ['pk0', 'pk1']
            pT4 = pst(st, "pT4", [128, 8, 128], BF16)
            ph2 = pst(st, "ph2", [128, 1024])
            pacc = pst(st, "pacc", [128, 1024])
            with contextlib.ExitStack() as st2:
                stg = [sbt(st2, "stg%d" % i, [128, 4096]) for i in range(2)]
                skb = sbt(st2, "skb", [128, 16, 128], BF16)
                ci = 0
                for (wsrc, wdst, nk, ncol, nm) in [(woa_d, woab, 4, 1024, 'woab'), (wom_d, womb, 4, 1024, 'womb'),
                                                    (wout_d, woutb, 8, 1024, 'woutb'), (wpq_d, wpqb, 8, 2048, 'wpqb')]:
                    wv_ = wsrc.rearrange("(k p) n -> p k n", p=128)
                    cw = 4096 // nk
                    for c0 in range(0, ncol, cw):
                        sl = ci % 2
                        sv = stg[sl][:, 0:nk * cw].rearrange("p (k n) -> p k n", k=nk)
                        dma(sv, wv_[:, :, c0:c0 + cw], [], ['stg%d' % sl], 'stg%d' % sl)
                        cp(wdst[:, :, c0:c0 + cw], sv, ['stg%d' % sl], [nm], eng=('pool' if ci % 2 == 0 else 'act'))
                        ci += 1
                sl = ci % 2
                sv = stg[sl][:, 0:2048].rearrange("p (c d) -> p c d", c=16)
                dma(sv, subk_d.rearrange("c n d -> n c d"), [], ['stg%d' % sl], 'stg%d' % sl)
                cp(skb[:], sv, ['stg%d' % sl], ['skb'])
                for c in range(16):
                    tr(pT4[:, c % 8, :], skb[:, c, :], identb[:], ['skb', 'identb'], ['pT4'])
                    if c % 8 == 7:
                        cp(subkT[:, c - 7:c + 1, :], pT4[:], ['pT4'], ['subkT'])
                P.barrier()

            for g in range(NG):
                gsl = slice(g * 512, (g + 1) * 512)
                with contextlib.ExitStack() as sg:
                    aT = sbt(sg, "aT%d" % g, [128, 4, 512], BF16); mT = sbt(sg, "mT%d" % g, [128, 4, 512], BF16)
                    gSg = sbt(sg, "gSg%d" % g, [128, 16, 512], BF16)
                    t1 = sbt(sg, "t1_%d" % g, [128, 512]); t2 = sbt(sg, "t2_%d" % g, [128, 512])
                    dma(aT[:], atA_d[:, :, gsl].rearrange("(c two) d t -> (two d) c t", two=2), ['at_dA'], ['aT'], 'aT')
                    dma(mT[:], atM_d[:, :, gsl].rearrange("(c two) d t -> (two d) c t", two=2), ['at_dM'], ['mT'], 'mT')
                    dma(gSg[:], gS_d[:, :, gsl].rearrange("c p t -> p c t"), ['gS_d'], ['gSg'], 'gSg')
                    for fc in range(8):
                        fsl = slice(fc * 128, (fc + 1) * 128)
                        for k in range(4):
                            mm(pbank[0], woab[:, k, fsl], aT[:, k, :], k == 0, k == 3, ['woab', 'aT'], ['pk0'])
                        for k in range(4):
                            mm(pbank[1], womb[:, k, fsl], mT[:, k, :], k == 0, k == 3, ['womb', 'mT'], ['pk1'])
                        tt(t1[:], pbank[0], gSg[:, fc, :], ALU.mult, ['pk0', 'gSg'], ['t1'])
                        tt(t2[:], pbank[1], gSg[:, 8 + fc, :], ALU.mult, ['pk1', 'gSg'], ['t2'])
                        tt(mixT[:, fc, :], t1[:], t2[:], ALU.add, ['t1', 't2'], ['mixT'], eng='pool')
                    P.barrier()
                with contextlib.ExitStack() as sl4:
                    def sb4(name, shape, dt=F32):
                        return sbt(sl4, "%s_%d" % (name, g), shape, dt)
                    xt2 = sb4("xt2", [128, D]); x1 = sb4("x1", [128, D]); tmpx = sb4("tmpx", [128, D])
                    junk4 = sb4("junk4", [128, D], BF16)
                    ssq = sb4("ssq4", [128, 1]); rstd = sb4("rstd4", [128, 1])
                    h2b = sb4("h2b", [128, D], BF16); h2T = sb4("h2T", [128, 8, 128], BF16)
                    qpT = sb4("qpT", [128, 16, 128], BF16)
                    s_all = sb4("s_all", [128, 16, 128]); s_wk = sb4("s_wk", [128, 128])
                    tops = sb4("tops", [128, 16, 16]); topi = sb4("topi", [128, 16, 16], U32); topf = sb4("topf", [128, 16, 16])
                    cand = sb4("cand", [128, 8, 256]); cwk = sb4("cwk", [128, 256])
                    best = sb4("best", [128, 8, 16]); bpos = sb4("bpos", [128, 8, 16], U32)
                    ak = sb4("ak", [128, 8, 16], U32); bk = sb4("bk", [128, 8, 16], U32)
                    akf = sb4("akf", [128, 8, 16]); bkf = sb4("bkf", [128, 8, 16])
                    oh = s_all[:].rearrange("p c (a b) -> p (c a) b", b=16).rearrange("p (h k) a -> p h k a", h=8)
                    i0 = sb4("i0", [128, 8, 16]); i1 = sb4("i1", [128, 8, 16])
                    idf = sb4("idf", [128, 128]); ids = sb4("ids", [128, 128], U32)
                    gate = sb4("gate", [128, 8, 16]); gz = sb4("gz", [128, 8]); ngmax = sb4("ngmax", [128, 8])
                    actv = sb4("actv", [128, 128]); coef = sb4("coef", [128, 128]); gtmp = sb4("gtmp", [128, 128])
                    NRING = 12
                    uvb = [sb4("uvb%d" % i, [128, 2 * D], BF16) for i in range(NRING)]
                    dgb = [sb4("dgb%d" % i, [128, 128], BF16) for i in range(4)]
                    junkf = sb4("junkf", [128, D], BF16)
                    ob = xt2
                    for j in range(4):
                        i = g * 4 + j
                        tsl = slice(j * 128, (j + 1) * 128)
                        dma(xt2[:], x_d[i * 128:(i + 1) * 128, :], [], ['xt2'], 'xt2')
                        for hf in range(2):
                            for k in range(8):
                                mm(pbank[hf], mixT[:, k, tsl], woutb[:, k, hf * 512:(hf + 1) * 512], k == 0, k == 7,
                                   ['mixT', 'woutb'], [PX[hf]])
                        tt(tmpx[:], pxo[:], G1[:], ALU.mult, PX + ['G1'], ['tmpx'])
                        tt(x1[:], tmpx[:], xt2[:], ALU.add, ['tmpx', 'xt2'], ['x1'], eng='pool')
                        act(junk4[:], x1[:], AF.Square, ['x1'], ['junk4', 'ssq4'], accum_out=ssq[:])
                        ts(ssq[:], ssq[:], float(1.0 / D), float(EPS), ALU.mult, ALU.add, ['ssq4'], ['ssq4'])
                        act(ssq[:], ssq[:], AF.Sqrt, ['ssq4'], ['ssq4'])
                        rcp(rstd[:], ssq[:], ['ssq4'], ['rstd4'])
                        stt(tmpx[:], x1[:], rstd[:, 0:1], A2[:], ALU.mult, ALU.mult, ['x1', 'rstd4', 'A2'], ['tmpx'])
                        tt(ph2[:], tmpx[:], B2[:], ALU.add, ['tmpx', 'B2'], ['ph2'])
                        cp(h2b[:], ph2[:], ['ph2'], ['h2b'], eng='act')
                        for k in range(8):
                            tr(pT4[:, k, :], h2b[:, k * 128:(k + 1) * 128], identb[:], ['h2b', 'identb'], ['pT4'])
                        cp(h2T[:], pT4[:], ['pT4'], ['h2T'])
                        for c4 in range(4):
                            pk = c4 % 2
                            for cc in range(4):
                                c = c4 * 4 + cc
                                for k in range(8):
                                    mm(pbank[pk][:, cc * 128:(cc + 1) * 128], wpqb[:, k, c * 128:(c + 1) * 128], h2T[:, k, :],
                                       k == 0, k == 7, ['wpqb', 'h2T'], [PX[pk]])
                            cp(qpT[:, c4 * 4:(c4 + 1) * 4, :].rearrange("p c t -> p (c t)"), pbank[pk], [PX[pk]], ['qpT'],
                               eng=('act' if c4 % 2 == 0 else 'dve'))
                        for c4 in range(4):
                            pk = c4 % 2
                            for cc in range(4):
                                c = c4 * 4 + cc
                                mm(pbank[pk][:, cc * 128:(cc + 1) * 128], qpT[:, c, :], subkT[:, c, :], True, True,
                                   ['qpT', 'subkT'], [PX[pk]])
                            cp(s_all[:, c4 * 4:(c4 + 1) * 4, :].rearrange("p c t -> p (c t)"), pbank[pk], [PX[pk]], ['s_all'],
                               eng=('act' if c4 % 2 == 0 else 'dve'))
                        for c in range(16):
                            sv_ = s_all[:, c, :]
                            P.op('dve', lambda e, c=c, sv_=sv_: e.max(out=tops[:, c, 0:8], in_=sv_), ['s_all'], ['tops'])
                            P.op('dve', lambda e, c=c, sv_=sv_: e.max_index(out=topi[:, c, 0:8], in_max=tops[:, c, 0:8], in_values=sv_),
                                 ['s_all', 'tops'], ['topi'])
                            P.op('dve', lambda e, c=c, sv_=sv_: e.match_replace(out=s_wk[:], in_to_replace=tops[:, c, 0:8], in_values=sv_,
                                                                            imm_value=-1e30), ['s_all', 'tops'], ['s_wk'])
                            P.op('dve', lambda e, c=c: e.max(out=tops[:, c, 8:16], in_=s_wk[:]), ['s_wk'], ['tops'])
                            P.op('dve', lambda e, c=c: e.max_index(out=topi[:, c, 8:16], in_max=tops[:, c, 8:16], in_values=s_wk[:]),
                                 ['s_wk', 'tops'], ['topi'])
                        cp(topf[:], topi[:], ['topi'], ['topf'])
                        t4 = tops[:].rearrange("p (h two) k -> p h two k", two=2)
                        shp4 = [128, 8, 16, 16]
                        c4v = cand[:].rearrange("p h (a b) -> p h a b", a=16)
                        tt(c4v, t4[:, :, 0, :].unsqueeze(3).to_broadcast(shp4), t4[:, :, 1, :].unsqueeze(2).to_broadcast(shp4), ALU.add,
                           ['tops'], ['cand'])
                        for h in range(8):
                            cv = cand[:, h, :]
                            P.op('dve', lambda e, h=h, cv=cv: e.max(out=best[:, h, 0:8], in_=cv), ['cand'], ['best'])
                            P.op('dve', lambda e, h=h, cv=cv: e.max_index(out=bpos[:, h, 0:8], in_max=best[:, h, 0:8], in_values=cv),
                                 ['cand', 'best'], ['bpos'])
                            P.op('dve', lambda e, h=h, cv=cv: e.match_replace(out=cwk[:], in_to_replace=best[:, h, 0:8], in_values=cv,
                                                                            imm_value=-1e30), ['cand', 'best'], ['cwk'])
                            P.op('dve', lambda e, h=h: e.max(out=best[:, h, 8:16], in_=cwk[:]), ['cwk'], ['best'])
                            P.op('dve', lambda e, h=h: e.max_index(out=bpos[:, h, 8:16], in_max=best[:, h, 8:16], in_values=cwk[:]),
                                 ['cwk', 'best'], ['bpos'])
                        ts(ak[:], bpos[:], 4, None, ALU.logical_shift_right, None, ['bpos'], ['ak'])
                        ts(bk[:], bpos[:], 15, None, ALU.bitwise_and, None, ['bpos'], ['bk'])
                        cp(akf[:], ak[:], ['ak'], ['akf'])
                        cp(bkf[:], bk[:], ['bk'], ['bkf'])
                        tf4 = topf[:].rearrange("p (h two) k -> p h two k", two=2)
                        io4 = iota16[:].unsqueeze(1).unsqueeze(1).to_broadcast(shp4)
                        for (kf_, half, dsti, nm) in [(akf, 0, i0, 'i0'), (bkf, 1, i1, 'i1')]:
                            tt(oh, kf_[:].unsqueeze(3).to_broadcast(shp4), io4, ALU.is_equal, ['akf', 'bkf', 'iota16'], ['s_all'])
                            tt(oh, oh, tf4[:, :, half, :].unsqueeze(2).to_broadcast(shp4), ALU.mult, ['s_all', 'topf'], ['s_all'])
                            red(dsti[:], oh, ALU.add, ['s_all'], [nm])
                        stt(idf[:].rearrange("p (h k) -> p h k", h=8), i0[:], 128.0, i1[:], ALU.mult, ALU.add, ['i0', 'i1'], ['idf'])
                        cp(ids[:], idf[:], ['idf'], ['ids'])
                        ts(ngmax[:], best[:, :, 0], -1.0, None, ALU.mult, None, ['best'], ['ngmax'])
                        tt(gate[:], best[:], bc2(ngmax[:], [128, 8, 16]), ALU.add, ['best', 'ngmax'], ['gate'])
                        act(gate[:], gate[:], AF.Exp, ['gate'], ['gate'])
                        red(gz[:], gate[:], ALU.add, ['gate'], ['gz'])
                        rcp(gz[:], gz[:], ['gz'], ['gz'])
                        tt(gate[:], gate[:], bc2(gz[:], [128, 8, 16]), ALU.mult, ['gate', 'gz'], ['gate'])
                        gate_f = gate[:].rearrange("p h k -> p (h k)")
                        GELU_S = float(2.0 * np.sqrt(2.0 / np.pi))

                        def pg_gather(gi):
                            for q_ in range(4):
                                sl_ = gi * 4 + q_
                                rb = sl_ % NRING
                                un = 'uvb%d' % rb
                                P.dma('pool', lambda e, sl_=sl_, rb=rb: e.indirect_dma_start(
                                    out=uvb[rb][:], out_offset=None, in_=uv_d,
                                    in_offset=bass.IndirectOffsetOnAxis(ap=ids[:, sl_:sl_ + 1], axis=0)), ['ids'], [un], un)

                        def pg_dot(gi):
                            for q_ in range(4):
                                sl_ = gi * 4 + q_
                                rb = sl_ % NRING
                                stt(junkf[:], uvb[rb][:, 0:D], 1.0, ph2[:], ALU.mult, ALU.mult, ['uvb%d' % rb, 'ph2'], ['junkf', 'actv'],
                                    accum_out=actv[:, sl_:sl_ + 1])

                        def pg_coef(gi):
                            s4 = slice(gi * 4, gi * 4 + 4)
                            a_ = actv[:, s4]; t_ = gtmp[:, s4]
                            stt(t_, a_, 0.044715, a_, ALU.mult, ALU.mult, ['actv'], ['gtmp'])
                            stt(t_, t_, 1.0, a_, ALU.add, ALU.mult, ['gtmp', 'actv'], ['gtmp'])
                            act(t_, t_, AF.Sigmoid, ['gtmp'], ['gtmp'], scale=GELU_S)
                            tt(t_, t_, a_, ALU.mult, ['gtmp', 'actv'], ['gtmp'])
                            tt(coef[:, s4], t_, gate_f[:, s4], ALU.mult, ['gtmp', 'gate'], ['coef'])

                        def pg_acc(gi):
                            for q_ in range(4):
                                sl_ = gi * 4 + q_
                                rb = sl_ % NRING
                                db = sl_ % 4
                                act(dgb[db][:], identb[:], AF.Identity, ['identb', 'coef'], ['dgb%d' % db], scale=coef[:, sl_:sl_ + 1])
                                for hf in range(2):
                                    mm(pacc[:, hf * 512:(hf + 1) * 512], dgb[db][:], uvb[rb][:, D + hf * 512:D + (hf + 1) * 512],
                                       sl_ == 0, sl_ == 127, ['dgb%d' % db, 'uvb%d' % rb], ['pacc'])

                        pg_gather(0); pg_gather(1); pg_gather(2)
                        pg_dot(0)
                        for gi in range(32):
                            if gi + 1 < 32:
                                pg_dot(gi + 1)
                            pg_coef(gi)
                            pg_acc(gi)
                            if gi + 3 < 32:
                                pg_gather(gi + 3)
                        tt(tmpx[:], pacc[:], G2[:], ALU.mult, ['pacc', 'G2'], ['tmpx'])
                        tt(ob[:], tmpx[:], x1[:], ALU.add, ['tmpx', 'x1'], ['xt2'], eng='pool')
                        dma(out_d[i * 128:(i + 1) * 128, :], ob[:], ['xt2'], ['out_d'], 'ob')
                    P.barrier()

    P.emit(nc)
    top.close()
    return nc


def make_inputs(inputs):
    f = lambda a: np.ascontiguousarray(np.asarray(a))
    common = {}
    for k in ["g_norm1", "g_norm2", "b_ada", "g_a_q", "g_a_k", "g_idx_k", "g_mq_a", "g_mkv_a", "g_m_q", "g_m_k"]:
        common[k] = f(np.asarray(inputs[k], np.float32).reshape(1, -1))
    for k in ["w_ada", "w_in", "w_mq_up", "w_mkv_up", "w_o_a", "w_o_m", "w_out", "w_peer_q", "peer_u", "peer_v"]:
        common[k] = f(np.asarray(inputs[k], np.float32)[0])
    common["peer_subkeys"] = f(np.asarray(inputs["peer_subkeys"], np.float32)[0].reshape(16, 128, 128))
    common["identb"] = np.eye(128, dtype=np.float32).astype(ml_dtypes.bfloat16)
    q = np.arange(128)[:, None]; s = np.arange(128)[None, :]
    common["trib"] = np.where(s <= q, 0.0, NEG).astype(np.float32).astype(ml_dtypes.bfloat16)
    inv64 = (10000.0 ** (-(np.arange(32, dtype=np.float32)) / np.float32(32))).astype(np.float32)
    inv32 = (10000.0 ** (-(np.arange(16, dtype=np.float32)) / np.float32(16))).astype(np.float32)
    common["inv64"] = f(np.broadcast_to(inv64[None, :], (128, 32)))
    common["inv32"] = f(np.broadcast_to(inv32[None, :], (128, 16)))
    common["iota16"] = f(np.broadcast_to(np.arange(16, dtype=np.float32)[None, :], (128, 16)))
    x = np.asarray(inputs["x"], np.float32)
    c = np.asarray(inputs["c"], np.float32)
    pos = np.asarray(inputs["positions"], np.int32)
    maps = []
    for b in range(x.shape[0]):
        m = dict(common)
        m["x"] = f(x[b])
        m["c_col"] = f(c[b].reshape(8, 128).T)
        m["pos_t"] = f(pos[b].reshape(NT, 128).T)
        maps.append(m)
    return maps


def kernel(**inputs):
    maps = make_inputs(inputs)
    nc = build_nc()
    res = run_bass_kernel_spmd(nc, maps, core_ids=list(range(8)))
    return np.stack([np.asarray(r["out"], np.float32) for r in res.results], axis=0)
```

```python
import contextlib
import numpy as np
import ml_dtypes
import concourse.bass as bass
import concourse.mybir as mybir
from concourse.bass_utils import run_bass_kernel_spmd

F32 = mybir.dt.float32
BF16 = mybir.dt.bfloat16
I32 = mybir.dt.int32
U32 = mybir.dt.uint32
AF = mybir.ActivationFunctionType
ALU = mybir.AluOpType
AX = mybir.AxisListType

S = 4096
D = 1024
NT = 32
NG = 8
EPS = 1e-6
NEG = -30000.0
NITER = 18
NSLOT = 4
TWO_PI = float(2 * np.pi)
NO_SELF_WAIT = False


class Prog:
    ENG = ('pe', 'act', 'dve', 'pool', 'sp')

    def __init__(self):
        self.ops = {e: [] for e in self.ENG}
        self.cnt = {e: 0 for e in self.ENG}
        self.known = {e: {} for e in self.ENG}
        self.res = {}
        self.dma_cnt = {}

    def _deps(self, eng, reads, writes):
        need = {}

        def add(sk, v):
            if sk == ('eng', 'pe') and eng == 'pe':
                return
            if NO_SELF_WAIT and sk == ('eng', eng):
                return
            if v > need.get(sk, 0):
                need[sk] = v
        for k in reads:
            st = self.res.get(k)
            if st and st[0] is not None:
                add(*st[0])
        for k in writes:
            st = self.res.get(k)
            if st:
                if st[0] is not None:
                    add(*st[0])
                for sk, v in st[1].items():
                    add(sk, v)
        waits = []
        kn = self.known[eng]
        for sk, v in need.items():
            if kn.get(sk, 0) >= v:
                continue
            kn[sk] = v
            waits.append((sk, v))
        return waits

    def _commit(self, tok, reads, writes):
        sk, v = tok
        for k in reads:
            st = self.res.setdefault(k, [None, {}])
            if v > st[1].get(sk, 0):
                st[1][sk] = v
        for k in writes:
            self.res[k] = [tok, {}]

    def op(self, eng, fn, r=(), w=()):
        waits = self._deps(eng, r, w)
        self.cnt[eng] += 1
        tok = (('eng', eng), self.cnt[eng])
        self.ops[eng].append((waits, fn, ('eng', eng), 1))
        self._commit(tok, r, w)

    def dma(self, eng, fn, r=(), w=(), sem=None):
        waits = self._deps(eng, r, w)
        c = self.dma_cnt.get(sem, 0) + 16
        self.dma_cnt[sem] = c
        tok = (('dma', sem), c)
        self.ops[eng].append((waits, fn, ('dma', sem), 16))
        self._commit(tok, r, w)

    def barrier(self):
        allk = [(('eng', e), self.cnt[e]) for e in self.ENG if self.cnt[e] > 0]
        allk += [(('dma', k), c) for k, c in self.dma_cnt.items()]
        for e in self.ENG:
            waits = []
            kn = self.known[e]
            for sk, v in allk:
                if sk == ('eng', e):
                    continue
                if kn.get(sk, 0) >= v:
                    continue
                kn[sk] = v
                waits.append((sk, v))
            if waits:
                self.ops[e].append((waits, None, None, 0))

    def emit(self, nc):
        self.barrier()
        with contextlib.ExitStack() as st:
            sems = {}
            for e in self.ENG:
                sems[('eng', e)] = st.enter_context(nc.semaphore("s_" + e))
            for i, k in enumerate(self.dma_cnt):
                sems[('dma', k)] = st.enter_context(nc.semaphore("d%d" % i))
            block = st.enter_context(nc.Block())

            def run(e, name):
                for (waits, fn, sk, inc) in self.ops[name]:
                    for (wk, v) in waits:
                        e.wait_ge(sems[wk], v)
                    if fn is not None:
                        fn(e).then_inc(sems[sk], inc)

            @block.tensor
            def _(e):
                run(e, 'pe')

            @block.scalar
            def _(e):
                run(e, 'act')

            @block.vector
            def _(e):
                run(e, 'dve')

            @block.gpsimd
            def _(e):
                run(e, 'pool')

            @block.sync
            def _(e):
                run(e, 'sp')


def build_nc(debug=False, phases=4, nt1=NT, skip=(), nslot=NSLOT, gcols=D):
    nc = bass.Bass("TRN2", target_bir_lowering=False)
    P = Prog()

    def din(name, shape, dt=F32):
        return nc.dram_tensor(name, list(shape), dt, kind="ExternalInput").ap()

    def dscr(name, shape, dt=BF16):
        return nc.dram_tensor(name, list(shape), dt, kind="ExternalOutput" if debug else "Internal").ap()

    x_d = din("x", [S, D])
    ccol_d = din("c_col", [128, 8])
    pos_d = din("pos_t", [128, NT], I32)
    g1_d = din("g_norm1", [1, D]); g2_d = din("g_norm2", [1, D])
    wada_d = din("w_ada", [D, 6 * D]); bada_d = din("b_ada", [1, 6 * D])
    win_d = din("w_in", [D, 4840])
    gaq_d = din("g_a_q", [1, 64]); gak_d = din("g_a_k", [1, 64]); gik_d = din("g_idx_k", [1, 64])
    gmqa_d = din("g_mq_a", [1, 384]); wmq_d = din("w_mq_up", [384, 768])
    gmkva_d = din("g_mkv_a", [1, 256]); wmkv_d = din("w_mkv_up", [256, 1024])
    gmq_d = din("g_m_q", [1, 96]); gmk_d = din("g_m_k", [1, 96])
    woa_d = din("w_o_a", [512, D]); wom_d = din("w_o_m", [512, D]); wout_d = din("w_out", [D, D])
    wpq_d = din("w_peer_q", [D, 2048]); subk_d = din("peer_subkeys", [16, 128, 128])
    pu_d = din("peer_u", [16384, D]); pv_d = din("peer_v", [16384, D])
    identb_d = din("identb", [128, 128], BF16)
    trib_d = din("trib", [128, 128], BF16)
    inv64_d = din("inv64", [128, 32]); inv32_d = din("inv32", [128, 16])
    iota16_d = din("iota16", [128, 16])
    out_d = nc.dram_tensor("out", [S, D], F32, kind="ExternalOutput").ap()

    qaT_d = dscr("qaT_s", [4, 128, S]); kaT_d = dscr("kaT_s", [4, 128, S]); qiT_d = dscr("qiT_s", [4, 128, S])
    kiT_d = dscr("kiT_s", [128, S])
    va_d = dscr("va_s", [S, 520]); vm_d = dscr("vm_s", [S, 520])
    qmT_d = dscr("qmT_s", [8, 96, S]); kmT_d = dscr("kmT_s", [8, 96, S])
    gS_d = dscr("gS_s", [16, 128, S])
    atA_d = dscr("atA_s", [8, 64, S]); atM_d = dscr("atM_s", [8, 64, S])
    mod_d = dscr("mod_s", [4, 128, D], F32)
    uv_d = nc.dram_tensor("uv_s", [16384, 2048], BF16, kind="Internal").ap()

    top = contextlib.ExitStack()

    uid = [0]

    def sbt(st, name, shape, dt=F32):
        uid[0] += 1
        return st.enter_context(nc.sbuf_tensor("sb%d_%s" % (uid[0], name), list(shape), dt))

    def pst(st, name, shape, dt=F32):
        uid[0] += 1
        return st.enter_context(nc.psum_tensor("ps%d_%s" % (uid[0], name), list(shape), dt))

    def tt(out, in0, in1, op, r, w, eng='dve'):
        P.op(eng, lambda e: e.tensor_tensor(out=out, in0=in0, in1=in1, op=op), r, w)

    def ts(out, in0, s1, s2, op0, op1, r, w, eng='dve', accum_out=None):
        if op1 is None:
            P.op(eng, lambda e: e.tensor_scalar(out=out, in0=in0, scalar1=s1, scalar2=None, op0=op0), r, w)
        elif accum_out is None:
            P.op(eng, lambda e: e.tensor_scalar(out=out, in0=in0, scalar1=s1, scalar2=s2, op0=op0, op1=op1), r, w)
        else:
            P.op(eng, lambda e: e.tensor_scalar(out=out, in0=in0, scalar1=s1, scalar2=s2, op0=op0, op1=op1, accum_out=accum_out), r, w)

    def stt(out, in0, scalar, in1, op0, op1, r, w, accum_out=None):
        if accum_out is None:
            P.op('dve', lambda e: e.scalar_tensor_tensor(out=out, in0=in0, scalar=scalar, in1=in1, op0=op0, op1=op1), r, w)
        else:
            P.op('dve', lambda e: e.scalar_tensor_tensor(out=out, in0=in0, scalar=scalar, in1=in1, op0=op0, op1=op1, accum_out=accum_out), r, w)

    def act(out, in_, func, r, w, scale=None, bias=None, accum_out=None):
        kw = {}
        if scale is not None:
            kw['scale'] = scale
        if bias is not None:
            kw['bias'] = bias
        if accum_out is not None:
            kw['accum_out'] = accum_out
        P.op('act', lambda e: e.activation(out=out, in_=in_, func=func, **kw), r, w)

    def cp(out, in_, r, w, eng='dve'):
        if eng == 'act':
            act(out, in_, AF.Copy, r, w)
        else:
            P.op(eng, lambda e: e.tensor_copy(out=out, in_=in_), r, w)

    def red(out, in_, op, r, w):
        P.op('dve', lambda e: e.tensor_reduce(out=out, in_=in_, axis=AX.X, op=op), r, w)

    def rcp(out, in_, r, w):
        P.op('dve', lambda e: e.reciprocal(out=out, in_=in_), r, w)

    def mm(out, lhsT, rhs, start, stop, r, w):
        P.op('pe', lambda e: e.matmul(out=out, lhsT=lhsT, rhs=rhs, start=start, stop=stop), r, w)

    def tr(out, in_, ident, r, w):
        P.op('pe', lambda e: e.transpose(out=out, in_=in_, identity=ident), r, w)

    def dma(out, in_, r, w, sem, eng='sp'):
        P.dma(eng, lambda e: e.dma_start(out=out, in_=in_), r, w, sem)

    def memset(ap, val, w, eng='dve'):
        P.op(eng, lambda e: e.memset(ap, val), (), w)

    def bc1(ap, shape):
        return ap.unsqueeze(1).to_broadcast(shape)

    def bc2(ap, shape):
        return ap.unsqueeze(2).to_broadcast(shape)

    identb = sbt(top, "identb", [128, 128], BF16)
    trib = sbt(top, "trib", [128, 128], BF16)
    wi_all = sbt(top, "wi_all", [128, NT, 8])
    st01 = contextlib.ExitStack()
    A1 = sbt(st01, "A1", [128, D]); B1 = sbt(st01, "B1", [128, D])
    dma(identb[:], identb_d, [], ['identb'], 'identb')
    dma(trib[:], trib_d, [], ['trib'], 'trib')

    with contextlib.ExitStack() as st:
        ccol = sbt(st, "ccol", [128, 8]); cact = sbt(st, "cact", [128, 8])
        cb = sbt(st, "cb", [128, 8, 128])
        g1b = sbt(st, "g1b", [128, D]); g2b = sbt(st, "g2b", [128, D])
        G1 = sbt(st, "G1", [128, D]); A2 = sbt(st, "A2", [128, D]); B2 = sbt(st, "B2", [128, D]); G2 = sbt(st, "G2", [128, D])
        wa = [sbt(st, "wa%d" % i, [128, 8, 512]) for i in range(2)]
        bb = [sbt(st, "bb%d" % i, [128, 512]) for i in range(2)]
        tmp0 = sbt(st, "tmp0", [128, 512])
        pm0 = [pst(st, "pm0_%d" % i, [128, 512]) for i in range(2)]
        dma(ccol[:], ccol_d, [], ['ccol'], 'ccol')
        dma(g1b[:], g1_d.to_broadcast([128, D]), [], ['g1b'], 'g1b')
        dma(g2b[:], g2_d.to_broadcast([128, D]), [], ['g2b'], 'g2b')
        act(cact[:], ccol[:], AF.Silu, ['ccol'], ['cact'])
        cp(cb[:], cact[:].unsqueeze(2).to_broadcast([128, 8, 128]), ['cact'], ['cb'])
        wada_v = wada_d.rearrange("(k p) n -> p k n", p=128)
        dests = [B1, A1, G1, B2, A2, G2]
        dnames = ['B1', 'A1', 'G1', 'B2', 'A2', 'G2']
        for n in range(12):
            sl = n % 2
            dma(wa[sl][:], wada_v[:, :, n * 512:(n + 1) * 512], [], ['wa%d' % sl], 'wa%d' % sl)
            dma(bb[sl][:], bada_d[:, n * 512:(n + 1) * 512].to_broadcast([128, 512]), [], ['bb%d' % sl], 'bb%d' % sl)
            for k in range(8):
                mm(pm0[sl][:], cb[:, k, :], wa[sl][:, k, :], k == 0, k == 7, ['cb', 'wa%d' % sl], ['pm0_%d' % sl])
            which = n // 2
            dst = dests[which][:, (n % 2) * 512:(n % 2 + 1) * 512]
            dn = dnames[which]
            if which in (1, 4):
                gsrc = (g1b if which == 1 else g2b)[:, (n % 2) * 512:(n % 2 + 1) * 512]
                tt(tmp0[:], pm0[sl][:], bb[sl][:], ALU.add, ['pm0_%d' % sl, 'bb%d' % sl], ['tmp0'])
                stt(dst, tmp0[:], 1.0, gsrc, ALU.add, ALU.mult, ['tmp0', 'g1b', 'g2b'], [dn])
            else:
                tt(dst, pm0[sl][:], bb[sl][:], ALU.add, ['pm0_%d' % sl, 'bb%d' % sl], [dn])
        for qi_, (tn_, nm_) in enumerate([(G1, 'G1'), (A2, 'A2'), (B2, 'B2'), (G2, 'G2')]):
            dma(mod_d[qi_], tn_[:], [nm_], ['mod_d'], 'mod' + nm_)
        P.barrier()

    if phases == 0:
        P.emit(nc)
        st01.close()
        top.close()
        return nc

    with contextlib.ExitStack() as st:
        winb = sbt(st, "winb", [128, 8, 4840], BF16)
        wmqb = sbt(st, "wmqb", [128, 3, 768], BF16)
        wmkvb = sbt(st, "wmkvb", [128, 2, 1024], BF16)
        gaq = sbt(st, "gaq", [128, 64]); gak = sbt(st, "gak", [128, 64]); gik = sbt(st, "gik", [128, 64])
        gmqa = sbt(st, "gmqa", [128, 384]); gmkva = sbt(st, "gmkva", [128, 256])
        gmq = sbt(st, "gmq", [128, 96]); gmk = sbt(st, "gmk", [128, 96])
        cos64 = sbt(st, "cos64", [128, NT, 32]); sin64 = sbt(st, "sin64", [128, NT, 32])
        cos32 = sbt(st, "cos32", [128, NT, 16]); sin32 = sbt(st, "sin32", [128, NT, 16])
        for tns, src, nm in [(gaq, gaq_d, 'gaq'), (gak, gak_d, 'gak'), (gik, gik_d, 'gik'), (gmqa, gmqa_d, 'gmqa'),
                             (gmkva, gmkva_d, 'gmkva'), (gmq, gmq_d, 'gmq'), (gmk, gmk_d, 'gmk')]:
            dma(tns[:], src.to_broadcast(list(tns[:].shape)), [], [nm], nm)
        with contextlib.ExitStack() as st2:
            stage = [sbt(st2, "stage%d" % i, [128, 4096]) for i in range(2)]
            win_v = win_d.rearrange("(k p) n -> p k n", p=128)
            ci = 0
            for c0 in range(0, 4840, 512):
                c1 = min(c0 + 512, 4840)
                wdt = c1 - c0
                sl = ci % 2
                sv = stage[sl][:, 0:8 * wdt].rearrange("p (k n) -> p k n", k=8)
                dma(sv, win_v[:, :, c0:c1], [], ['stage%d' % sl], 'stage%d' % sl)
                cp(winb[:, :, c0:c1], sv, ['stage%d' % sl], ['winb'], eng=('pool' if ci % 2 == 0 else 'act'))
                ci += 1
            sv = stage[ci % 2][:, 0:3 * 768].rearrange("p (k n) -> p k n", k=3)
            dma(sv, wmq_d.rearrange("(k p) n -> p k n", p=128), [], ['stage%d' % (ci % 2)], 'stage%d' % (ci % 2))
            cp(wmqb[:], sv, ['stage%d' % (ci % 2)], ['wmqb'], eng='pool')
            ci += 1
            sv = stage[ci % 2][:, 0:2 * 1024].rearrange("p (k n) -> p k n", k=2)
            dma(sv, wmkv_d.rearrange("(k p) n -> p k n", p=128), [], ['stage%d' % (ci % 2)], 'stage%d' % (ci % 2))
            cp(wmkvb[:], sv, ['stage%d' % (ci % 2)], ['wmkvb'], eng='pool')
            posi = sbt(st2, "posi", [128, NT], I32); posf = sbt(st2, "posf", [128, NT])
            inv64 = sbt(st2, "inv64", [128, 32]); inv32 = sbt(st2, "inv32", [128, 16])
            rt_a = sbt(st2, "rt_a", [128, NT, 32]); rt_k = sbt(st2, "rt_k", [128, NT, 32])
            rt_i = sbt(st2, "rt_i", [128, NT, 32], I32); rt_y = sbt(st2, "rt_y", [128, NT, 32])
            dma(posi[:], pos_d, [], ['posi'], 'posi')
            dma(inv64[:], inv64_d, [], ['inv64'], 'inv64')
            dma(inv32[:], inv32_d, [], ['inv32'], 'inv32')
            cp(posf[:], posi[:], ['posi'], ['posf'])
            for (inv, hf, cs, sn, nm) in [(inv64, 32, cos64, sin64, '64'), (inv32, 16, cos32, sin32, '32')]:
                a = rt_a[:, :, 0:hf]; kk = rt_k[:, :, 0:hf]; ii = rt_i[:, :, 0:hf]; y = rt_y[:, :, 0:hf]
                shp = [128, NT, hf]
                tt(a, bc2(posf[:], shp), bc1(inv[:], shp), ALU.mult, ['posf', 'inv' + nm], ['rt_a'])
                ts(kk, a, float(1.0 / TWO_PI), None, ALU.mult, None, ['rt_a'], ['rt_k'])
                cp(ii, kk, ['rt_k'], ['rt_i'])
                cp(kk, ii, ['rt_i'], ['rt_k'])
                stt(a, kk, -TWO_PI, a, ALU.mult, ALU.add, ['rt_k', 'rt_a'], ['rt_a'])
                ts(kk, a, float(np.pi / 2), float(np.pi), ALU.add, ALU.is_gt, ['rt_a'], ['rt_k'])
                stt(y, kk, -TWO_PI, a, ALU.mult, ALU.add, ['rt_k', 'rt_a'], ['rt_y'])
                ts(y, y, float(np.pi / 2), None, ALU.add, None, ['rt_y'], ['rt_y'])
                ts(y, y, float(np.pi), float(-np.pi), ALU.min, ALU.max, ['rt_y'], ['rt_y'])
                ts(a, a, float(np.pi), float(-np.pi), ALU.min, ALU.max, ['rt_a'], ['rt_a'])
                act(sn[:], a, AF.Sin, ['rt_a'], ['sin' + nm])
                act(cs[:], y, AF.Sin, ['rt_y'], ['cos' + nm])
            P.barrier()

        xt = [sbt(st, "xt%d" % i, [128, D]) for i in range(2)]
        junkb = sbt(st, "junkb", [128, D], BF16)
        ssq = sbt(st, "ssq", [128, 1]); rstd = sbt(st, "rstd", [128, 1])
        htmp = sbt(st, "htmp", [128, D]); hb = sbt(st, "hb", [128, D], BF16)
        hT = [sbt(st, "hT%d" % i, [128, 8, 128], BF16) for i in range(2)]
        proj = sbt(st, "proj", [128, 2792])
        sq = sbt(st, "sq", [128, 1024]); nrm = sbt(st, "nrm", [128, 1024])
        s8 = sbt(st, "s8", [128, 8]); r8 = sbt(st, "r8", [128, 8])
        rp = [sbt(st, "rp%d" % i, [128, 8, 32]) for i in range(4)]
        tokb = sbt(st, "tokb", [128, 768], BF16)
        kib = sbt(st, "kib", [128, 128], BF16)
        cqT = sbt(st, "cqT", [128, 3, 128], BF16); ckvT = sbt(st, "ckvT", [128, 2, 128], BF16)
        qmf = sbt(st, "qmf", [128, 768]); kvf = sbt(st, "kvf", [128, 8, 128]); kmpre = sbt(st, "kmpre", [128, 8, 96])
        vaug = sbt(st, "vaug", [128, 8, 65], BF16); vmaug = sbt(st, "vmaug", [128, 8, 65], BF16)
        qaT_t = sbt(st, "qaT_t", [128, 4, 128], BF16); kaT_t = sbt(st, "kaT_t", [128, 4, 128], BF16)
        qiT_t = sbt(st, "qiT_t", [128, 4, 128], BF16); kiT_t = sbt(st, "kiT_t", [128, 128], BF16)
        qmT_t = sbt(st, "qmT_t", [128, 8, 128], BF16); kmT_t = sbt(st, "kmT_t", [128, 8, 128], BF16)
        gS_t = sbt(st, "gS_t", [128, 16, 128], BF16)
        pT = pst(st, "pT", [128, 8, 128], BF16)
        pT2 = pst(st, "pT2", [128, 8, 128], BF16)
        pproj = [pst(st, "pproj%d" % i, [128, 512]) for i in range(2)]
        pgate = pst(st, "pgate", [128, 512])
        pm = pst(st, "pm", [128, 1024])
        memset(vaug[:], 1.0, ['vaug'], eng='pool')
        memset(vmaug[:], 1.0, ['vmaug'], eng='pool')

        def rms_heads(src3, H, Dh, gain, dst3, rs, ws, gname):
            shp = [128, H, Dh]
            sqv = sq[:, 0:H * Dh].rearrange("p (h d) -> p h d", h=H)
            tt(sqv, src3, src3, ALU.mult, rs, ['sq'])
            red(s8[:, 0:H], sqv, ALU.add, ['sq'], ['s8'])
            ts(s8[:, 0:H], s8[:, 0:H], float(1.0 / Dh), float(EPS), ALU.mult, ALU.add, ['s8'], ['s8'])
            act(s8[:, 0:H], s8[:, 0:H], AF.Sqrt, ['s8'], ['s8'])
            rcp(r8[:, 0:H], s8[:, 0:H], ['s8'], ['r8'])
            nv = nrm[:, 0:H * Dh].rearrange("p (h d) -> p h d", h=H)
            tt(nv, src3, bc2(r8[:, 0:H], shp), ALU.mult, rs + ['r8'], ['nrm'])
            tt(dst3, nv, bc1(gain[:], shp), ALU.mult, ['nrm', gname], ws)

        def rope(src3, H, hf, cosv, sinv, dst3, rs, ws, cname):
            shp = [128, H, hf]
            x1 = src3[:, :, 0:hf]; x2 = src3[:, :, hf:2 * hf]
            cb_ = bc1(cosv, shp); sb_ = bc1(sinv, shp)
            t = [rp[i][:, 0:H, 0:hf] for i in range(4)]
            tt(t[0], x1, cb_, ALU.mult, rs + ['cos' + cname], ['rp0'])
            tt(t[1], x2, sb_, ALU.mult, rs + ['sin' + cname], ['rp1'], eng='pool')
            tt(dst3[:, :, 0:hf], t[0], t[1], ALU.subtract, ['rp0', 'rp1'], ws)
            tt(t[2], x2, cb_, ALU.mult, rs + ['cos' + cname], ['rp2'])
            tt(t[3], x1, sb_, ALU.mult, rs + ['sin' + cname], ['rp3'], eng='pool')
            tt(dst3[:, :, hf:2 * hf], t[2], t[3], ALU.add, ['rp2', 'rp3'], ws)

        for i in range(nt1):
            hs = i % 2
            hTn = 'hT%d' % hs
            xs = i % 2
            xn = 'xt%d' % xs
            tok = slice(i * 128, (i + 1) * 128)
            dma(xt[xs][:], x_d[tok, :], [], [xn], xn)
            act(junkb[:], xt[xs][:], AF.Square, [xn], ['junkb', 'ssq'], accum_out=ssq[:])
            ts(ssq[:], ssq[:], float(1.0 / D), float(EPS), ALU.mult, ALU.add, ['ssq'], ['ssq'])
            act(ssq[:], ssq[:], AF.Sqrt, ['ssq'], ['ssq'])
            rcp(rstd[:], ssq[:], ['ssq'], ['rstd'])
            stt(htmp[:], xt[xs][:], rstd[:, 0:1], A1[:], ALU.mult, ALU.mult, [xn, 'rstd', 'A1'], ['htmp'])
            tt(hb[:], htmp[:], B1[:], ALU.add, ['htmp', 'B1'], ['hb'], eng='pool')
            for k in range(8):
                tr(pT[:, k, :], hb[:, k * 128:(k + 1) * 128], identb[:], ['hb', 'identb'], ['pT'])
            cp(hT[hs][:], pT[:], ['pT'], [hTn], eng='act')
            for cc in range(6):
                c0 = cc * 512
                c1 = min(c0 + 512, 2792)
                pp = cc % 2
                for k in range(8):
                    mm(pproj[pp][:, 0:c1 - c0], hT[hs][:, k, :], winb[:, k, c0:c1], k == 0, k == 7,
                       [hTn, 'winb'], ['pproj%d' % pp])
                cp(proj[:, c0:c1], pproj[pp][:, 0:c1 - c0], ['pproj%d' % pp], ['proj'], eng='act')
            for fc in range(16):
                c0 = 2792 + fc * 128
                for k in range(8):
                    mm(pgate[:, (fc % 4) * 128:(fc % 4 + 1) * 128], winb[:, k, c0:c0 + 128], hT[hs][:, k, :], k == 0, k == 7,
                       [hTn, 'winb'], ['pgate'])
                if fc % 4 == 3:
                    act(gS_t[:, fc - 3:fc + 1, :].rearrange("p c t -> p (c t)"), pgate[:], AF.Sigmoid, ['pgate'], ['gS_t'])
            dma(gS_d[:, :, tok].rearrange("c p t -> p c t"), gS_t[:], ['gS_t'], ['gS_d'], 'gS_t')
            cs64 = cos64[:, i, :]; sn64 = sin64[:, i, :]; cs32 = cos32[:, i, :]; sn32 = sin32[:, i, :]
            for (c0, gn, gt_, dstT, dn, dd_) in [(0, 'gaq', gaq, qaT_t, 'qaT_t', qaT_d), (512, 'gak', gak, kaT_t, 'kaT_t', kaT_d)]:
                src3 = proj[:, c0:c0 + 512].rearrange("p (h d) -> p h d", h=8)
                n3 = sq[:, 0:512].rearrange("p (h d) -> p h d", h=8)
                rms_heads(src3, 8, 64, gt_, n3, ['proj'], ['sq'], gn)
                rope(n3, 8, 32, cs64, sn64, tokb[:, 0:512].rearrange("p (h d) -> p h d", h=8), ['sq'], ['tokb'], '64')
                for k in range(4):
                    tr(pT2[:, k, :], tokb[:, k * 128:(k + 1) * 128], identb[:], ['tokb', 'identb'], ['pT2'])
                cp(dstT[:], pT2[:, 0:4, :], ['pT2'], [dn])
                dma(dd_[:, :, tok].rearrange("c p t -> p c t"), dstT[:], [dn], [dn + '_d'], dn)
            rope(proj[:, 1536:2048].rearrange("p (h d) -> p h d", h=8), 8, 32, cs64, sn64,
                 tokb[:, 0:512].rearrange("p (h d) -> p h d", h=8), ['proj'], ['tokb'], '64')
            for k in range(4):
                tr(pT2[:, k, :], tokb[:, k * 128:(k + 1) * 128], identb[:], ['tokb', 'identb'], ['pT2'])
            cp(qiT_t[:], pT2[:, 0:4, :], ['pT2'], ['qiT_t'])
            dma(qiT_d[:, :, tok].rearrange("c p t -> p c t"), qiT_t[:], ['qiT_t'], ['qiT_t_d'], 'qiT_t')
            n3 = sq[:, 0:64].rearrange("p (h d) -> p h d", h=1)
            rms_heads(proj[:, 2048:2112].rearrange("p (h d) -> p h d", h=1), 1, 64, gik, n3, ['proj'], ['sq'], 'gik')
            rope(n3, 1, 32, cs64, sn64, kib[:, 0:64].rearrange("p (h d) -> p h d", h=1), ['sq'], ['kib'], '64')
            cp(kib[:, 64:128], kib[:, 0:64], ['kib'], ['kib'])
            tr(pT2[:, 0, :], kib[:], identb[:], ['kib', 'identb'], ['pT2'])
            cp(kiT_t[:], pT2[:, 0, :], ['pT2'], ['kiT_t'])
            dma(kiT_d[:, tok], kiT_t[:], ['kiT_t'], ['kiT_t_d'], 'kiT_t')
            ts(wi_all[:, i, :], proj[:, 2112:2120], float(512 ** -0.5), None, ALU.mult, None, ['proj'], ['wi_all'])
            cp(vaug[:, :, 0:64], proj[:, 1024:1536].rearrange("p (h d) -> p h d", h=8), ['proj'], ['vaug'], eng='pool')
            dma(va_d[tok, :], vaug[:].rearrange("p h d -> p (h d)"), ['vaug'], ['va_d'], 'vaug')
            rms_heads(proj[:, 2120:2504].rearrange("p (h d) -> p h d", h=1), 1, 384, gmqa,
                      tokb[:, 0:384].rearrange("p (h d) -> p h d", h=1), ['proj'], ['tokb'], 'gmqa')
            for k in range(3):
                tr(pT2[:, k, :], tokb[:, k * 128:(k + 1) * 128], identb[:], ['tokb', 'identb'], ['pT2'])
            cp(cqT[:], pT2[:, 0:3, :], ['pT2'], ['cqT'])
            for (c0, c1) in [(0, 512), (512, 768)]:
                for k in range(3):
                    mm(pm[:, c0:c1], cqT[:, k, :], wmqb[:, k, c0:c1], k == 0, k == 2, ['cqT', 'wmqb'], ['pm'])
            cp(qmf[:], pm[:, 0:768], ['pm'], ['qmf'], eng='act')
            q3 = qmf[:].rearrange("p (h d) -> p h d", h=8)
            n96 = sq[:, 0:768].rearrange("p (h d) -> p h d", h=8)
            rms_heads(q3, 8, 96, gmq, n96, ['qmf'], ['sq'], 'gmq')
            tb96 = tokb[:, 0:768].rearrange("p (h d) -> p h d", h=8)
            cp(tb96[:, :, 0:64], n96[:, :, 0:64], ['sq'], ['tokb'], eng='pool')
            rope(n96[:, :, 64:96], 8, 16, cs32, sn32, tb96[:, :, 64:96], ['sq'], ['tokb'], '32')
            for h in range(8):
                tr(pT2[0:96, h, :], tokb[:, h * 96:(h + 1) * 96], identb[:], ['tokb', 'identb'], ['pT2'])
            cp(qmT_t[0:96, :, :], pT2[0:96, :, :], ['pT2'], ['qmT_t'])
            dma(qmT_d[:, :, tok].rearrange("h d t -> d h t"), qmT_t[0:96, :, :], ['qmT_t'], ['qmT_t_d'], 'qmT_t')
            rms_heads(proj[:, 2504:2760].rearrange("p (h d) -> p h d", h=1), 1, 256, gmkva,
                      tokb[:, 0:256].rearrange("p (h d) -> p h d", h=1), ['proj'], ['tokb'], 'gmkva')
            for k in range(2):
                tr(pT2[:, k, :], tokb[:, k * 128:(k + 1) * 128], identb[:], ['tokb', 'identb'], ['pT2'])
            cp(ckvT[:], pT2[:, 0:2, :], ['pT2'], ['ckvT'])
            for (c0, c1) in [(0, 512), (512, 1024)]:
                for k in range(2):
                    mm(pm[:, c0:c1], ckvT[:, k, :], wmkvb[:, k, c0:c1], k == 0, k == 1, ['ckvT', 'wmkvb'], ['pm'])
            cp(kvf[:].rearrange("p h d -> p (h d)"), pm[:], ['pm'], ['kvf'], eng='act')
            cp(kmpre[:, :, 0:64], kvf[:, :, 0:64], ['kvf'], ['kmpre'], eng='pool')
            cp(kmpre[:, :, 64:96], bc1(proj[:, 2760:2792], [128, 8, 32]), ['proj'], ['kmpre'], eng='pool')
            cp(vmaug[:, :, 0:64], kvf[:, :, 64:128], ['kvf'], ['vmaug'], eng='pool')
            dma(vm_d[tok, :], vmaug[:].rearrange("p h d -> p (h d)"), ['vmaug'], ['vm_d'], 'vmaug')
            rms_heads(kmpre[:], 8, 96, gmk, n96, ['kmpre'], ['sq'], 'gmk')
            cp(tb96[:, :, 0:64], n96[:, :, 0:64], ['sq'], ['tokb'], eng='pool')
            rope(n96[:, :, 64:96], 8, 16, cs32, sn32, tb96[:, :, 64:96], ['sq'], ['tokb'], '32')
            for h in range(8):
                tr(pT2[0:96, h, :], tokb[:, h * 96:(h + 1) * 96], identb[:], ['tokb', 'identb'], ['pT2'])
            cp(kmT_t[0:96, :, :], pT2[0:96, :, :], ['pT2'], ['kmT_t'])
            dma(kmT_d[:, :, tok].rearrange("h d t -> d h t"), kmT_t[0:96, :, :], ['kmT_t'], ['kmT_t_d'], 'kmT_t')
        P.barrier()
    st01.close()

    def attention_phase(tag, dsa):
        with contextlib.ExitStack() as st:
            ones64 = sbt(st, "ones64" + tag, [128, 64])
            memset(ones64[:], 1.0, ['ones64'])
            if dsa:
                kT = sbt(st, "kaT", [128, 4, S], BF16)
                kiT = sbt(st, "kiT", [128, S], BF16)
                for c in range(4):
                    dma(kT[:, c, :], kaT_d[c], ['kaT_t_d'], ['kT%d' % c], 'kT%d' % c)
                dma(kiT[:], kiT_d, ['kiT_t_d'], ['kiT'], 'kiT')
                v_src = va_d
                qT_g = sbt(st, "qaTg", [128, 4, 512], BF16)
                qiTg = [sbt(st, "qiTg%d" % i, [128, 4, 512], BF16) for i in range(2)]
                score = sbt(st, "score", [128, S])
                junk = sbt(st, "junkc", [128, S], BF16)
                bias_g = [sbt(st, "bias_g%d" % i, [128, 4, S], BF16) for i in range(2)]
                dg = sbt(st, "dg", [128, 8, 128], BF16)
                rbuf = [sbt(st, "rbuf%d" % i, [128, 512], BF16) for i in range(2)]
                lo = sbt(st, "lo", [128, 1]); hi = sbt(st, "hi", [128, 1]); mid = sbt(st, "mid", [128, 1])
                wv = sbt(st, "wv", [128, NITER + 1]); cnt = sbt(st, "cnt", [128, 1]); stp = sbt(st, "stp", [128, 1])
                pd = [pst(st, "pd%d" % i, [128, 512]) for i in range(2)]
                psc = pst(st, "psc", [128, 512])
                scale = 64 ** -0.5
                Kd = 64
                out_d = atA_d
            else:
                kT = sbt(st, "kmT", [128, 8, S], BF16)
                for h in range(8):
                    dma(kT[0:96, h, :], kmT_d[h], ['kmT_t_d'], ['kT%d' % h], 'kT%d' % h)
                v_src = vm_d
                qT_g = sbt(st, "qmTg", [128, 8, 512], BF16)
                scale = 96 ** -0.5
                Kd = 96
                out_d = atM_d
            vv = sbt(st, "vv" + tag, [128, NT, 520], BF16)
            v_v = v_src.rearrange("(t p) c -> p t c", p=128)
            for q in range(4):
                dma(vv[:, q * 8:(q + 1) * 8, :], v_v[:, q * 8:(q + 1) * 8, :], ['va_d', 'vm_d'], ['vv%d' % q], 'vv%d' % q)
            vres = ['vv%d' % q for q in range(4)]
            ptb = [sbt(st, "ptb%d%s" % (i, tag), [128, 512], BF16) for i in range(2)]
            rz = sbt(st, "rz" + tag, [128, 512]); of = sbt(st, "of" + tag, [128, 512])
            yb = [sbt(st, "yb%d%s" % (i, tag), [128, 512], BF16) for i in range(2)]
            pS = [pst(st, "pS%d%s" % (i, tag), [128, 512]) for i in range(2)]
            pO = [pst(st, "pO%d%s" % (i, tag), [128, 512]) for i in range(2)]
            pB = pst(st, "pB" + tag, [128, 512])
            kres = ['kT%d' % c for c in range(8)]
            ctr = 0
            def idx_block(g, b):
                bg = bias_g[g % 2]
                bgn = 'bias_g%d' % (g % 2)
                jq = 4 * g + b
                Sp = (jq + 1) * 128
                shp = [128, 8, 128]
                tt(dg[:], bc1(identb[:], shp), bc2(wi_all[:, jq, :], shp), ALU.mult, ['identb', 'wi_all'], ['dg'])
                items_i = [(c, h) for c in range(g + 1) for h in range(8)]
                qn = 'qiTg%d' % (g % 2)
                qi_ = qiTg[g % 2]

                def idx_dots(k_):
                    c, h = items_i[k_]
                    wc = 512 if c < g else (b + 1) * 128
                    k0 = c * 512
                    hp = h % 2; hc = h // 2
                    prt = slice(hp * 64, hp * 64 + 64)
                    dd = k_ % 2
                    mm(pd[dd][:, 0:wc], qi_[prt, hc, b * 128:(b + 1) * 128], kiT[prt, k0:k0 + wc], True, True,
                       [qn, 'kiT'], ['pd%d' % dd])
                    act(rbuf[dd][:, 0:wc], pd[dd][:, 0:wc], AF.Relu, ['pd%d' % dd], ['rbuf%d' % dd])

                def idx_acc(k_):
                    c, h = items_i[k_]
                    wc = 512 if c < g else (b + 1) * 128
                    k0 = c * 512
                    dd = k_ % 2
                    mm(psc[:, 0:wc], dg[:, h, :], rbuf[dd][:, 0:wc], h == 0, h == 7, ['dg', 'rbuf%d' % dd], ['psc'])
                    if h == 7:
                        if c < g:
                            cp(score[:, k0:k0 + 512], psc[:], ['psc'], ['score'])
                        else:
                            if b > 0:
                                cp(score[:, k0:k0 + b * 128], psc[:, 0:b * 128], ['psc'], ['score'])
                            tt(score[:, jq * 128:(jq + 1) * 128], psc[:, b * 128:(b + 1) * 128], trib[:], ALU.add,
                               ['psc', 'trib'], ['score'])
                idx_dots(0)
                for k_ in range(len(items_i)):
                    if k_ + 1 < len(items_i):
                        idx_dots(k_ + 1)
                    idx_acc(k_)
                if jq >= 2:
                    red(hi[:], score[:, 0:Sp], ALU.max, ['score'], ['hi'])
                    red(lo[:], score[:, 0:jq * 128], ALU.min, ['score'], ['lo'])
                    tt(stp[:], hi[:], lo[:], ALU.subtract, ['hi', 'lo'], ['stp'])
                    for k in range(NITER + 1):
                        ts(wv[:, k:k + 1], stp[:], float(2.0 ** -(k + 1)), None, ALU.mult, None, ['stp'], ['wv'])
                    tt(mid[:], lo[:], wv[:, 0:1], ALU.add, ['lo', 'wv'], ['mid'])
                    for k in range(NITER):
                        ts(junk[:, 0:Sp], score[:, 0:Sp], mid[:, 0:1], 0.0, ALU.is_ge, ALU.add, ['score', 'mid'],
                           ['junk', 'cnt'], accum_out=cnt[:])
                        ts(stp[:], cnt[:], 255.5, wv[:, k:k + 1], ALU.is_ge, ALU.mult, ['cnt', 'wv'], ['stp'])
                        tt(lo[:], lo[:], stp[:], ALU.add, ['lo', 'stp'], ['lo'])
                        tt(mid[:], lo[:], wv[:, k + 1:k + 2], ALU.add, ['lo', 'wv'], ['mid'])
                else:
                    memset(lo[:], -10000.0, ['lo'])
                ts(bg[:, b, 0:Sp], score[:, 0:Sp], lo[:, 0:1], NEG, ALU.is_lt, ALU.mult, ['score', 'lo'], [bgn])

            def att_part(g, heads):
                gsl = slice(g * 512, (g + 1) * 512)
                nsc = 4 * g + 4
                items_a = [(h, sc) for h in heads for sc in range(nsc)]
                if dsa:
                    bg = bias_g[g % 2]
                    bgn = 'bias_g%d' % (g % 2)

                def att_S(k_):
                    h, sc = items_a[k_]
                    r_ = sc - 4 * g
                    qlo = max(r_, 0) * 128
                    ss = k_ % 2
                    ssl = slice(sc * 128, (sc + 1) * 128)
                    if dsa:
                        hp = h % 2; hc = h // 2
                        prt = slice(hp * 64, hp * 64 + 64)
                        mm(pS[ss][:, qlo:512], kT[prt, hc, ssl], qT_g[prt, hc, qlo:512], True, False,
                           ['kT%d' % hc, 'qT_g'], ['pS%d' % ss])
                        b0 = max(r_, 0)
                        for b in range(b0, 4):
                            mm(pS[ss][:, b * 128:(b + 1) * 128], bg[:, b, ssl], identb[:], False, b == 3,
                               [bgn, 'identb'], ['pS%d' % ss])
                    else:
                        last = r_ < 0
                        mm(pS[ss][:, qlo:512], kT[0:96, h, ssl], qT_g[0:96, h, qlo:512], True, last,
                           ['kT%d' % h, 'qT_g'], ['pS%d' % ss])
                        if r_ >= 0:
                            mm(pS[ss][:, qlo:qlo + 128], trib[:], identb[:], False, True, ['trib', 'identb'], ['pS%d' % ss])
                    act(ptb[ss][:, qlo:512], pS[ss][:, qlo:512], AF.Exp, ['pS%d' % ss], ['ptb%d' % ss], scale=float(scale))

                def att_PV(k_):
                    h, sc = items_a[k_]
                    r_ = sc - 4 * g
                    qlo = max(r_, 0) * 128
                    ss = k_ % 2
                    po = h % 2
                    pon = 'pO%d' % po
                    mm(pO[po][0:65, qlo:512], vv[:, sc, h * 65:(h + 1) * 65], ptb[ss][:, qlo:512], sc == 0, sc == nsc - 1,
                       ['ptb%d' % ss] + vres, [pon])
                    if sc == nsc - 1:
                        rcp(rz[64:65, :], pO[po][64:65, :], [pon], ['rz'])
                        mm(pB[0:64, :], ones64[64:65, 0:64], rz[64:65, :], True, True, ['ones64', 'rz'], ['pB'])
                        cp(of[0:64, :], pO[po][0:64, :], [pon], ['of'], eng='act')
                        ybn = 'yb%d' % po
                        tt(yb[po][0:64, :], of[0:64, :], pB[0:64, :], ALU.mult, ['of', 'pB'], [ybn])
                        dma(out_d[h, :, gsl], yb[po][0:64, :], [ybn], ['at_d' + tag], ybn + tag)
                att_S(0)
                for k_ in range(len(items_a)):
                    if k_ + 1 < len(items_a):
                        att_S(k_ + 1)
                    att_PV(k_)

            def load_q(g):
                gsl = slice(g * 512, (g + 1) * 512)
                if dsa:
                    dma(qT_g[:], qaT_d[:, :, gsl].rearrange("c p t -> p c t"), ['qaT_t_d'], ['qT_g'], 'qT_g')
                else:
                    dma(qT_g[0:96, :, :], qmT_d[:, :, gsl].rearrange("h d t -> d h t"), ['qmT_t_d'], ['qT_g'], 'qT_g')

            def load_qi(g):
                gsl = slice(g * 512, (g + 1) * 512)
                dma(qiTg[g % 2][:], qiT_d[:, :, gsl].rearrange("c p t -> p c t"), ['qiT_t_d'], ['qiTg%d' % (g % 2)], 'qiTg%d' % (g % 2))

            if dsa:
                load_qi(0)
                for b in range(4):
                    idx_block(0, b)
                for g in range(NG):
                    load_q(g)
                    if g + 1 < NG:
                        load_qi(g + 1)
                    for part in range(4):
                        if g + 1 < NG:
                            idx_block(g + 1, part)
                        att_part(g, [2 * part, 2 * part + 1])
            else:
                stgc = [sbt(st, "stgc%d" % i, [128, 4096]) for i in range(2)]
                cvb = [sbt(st, "cvb%d" % i, [128, 4, D], BF16) for i in range(3)]
                uv_v = uv_d.rearrange("(p r) d -> p r d", p=128)
                conv = [(tsrc, col, rc) for (tsrc, col) in [(pu_d, 0), (pv_d, D)] for rc in range(32)]

                def conv_iter(it):
                    tsrc, col, rc = conv[it]
                    t_v = tsrc.rearrange("(p r) d -> p r d", p=128)
                    sl = it % 2
                    cs_ = it % 3
                    sv = stgc[sl][:].rearrange("p (r d) -> p r d", r=4)
                    dma(sv, t_v[:, rc * 4:(rc + 1) * 4, :], [], ['stgc%d' % sl], 'stgc%d' % sl)
                    cp(cvb[cs_][:], sv, ['stgc%d' % sl], ['cvb%d' % cs_], eng=('pool', 'dve')[it % 2])
                    dma(uv_v[:, rc * 4:(rc + 1) * 4, col:col + D], cvb[cs_][:], ['cvb%d' % cs_], ['uv_d%d' % cs_], 'cvb%d' % cs_)
                for g in range(NG):
                    load_q(g)
                    for it in range(g * 8, g * 8 + 8):
                        conv_iter(it)
                    att_part(g, list(range(8)))
            P.barrier()

    if phases >= 2:
        attention_phase("A", True)
    if phases >= 3:
        attention_phase("M", False)

    if phases >= 4:
        with contextlib.ExitStack() as st:
            woab = sbt(st, "woab", [128, 4, D], BF16); womb = sbt(st, "womb", [128, 4, D], BF16)
            woutb = sbt(st, "woutb", [128, 8, D], BF16); wpqb = sbt(st, "wpqb", [128, 8, 2048], BF16)
            subkT = sbt(st, "subkT", [128, 16, 128], BF16)
            iota16 = sbt(st, "iota16", [128, 16])
            G1 = sbt(st, "G1p", [128, D]); A2 = sbt(st, "A2p", [128, D]); B2 = sbt(st, "B2p", [128, D]); G2 = sbt(st, "G2p", [128, D])
            dma(iota16[:], iota16_d, [], ['iota16'], 'iota16')
            for qi_, (tn_, nm_) in enumerate([(G1, 'G1'), (A2, 'A2'), (B2, 'B2'), (G2, 'G2')]):
                dma(tn_[:], mod_d[qi_], ['mod_d'], [nm_], 'ld' + nm_)
            pk = pst(st, "pk", [128, 512])
            pT4 = pst(st, "pT4", [128, 8, 128], BF16)
            ph2 = [pst(st, "ph2_%d" % i, [128, 1024]) for i in range(2)]
            pacc = pst(st, "pacc", [128, 1024])
            with contextlib.ExitStack() as st2:
                stg = [sbt(st2, "stg%d" % i, [128, 4096]) for i in range(2)]
                skb = sbt(st2, "skb", [128, 16, 128], BF16)
                ci = 0
                for (wsrc, wdst, nk, ncol, nm) in [(woa_d, woab, 4, 1024, 'woab'), (wom_d, womb, 4, 1024, 'womb'),
                                                    (wout_d, woutb, 8, 1024, 'woutb'), (wpq_d, wpqb, 8, 2048, 'wpqb')]:
                    wv_ = wsrc.rearrange("(k p) n -> p k n", p=128)
                    cw = 4096 // nk
                    for c0 in range(0, ncol, cw):
                        sl = ci % 2
                        sv = stg[sl][:, 0:nk * cw].rearrange("p (k n) -> p k n", k=nk)
                        dma(sv, wv_[:, :, c0:c0 + cw], [], ['stg%d' % sl], 'stg%d' % sl)
                        cp(wdst[:, :, c0:c0 + cw], sv, ['stg%d' % sl], [nm], eng=('pool' if ci % 2 == 0 else 'act'))
                        ci += 1
                sl = ci % 2
                sv = stg[sl][:, 0:2048].rearrange("p (c d) -> p c d", c=16)
                dma(sv, subk_d.rearrange("c n d -> n c d"), [], ['stg%d' % sl], 'stg%d' % sl)
                cp(skb[:], sv, ['stg%d' % sl], ['skb'])
                for c in range(16):
                    tr(pT4[:, c % 8, :], skb[:, c, :], identb[:], ['skb', 'identb'], ['pT4'])
                    if c % 8 == 7:
                        cp(subkT[:, c - 7:c + 1, :], pT4[:], ['pT4'], ['subkT'])
                P.barrier()

            def sb4(name, shape, dt=F32):
                return sbt(st, name, shape, dt)
            aT_t = sb4("aT_t", [128, 4, 128], BF16); mT_t = sb4("mT_t", [128, 4, 128], BF16)
            gS4 = sb4("gS4", [128, 16, 128], BF16)
            t1 = sb4("t1", [128, 128]); t2 = sb4("t2", [128, 128])
            mixT = sb4("mixT", [128, 8, 128], BF16)
            xt2 = sb4("xt2", [128, D]); tmpx = sb4("tmpx", [128, D])
            x1 = [sb4("x1_%d" % i, [128, D]) for i in range(2)]
            junk4 = sb4("junk4", [128, D], BF16)
            ssq = sb4("ssq4", [128, 1]); rstd = sb4("rstd4", [128, 1])
            h2b = sb4("h2b", [128, D], BF16); h2T = sb4("h2T", [128, 8, 128], BF16)
            qpT = sb4("qpT", [128, 16, 128], BF16)
            s_all = sb4("s_all", [128, 16, 128]); s_wk = sb4("s_wk", [128, 128])
            tops = sb4("tops", [128, 16, 16]); topi = sb4("topi", [128, 16, 16], U32); topf = sb4("topf", [128, 16, 16])
            cand = s_all[:].rearrange("p (h two) n -> p h (two n)", two=2)
            cwk = sb4("cwk", [128, 256])
            best = sb4("best", [128, 8, 16]); bpos = sb4("bpos", [128, 8, 16], U32)
            ak = sb4("ak", [128, 8, 16], U32); bk = sb4("bk", [128, 8, 16], U32)
            akf = sb4("akf", [128, 8, 16]); bkf = sb4("bkf", [128, 8, 16])
            oh = s_all[:].rearrange("p c (a b) -> p (c a) b", b=16).rearrange("p (h k) a -> p h k a", h=8)
            i0 = sb4("i0", [128, 8, 16]); i1 = sb4("i1", [128, 8, 16])
            idf = sb4("idf", [128, 128])
            ids = [sb4("ids%d" % i, [128, 128], U32) for i in range(2)]
            gate = [sb4("gate%d" % i, [128, 8, 16]) for i in range(2)]
            gz = sb4("gz", [128, 8]); ngmax = sb4("ngmax", [128, 8])
            actv = [sb4("actv%d" % i, [128, 128]) for i in range(2)]
            coef = [sb4("coef%d" % i, [128, 128]) for i in range(2)]
            gtmp = [sb4("gtmp%d" % i, [128, 128]) for i in range(2)]
            NRING = 12
            uvb = [sb4("uvb%d" % i, [128, 2 * D], BF16) for i in range(NRING)]
            dgb = [sb4("dgb%d" % i, [128, 128], BF16) for i in range(4)]
            junkf = sb4("junkf", [128, D], BF16)
            ob = xt2
            GELU_S = float(2.0 * np.sqrt(2.0 / np.pi))
            shp4 = [128, 8, 16, 16]

            def prologue(i):
                p = i % 2
                tok = slice(i * 128, (i + 1) * 128)
                X1 = 'x1_%d' % p
                PH = 'ph2_%d' % p
                dma(aT_t[:], atA_d[:, :, tok].rearrange("(c two) d t -> (two d) c t", two=2), ['at_dA'], ['aT_t'], 'aT_t')
                dma(mT_t[:], atM_d[:, :, tok].rearrange("(c two) d t -> (two d) c t", two=2), ['at_dM'], ['mT_t'], 'mT_t')
                dma(gS4[:], gS_d[:, :, tok].rearrange("c p t -> p c t"), ['gS_d'], ['gS4'], 'gS4')
                dma(xt2[:], x_d[tok, :], [], ['xt2'], 'xt2')
                for fc in range(8):
                    fsl = slice(fc * 128, (fc + 1) * 128)
                    for k in range(4):
                        mm(pk[:, 0:128], woab[:, k, fsl], aT_t[:, k, :], k == 0, k == 3, ['woab', 'aT_t'], ['pk'])
                    for k in range(4):
                        mm(pk[:, 128:256], womb[:, k, fsl], mT_t[:, k, :], k == 0, k == 3, ['womb', 'mT_t'], ['pk'])
                    tt(t1[:], pk[:, 0:128], gS4[:, fc, :], ALU.mult, ['pk', 'gS4'], ['t1'])
                    tt(t2[:], pk[:, 128:256], gS4[:, 8 + fc, :], ALU.mult, ['pk', 'gS4'], ['t2'])
                    tt(mixT[:, fc, :], t1[:], t2[:], ALU.add, ['t1', 't2'], ['mixT'])
                    if fc % 2 == 1:
                        yield
                for hf in range(2):
                    hsl = slice(hf * 512, (hf + 1) * 512)
                    for k in range(8):
                        mm(pk[:], mixT[:, k, :], woutb[:, k, hsl], k == 0, k == 7, ['mixT', 'woutb'], ['pk'])
                    tt(tmpx[:, hsl], pk[:], G1[:, hsl], ALU.mult, ['pk', 'G1'], ['tmpx'])
                tt(x1[p][:], tmpx[:], xt2[:], ALU.add, ['tmpx', 'xt2'], [X1])
                yield
                act(junk4[:], x1[p][:], AF.Square, [X1], ['junk4', 'ssq4'], accum_out=ssq[:])
                ts(ssq[:], ssq[:], float(1.0 / D), float(EPS), ALU.mult, ALU.add, ['ssq4'], ['ssq4'])
                act(ssq[:], ssq[:], AF.Sqrt, ['ssq4'], ['ssq4'])
                rcp(rstd[:], ssq[:], ['ssq4'], ['rstd4'])
                stt(tmpx[:], x1[p][:], rstd[:, 0:1], A2[:], ALU.mult, ALU.mult, [X1, 'rstd4', 'A2'], ['tmpx'])
                tt(ph2[p][:], tmpx[:], B2[:], ALU.add, ['tmpx', 'B2'], [PH])
                cp(h2b[:], ph2[p][:], [PH], ['h2b'], eng='act')
                for k in range(8):
                    tr(pT4[:, k, :], h2b[:, k * 128:(k + 1) * 128], identb[:], ['h2b', 'identb'], ['pT4'])
                cp(h2T[:], pT4[:], ['pT4'], ['h2T'], eng='act')
                yield
                for c4 in range(4):
                    for cc in range(4):
                        c = c4 * 4 + cc
                        for k in range(8):
                            mm(pk[:, cc * 128:(cc + 1) * 128], wpqb[:, k, c * 128:(c + 1) * 128], h2T[:, k, :],
                               k == 0, k == 7, ['wpqb', 'h2T'], ['pk'])
                    cp(qpT[:, c4 * 4:(c4 + 1) * 4, :].rearrange("p c t -> p (c t)"), pk[:], ['pk'], ['qpT'], eng='act')
                    yield
                for c4 in range(4):
                    for cc in range(4):
                        c = c4 * 4 + cc
                        mm(pk[:, cc * 128:(cc + 1) * 128], qpT[:, c, :], subkT[:, c, :], True, True, ['qpT', 'subkT'], ['pk'])
                    cp(s_all[:, c4 * 4:(c4 + 1) * 4, :].rearrange("p c t -> p (c t)"), pk[:], ['pk'], ['s_all'], eng='act')
                yield
                for c in range(16):
                    sv_ = s_all[:, c, :]
                    P.op('dve', lambda e, c=c, sv_=sv_: e.max(out=tops[:, c, 0:8], in_=sv_), ['s_all'], ['tops'])
                    P.op('dve', lambda e, c=c, sv_=sv_: e.max_index(out=topi[:, c, 0:8], in_max=tops[:, c, 0:8], in_values=sv_),
                         ['s_all', 'tops'], ['topi'])
                    P.op('dve', lambda e, c=c, sv_=sv_: e.match_replace(out=s_wk[:], in_to_replace=tops[:, c, 0:8], in_values=sv_,
                                                                    imm_value=-1e30), ['s_all', 'tops'], ['s_wk'])
                    P.op('dve', lambda e, c=c: e.max(out=tops[:, c, 8:16], in_=s_wk[:]), ['s_wk'], ['tops'])
                    P.op('dve', lambda e, c=c: e.max_index(out=topi[:, c, 8:16], in_max=tops[:, c, 8:16], in_values=s_wk[:]),
                         ['s_wk', 'tops'], ['topi'])
                    if c % 2 == 1:
                        yield
                cp(topf[:], topi[:], ['topi'], ['topf'])
                t4 = tops[:].rearrange("p (h two) k -> p h two k", two=2)
                c4v = cand.rearrange("p h (a b) -> p h a b", a=16)
                tt(c4v, t4[:, :, 0, :].unsqueeze(3).to_broadcast(shp4), t4[:, :, 1, :].unsqueeze(2).to_broadcast(shp4), ALU.add,
                   ['tops'], ['s_all'])
                for h in range(8):
                    cv = cand[:, h, :]
                    P.op('dve', lambda e, h=h, cv=cv: e.max(out=best[:, h, 0:8], in_=cv), ['s_all'], ['best'])
                    P.op('dve', lambda e, h=h, cv=cv: e.max_index(out=bpos[:, h, 0:8], in_max=best[:, h, 0:8], in_values=cv),
                         ['s_all', 'best'], ['bpos'])
                    P.op('dve', lambda e, h=h, cv=cv: e.match_replace(out=cwk[:], in_to_replace=best[:, h, 0:8], in_values=cv,
                                                                    imm_value=-1e30), ['s_all', 'best'], ['cwk'])
                    P.op('dve', lambda e, h=h: e.max(out=best[:, h, 8:16], in_=cwk[:]), ['cwk'], ['best'])
                    P.op('dve', lambda e, h=h: e.max_index(out=bpos[:, h, 8:16], in_max=best[:, h, 8:16], in_values=cwk[:]),
                         ['cwk', 'best'], ['bpos'])
                    if h % 2 == 1:
                        yield
                ts(ak[:], bpos[:], 4, None, ALU.logical_shift_right, None, ['bpos'], ['ak'])
                ts(bk[:], bpos[:], 15, None, ALU.bitwise_and, None, ['bpos'], ['bk'])
                cp(akf[:], ak[:], ['ak'], ['akf'])
                cp(bkf[:], bk[:], ['bk'], ['bkf'])
                tf4 = topf[:].rearrange("p (h two) k -> p h two k", two=2)
                io4 = iota16[:].unsqueeze(1).unsqueeze(1).to_broadcast(shp4)
                for (kf_, half, dsti, nm) in [(akf, 0, i0, 'i0'), (bkf, 1, i1, 'i1')]:
                    tt(oh, kf_[:].unsqueeze(3).to_broadcast(shp4), io4, ALU.is_equal, ['akf', 'bkf', 'iota16'], ['s_all'])
                    tt(oh, oh, tf4[:, :, half, :].unsqueeze(2).to_broadcast(shp4), ALU.mult, ['s_all', 'topf'], ['s_all'])
                    red(dsti[:], oh, ALU.add, ['s_all'], [nm])
                    yield
                stt(idf[:].rearrange("p (h k) -> p h k", h=8), i0[:], 128.0, i1[:], ALU.mult, ALU.add, ['i0', 'i1'], ['idf'])
                cp(ids[p][:], idf[:], ['idf'], ['ids%d' % p])
                gt_ = gate[p]; GN = 'gate%d' % p
                ts(ngmax[:], best[:, :, 0], -1.0, None, ALU.mult, None, ['best'], ['ngmax'])
                tt(gt_[:], best[:], bc2(ngmax[:], [128, 8, 16]), ALU.add, ['best', 'ngmax'], [GN])
                act(gt_[:], gt_[:], AF.Exp, [GN], [GN])
                red(gz[:], gt_[:], ALU.add, [GN], ['gz'])
                rcp(gz[:], gz[:], ['gz'], ['gz'])
                tt(gt_[:], gt_[:], bc2(gz[:], [128, 8, 16]), ALU.mult, [GN, 'gz'], [GN])

            NTOT = NT * 32

            def pg_gather(G):
                i, gi = divmod(G, 32)
                p = i % 2
                for q_ in range(4):
                    sl_ = gi * 4 + q_
                    rb = (G * 4 + q_) % NRING
                    un = 'uvb%d' % rb
                    P.dma('pool', lambda e, sl_=sl_, rb=rb, p=p: e.indirect_dma_start(
                        out=uvb[rb][:], out_offset=None, in_=uv_d,
                        in_offset=bass.IndirectOffsetOnAxis(ap=ids[p][:, sl_:sl_ + 1], axis=0)), ['ids%d' % p], [un], un)

            def pg_dot(G):
                i, gi = divmod(G, 32)
                p = i % 2
                for q_ in range(4):
                    sl_ = gi * 4 + q_
                    rb = (G * 4 + q_) % NRING
                    stt(junkf[:], uvb[rb][:, 0:D], 1.0, ph2[p][:], ALU.mult, ALU.mult, ['uvb%d' % rb, 'ph2_%d' % p],
                        ['junkf', 'actv%d' % p], accum_out=actv[p][:, sl_:sl_ + 1])

            def pg_coef(G):
                i, gi = divmod(G, 32)
                p = i % 2
                s4 = slice(gi * 4, gi * 4 + 4)
                a_ = actv[p][:, s4]; t_ = gtmp[p][:, s4]
                AN = 'actv%d' % p; TN = 'gtmp%d' % p
                gate_f = gate[p][:].rearrange("p h k -> p (h k)")
                stt(t_, a_, 0.044715, a_, ALU.mult, ALU.mult, [AN], [TN])
                stt(t_, t_, 1.0, a_, ALU.add, ALU.mult, [TN, AN], [TN])
                act(t_, t_, AF.Sigmoid, [TN], [TN], scale=GELU_S)
                tt(t_, t_, a_, ALU.mult, [TN, AN], [TN])
                tt(coef[p][:, s4], t_, gate_f[:, s4], ALU.mult, [TN, 'gate%d' % p], ['coef%d' % p])

            def pg_acc(G):
                i, gi = divmod(G, 32)
                p = i % 2
                for q_ in range(4):
                    sl_ = gi * 4 + q_
                    rb = (G * 4 + q_) % NRING
                    db = sl_ % 4
                    act(dgb[db][:], identb[:], AF.Identity, ['identb', 'coef%d' % p], ['dgb%d' % db], scale=coef[p][:, sl_:sl_ + 1])
                    for hf in range(2):
                        mm(pacc[:, hf * 512:(hf + 1) * 512], dgb[db][:], uvb[rb][:, D + hf * 512:D + (hf + 1) * 512],
                           sl_ == 0, sl_ == 127, ['dgb%d' % db, 'uvb%d' % rb], ['pacc'])

            def epilogue(i):
                p = i % 2
                tt(tmpx[:], pacc[:], G2[:], ALU.mult, ['pacc', 'G2'], ['tmpx'])
                tt(ob[:], tmpx[:], x1[p][:], ALU.add, ['tmpx', 'x1_%d' % p], ['xt2'])
                dma(out_d[i * 128:(i + 1) * 128, :], ob[:], ['xt2'], ['out_d'], 'ob')

            for _ in prologue(0):
                pass
            pg_gather(0); pg_gather(1); pg_gather(2)
            pg_dot(0)
            gen = None
            for G in range(NTOT):
                i, gi = divmod(G, 32)
                if gi == 0:
                    gen = prologue(i + 1) if i + 1 < NT else None
                if G + 1 < NTOT:
                    pg_dot(G + 1)
                pg_coef(G)
                pg_acc(G)
                if gi == 31:
                    epilogue(i)
                if gen is not None:
                    if gi < 27:
                        next(gen, None)
                    elif gi == 27:
                        for _ in gen:
                            pass
                if G + 3 < NTOT:
                    pg_gather(G + 3)
            P.barrier()

    P.emit(nc)
    top.close()
    return nc


def make_inputs(inputs):
    f = lambda a: np.ascontiguousarray(np.asarray(a))
    common = {}
    for k in ["g_norm1", "g_norm2", "b_ada", "g_a_q", "g_a_k", "g_idx_k", "g_mq_a", "g_mkv_a", "g_m_q", "g_m_k"]:
        common[k] = f(np.asarray(inputs[k], np.float32).reshape(1, -1))
    for k in ["w_ada", "w_in", "w_mq_up", "w_mkv_up", "w_o_a", "w_o_m", "w_out", "w_peer_q", "peer_u", "peer_v"]:
        common[k] = f(np.asarray(inputs[k], np.float32)[0])
    common["peer_subkeys"] = f(np.asarray(inputs["peer_subkeys"], np.float32)[0].reshape(16, 128, 128))
    common["identb"] = np.eye(128, dtype=np.float32).astype(ml_dtypes.bfloat16)
    q = np.arange(128)[:, None]; s = np.arange(128)[None, :]
    common["trib"] = np.where(s <= q, 0.0, NEG).astype(np.float32).astype(ml_dtypes.bfloat16)
    inv64 = (10000.0 ** (-(np.arange(32, dtype=np.float32)) / np.float32(32))).astype(np.float32)
    inv32 = (10000.0 ** (-(np.arange(16, dtype=np.float32)) / np.float32(16))).astype(np.float32)
    common["inv64"] = f(np.broadcast_to(inv64[None, :], (128, 32)))
    common["inv32"] = f(np.broadcast_to(inv32[None, :], (128, 16)))
    common["iota16"] = f(np.broadcast_to(np.arange(16, dtype=np.float32)[None, :], (128, 16)))
    x = np.asarray(inputs["x"], np.float32)
    c = np.asarray(inputs["c"], np.float32)
    pos = np.asarray(inputs["positions"], np.int32)
    maps = []
    for b in range(x.shape[0]):
        m = dict(common)
        m["x"] = f(x[b])
        m["c_col"] = f(c[b].reshape(8, 128).T)
        m["pos_t"] = f(pos[b].reshape(NT, 128).T)
        maps.append(m)
    return maps


def kernel(**inputs):
    maps = make_inputs(inputs)
    nc = build_nc()
    res = run_bass_kernel_spmd(nc, maps, core_ids=list(range(8)))
    return np.stack([np.asarray(r["out"], np.float32) for r in res.results], axis=0)
```

```python
import contextlib
import numpy as np
import ml_dtypes
import concourse.bass as bass
import concourse.mybir as mybir
from concourse.bass_utils import run_bass_kernel_spmd

F32 = mybir.dt.float32
BF16 = mybir.dt.bfloat16
I32 = mybir.dt.int32
U32 = mybir.dt.uint32
AF = mybir.ActivationFunctionType
ALU = mybir.AluOpType
AX = mybir.AxisListType

S = 4096
D = 1024
NT = 32
NG = 8
EPS = 1e-6
NEG = -30000.0
NITER = 18
NSLOT = 4
TWO_PI = float(2 * np.pi)
NO_SELF_WAIT = False


class Prog:
    ENG = ('pe', 'act', 'dve', 'pool', 'sp')

    def __init__(self):
        self.ops = {e: [] for e in self.ENG}
        self.cnt = {e: 0 for e in self.ENG}
        self.known = {e: {} for e in self.ENG}
        self.res = {}
        self.dma_cnt = {}

    def _deps(self, eng, reads, writes):
        need = {}

        def add(sk, v):
            if sk == ('eng', 'pe') and eng == 'pe':
                return
            if NO_SELF_WAIT and sk == ('eng', eng):
                return
            if v > need.get(sk, 0):
                need[sk] = v
        for k in reads:
            st = self.res.get(k)
            if st and st[0] is not None:
                add(*st[0])
        for k in writes:
            st = self.res.get(k)
            if st:
                if st[0] is not None:
                    add(*st[0])
                for sk, v in st[1].items():
                    add(sk, v)
        waits = []
        kn = self.known[eng]
        for sk, v in need.items():
            if kn.get(sk, 0) >= v:
                continue
            kn[sk] = v
            waits.append((sk, v))
        return waits

    def _commit(self, tok, reads, writes):
        sk, v = tok
        for k in reads:
            st = self.res.setdefault(k, [None, {}])
            if v > st[1].get(sk, 0):
                st[1][sk] = v
        for k in writes:
            self.res[k] = [tok, {}]

    def op(self, eng, fn, r=(), w=()):
        waits = self._deps(eng, r, w)
        self.cnt[eng] += 1
        tok = (('eng', eng), self.cnt[eng])
        self.ops[eng].append((waits, fn, ('eng', eng), 1))
        self._commit(tok, r, w)

    def dma(self, eng, fn, r=(), w=(), sem=None):
        waits = self._deps(eng, r, w)
        c = self.dma_cnt.get(sem, 0) + 16
        self.dma_cnt[sem] = c
        tok = (('dma', sem), c)
        self.ops[eng].append((waits, fn, ('dma', sem), 16))
        self._commit(tok, r, w)

    def barrier(self):
        allk = [(('eng', e), self.cnt[e]) for e in self.ENG if self.cnt[e] > 0]
        allk += [(('dma', k), c) for k, c in self.dma_cnt.items()]
        for e in self.ENG:
            waits = []
            kn = self.known[e]
            for sk, v in allk:
                if sk == ('eng', e):
                    continue
                if kn.get(sk, 0) >= v:
                    continue
                kn[sk] = v
                waits.append((sk, v))
            if waits:
                self.ops[e].append((waits, None, None, 0))

    def emit(self, nc):
        self.barrier()
        with contextlib.ExitStack() as st:
            sems = {}
            for e in self.ENG:
                sems[('eng', e)] = st.enter_context(nc.semaphore("s_" + e))
            for i, k in enumerate(self.dma_cnt):
                sems[('dma', k)] = st.enter_context(nc.semaphore("d%d" % i))
            block = st.enter_context(nc.Block())

            def run(e, name):
                for (waits, fn, sk, inc) in self.ops[name]:
                    for (wk, v) in waits:
                        e.wait_ge(sems[wk], v)
                    if fn is not None:
                        fn(e).then_inc(sems[sk], inc)

            @block.tensor
            def _(e):
                run(e, 'pe')

            @block.scalar
            def _(e):
                run(e, 'act')

            @block.vector
            def _(e):
                run(e, 'dve')

            @block.gpsimd
            def _(e):
                run(e, 'pool')

            @block.sync
            def _(e):
                run(e, 'sp')


def build_nc(debug=False, phases=4, nt1=NT, skip=(), nslot=NSLOT, gcols=D):
    nc = bass.Bass("TRN2", target_bir_lowering=False)
    P = Prog()

    def din(name, shape, dt=F32):
        return nc.dram_tensor(name, list(shape), dt, kind="ExternalInput").ap()

    def dscr(name, shape, dt=BF16):
        return nc.dram_tensor(name, list(shape), dt, kind="ExternalOutput" if debug else "Internal").ap()

    x_d = din("x", [S, D])
    ccol_d = din("c_col", [128, 8])
    pos_d = din("pos_t", [128, NT], I32)
    g1_d = din("g_norm1", [1, D]); g2_d = din("g_norm2", [1, D])
    wada_d = din("w_ada", [D, 6 * D]); bada_d = din("b_ada", [1, 6 * D])
    win_d = din("w_in", [D, 4840])
    gaq_d = din("g_a_q", [1, 64]); gak_d = din("g_a_k", [1, 64]); gik_d = din("g_idx_k", [1, 64])
    gmqa_d = din("g_mq_a", [1, 384]); wmq_d = din("w_mq_up", [384, 768])
    gmkva_d = din("g_mkv_a", [1, 256]); wmkv_d = din("w_mkv_up", [256, 1024])
    gmq_d = din("g_m_q", [1, 96]); gmk_d = din("g_m_k", [1, 96])
    woa_d = din("w_o_a", [512, D]); wom_d = din("w_o_m", [512, D]); wout_d = din("w_out", [D, D])
    wpq_d = din("w_peer_q", [D, 2048]); subk_d = din("peer_subkeys", [16, 128, 128])
    pu_d = din("peer_u", [16384, D]); pv_d = din("peer_v", [16384, D])
    identb_d = din("identb", [128, 128], BF16)
    trib_d = din("trib", [128, 128], BF16)
    inv64_d = din("inv64", [128, 32]); inv32_d = din("inv32", [128, 16])
    iota16_d = din("iota16", [128, 16])
    out_d = nc.dram_tensor("out", [S, D], F32, kind="ExternalOutput").ap()

    qaT_d = dscr("qaT_s", [4, 128, S]); kaT_d = dscr("kaT_s", [4, 128, S]); qiT_d = dscr("qiT_s", [4, 128, S])
    kiT_d = dscr("kiT_s", [128, S])
    va_d = dscr("va_s", [S, 520]); vm_d = dscr("vm_s", [S, 520])
    qmT_d = dscr("qmT_s", [8, 96, S]); kmT_d = dscr("kmT_s", [8, 96, S])
    gS_d = dscr("gS_s", [16, 128, S])
    atA_d = dscr("atA_s", [8, 64, S]); atM_d = dscr("atM_s", [8, 64, S])
    mod_d = dscr("mod_s", [4, 128, D], F32)
    uv_d = nc.dram_tensor("uv_s", [16384, 2048], BF16, kind="Internal").ap()

    top = contextlib.ExitStack()

    uid = [0]

    def sbt(st, name, shape, dt=F32):
        uid[0] += 1
        return st.enter_context(nc.sbuf_tensor("sb%d_%s" % (uid[0], name), list(shape), dt))

    def pst(st, name, shape, dt=F32):
        uid[0] += 1
        return st.enter_context(nc.psum_tensor("ps%d_%s" % (uid[0], name), list(shape), dt))

    def tt(out, in0, in1, op, r, w, eng='dve'):
        P.op(eng, lambda e: e.tensor_tensor(out=out, in0=in0, in1=in1, op=op), r, w)

    def ts(out, in0, s1, s2, op0, op1, r, w, eng='dve', accum_out=None):
        if op1 is None:
            P.op(eng, lambda e: e.tensor_scalar(out=out, in0=in0, scalar1=s1, scalar2=None, op0=op0), r, w)
        elif accum_out is None:
            P.op(eng, lambda e: e.tensor_scalar(out=out, in0=in0, scalar1=s1, scalar2=s2, op0=op0, op1=op1), r, w)
        else:
            P.op(eng, lambda e: e.tensor_scalar(out=out, in0=in0, scalar1=s1, scalar2=s2, op0=op0, op1=op1, accum_out=accum_out), r, w)

    def stt(out, in0, scalar, in1, op0, op1, r, w, accum_out=None):
        if accum_out is None:
            P.op('dve', lambda e: e.scalar_tensor_tensor(out=out, in0=in0, scalar=scalar, in1=in1, op0=op0, op1=op1), r, w)
        else:
            P.op('dve', lambda e: e.scalar_tensor_tensor(out=out, in0=in0, scalar=scalar, in1=in1, op0=op0, op1=op1, accum_out=accum_out), r, w)

    def act(out, in_, func, r, w, scale=None, bias=None, accum_out=None):
        kw = {}
        if scale is not None:
            kw['scale'] = scale
        if bias is not None:
            kw['bias'] = bias
        if accum_out is not None:
            kw['accum_out'] = accum_out
        P.op('act', lambda e: e.activation(out=out, in_=in_, func=func, **kw), r, w)

    def cp(out, in_, r, w, eng='dve'):
        if eng == 'act':
            act(out, in_, AF.Copy, r, w)
        else:
            P.op(eng, lambda e: e.tensor_copy(out=out, in_=in_), r, w)

    def red(out, in_, op, r, w):
        P.op('dve', lambda e: e.tensor_reduce(out=out, in_=in_, axis=AX.X, op=op), r, w)

    def rcp(out, in_, r, w):
        P.op('dve', lambda e: e.reciprocal(out=out, in_=in_), r, w)

    def mm(out, lhsT, rhs, start, stop, r, w):
        P.op('pe', lambda e: e.matmul(out=out, lhsT=lhsT, rhs=rhs, start=start, stop=stop), r, w)

    def tr(out, in_, ident, r, w):
        P.op('pe', lambda e: e.transpose(out=out, in_=in_, identity=ident), r, w)

    def dma(out, in_, r, w, sem, eng='sp'):
        P.dma(eng, lambda e: e.dma_start(out=out, in_=in_), r, w, sem)

    def memset(ap, val, w, eng='dve'):
        P.op(eng, lambda e: e.memset(ap, val), (), w)

    def bc1(ap, shape):
        return ap.unsqueeze(1).to_broadcast(shape)

    def bc2(ap, shape):
        return ap.unsqueeze(2).to_broadcast(shape)

    identb = sbt(top, "identb", [128, 128], BF16)
    trib = sbt(top, "trib", [128, 128], BF16)
    wi_all = sbt(top, "wi_all", [128, NT, 8])
    st01 = contextlib.ExitStack()
    A1 = sbt(st01, "A1", [128, D]); B1 = sbt(st01, "B1", [128, D])
    dma(identb[:], identb_d, [], ['identb'], 'identb')
    dma(trib[:], trib_d, [], ['trib'], 'trib')

    with contextlib.ExitStack() as st:
        ccol = sbt(st, "ccol", [128, 8]); cact = sbt(st, "cact", [128, 8])
        cb = sbt(st, "cb", [128, 8, 128])
        g1b = sbt(st, "g1b", [128, D]); g2b = sbt(st, "g2b", [128, D])
        G1 = sbt(st, "G1", [128, D]); A2 = sbt(st, "A2", [128, D]); B2 = sbt(st, "B2", [128, D]); G2 = sbt(st, "G2", [128, D])
        wa = [sbt(st, "wa%d" % i, [128, 8, 512]) for i in range(2)]
        bb = [sbt(st, "bb%d" % i, [128, 512]) for i in range(2)]
        tmp0 = sbt(st, "tmp0", [128, 512])
        pm0 = [pst(st, "pm0_%d" % i, [128, 512]) for i in range(2)]
        dma(ccol[:], ccol_d, [], ['ccol'], 'ccol')
        dma(g1b[:], g1_d.to_broadcast([128, D]), [], ['g1b'], 'g1b')
        dma(g2b[:], g2_d.to_broadcast([128, D]), [], ['g2b'], 'g2b')
        act(cact[:], ccol[:], AF.Silu, ['ccol'], ['cact'])
        cp(cb[:], cact[:].unsqueeze(2).to_broadcast([128, 8, 128]), ['cact'], ['cb'])
        wada_v = wada_d.rearrange("(k p) n -> p k n", p=128)
        dests = [B1, A1, G1, B2, A2, G2]
        dnames = ['B1', 'A1', 'G1', 'B2', 'A2', 'G2']
        for n in range(12):
            sl = n % 2
            dma(wa[sl][:], wada_v[:, :, n * 512:(n + 1) * 512], [], ['wa%d' % sl], 'wa%d' % sl)
            dma(bb[sl][:], bada_d[:, n * 512:(n + 1) * 512].to_broadcast([128, 512]), [], ['bb%d' % sl], 'bb%d' % sl)
            for k in range(8):
                mm(pm0[sl][:], cb[:, k, :], wa[sl][:, k, :], k == 0, k == 7, ['cb', 'wa%d' % sl], ['pm0_%d' % sl])
            which = n // 2
            dst = dests[which][:, (n % 2) * 512:(n % 2 + 1) * 512]
            dn = dnames[which]
            if which in (1, 4):
                gsrc = (g1b if which == 1 else g2b)[:, (n % 2) * 512:(n % 2 + 1) * 512]
                tt(tmp0[:], pm0[sl][:], bb[sl][:], ALU.add, ['pm0_%d' % sl, 'bb%d' % sl], ['tmp0'])
                stt(dst, tmp0[:], 1.0, gsrc, ALU.add, ALU.mult, ['tmp0', 'g1b', 'g2b'], [dn])
            else:
                tt(dst, pm0[sl][:], bb[sl][:], ALU.add, ['pm0_%d' % sl, 'bb%d' % sl], [dn])
        for qi_, (tn_, nm_) in enumerate([(G1, 'G1'), (A2, 'A2'), (B2, 'B2'), (G2, 'G2')]):
            dma(mod_d[qi_], tn_[:], [nm_], ['mod_d'], 'mod' + nm_)
        P.barrier()

    if phases == 0:
        P.emit(nc)
        st01.close()
        top.close()
        return nc

    with contextlib.ExitStack() as st:
        winb = sbt(st, "winb", [128, 8, 4840], BF16)
        wmqb = sbt(st, "wmqb", [128, 3, 768], BF16)
        wmkvb = sbt(st, "wmkvb", [128, 2, 1024], BF16)
        gaq = sbt(st, "gaq", [128, 64]); gak = sbt(st, "gak", [128, 64]); gik = sbt(st, "gik", [128, 64])
        gmqa = sbt(st, "gmqa", [128, 384]); gmkva = sbt(st, "gmkva", [128, 256])
        gmq = sbt(st, "gmq", [128, 96]); gmk = sbt(st, "gmk", [128, 96])
        cos64 = sbt(st, "cos64", [128, NT, 32]); sin64 = sbt(st, "sin64", [128, NT, 32])
        cos32 = sbt(st, "cos32", [128, NT, 16]); sin32 = sbt(st, "sin32", [128, NT, 16])
        for tns, src, nm in [(gaq, gaq_d, 'gaq'), (gak, gak_d, 'gak'), (gik, gik_d, 'gik'), (gmqa, gmqa_d, 'gmqa'),
                             (gmkva, gmkva_d, 'gmkva'), (gmq, gmq_d, 'gmq'), (gmk, gmk_d, 'gmk')]:
            dma(tns[:], src.to_broadcast(list(tns[:].shape)), [], [nm], nm)
        with contextlib.ExitStack() as st2:
            stage = [sbt(st2, "stage%d" % i, [128, 4096]) for i in range(2)]
            win_v = win_d.rearrange("(k p) n -> p k n", p=128)
            ci = 0
            for c0 in range(0, 4840, 512):
                c1 = min(c0 + 512, 4840)
                wdt = c1 - c0
                sl = ci % 2
                sv = stage[sl][:, 0:8 * wdt].rearrange("p (k n) -> p k n", k=8)
                dma(sv, win_v[:, :, c0:c1], [], ['stage%d' % sl], 'stage%d' % sl)
                cp(winb[:, :, c0:c1], sv, ['stage%d' % sl], ['winb'], eng=('pool' if ci % 2 == 0 else 'act'))
                ci += 1
            sv = stage[ci % 2][:, 0:3 * 768].rearrange("p (k n) -> p k n", k=3)
            dma(sv, wmq_d.rearrange("(k p) n -> p k n", p=128), [], ['stage%d' % (ci % 2)], 'stage%d' % (ci % 2))
            cp(wmqb[:], sv, ['stage%d' % (ci % 2)], ['wmqb'], eng='pool')
            ci += 1
            sv = stage[ci % 2][:, 0:2 * 1024].rearrange("p (k n) -> p k n", k=2)
            dma(sv, wmkv_d.rearrange("(k p) n -> p k n", p=128), [], ['stage%d' % (ci % 2)], 'stage%d' % (ci % 2))
            cp(wmkvb[:], sv, ['stage%d' % (ci % 2)], ['wmkvb'], eng='pool')
            posi = sbt(st2, "posi", [128, NT], I32); posf = sbt(st2, "posf", [128, NT])
            inv64 = sbt(st2, "inv64", [128, 32]); inv32 = sbt(st2, "inv32", [128, 16])
            rt_a = sbt(st2, "rt_a", [128, NT, 32]); rt_k = sbt(st2, "rt_k", [128, NT, 32])
            rt_i = sbt(st2, "rt_i", [128, NT, 32], I32); rt_y = sbt(st2, "rt_y", [128, NT, 32])
            dma(posi[:], pos_d, [], ['posi'], 'posi')
            dma(inv64[:], inv64_d, [], ['inv64'], 'inv64')
            dma(inv32[:], inv32_d, [], ['inv32'], 'inv32')
            cp(posf[:], posi[:], ['posi'], ['posf'])
            for (inv, hf, cs, sn, nm) in [(inv64, 32, cos64, sin64, '64'), (inv32, 16, cos32, sin32, '32')]:
                a = rt_a[:, :, 0:hf]; kk = rt_k[:, :, 0:hf]; ii = rt_i[:, :, 0:hf]; y = rt_y[:, :, 0:hf]
                shp = [128, NT, hf]
                tt(a, bc2(posf[:], shp), bc1(inv[:], shp), ALU.mult, ['posf', 'inv' + nm], ['rt_a'])
                ts(kk, a, float(1.0 / TWO_PI), None, ALU.mult, None, ['rt_a'], ['rt_k'])
                cp(ii, kk, ['rt_k'], ['rt_i'])
                cp(kk, ii, ['rt_i'], ['rt_k'])
                stt(a, kk, -TWO_PI, a, ALU.mult, ALU.add, ['rt_k', 'rt_a'], ['rt_a'])
                ts(kk, a, float(np.pi / 2), float(np.pi), ALU.add, ALU.is_gt, ['rt_a'], ['rt_k'])
                stt(y, kk, -TWO_PI, a, ALU.mult, ALU.add, ['rt_k', 'rt_a'], ['rt_y'])
                ts(y, y, float(np.pi / 2), None, ALU.add, None, ['rt_y'], ['rt_y'])
                ts(y, y, float(np.pi), float(-np.pi), ALU.min, ALU.max, ['rt_y'], ['rt_y'])
                ts(a, a, float(np.pi), float(-np.pi), ALU.min, ALU.max, ['rt_a'], ['rt_a'])
                act(sn[:], a, AF.Sin, ['rt_a'], ['sin' + nm])
                act(cs[:], y, AF.Sin, ['rt_y'], ['cos' + nm])
            P.barrier()

        xt = [sbt(st, "xt%d" % i, [128, D]) for i in range(2)]
        junkb = sbt(st, "junkb", [128, D], BF16)
        ssq = sbt(st, "ssq", [128, 1]); rstd = sbt(st, "rstd", [128, 1])
        htmp = sbt(st, "htmp", [128, D]); hb = sbt(st, "hb", [128, D], BF16)
        hT = [sbt(st, "hT%d" % i, [128, 8, 128], BF16) for i in range(2)]
        proj = sbt(st, "proj", [128, 2792])
        sq = sbt(st, "sq", [128, 1024]); nrm = sbt(st, "nrm", [128, 1024])
        s8 = sbt(st, "s8", [128, 8]); r8 = sbt(st, "r8", [128, 8])
        rp = [sbt(st, "rp%d" % i, [128, 8, 32]) for i in range(4)]
        tokb = sbt(st, "tokb", [128, 768], BF16)
        kib = sbt(st, "kib", [128, 128], BF16)
        cqT = sbt(st, "cqT", [128, 3, 128], BF16); ckvT = sbt(st, "ckvT", [128, 2, 128], BF16)
        qmf = sbt(st, "qmf", [128, 768]); kvf = sbt(st, "kvf", [128, 8, 128]); kmpre = sbt(st, "kmpre", [128, 8, 96])
        vaug = sbt(st, "vaug", [128, 8, 65], BF16); vmaug = sbt(st, "vmaug", [128, 8, 65], BF16)
        qaT_t = sbt(st, "qaT_t", [128, 4, 128], BF16); kaT_t = sbt(st, "kaT_t", [128, 4, 128], BF16)
        qiT_t = sbt(st, "qiT_t", [128, 4, 128], BF16); kiT_t = sbt(st, "kiT_t", [128, 128], BF16)
        qmT_t = sbt(st, "qmT_t", [128, 8, 128], BF16); kmT_t = sbt(st, "kmT_t", [128, 8, 128], BF16)
        gS_t = sbt(st, "gS_t", [128, 16, 128], BF16)
        pT = pst(st, "pT", [128, 8, 128], BF16)
        pT2 = pst(st, "pT2", [128, 8, 128], BF16)
        pproj = [pst(st, "pproj%d" % i, [128, 512]) for i in range(2)]
        pgate = pst(st, "pgate", [128, 512])
        pm = pst(st, "pm", [128, 1024])
        memset(vaug[:], 1.0, ['vaug'], eng='pool')
        memset(vmaug[:], 1.0, ['vmaug'], eng='pool')

        def rms_heads(src3, H, Dh, gain, dst3, rs, ws, gname):
            shp = [128, H, Dh]
            sqv = sq[:, 0:H * Dh].rearrange("p (h d) -> p h d", h=H)
            tt(sqv, src3, src3, ALU.mult, rs, ['sq'])
            red(s8[:, 0:H], sqv, ALU.add, ['sq'], ['s8'])
            ts(s8[:, 0:H], s8[:, 0:H], float(1.0 / Dh), float(EPS), ALU.mult, ALU.add, ['s8'], ['s8'])
            act(s8[:, 0:H], s8[:, 0:H], AF.Sqrt, ['s8'], ['s8'])
            rcp(r8[:, 0:H], s8[:, 0:H], ['s8'], ['r8'])
            nv = nrm[:, 0:H * Dh].rearrange("p (h d) -> p h d", h=H)
            tt(nv, src3, bc2(r8[:, 0:H], shp), ALU.mult, rs + ['r8'], ['nrm'])
            tt(dst3, nv, bc1(gain[:], shp), ALU.mult, ['nrm', gname], ws)

        def rope(src3, H, hf, cosv, sinv, dst3, rs, ws, cname):
            shp = [128, H, hf]
            x1 = src3[:, :, 0:hf]; x2 = src3[:, :, hf:2 * hf]
            cb_ = bc1(cosv, shp); sb_ = bc1(sinv, shp)
            t = [rp[i][:, 0:H, 0:hf] for i in range(4)]
            tt(t[0], x1, cb_, ALU.mult, rs + ['cos' + cname], ['rp0'])
            tt(t[1], x2, sb_, ALU.mult, rs + ['sin' + cname], ['rp1'], eng='pool')
            tt(dst3[:, :, 0:hf], t[0], t[1], ALU.subtract, ['rp0', 'rp1'], ws)
            tt(t[2], x2, cb_, ALU.mult, rs + ['cos' + cname], ['rp2'])
            tt(t[3], x1, sb_, ALU.mult, rs + ['sin' + cname], ['rp3'], eng='pool')
            tt(dst3[:, :, hf:2 * hf], t[2], t[3], ALU.add, ['rp2', 'rp3'], ws)

        for i in range(nt1):
            hs = i % 2
            hTn = 'hT%d' % hs
            xs = i % 2
            xn = 'xt%d' % xs
            tok = slice(i * 128, (i + 1) * 128)
            dma(xt[xs][:], x_d[tok, :], [], [xn], xn)
            act(junkb[:], xt[xs][:], AF.Square, [xn], ['junkb', 'ssq'], accum_out=ssq[:])
            ts(ssq[:], ssq[:], float(1.0 / D), float(EPS), ALU.mult, ALU.add, ['ssq'], ['ssq'])
            act(ssq[:], ssq[:], AF.Sqrt, ['ssq'], ['ssq'])
            rcp(rstd[:], ssq[:], ['ssq'], ['rstd'])
            stt(htmp[:], xt[xs][:], rstd[:, 0:1], A1[:], ALU.mult, ALU.mult, [xn, 'rstd', 'A1'], ['htmp'])
            tt(hb[:], htmp[:], B1[:], ALU.add, ['htmp', 'B1'], ['hb'], eng='pool')
            for k in range(8):
                tr(pT[:, k, :], hb[:, k * 128:(k + 1) * 128], identb[:], ['hb', 'identb'], ['pT'])
            cp(hT[hs][:], pT[:], ['pT'], [hTn], eng='act')
            for cc in range(6):
                c0 = cc * 512
                c1 = min(c0 + 512, 2792)
                pp = cc % 2
                for k in range(8):
                    mm(pproj[pp][:, 0:c1 - c0], hT[hs][:, k, :], winb[:, k, c0:c1], k == 0, k == 7,
                       [hTn, 'winb'], ['pproj%d' % pp])
                cp(proj[:, c0:c1], pproj[pp][:, 0:c1 - c0], ['pproj%d' % pp], ['proj'], eng='act')
            for fc in range(16):
                c0 = 2792 + fc * 128
                for k in range(8):
                    mm(pgate[:, (fc % 4) * 128:(fc % 4 + 1) * 128], winb[:, k, c0:c0 + 128], hT[hs][:, k, :], k == 0, k == 7,
                       [hTn, 'winb'], ['pgate'])
                if fc % 4 == 3:
                    act(gS_t[:, fc - 3:fc + 1, :].rearrange("p c t -> p (c t)"), pgate[:], AF.Sigmoid, ['pgate'], ['gS_t'])
            dma(gS_d[:, :, tok].rearrange("c p t -> p c t"), gS_t[:], ['gS_t'], ['gS_d'], 'gS_t')
            cs64 = cos64[:, i, :]; sn64 = sin64[:, i, :]; cs32 = cos32[:, i, :]; sn32 = sin32[:, i, :]
            for (c0, gn, gt_, dstT, dn, dd_) in [(0, 'gaq', gaq, qaT_t, 'qaT_t', qaT_d), (512, 'gak', gak, kaT_t, 'kaT_t', kaT_d)]:
                src3 = proj[:, c0:c0 + 512].rearrange("p (h d) -> p h d", h=8)
                n3 = sq[:, 0:512].rearrange("p (h d) -> p h d", h=8)
                rms_heads(src3, 8, 64, gt_, n3, ['proj'], ['sq'], gn)
                rope(n3, 8, 32, cs64, sn64, tokb[:, 0:512].rearrange("p (h d) -> p h d", h=8), ['sq'], ['tokb'], '64')
                for k in range(4):
                    tr(pT2[:, k, :], tokb[:, k * 128:(k + 1) * 128], identb[:], ['tokb', 'identb'], ['pT2'])
                cp(dstT[:], pT2[:, 0:4, :], ['pT2'], [dn])
                dma(dd_[:, :, tok].rearrange("c p t -> p c t"), dstT[:], [dn], [dn + '_d'], dn)
            rope(proj[:, 1536:2048].rearrange("p (h d) -> p h d", h=8), 8, 32, cs64, sn64,
                 tokb[:, 0:512].rearrange("p (h d) -> p h d", h=8), ['proj'], ['tokb'], '64')
            for k in range(4):
                tr(pT2[:, k, :], tokb[:, k * 128:(k + 1) * 128], identb[:], ['tokb', 'identb'], ['pT2'])
            cp(qiT_t[:], pT2[:, 0:4, :], ['pT2'], ['qiT_t'])
            dma(qiT_d[:, :, tok].rearrange("c p t -> p c t"), qiT_t[:], ['qiT_t'], ['qiT_t_d'], 'qiT_t')
            n3 = sq[:, 0:64].rearrange("p (h d) -> p h d", h=1)
            rms_heads(proj[:, 2048:2112].rearrange("p (h d) -> p h d", h=1), 1, 64, gik, n3, ['proj'], ['sq'], 'gik')
            rope(n3, 1, 32, cs64, sn64, kib[:, 0:64].rearrange("p (h d) -> p h d", h=1), ['sq'], ['kib'], '64')
            cp(kib[:, 64:128], kib[:, 0:64], ['kib'], ['kib'])
            tr(pT2[:, 0, :], kib[:], identb[:], ['kib', 'identb'], ['pT2'])
            cp(kiT_t[:], pT2[:, 0, :], ['pT2'], ['kiT_t'])
            dma(kiT_d[:, tok], kiT_t[:], ['kiT_t'], ['kiT_t_d'], 'kiT_t')
            ts(wi_all[:, i, :], proj[:, 2112:2120], float(512 ** -0.5), None, ALU.mult, None, ['proj'], ['wi_all'])
            cp(vaug[:, :, 0:64], proj[:, 1024:1536].rearrange("p (h d) -> p h d", h=8), ['proj'], ['vaug'], eng='pool')
            dma(va_d[tok, :], vaug[:].rearrange("p h d -> p (h d)"), ['vaug'], ['va_d'], 'vaug')
            rms_heads(proj[:, 2120:2504].rearrange("p (h d) -> p h d", h=1), 1, 384, gmqa,
                      tokb[:, 0:384].rearrange("p (h d) -> p h d", h=1), ['proj'], ['tokb'], 'gmqa')
            for k in range(3):
                tr(pT2[:, k, :], tokb[:, k * 128:(k + 1) * 128], identb[:], ['tokb', 'identb'], ['pT2'])
            cp(cqT[:], pT2[:, 0:3, :], ['pT2'], ['cqT'])
            for (c0, c1) in [(0, 512), (512, 768)]:
                for k in range(3):
                    mm(pm[:, c0:c1], cqT[:, k, :], wmqb[:, k, c0:c1], k == 0, k == 2, ['cqT', 'wmqb'], ['pm'])
            cp(qmf[:], pm[:, 0:768], ['pm'], ['qmf'], eng='act')
            q3 = qmf[:].rearrange("p (h d) -> p h d", h=8)
            n96 = sq[:, 0:768].rearrange("p (h d) -> p h d", h=8)
            rms_heads(q3, 8, 96, gmq, n96, ['qmf'], ['sq'], 'gmq')
            tb96 = tokb[:, 0:768].rearrange("p (h d) -> p h d", h=8)
            cp(tb96[:, :, 0:64], n96[:, :, 0:64], ['sq'], ['tokb'], eng='pool')
            rope(n96[:, :, 64:96], 8, 16, cs32, sn32, tb96[:, :, 64:96], ['sq'], ['tokb'], '32')
            for h in range(8):
                tr(pT2[0:96, h, :], tokb[:, h * 96:(h + 1) * 96], identb[:], ['tokb', 'identb'], ['pT2'])
            cp(qmT_t[0:96, :, :], pT2[0:96, :, :], ['pT2'], ['qmT_t'])
            dma(qmT_d[:, :, tok].rearrange("h d t -> d h t"), qmT_t[0:96, :, :], ['qmT_t'], ['qmT_t_d'], 'qmT_t')
            rms_heads(proj[:, 2504:2760].rearrange("p (h d) -> p h d", h=1), 1, 256, gmkva,
                      tokb[:, 0:256].rearrange("p (h d) -> p h d", h=1), ['proj'], ['tokb'], 'gmkva')
            for k in range(2):
                tr(pT2[:, k, :], tokb[:, k * 128:(k + 1) * 128], identb[:], ['tokb', 'identb'], ['pT2'])
            cp(ckvT[:], pT2[:, 0:2, :], ['pT2'], ['ckvT'])
            for (c0, c1) in [(0, 512), (512, 1024)]:
                for k in range(2):
                    mm(pm[:, c0:c1], ckvT[:, k, :], wmkvb[:, k, c0:c1], k == 0, k == 1, ['ckvT', 'wmkvb'], ['pm'])
            cp(kvf[:].rearrange("p h d -> p (h d)"), pm[:], ['pm'], ['kvf'], eng='act')
            cp(kmpre[:, :, 0:64], kvf[:, :, 0:64], ['kvf'], ['kmpre'], eng='pool')
            cp(kmpre[:, :, 64:96], bc1(proj[:, 2760:2792], [128, 8, 32]), ['proj'], ['kmpre'], eng='pool')
            cp(vmaug[:, :, 0:64], kvf[:, :, 64:128], ['kvf'], ['vmaug'], eng='pool')
            dma(vm_d[tok, :], vmaug[:].rearrange("p h d -> p (h d)"), ['vmaug'], ['vm_d'], 'vmaug')
            rms_heads(kmpre[:], 8, 96, gmk, n96, ['kmpre'], ['sq'], 'gmk')
            cp(tb96[:, :, 0:64], n96[:, :, 0:64], ['sq'], ['tokb'], eng='pool')
            rope(n96[:, :, 64:96], 8, 16, cs32, sn32, tb96[:, :, 64:96], ['sq'], ['tokb'], '32')
            for h in range(8):
                tr(pT2[0:96, h, :], tokb[:, h * 96:(h + 1) * 96], identb[:], ['tokb', 'identb'], ['pT2'])
            cp(kmT_t[0:96, :, :], pT2[0:96, :, :], ['pT2'], ['kmT_t'])
            dma(kmT_d[:, :, tok].rearrange("h d t -> d h t"), kmT_t[0:96, :, :], ['kmT_t'], ['kmT_t_d'], 'kmT_t')
        P.barrier()
    st01.close()

    def attention_phase(tag, dsa):
        with contextlib.ExitStack() as st:
            ones64 = sbt(st, "ones64" + tag, [128, 64])
            memset(ones64[:], 1.0, ['ones64'])
            if dsa:
                kT = sbt(st, "kaT", [128, 4, S], BF16)
                kiT = sbt(st, "kiT", [128, S], BF16)
                for c in range(4):
                    dma(kT[:, c, :], kaT_d[c], ['kaT_t_d'], ['kT%d' % c], 'kT%d' % c)
                dma(kiT[:], kiT_d, ['kiT_t_d'], ['kiT'], 'kiT')
                v_src = va_d
                qT_g = sbt(st, "qaTg", [128, 4, 512], BF16)
                qiTg = [sbt(st, "qiTg%d" % i, [128, 4, 512], BF16) for i in range(2)]
                score = sbt(st, "score", [128, S])
                junk = sbt(st, "junkc", [128, S], BF16)
                bias_g = [sbt(st, "bias_g%d" % i, [128, 4, S], BF16) for i in range(2)]
                dg = sbt(st, "dg", [128, 8, 128], BF16)
                rbuf = [sbt(st, "rbuf%d" % i, [128, 512], BF16) for i in range(2)]
                lo = sbt(st, "lo", [128, 1]); hi = sbt(st, "hi", [128, 1]); mid = sbt(st, "mid", [128, 1])
                wv = sbt(st, "wv", [128, NITER + 1]); cnt = sbt(st, "cnt", [128, 1]); stp = sbt(st, "stp", [128, 1])
                pd = [pst(st, "pd%d" % i, [128, 512]) for i in range(2)]
                psc = pst(st, "psc", [128, 512])
                scale = 64 ** -0.5
                Kd = 64
                out_d = atA_d
            else:
                kT = sbt(st, "kmT", [128, 8, S], BF16)
                for h in range(8):
                    dma(kT[0:96, h, :], kmT_d[h], ['kmT_t_d'], ['kT%d' % h], 'kT%d' % h)
                v_src = vm_d
                qT_g = sbt(st, "qmTg", [128, 8, 512], BF16)
                scale = 96 ** -0.5
                Kd = 96
                out_d = atM_d
            vv = sbt(st, "vv" + tag, [128, NT, 520], BF16)
            v_v = v_src.rearrange("(t p) c -> p t c", p=128)
            for q in range(4):
                dma(vv[:, q * 8:(q + 1) * 8, :], v_v[:, q * 8:(q + 1) * 8, :], ['va_d', 'vm_d'], ['vv%d' % q], 'vv%d' % q)
            vres = ['vv%d' % q for q in range(4)]
            ptb = [sbt(st, "ptb%d%s" % (i, tag), [128, 512], BF16) for i in range(2)]
            rz = sbt(st, "rz" + tag, [128, 512]); of = sbt(st, "of" + tag, [128, 512])
            yb = [sbt(st, "yb%d%s" % (i, tag), [128, 512], BF16) for i in range(2)]
            pS = [pst(st, "pS%d%s" % (i, tag), [128, 512]) for i in range(2)]
            pO = [pst(st, "pO%d%s" % (i, tag), [128, 512]) for i in range(2)]
            pB = pst(st, "pB" + tag, [128, 512])
            kres = ['kT%d' % c for c in range(8)]
            ctr = 0
            def idx_block(g, b):
                bg = bias_g[g % 2]
                bgn = 'bias_g%d' % (g % 2)
                jq = 4 * g + b
                Sp = (jq + 1) * 128
                shp = [128, 8, 128]
                tt(dg[:], bc1(identb[:], shp), bc2(wi_all[:, jq, :], shp), ALU.mult, ['identb', 'wi_all'], ['dg'])
                items_i = [(c, h) for c in range(g + 1) for h in range(8)]
                qn = 'qiTg%d' % (g % 2)
                qi_ = qiTg[g % 2]

                def idx_dots(k_):
                    c, h = items_i[k_]
                    wc = 512 if c < g else (b + 1) * 128
                    k0 = c * 512
                    hp = h % 2; hc = h // 2
                    prt = slice(hp * 64, hp * 64 + 64)
                    dd = k_ % 2
                    mm(pd[dd][:, 0:wc], qi_[prt, hc, b * 128:(b + 1) * 128], kiT[prt, k0:k0 + wc], True, True,
                       [qn, 'kiT'], ['pd%d' % dd])
                    act(rbuf[dd][:, 0:wc], pd[dd][:, 0:wc], AF.Relu, ['pd%d' % dd], ['rbuf%d' % dd])

                def idx_acc(k_):
                    c, h = items_i[k_]
                    wc = 512 if c < g else (b + 1) * 128
                    k0 = c * 512
                    dd = k_ % 2
                    mm(psc[:, 0:wc], dg[:, h, :], rbuf[dd][:, 0:wc], h == 0, h == 7, ['dg', 'rbuf%d' % dd], ['psc'])
                    if h == 7:
                        if c < g:
                            cp(score[:, k0:k0 + 512], psc[:], ['psc'], ['score'])
                        else:
                            if b > 0:
                                cp(score[:, k0:k0 + b * 128], psc[:, 0:b * 128], ['psc'], ['score'])
                            tt(score[:, jq * 128:(jq + 1) * 128], psc[:, b * 128:(b + 1) * 128], trib[:], ALU.add,
                               ['psc', 'trib'], ['score'])
                idx_dots(0)
                for k_ in range(len(items_i)):
                    if k_ + 1 < len(items_i):
                        idx_dots(k_ + 1)
                    idx_acc(k_)
                if jq >= 2:
                    red(hi[:], score[:, 0:Sp], ALU.max, ['score'], ['hi'])
                    red(lo[:], score[:, 0:jq * 128], ALU.min, ['score'], ['lo'])
                    tt(stp[:], hi[:], lo[:], ALU.subtract, ['hi', 'lo'], ['stp'])
                    for k in range(NITER + 1):
                        ts(wv[:, k:k + 1], stp[:], float(2.0 ** -(k + 1)), None, ALU.mult, None, ['stp'], ['wv'])
                    tt(mid[:], lo[:], wv[:, 0:1], ALU.add, ['lo', 'wv'], ['mid'])
                    for k in range(NITER):
                        ts(junk[:, 0:Sp], score[:, 0:Sp], mid[:, 0:1], 0.0, ALU.is_ge, ALU.add, ['score', 'mid'],
                           ['junk', 'cnt'], accum_out=cnt[:])
                        ts(stp[:], cnt[:], 255.5, wv[:, k:k + 1], ALU.is_ge, ALU.mult, ['cnt', 'wv'], ['stp'])
                        tt(lo[:], lo[:], stp[:], ALU.add, ['lo', 'stp'], ['lo'])
                        tt(mid[:], lo[:], wv[:, k + 1:k + 2], ALU.add, ['lo', 'wv'], ['mid'])
                else:
                    memset(lo[:], -10000.0, ['lo'])
                ts(bg[:, b, 0:Sp], score[:, 0:Sp], lo[:, 0:1], NEG, ALU.is_lt, ALU.mult, ['score', 'lo'], [bgn])

            def att_part(g, heads):
                gsl = slice(g * 512, (g + 1) * 512)
                nsc = 4 * g + 4
                items_a = [(h, sc) for h in heads for sc in range(nsc)]
                if dsa:
                    bg = bias_g[g % 2]
                    bgn = 'bias_g%d' % (g % 2)

                def att_S(k_):
                    h, sc = items_a[k_]
                    r_ = sc - 4 * g
                    qlo = max(r_, 0) * 128
                    ss = k_ % 2
                    ssl = slice(sc * 128, (sc + 1) * 128)
                    if dsa:
                        hp = h % 2; hc = h // 2
                        prt = slice(hp * 64, hp * 64 + 64)
                        mm(pS[ss][:, qlo:512], kT[prt, hc, ssl], qT_g[prt, hc, qlo:512], True, False,
                           ['kT%d' % hc, 'qT_g'], ['pS%d' % ss])
                        b0 = max(r_, 0)
                        for b in range(b0, 4):
                            mm(pS[ss][:, b * 128:(b + 1) * 128], bg[:, b, ssl], identb[:], False, b == 3,
                               [bgn, 'identb'], ['pS%d' % ss])
                    else:
                        last = r_ < 0
                        mm(pS[ss][:, qlo:512], kT[0:96, h, ssl], qT_g[0:96, h, qlo:512], True, last,
                           ['kT%d' % h, 'qT_g'], ['pS%d' % ss])
                        if r_ >= 0:
                            mm(pS[ss][:, qlo:qlo + 128], trib[:], identb[:], False, True, ['trib', 'identb'], ['pS%d' % ss])
                    act(ptb[ss][:, qlo:512], pS[ss][:, qlo:512], AF.Exp, ['pS%d' % ss], ['ptb%d' % ss], scale=float(scale))

                def att_PV(k_):
                    h, sc = items_a[k_]
                    r_ = sc - 4 * g
                    qlo = max(r_, 0) * 128
                    ss = k_ % 2
                    po = h % 2
                    pon = 'pO%d' % po
                    mm(pO[po][0:65, qlo:512], vv[:, sc, h * 65:(h + 1) * 65], ptb[ss][:, qlo:512], sc == 0, sc == nsc - 1,
                       ['ptb%d' % ss] + vres, [pon])
                    if sc == nsc - 1:
                        rcp(rz[64:65, :], pO[po][64:65, :], [pon], ['rz'])
                        mm(pB[0:64, :], ones64[64:65, 0:64], rz[64:65, :], True, True, ['ones64', 'rz'], ['pB'])
                        cp(of[0:64, :], pO[po][0:64, :], [pon], ['of'], eng='act')
                        ybn = 'yb%d' % po
                        tt(yb[po][0:64, :], of[0:64, :], pB[0:64, :], ALU.mult, ['of', 'pB'], [ybn])
                        dma(out_d[h, :, gsl], yb[po][0:64, :], [ybn], ['at_d' + tag], ybn + tag)
                att_S(0)
                for k_ in range(len(items_a)):
                    if k_ + 1 < len(items_a):
                        att_S(k_ + 1)
                    att_PV(k_)

            def load_q(g):
                gsl = slice(g * 512, (g + 1) * 512)
                if dsa:
                    dma(qT_g[:], qaT_d[:, :, gsl].rearrange("c p t -> p c t"), ['qaT_t_d'], ['qT_g'], 'qT_g')
                else:
                    dma(qT_g[0:96, :, :], qmT_d[:, :, gsl].rearrange("h d t -> d h t"), ['qmT_t_d'], ['qT_g'], 'qT_g')

            def load_qi(g):
                gsl = slice(g * 512, (g + 1) * 512)
                dma(qiTg[g % 2][:], qiT_d[:, :, gsl].rearrange("c p t -> p c t"), ['qiT_t_d'], ['qiTg%d' % (g % 2)], 'qiTg%d' % (g % 2))

            if dsa:
                load_qi(0)
                for b in range(4):
                    idx_block(0, b)
                for g in range(NG):
                    load_q(g)
                    if g + 1 < NG:
                        load_qi(g + 1)
                    for part in range(4):
                        if g + 1 < NG:
                            idx_block(g + 1, part)
                        att_part(g, [2 * part, 2 * part + 1])
            else:
                stgc = [sbt(st, "stgc%d" % i, [128, 4096]) for i in range(2)]
                cvb = [sbt(st, "cvb%d" % i, [128, 4, D], BF16) for i in range(3)]
                uv_v = uv_d.rearrange("(p r) d -> p r d", p=128)
                conv = [(tsrc, col, rc) for (tsrc, col) in [(pu_d, 0), (pv_d, D)] for rc in range(32)]

                def conv_iter(it):
                    tsrc, col, rc = conv[it]
                    t_v = tsrc.rearrange("(p r) d -> p r d", p=128)
                    sl = it % 2
                    cs_ = it % 3
                    sv = stgc[sl][:].rearrange("p (r d) -> p r d", r=4)
                    dma(sv, t_v[:, rc * 4:(rc + 1) * 4, :], [], ['stgc%d' % sl], 'stgc%d' % sl)
                    cp(cvb[cs_][:], sv, ['stgc%d' % sl], ['cvb%d' % cs_], eng=('pool', 'dve')[it % 2])
                    dma(uv_v[:, rc * 4:(rc + 1) * 4, col:col + D], cvb[cs_][:], ['cvb%d' % cs_], ['uv_d%d' % cs_], 'cvb%d' % cs_)
                for g in range(NG):
                    load_q(g)
                    for it in range(g * 8, g * 8 + 8):
                        conv_iter(it)
                    att_part(g, list(range(8)))
            P.barrier()

    if phases >= 2:
        attention_phase("A", True)
    if phases >= 3:
        attention_phase("M", False)

    if phases >= 4:
        with contextlib.ExitStack() as st:
            woab = sbt(st, "woab", [128, 4, D], BF16); womb = sbt(st, "womb", [128, 4, D], BF16)
            woutb = sbt(st, "woutb", [128, 8, D], BF16); wpqb = sbt(st, "wpqb", [128, 8, 2048], BF16)
            subkT = sbt(st, "subkT", [128, 16, 128], BF16)
            iota16 = sbt(st, "iota16", [128, 16])
            G1 = sbt(st, "G1p", [128, D]); A2 = sbt(st, "A2p", [128, D]); B2 = sbt(st, "B2p", [128, D]); G2 = sbt(st, "G2p", [128, D])
            dma(iota16[:], iota16_d, [], ['iota16'], 'iota16')
            for qi_, (tn_, nm_) in enumerate([(G1, 'G1'), (A2, 'A2'), (B2, 'B2'), (G2, 'G2')]):
                dma(tn_[:], mod_d[qi_], ['mod_d'], [nm_], 'ld' + nm_)
            pkb = [pst(st, "pk%d" % i, [128, 512]) for i in range(2)]
            pk = pkb[0]
            pT4 = pst(st, "pT4", [128, 8, 128], BF16)
            pacc = pst(st, "pacc", [128, 1024])
            with contextlib.ExitStack() as st2:
                stg = [sbt(st2, "stg%d" % i, [128, 4096]) for i in range(2)]
                skb = sbt(st2, "skb", [128, 16, 128], BF16)
                ci = 0
                for (wsrc, wdst, nk, ncol, nm) in [(woa_d, woab, 4, 1024, 'woab'), (wom_d, womb, 4, 1024, 'womb'),
                                                    (wout_d, woutb, 8, 1024, 'woutb'), (wpq_d, wpqb, 8, 2048, 'wpqb')]:
                    wv_ = wsrc.rearrange("(k p) n -> p k n", p=128)
                    cw = 4096 // nk
                    for c0 in range(0, ncol, cw):
                        sl = ci % 2
                        sv = stg[sl][:, 0:nk * cw].rearrange("p (k n) -> p k n", k=nk)
                        dma(sv, wv_[:, :, c0:c0 + cw], [], ['stg%d' % sl], 'stg%d' % sl)
                        cp(wdst[:, :, c0:c0 + cw], sv, ['stg%d' % sl], [nm], eng=('pool' if ci % 2 == 0 else 'act'))
                        ci += 1
                sl = ci % 2
                sv = stg[sl][:, 0:2048].rearrange("p (c d) -> p c d", c=16)
                dma(sv, subk_d.rearrange("c n d -> n c d"), [], ['stg%d' % sl], 'stg%d' % sl)
                cp(skb[:], sv, ['stg%d' % sl], ['skb'])
                for c in range(16):
                    tr(pT4[:, c % 8, :], skb[:, c, :], identb[:], ['skb', 'identb'], ['pT4'])
                    if c % 8 == 7:
                        cp(subkT[:, c - 7:c + 1, :], pT4[:], ['pT4'], ['subkT'])
                P.barrier()

            def sb4(name, shape, dt=F32):
                return sbt(st, name, shape, dt)
            aT_t = sb4("aT_t", [128, 4, 128], BF16); mT_t = sb4("mT_t", [128, 4, 128], BF16)
            gS4 = sb4("gS4", [128, 16, 128], BF16)
            t1 = sb4("t1", [128, 128]); t2 = sb4("t2", [128, 128])
            mixT = sb4("mixT", [128, 8, 128], BF16)
            xt2 = sb4("xt2", [128, D])
            tmpx = xt2
            x1 = [sb4("x1_%d" % i, [128, D]) for i in range(2)]
            ssq = sb4("ssq4", [128, 1]); rstd = sb4("rstd4", [128, 1])
            h2b = [sb4("h2b%d" % i, [128, D], BF16) for i in range(2)]; h2T = sb4("h2T", [128, 8, 128], BF16)
            prod = [sb4("prod%d" % i, [128, D], BF16) for i in range(2)]
            qpT = sb4("qpT", [128, 16, 128], BF16)
            s_all = sb4("s_all", [128, 16, 128]); s_wk = sb4("s_wk", [128, 128])
            tops = sb4("tops", [128, 16, 16]); topi = sb4("topi", [128, 16, 16], U32); topf = sb4("topf", [128, 16, 16])
            cand = s_all[:].rearrange("p (h two) n -> p h (two n)", two=2)
            cwk = sb4("cwk", [128, 256])
            best = sb4("best", [128, 8, 16]); bpos = sb4("bpos", [128, 8, 16], U32)
            ak = sb4("ak", [128, 8, 16], U32); bk = sb4("bk", [128, 8, 16], U32)
            akf = sb4("akf", [128, 8, 16]); bkf = sb4("bkf", [128, 8, 16])
            oh = s_all[:].rearrange("p c (a b) -> p (c a) b", b=16).rearrange("p (h k) a -> p h k a", h=8)
            i0 = sb4("i0", [128, 8, 16]); i1 = sb4("i1", [128, 8, 16])
            idf = sb4("idf", [128, 128])
            ids = [sb4("ids%d" % i, [128, 128], U32) for i in range(2)]
            gate = [sb4("gate%d" % i, [128, 8, 16]) for i in range(2)]
            gz = sb4("gz", [128, 8]); ngmax = sb4("ngmax", [128, 8])
            actv = [sb4("actv%d" % i, [128, 128]) for i in range(2)]
            coef = [sb4("coef%d" % i, [128, 128]) for i in range(2)]
            gtmp = [sb4("gtmp%d" % i, [128, 128]) for i in range(2)]
            NRING = 16
            uvb = [sb4("uvb%d" % i, [128, 2 * D], BF16) for i in range(NRING)]
            dgb = [sb4("dgb%d" % i, [128, 128], BF16) for i in range(4)]
            ob = xt2
            GELU_S = float(2.0 * np.sqrt(2.0 / np.pi))
            shp4 = [128, 8, 16, 16]

            def prologue(i):
                p = i % 2
                tok = slice(i * 128, (i + 1) * 128)
                X1 = 'x1_%d' % p
                PH = 'ph2_%d' % p
                dma(aT_t[:], atA_d[:, :, tok].rearrange("(c two) d t -> (two d) c t", two=2), ['at_dA'], ['aT_t'], 'aT_t')
                dma(mT_t[:], atM_d[:, :, tok].rearrange("(c two) d t -> (two d) c t", two=2), ['at_dM'], ['mT_t'], 'mT_t')
                dma(gS4[:], gS_d[:, :, tok].rearrange("c p t -> p c t"), ['gS_d'], ['gS4'], 'gS4')
                dma(xt2[:], x_d[tok, :], [], ['xt2'], 'xt2')
                yield

                def mix_mm(fc):
                    fsl = slice(fc * 128, (fc + 1) * 128)
                    pkx = pkb[fc % 2]
                    pn = 'pk%d' % (fc % 2)
                    for k in range(4):
                        mm(pkx[:, 0:128], woab[:, k, fsl], aT_t[:, k, :], k == 0, k == 3, ['woab', 'aT_t'], [pn])
                    for k in range(4):
                        mm(pkx[:, 128:256], womb[:, k, fsl], mT_t[:, k, :], k == 0, k == 3, ['womb', 'mT_t'], [pn])

                def mix_dve(fc):
                    pkx = pkb[fc % 2]
                    pn = 'pk%d' % (fc % 2)
                    tt(t1[:], pkx[:, 0:128], gS4[:, fc, :], ALU.mult, [pn, 'gS4'], ['t1'])
                    tt(t2[:], pkx[:, 128:256], gS4[:, 8 + fc, :], ALU.mult, [pn, 'gS4'], ['t2'])
                    tt(mixT[:, fc, :], t1[:], t2[:], ALU.add, ['t1', 't2'], ['mixT'])
                mix_mm(0); mix_mm(1)
                yield
                for s_ in range(1, 4):
                    mix_dve(2 * s_ - 2); mix_dve(2 * s_ - 1)
                    mix_mm(2 * s_); mix_mm(2 * s_ + 1)
                    yield
                mix_dve(6); mix_dve(7)
                PKB = ['pk0', 'pk1']
                for k in range(8):
                    mm(pkb[0][:], mixT[:, k, :], woutb[:, k, 0:512], k == 0, k == 7, ['mixT', 'woutb'], ['pk0'])
                for k in range(8):
                    mm(pkb[1][:], mixT[:, k, :], woutb[:, k, 512:1024], k == 0, k == 7, ['mixT', 'woutb'], ['pk1'])
                yield
                tt(x1[p][:, 0:512], pkb[0][:], G1[:, 0:512], ALU.mult, ['pk0', 'G1'], [X1])
                tt(x1[p][:, 512:1024], pkb[1][:], G1[:, 512:1024], ALU.mult, ['pk1', 'G1'], [X1])
                tt(x1[p][:], x1[p][:], xt2[:], ALU.add, [X1, 'xt2'], [X1])
                yield
                act(prod[0][:], x1[p][:], AF.Square, [X1], ['prod0', 'ssq4'], accum_out=ssq[:])
                act(ssq[:], ssq[:], AF.Sqrt, ['ssq4'], ['ssq4'], scale=float(1.0 / D), bias=float(EPS))
                yield
                rcp(rstd[:], ssq[:], ['ssq4'], ['rstd4'])
                stt(tmpx[:], x1[p][:], rstd[:, 0:1], A2[:], ALU.mult, ALU.mult, [X1, 'rstd4', 'A2'], ['xt2'])
                tt(h2b[p][:], tmpx[:], B2[:], ALU.add, ['xt2', 'B2'], [PH])
                yield
                for k in range(8):
                    tr(pT4[:, k, :], h2b[p][:, k * 128:(k + 1) * 128], identb[:], [PH, 'identb'], ['pT4'])
                yield
                cp(h2T[:], pT4[:], ['pT4'], ['h2T'], eng='act')

                def q_mm(c4):
                    for cc in range(4):
                        c = c4 * 4 + cc
                        for k in range(8):
                            mm(pkb[c4 % 2][:, cc * 128:(cc + 1) * 128], wpqb[:, k, c * 128:(c + 1) * 128], h2T[:, k, :],
                               k == 0, k == 7, ['wpqb', 'h2T'], ['pk%d' % (c4 % 2)])

                def q_ev(c4):
                    cp(qpT[:, c4 * 4:(c4 + 1) * 4, :].rearrange("p c t -> p (c t)"), pkb[c4 % 2][:], ['pk%d' % (c4 % 2)], ['qpT'], eng='act')

                def s_mm(c4):
                    for cc in range(4):
                        c = c4 * 4 + cc
                        mm(pkb[c4 % 2][:, cc * 128:(cc + 1) * 128], qpT[:, c, :], subkT[:, c, :], True, True, ['qpT', 'subkT'],
                           ['pk%d' % (c4 % 2)])

                def s_ev(c4):
                    cp(s_all[:, c4 * 4:(c4 + 1) * 4, :].rearrange("p c t -> p (c t)"), pkb[c4 % 2][:], ['pk%d' % (c4 % 2)], ['s_all'], eng='act')

                def topk1(c):
                    sv_ = s_all[:, c, :]
                    P.op('dve', lambda e, c=c, sv_=sv_: e.max(out=tops[:, c, 0:8], in_=sv_), ['s_all'], ['tops'])
                    P.op('dve', lambda e, c=c, sv_=sv_: e.max_index(out=topi[:, c, 0:8], in_max=tops[:, c, 0:8], in_values=sv_),
                         ['s_all', 'tops'], ['topi'])
                    P.op('dve', lambda e, c=c, sv_=sv_: e.match_replace(out=s_wk[:], in_to_replace=tops[:, c, 0:8], in_values=sv_,
                                                                    imm_value=-1e30), ['s_all', 'tops'], ['s_wk'])
                    P.op('dve', lambda e, c=c: e.max(out=tops[:, c, 8:16], in_=s_wk[:]), ['s_wk'], ['tops'])
                    P.op('dve', lambda e, c=c: e.max_index(out=topi[:, c, 8:16], in_max=tops[:, c, 8:16], in_values=s_wk[:]),
                         ['s_wk', 'tops'], ['topi'])
                q_mm(0)
                yield
                for c4 in range(1, 4):
                    q_ev(c4 - 1); q_mm(c4)
                    yield
                q_ev(3); s_mm(0)
                yield
                s_ev(0); s_mm(1)
                yield
                s_ev(1); s_mm(2)
                for c in range(0, 4):
                    topk1(c)
                yield
                s_ev(2); s_mm(3)
                for c in range(4, 8):
                    topk1(c)
                yield
                s_ev(3)
                for c in range(8, 12):
                    topk1(c)
                yield
                for c in range(12, 16):
                    topk1(c)
                cp(topf[:], topi[:], ['topi'], ['topf'])
                t4 = tops[:].rearrange("p (h two) k -> p h two k", two=2)
                c4v = cand.rearrange("p h (a b) -> p h a b", a=16)
                tt(c4v, t4[:, :, 0, :].unsqueeze(3).to_broadcast(shp4), t4[:, :, 1, :].unsqueeze(2).to_broadcast(shp4), ALU.add,
                   ['tops'], ['s_all'])
                yield
                for h in range(8):
                    cv = cand[:, h, :]
                    P.op('dve', lambda e, h=h, cv=cv: e.max(out=best[:, h, 0:8], in_=cv), ['s_all'], ['best'])
                    P.op('dve', lambda e, h=h, cv=cv: e.max_index(out=bpos[:, h, 0:8], in_max=best[:, h, 0:8], in_values=cv),
                         ['s_all', 'best'], ['bpos'])
                    P.op('dve', lambda e, h=h, cv=cv: e.match_replace(out=cwk[:], in_to_replace=best[:, h, 0:8], in_values=cv,
                                                                    imm_value=-1e30), ['s_all', 'best'], ['cwk'])
                    P.op('dve', lambda e, h=h: e.max(out=best[:, h, 8:16], in_=cwk[:]), ['cwk'], ['best'])
                    P.op('dve', lambda e, h=h: e.max_index(out=bpos[:, h, 8:16], in_max=best[:, h, 8:16], in_values=cwk[:]),
                         ['cwk', 'best'], ['bpos'])
                    if h == 3:
                        yield
                gt_ = gate[p]; GN = 'gate%d' % p
                ts(ngmax[:], best[:, :, 0], -1.0, None, ALU.mult, None, ['best'], ['ngmax'])
                tt(gt_[:], best[:], bc2(ngmax[:], [128, 8, 16]), ALU.add, ['best', 'ngmax'], [GN])
                act(gt_[:], gt_[:], AF.Exp, [GN], [GN])
                yield
                ts(ak[:], bpos[:], 4, None, ALU.logical_shift_right, None, ['bpos'], ['ak'])
                ts(bk[:], bpos[:], 15, None, ALU.bitwise_and, None, ['bpos'], ['bk'])
                cp(akf[:], ak[:], ['ak'], ['akf'])
                cp(bkf[:], bk[:], ['bk'], ['bkf'])
                tf4 = topf[:].rearrange("p (h two) k -> p h two k", two=2)
                io4 = iota16[:].unsqueeze(1).unsqueeze(1).to_broadcast(shp4)
                for (kf_, half, dsti, nm) in [(akf, 0, i0, 'i0'), (bkf, 1, i1, 'i1')]:
                    tt(oh, kf_[:].unsqueeze(3).to_broadcast(shp4), io4, ALU.is_equal, ['akf', 'bkf', 'iota16'], ['s_all'])
                    tt(oh, oh, tf4[:, :, half, :].unsqueeze(2).to_broadcast(shp4), ALU.mult, ['s_all', 'topf'], ['s_all'])
                    red(dsti[:], oh, ALU.add, ['s_all'], [nm])
                stt(idf[:].rearrange("p (h k) -> p h k", h=8), i0[:], 128.0, i1[:], ALU.mult, ALU.add, ['i0', 'i1'], ['idf'])
                cp(ids[p][:], idf[:], ['idf'], ['ids%d' % p])
                red(gz[:], gt_[:], ALU.add, [GN], ['gz'])
                rcp(gz[:], gz[:], ['gz'], ['gz'])
                tt(gt_[:], gt_[:], bc2(gz[:], [128, 8, 16]), ALU.mult, [GN, 'gz'], [GN])

            NTOT = NT * 32

            def pg_gather(G):
                i, gi = divmod(G, 32)
                p = i % 2
                for q_ in range(4):
                    sl_ = gi * 4 + q_
                    rb = (G * 4 + q_) % NRING
                    un = 'uvb%d' % rb
                    P.dma('pool', lambda e, sl_=sl_, rb=rb, p=p: e.indirect_dma_start(
                        out=uvb[rb][:], out_offset=None, in_=uv_d,
                        in_offset=bass.IndirectOffsetOnAxis(ap=ids[p][:, sl_:sl_ + 1], axis=0)), ['ids%d' % p], [un], un)

            def pg_dot(G):
                i, gi = divmod(G, 32)
                p = i % 2
                for q_ in range(4):
                    sl_ = gi * 4 + q_
                    rb = (G * 4 + q_) % NRING
                    pr = (G * 4 + q_) % 2
                    tt(prod[pr][:], uvb[rb][:, 0:D], h2b[p][:], ALU.mult, ['uvb%d' % rb, 'ph2_%d' % p], ['prod%d' % pr])
                    act(prod[pr][:], prod[pr][:], AF.Identity, ['prod%d' % pr], ['prod%d' % pr, 'actv%d' % p], accum_out=actv[p][:, sl_:sl_ + 1])

            def pg_coefA(G):
                i, gi = divmod(G, 32)
                p = i % 2
                s4 = slice(gi * 4, gi * 4 + 4)
                a_ = actv[p][:, s4]; t_ = gtmp[p][:, s4]
                AN = 'actv%d' % p; TN = 'gtmp%d' % p
                stt(t_, a_, 0.044715, a_, ALU.mult, ALU.mult, [AN], [TN])
                stt(t_, t_, 1.0, a_, ALU.add, ALU.mult, [TN, AN], [TN])
                act(t_, t_, AF.Sigmoid, [TN], [TN], scale=GELU_S)

            def pg_coefB(G):
                i, gi = divmod(G, 32)
                p = i % 2
                s4 = slice(gi * 4, gi * 4 + 4)
                a_ = actv[p][:, s4]; t_ = gtmp[p][:, s4]
                AN = 'actv%d' % p; TN = 'gtmp%d' % p
                gate_f = gate[p][:].rearrange("p h k -> p (h k)")
                tt(t_, t_, a_, ALU.mult, [TN, AN], [TN])
                tt(coef[p][:, s4], t_, gate_f[:, s4], ALU.mult, [TN, 'gate%d' % p], ['coef%d' % p])

            def pg_acc(G):
                i, gi = divmod(G, 32)
                p = i % 2
                for q_ in range(4):
                    sl_ = gi * 4 + q_
                    rb = (G * 4 + q_) % NRING
                    db = sl_ % 4
                    act(dgb[db][:], identb[:], AF.Identity, ['identb', 'coef%d' % p], ['dgb%d' % db], scale=coef[p][:, sl_:sl_ + 1])
                    for hf in range(2):
                        mm(pacc[:, hf * 512:(hf + 1) * 512], dgb[db][:], uvb[rb][:, D + hf * 512:D + (hf + 1) * 512],
                           sl_ == 0, sl_ == 127, ['dgb%d' % db, 'uvb%d' % rb], ['pacc'])

            def epilogue(i):
                p = i % 2
                tt(tmpx[:], pacc[:], G2[:], ALU.mult, ['pacc', 'G2'], ['xt2'])
                tt(ob[:], tmpx[:], x1[p][:], ALU.add, ['xt2', 'x1_%d' % p], ['xt2'])
                dma(out_d[i * 128:(i + 1) * 128, :], ob[:], ['xt2'], ['out_d'], 'ob')

            for _ in prologue(0):
                pass
            NLEAD = NRING // 4
            for G0 in range(NLEAD):
                pg_gather(G0)
            pg_dot(0)
            gen = None
            for G in range(NTOT + 1):
                i, gi = divmod(G, 32)
                if gi == 0 and G < NTOT:
                    gen = prologue(i + 1) if i + 1 < NT else None
                if G + 1 < NTOT:
                    pg_dot(G + 1)
                if G < NTOT:
                    pg_coefA(G)
                if G >= 1:
                    pg_coefB(G - 1)
                    pg_acc(G - 1)
                    if (G - 1) % 32 == 31:
                        epilogue((G - 1) // 32)
                if gen is not None:
                    if gi < 26:
                        next(gen, None)
                    elif gi == 26:
                        for _ in gen:
                            pass
                        gen = None
                if G >= 1 and G - 1 + NRING // 4 < NTOT:
                    pg_gather(G - 1 + NRING // 4)
            P.barrier()

    P.emit(nc)
    top.close()
    return nc


def make_inputs(inputs):
    f = lambda a: np.ascontiguousarray(np.asarray(a))
    common = {}
    for k in ["g_norm1", "g_norm2", "b_ada", "g_a_q", "g_a_k", "g_idx_k", "g_mq_a", "g_mkv_a", "g_m_q", "g_m_k"]:
        common[k] = f(np.asarray(inputs[k], np.float32).reshape(1, -1))
    for k in ["w_ada", "w_in", "w_mq_up", "w_mkv_up", "w_o_a", "w_o_m", "w_out", "w_peer_q", "peer_u", "peer_v"]:
        common[k] = f(np.asarray(inputs[k], np.float32)[0])
    common["peer_subkeys"] = f(np.asarray(inputs["peer_subkeys"], np.float32)[0].reshape(16, 128, 128))
    common["identb"] = np.eye(128, dtype=np.float32).astype(ml_dtypes.bfloat16)
    q = np.arange(128)[:, None]; s = np.arange(128)[None, :]
    common["trib"] = np.where(s <= q, 0.0, NEG).astype(np.float32).astype(ml_dtypes.bfloat16)
    inv64 = (10000.0 ** (-(np.arange(32, dtype=np.float32)) / np.float32(32))).astype(np.float32)
    inv32 = (10000.0 ** (-(np.arange(16, dtype=np.float32)) / np.float32(16))).astype(np.float32)
    common["inv64"] = f(np.broadcast_to(inv64[None, :], (128, 32)))
    common["inv32"] = f(np.broadcast_to(inv32[None, :], (128, 16)))
    common["iota16"] = f(np.broadcast_to(np.arange(16, dtype=np.float32)[None, :], (128, 16)))
    x = np.asarray(inputs["x"], np.float32)
    c = np.asarray(inputs["c"], np.float32)
    pos = np.asarray(inputs["positions"], np.int32)
    maps = []
    for b in range(x.shape[0]):
        m = dict(common)
        m["x"] = f(x[b])
        m["c_col"] = f(c[b].reshape(8, 128).T)
        m["pos_t"] = f(pos[b].reshape(NT, 128).T)
        maps.append(m)
    return maps


def kernel(**inputs):
    maps = make_inputs(inputs)
    nc = build_nc()
    res = run_bass_kernel_spmd(nc, maps, core_ids=list(range(8)))
    return np.stack([np.asarray(r["out"], np.float32) for r in res.results], axis=0)
```

```python
import contextlib
import numpy as np
import ml_dtypes
import concourse.bass as bass
import concourse.mybir as mybir
from concourse.bass_utils import run_bass_kernel_spmd

F32 = mybir.dt.float32
BF16 = mybir.dt.bfloat16
I32 = mybir.dt.int32
U32 = mybir.dt.uint32
AF = mybir.ActivationFunctionType
ALU = mybir.AluOpType
AX = mybir.AxisListType

S = 4096
D = 1024
NT = 32
NG = 8
EPS = 1e-6
NEG = -30000.0
NITER = 18
NSLOT = 4
TWO_PI = float(2 * np.pi)
NO_SELF_WAIT = False


class Prog:
    ENG = ('pe', 'act', 'dve', 'pool', 'sp')

    def __init__(self):
        self.ops = {e: [] for e in self.ENG}
        self.cnt = {e: 0 for e in self.ENG}
        self.known = {e: {} for e in self.ENG}
        self.res = {}
        self.dma_cnt = {}

    def _deps(self, eng, reads, writes):
        need = {}

        def add(sk, v):
            if sk == ('eng', 'pe') and eng == 'pe':
                return
            if NO_SELF_WAIT and sk == ('eng', eng):
                return
            if v > need.get(sk, 0):
                need[sk] = v
        for k in reads:
            st = self.res.get(k)
            if st and st[0] is not None:
                add(*st[0])
        for k in writes:
            st = self.res.get(k)
            if st:
                if st[0] is not None:
                    add(*st[0])
                for sk, v in st[1].items():
                    add(sk, v)
        waits = []
        kn = self.known[eng]
        for sk, v in need.items():
            if kn.get(sk, 0) >= v:
                continue
            kn[sk] = v
            waits.append((sk, v))
        return waits

    def _commit(self, tok, reads, writes):
        sk, v = tok
        for k in reads:
            st = self.res.setdefault(k, [None, {}])
            if v > st[1].get(sk, 0):
                st[1][sk] = v
        for k in writes:
            self.res[k] = [tok, {}]

    def op(self, eng, fn, r=(), w=()):
        waits = self._deps(eng, r, w)
        self.cnt[eng] += 1
        tok = (('eng', eng), self.cnt[eng])
        self.ops[eng].append((waits, fn, ('eng', eng), 1))
        self._commit(tok, r, w)

    def dma(self, eng, fn, r=(), w=(), sem=None):
        waits = self._deps(eng, r, w)
        c = self.dma_cnt.get(sem, 0) + 16
        self.dma_cnt[sem] = c
        tok = (('dma', sem), c)
        self.ops[eng].append((waits, fn, ('dma', sem), 16))
        self._commit(tok, r, w)

    def barrier(self):
        allk = [(('eng', e), self.cnt[e]) for e in self.ENG if self.cnt[e] > 0]
        allk += [(('dma', k), c) for k, c in self.dma_cnt.items()]
        for e in self.ENG:
            waits = []
            kn = self.known[e]
            for sk, v in allk:
                if sk == ('eng', e):
                    continue
                if kn.get(sk, 0) >= v:
                    continue
                kn[sk] = v
                waits.append((sk, v))
            if waits:
                self.ops[e].append((waits, None, None, 0))

    def emit(self, nc):
        self.barrier()
        with contextlib.ExitStack() as st:
            sems = {}
            for e in self.ENG:
                sems[('eng', e)] = st.enter_context(nc.semaphore("s_" + e))
            for i, k in enumerate(self.dma_cnt):
                sems[('dma', k)] = st.enter_context(nc.semaphore("d%d" % i))
            block = st.enter_context(nc.Block())

            def run(e, name):
                for (waits, fn, sk, inc) in self.ops[name]:
                    for (wk, v) in waits:
                        e.wait_ge(sems[wk], v)
                    if fn is not None:
                        fn(e).then_inc(sems[sk], inc)

            @block.tensor
            def _(e):
                run(e, 'pe')

            @block.scalar
            def _(e):
                run(e, 'act')

            @block.vector
            def _(e):
                run(e, 'dve')

            @block.gpsimd
            def _(e):
                run(e, 'pool')

            @block.sync
            def _(e):
                run(e, 'sp')


def build_nc(debug=False, phases=4, nt1=NT, skip=(), nslot=NSLOT, gcols=D):
    nc = bass.Bass("TRN2", target_bir_lowering=False)
    P = Prog()

    def din(name, shape, dt=F32):
        return nc.dram_tensor(name, list(shape), dt, kind="ExternalInput").ap()

    def dscr(name, shape, dt=BF16):
        return nc.dram_tensor(name, list(shape), dt, kind="ExternalOutput" if debug else "Internal").ap()

    x_d = din("x", [S, D])
    ccol_d = din("c_col", [128, 8])
    pos_d = din("pos_t", [128, NT], I32)
    g1_d = din("g_norm1", [1, D]); g2_d = din("g_norm2", [1, D])
    wada_d = din("w_ada", [D, 6 * D]); bada_d = din("b_ada", [1, 6 * D])
    win_d = din("w_in", [D, 4840])
    gaq_d = din("g_a_q", [1, 64]); gak_d = din("g_a_k", [1, 64]); gik_d = din("g_idx_k", [1, 64])
    gmqa_d = din("g_mq_a", [1, 384]); wmq_d = din("w_mq_up", [384, 768])
    gmkva_d = din("g_mkv_a", [1, 256]); wmkv_d = din("w_mkv_up", [256, 1024])
    gmq_d = din("g_m_q", [1, 96]); gmk_d = din("g_m_k", [1, 96])
    woa_d = din("w_o_a", [512, D]); wom_d = din("w_o_m", [512, D]); wout_d = din("w_out", [D, D])
    wpq_d = din("w_peer_q", [D, 2048]); subk_d = din("peer_subkeys", [16, 128, 128])
    pu_d = din("peer_u", [16384, D]); pv_d = din("peer_v", [16384, D])
    identb_d = din("identb", [128, 128], BF16)
    trib_d = din("trib", [128, 128], BF16)
    inv64_d = din("inv64", [128, 32]); inv32_d = din("inv32", [128, 16])
    iota16_d = din("iota16", [128, 16])
    out_d = nc.dram_tensor("out", [S, D], F32, kind="ExternalOutput").ap()

    qaT_d = dscr("qaT_s", [4, 128, S]); kaT_d = dscr("kaT_s", [4, 128, S]); qiT_d = dscr("qiT_s", [4, 128, S])
    kiT_d = dscr("kiT_s", [128, S])
    va_d = dscr("va_s", [S, 520]); vm_d = dscr("vm_s", [S, 520])
    qmT_d = dscr("qmT_s", [8, 96, S]); kmT_d = dscr("kmT_s", [8, 96, S])
    gS_d = dscr("gS_s", [16, 128, S])
    atA_d = dscr("atA_s", [8, 64, S]); atM_d = dscr("atM_s", [8, 64, S])
    mod_d = dscr("mod_s", [4, 128, D], F32)
    uv_d = nc.dram_tensor("uv_s", [16384, 2048], BF16, kind="Internal").ap()

    top = contextlib.ExitStack()

    uid = [0]

    def sbt(st, name, shape, dt=F32):
        uid[0] += 1
        return st.enter_context(nc.sbuf_tensor("sb%d_%s" % (uid[0], name), list(shape), dt))

    def pst(st, name, shape, dt=F32):
        uid[0] += 1
        return st.enter_context(nc.psum_tensor("ps%d_%s" % (uid[0], name), list(shape), dt))

    def tt(out, in0, in1, op, r, w, eng='dve'):
        P.op(eng, lambda e: e.tensor_tensor(out=out, in0=in0, in1=in1, op=op), r, w)

    def ts(out, in0, s1, s2, op0, op1, r, w, eng='dve', accum_out=None):
        if op1 is None:
            P.op(eng, lambda e: e.tensor_scalar(out=out, in0=in0, scalar1=s1, scalar2=None, op0=op0), r, w)
        elif accum_out is None:
            P.op(eng, lambda e: e.tensor_scalar(out=out, in0=in0, scalar1=s1, scalar2=s2, op0=op0, op1=op1), r, w)
        else:
            P.op(eng, lambda e: e.tensor_scalar(out=out, in0=in0, scalar1=s1, scalar2=s2, op0=op0, op1=op1, accum_out=accum_out), r, w)

    def stt(out, in0, scalar, in1, op0, op1, r, w, accum_out=None):
        if accum_out is None:
            P.op('dve', lambda e: e.scalar_tensor_tensor(out=out, in0=in0, scalar=scalar, in1=in1, op0=op0, op1=op1), r, w)
        else:
            P.op('dve', lambda e: e.scalar_tensor_tensor(out=out, in0=in0, scalar=scalar, in1=in1, op0=op0, op1=op1, accum_out=accum_out), r, w)

    def act(out, in_, func, r, w, scale=None, bias=None, accum_out=None):
        kw = {}
        if scale is not None:
            kw['scale'] = scale
        if bias is not None:
            kw['bias'] = bias
        if accum_out is not None:
            kw['accum_out'] = accum_out
        P.op('act', lambda e: e.activation(out=out, in_=in_, func=func, **kw), r, w)

    def cp(out, in_, r, w, eng='dve'):
        if eng == 'act':
            act(out, in_, AF.Copy, r, w)
        else:
            P.op(eng, lambda e: e.tensor_copy(out=out, in_=in_), r, w)

    def red(out, in_, op, r, w):
        P.op('dve', lambda e: e.tensor_reduce(out=out, in_=in_, axis=AX.X, op=op), r, w)

    def rcp(out, in_, r, w):
        P.op('dve', lambda e: e.reciprocal(out=out, in_=in_), r, w)

    def mm(out, lhsT, rhs, start, stop, r, w):
        P.op('pe', lambda e: e.matmul(out=out, lhsT=lhsT, rhs=rhs, start=start, stop=stop), r, w)

    def tr(out, in_, ident, r, w):
        P.op('pe', lambda e: e.transpose(out=out, in_=in_, identity=ident), r, w)

    def dma(out, in_, r, w, sem, eng='sp'):
        P.dma(eng, lambda e: e.dma_start(out=out, in_=in_), r, w, sem)

    def memset(ap, val, w, eng='dve'):
        P.op(eng, lambda e: e.memset(ap, val), (), w)

    def bc1(ap, shape):
        return ap.unsqueeze(1).to_broadcast(shape)

    def bc2(ap, shape):
        return ap.unsqueeze(2).to_broadcast(shape)

    identb = sbt(top, "identb", [128, 128], BF16)
    trib = sbt(top, "trib", [128, 128], BF16)
    wi_all = sbt(top, "wi_all", [128, NT, 8])
    st01 = contextlib.ExitStack()
    A1 = sbt(st01, "A1", [128, D]); B1 = sbt(st01, "B1", [128, D])
    dma(identb[:], identb_d, [], ['identb'], 'identb')
    dma(trib[:], trib_d, [], ['trib'], 'trib')

    with contextlib.ExitStack() as st:
        ccol = sbt(st, "ccol", [128, 8]); cact = sbt(st, "cact", [128, 8])
        cb = sbt(st, "cb", [128, 8, 128])
        g1b = sbt(st, "g1b", [128, D]); g2b = sbt(st, "g2b", [128, D])
        G1 = sbt(st, "G1", [128, D]); A2 = sbt(st, "A2", [128, D]); B2 = sbt(st, "B2", [128, D]); G2 = sbt(st, "G2", [128, D])
        wa = [sbt(st, "wa%d" % i, [128, 8, 512]) for i in range(2)]
        bb = [sbt(st, "bb%d" % i, [128, 512]) for i in range(2)]
        tmp0 = sbt(st, "tmp0", [128, 512])
        pm0 = [pst(st, "pm0_%d" % i, [128, 512]) for i in range(2)]
        dma(ccol[:], ccol_d, [], ['ccol'], 'ccol')
        dma(g1b[:], g1_d.to_broadcast([128, D]), [], ['g1b'], 'g1b')
        dma(g2b[:], g2_d.to_broadcast([128, D]), [], ['g2b'], 'g2b')
        act(cact[:], ccol[:], AF.Silu, ['ccol'], ['cact'])
        cp(cb[:], cact[:].unsqueeze(2).to_broadcast([128, 8, 128]), ['cact'], ['cb'])
        wada_v = wada_d.rearrange("(k p) n -> p k n", p=128)
        dests = [B1, A1, G1, B2, A2, G2]
        dnames = ['B1', 'A1', 'G1', 'B2', 'A2', 'G2']
        for n in range(12):
            sl = n % 2
            dma(wa[sl][:], wada_v[:, :, n * 512:(n + 1) * 512], [], ['wa%d' % sl], 'wa%d' % sl)
            dma(bb[sl][:], bada_d[:, n * 512:(n + 1) * 512].to_broadcast([128, 512]), [], ['bb%d' % sl], 'bb%d' % sl)
            for k in range(8):
                mm(pm0[sl][:], cb[:, k, :], wa[sl][:, k, :], k == 0, k == 7, ['cb', 'wa%d' % sl], ['pm0_%d' % sl])
            which = n // 2
            dst = dests[which][:, (n % 2) * 512:(n % 2 + 1) * 512]
            dn = dnames[which]
            if which in (1, 4):
                gsrc = (g1b if which == 1 else g2b)[:, (n % 2) * 512:(n % 2 + 1) * 512]
                tt(tmp0[:], pm0[sl][:], bb[sl][:], ALU.add, ['pm0_%d' % sl, 'bb%d' % sl], ['tmp0'])
                stt(dst, tmp0[:], 1.0, gsrc, ALU.add, ALU.mult, ['tmp0', 'g1b', 'g2b'], [dn])
            else:
                tt(dst, pm0[sl][:], bb[sl][:], ALU.add, ['pm0_%d' % sl, 'bb%d' % sl], [dn])
        for qi_, (tn_, nm_) in enumerate([(G1, 'G1'), (A2, 'A2'), (B2, 'B2'), (G2, 'G2')]):
            dma(mod_d[qi_], tn_[:], [nm_], ['mod_d'], 'mod' + nm_)
        P.barrier()

    if phases == 0:
        P.emit(nc)
        st01.close()
        top.close()
        return nc

    with contextlib.ExitStack() as st:
        winb = sbt(st, "winb", [128, 8, 4840], BF16)
        wmqb = sbt(st, "wmqb", [128, 3, 768], BF16)
        wmkvb = sbt(st, "wmkvb", [128, 2, 1024], BF16)
        gaq = sbt(st, "gaq", [128, 64]); gak = sbt(st, "gak", [128, 64]); gik = sbt(st, "gik", [128, 64])
        gmqa = sbt(st, "gmqa", [128, 384]); gmkva = sbt(st, "gmkva", [128, 256])
        gmq = sbt(st, "gmq", [128, 96]); gmk = sbt(st, "gmk", [128, 96])
        cos64 = sbt(st, "cos64", [128, NT, 32]); sin64 = sbt(st, "sin64", [128, NT, 32])
        cos32 = sbt(st, "cos32", [128, NT, 16]); sin32 = sbt(st, "sin32", [128, NT, 16])
        for tns, src, nm in [(gaq, gaq_d, 'gaq'), (gak, gak_d, 'gak'), (gik, gik_d, 'gik'), (gmqa, gmqa_d, 'gmqa'),
                             (gmkva, gmkva_d, 'gmkva'), (gmq, gmq_d, 'gmq'), (gmk, gmk_d, 'gmk')]:
            dma(tns[:], src.to_broadcast(list(tns[:].shape)), [], [nm], nm)
        with contextlib.ExitStack() as st2:
            stage = [sbt(st2, "stage%d" % i, [128, 4096]) for i in range(2)]
            win_v = win_d.rearrange("(k p) n -> p k n", p=128)
            ci = 0
            for c0 in range(0, 4840, 512):
                c1 = min(c0 + 512, 4840)
                wdt = c1 - c0
                sl = ci % 2
                sv = stage[sl][:, 0:8 * wdt].rearrange("p (k n) -> p k n", k=8)
                dma(sv, win_v[:, :, c0:c1], [], ['stage%d' % sl], 'stage%d' % sl)
                cp(winb[:, :, c0:c1], sv, ['stage%d' % sl], ['winb'], eng=('pool' if ci % 2 == 0 else 'act'))
                ci += 1
            sv = stage[ci % 2][:, 0:3 * 768].rearrange("p (k n) -> p k n", k=3)
            dma(sv, wmq_d.rearrange("(k p) n -> p k n", p=128), [], ['stage%d' % (ci % 2)], 'stage%d' % (ci % 2))
            cp(wmqb[:], sv, ['stage%d' % (ci % 2)], ['wmqb'], eng='pool')
            ci += 1
            sv = stage[ci % 2][:, 0:2 * 1024].rearrange("p (k n) -> p k n", k=2)
            dma(sv, wmkv_d.rearrange("(k p) n -> p k n", p=128), [], ['stage%d' % (ci % 2)], 'stage%d' % (ci % 2))
            cp(wmkvb[:], sv, ['stage%d' % (ci % 2)], ['wmkvb'], eng='pool')
            posi = sbt(st2, "posi", [128, NT], I32); posf = sbt(st2, "posf", [128, NT])
            inv64 = sbt(st2, "inv64", [128, 32]); inv32 = sbt(st2, "inv32", [128, 16])
            rt_a = sbt(st2, "rt_a", [128, NT, 32]); rt_k = sbt(st2, "rt_k", [128, NT, 32])
            rt_i = sbt(st2, "rt_i", [128, NT, 32], I32); rt_y = sbt(st2, "rt_y", [128, NT, 32])
            dma(posi[:], pos_d, [], ['posi'], 'posi')
            dma(inv64[:], inv64_d, [], ['inv64'], 'inv64')
            dma(inv32[:], inv32_d, [], ['inv32'], 'inv32')
            cp(posf[:], posi[:], ['posi'], ['posf'])
            for (inv, hf, cs, sn, nm) in [(inv64, 32, cos64, sin64, '64'), (inv32, 16, cos32, sin32, '32')]:
                a = rt_a[:, :, 0:hf]; kk = rt_k[:, :, 0:hf]; ii = rt_i[:, :, 0:hf]; y = rt_y[:, :, 0:hf]
                shp = [128, NT, hf]
                tt(a, bc2(posf[:], shp), bc1(inv[:], shp), ALU.mult, ['posf', 'inv' + nm], ['rt_a'])
                ts(kk, a, float(1.0 / TWO_PI), None, ALU.mult, None, ['rt_a'], ['rt_k'])
                cp(ii, kk, ['rt_k'], ['rt_i'])
                cp(kk, ii, ['rt_i'], ['rt_k'])
                stt(a, kk, -TWO_PI, a, ALU.mult, ALU.add, ['rt_k', 'rt_a'], ['rt_a'])
                ts(kk, a, float(np.pi / 2), float(np.pi), ALU.add, ALU.is_gt, ['rt_a'], ['rt_k'])
                stt(y, kk, -TWO_PI, a, ALU.mult, ALU.add, ['rt_k', 'rt_a'], ['rt_y'])
                ts(y, y, float(np.pi / 2), None, ALU.add, None, ['rt_y'], ['rt_y'])
                ts(y, y, float(np.pi), float(-np.pi), ALU.min, ALU.max, ['rt_y'], ['rt_y'])
                ts(a, a, float(np.pi), float(-np.pi), ALU.min, ALU.max, ['rt_a'], ['rt_a'])
                act(sn[:], a, AF.Sin, ['rt_a'], ['sin' + nm])
                act(cs[:], y, AF.Sin, ['rt_y'], ['cos' + nm])
            P.barrier()

        xt = [sbt(st, "xt%d" % i, [128, D]) for i in range(2)]
        junkb = sbt(st, "junkb", [128, D], BF16)
        ssq = sbt(st, "ssq", [128, 1]); rstd = sbt(st, "rstd", [128, 1])
        htmp = sbt(st, "htmp", [128, D]); hb = sbt(st, "hb", [128, D], BF16)
        hT = [sbt(st, "hT%d" % i, [128, 8, 128], BF16) for i in range(2)]
        proj = sbt(st, "proj", [128, 2792])
        sq = sbt(st, "sq", [128, 1024]); nrm = sbt(st, "nrm", [128, 1024])
        s8 = sbt(st, "s8", [128, 8]); r8 = sbt(st, "r8", [128, 8])
        rp = [sbt(st, "rp%d" % i, [128, 8, 32]) for i in range(4)]
        tokb = sbt(st, "tokb", [128, 768], BF16)
        kib = sbt(st, "kib", [128, 128], BF16)
        cqT = sbt(st, "cqT", [128, 3, 128], BF16); ckvT = sbt(st, "ckvT", [128, 2, 128], BF16)
        qmf = sbt(st, "qmf", [128, 768]); kvf = sbt(st, "kvf", [128, 8, 128]); kmpre = sbt(st, "kmpre", [128, 8, 96])
        vaug = sbt(st, "vaug", [128, 8, 65], BF16); vmaug = sbt(st, "vmaug", [128, 8, 65], BF16)
        qaT_t = sbt(st, "qaT_t", [128, 4, 128], BF16); kaT_t = sbt(st, "kaT_t", [128, 4, 128], BF16)
        qiT_t = sbt(st, "qiT_t", [128, 4, 128], BF16); kiT_t = sbt(st, "kiT_t", [128, 128], BF16)
        qmT_t = sbt(st, "qmT_t", [128, 8, 128], BF16); kmT_t = sbt(st, "kmT_t", [128, 8, 128], BF16)
        gS_t = sbt(st, "gS_t", [128, 16, 128], BF16)
        pT = pst(st, "pT", [128, 8, 128], BF16)
        pT2 = pst(st, "pT2", [128, 8, 128], BF16)
        pproj = [pst(st, "pproj%d" % i, [128, 512]) for i in range(2)]
        pgate = pst(st, "pgate", [128, 512])
        pm = pst(st, "pm", [128, 1024])
        memset(vaug[:], 1.0, ['vaug'], eng='pool')
        memset(vmaug[:], 1.0, ['vmaug'], eng='pool')

        def rms_heads(src3, H, Dh, gain, dst3, rs, ws, gname):
            shp = [128, H, Dh]
            sqv = sq[:, 0:H * Dh].rearrange("p (h d) -> p h d", h=H)
            tt(sqv, src3, src3, ALU.mult, rs, ['sq'])
            red(s8[:, 0:H], sqv, ALU.add, ['sq'], ['s8'])
            ts(s8[:, 0:H], s8[:, 0:H], float(1.0 / Dh), float(EPS), ALU.mult, ALU.add, ['s8'], ['s8'])
            act(s8[:, 0:H], s8[:, 0:H], AF.Sqrt, ['s8'], ['s8'])
            rcp(r8[:, 0:H], s8[:, 0:H], ['s8'], ['r8'])
            nv = nrm[:, 0:H * Dh].rearrange("p (h d) -> p h d", h=H)
            tt(nv, src3, bc2(r8[:, 0:H], shp), ALU.mult, rs + ['r8'], ['nrm'])
            tt(dst3, nv, bc1(gain[:], shp), ALU.mult, ['nrm', gname], ws)

        def rope(src3, H, hf, cosv, sinv, dst3, rs, ws, cname):
            shp = [128, H, hf]
            x1 = src3[:, :, 0:hf]; x2 = src3[:, :, hf:2 * hf]
            cb_ = bc1(cosv, shp); sb_ = bc1(sinv, shp)
            t = [rp[i][:, 0:H, 0:hf] for i in range(4)]
            tt(t[0], x1, cb_, ALU.mult, rs + ['cos' + cname], ['rp0'])
            tt(t[1], x2, sb_, ALU.mult, rs + ['sin' + cname], ['rp1'], eng='pool')
            tt(dst3[:, :, 0:hf], t[0], t[1], ALU.subtract, ['rp0', 'rp1'], ws)
            tt(t[2], x2, cb_, ALU.mult, rs + ['cos' + cname], ['rp2'])
            tt(t[3], x1, sb_, ALU.mult, rs + ['sin' + cname], ['rp3'], eng='pool')
            tt(dst3[:, :, hf:2 * hf], t[2], t[3], ALU.add, ['rp2', 'rp3'], ws)

        for i in range(nt1):
            hs = i % 2
            hTn = 'hT%d' % hs
            xs = i % 2
            xn = 'xt%d' % xs
            tok = slice(i * 128, (i + 1) * 128)
            dma(xt[xs][:], x_d[tok, :], [], [xn], xn)
            act(junkb[:], xt[xs][:], AF.Square, [xn], ['junkb', 'ssq'], accum_out=ssq[:])
            ts(ssq[:], ssq[:], float(1.0 / D), float(EPS), ALU.mult, ALU.add, ['ssq'], ['ssq'])
            act(ssq[:], ssq[:], AF.Sqrt, ['ssq'], ['ssq'])
            rcp(rstd[:], ssq[:], ['ssq'], ['rstd'])
            stt(htmp[:], xt[xs][:], rstd[:, 0:1], A1[:], ALU.mult, ALU.mult, [xn, 'rstd', 'A1'], ['htmp'])
            tt(hb[:], htmp[:], B1[:], ALU.add, ['htmp', 'B1'], ['hb'], eng='pool')
            for k in range(8):
                tr(pT[:, k, :], hb[:, k * 128:(k + 1) * 128], identb[:], ['hb', 'identb'], ['pT'])
            cp(hT[hs][:], pT[:], ['pT'], [hTn], eng='act')
            for cc in range(6):
                c0 = cc * 512
                c1 = min(c0 + 512, 2792)
                pp = cc % 2
                for k in range(8):
                    mm(pproj[pp][:, 0:c1 - c0], hT[hs][:, k, :], winb[:, k, c0:c1], k == 0, k == 7,
                       [hTn, 'winb'], ['pproj%d' % pp])
                cp(proj[:, c0:c1], pproj[pp][:, 0:c1 - c0], ['pproj%d' % pp], ['proj'], eng='act')
            for fc in range(16):
                c0 = 2792 + fc * 128
                for k in range(8):
                    mm(pgate[:, (fc % 4) * 128:(fc % 4 + 1) * 128], winb[:, k, c0:c0 + 128], hT[hs][:, k, :], k == 0, k == 7,
                       [hTn, 'winb'], ['pgate'])
                if fc % 4 == 3:
                    act(gS_t[:, fc - 3:fc + 1, :].rearrange("p c t -> p (c t)"), pgate[:], AF.Sigmoid, ['pgate'], ['gS_t'])
            dma(gS_d[:, :, tok].rearrange("c p t -> p c t"), gS_t[:], ['gS_t'], ['gS_d'], 'gS_t')
            cs64 = cos64[:, i, :]; sn64 = sin64[:, i, :]; cs32 = cos32[:, i, :]; sn32 = sin32[:, i, :]
            for (c0, gn, gt_, dstT, dn, dd_) in [(0, 'gaq', gaq, qaT_t, 'qaT_t', qaT_d), (512, 'gak', gak, kaT_t, 'kaT_t', kaT_d)]:
                src3 = proj[:, c0:c0 + 512].rearrange("p (h d) -> p h d", h=8)
                n3 = sq[:, 0:512].rearrange("p (h d) -> p h d", h=8)
                rms_heads(src3, 8, 64, gt_, n3, ['proj'], ['sq'], gn)
                rope(n3, 8, 32, cs64, sn64, tokb[:, 0:512].rearrange("p (h d) -> p h d", h=8), ['sq'], ['tokb'], '64')
                for k in range(4):
                    tr(pT2[:, k, :], tokb[:, k * 128:(k + 1) * 128], identb[:], ['tokb', 'identb'], ['pT2'])
                cp(dstT[:], pT2[:, 0:4, :], ['pT2'], [dn])
                dma(dd_[:, :, tok].rearrange("c p t -> p c t"), dstT[:], [dn], [dn + '_d'], dn)
            rope(proj[:, 1536:2048].rearrange("p (h d) -> p h d", h=8), 8, 32, cs64, sn64,
                 tokb[:, 0:512].rearrange("p (h d) -> p h d", h=8), ['proj'], ['tokb'], '64')
            for k in range(4):
                tr(pT2[:, k, :], tokb[:, k * 128:(k + 1) * 128], identb[:], ['tokb', 'identb'], ['pT2'])
            cp(qiT_t[:], pT2[:, 0:4, :], ['pT2'], ['qiT_t'])
            dma(qiT_d[:, :, tok].rearrange("c p t -> p c t"), qiT_t[:], ['qiT_t'], ['qiT_t_d'], 'qiT_t')
            n3 = sq[:, 0:64].rearrange("p (h d) -> p h d", h=1)
            rms_heads(proj[:, 2048:2112].rearrange("p (h d) -> p h d", h=1), 1, 64, gik, n3, ['proj'], ['sq'], 'gik')
            rope(n3, 1, 32, cs64, sn64, kib[:, 0:64].rearrange("p (h d) -> p h d", h=1), ['sq'], ['kib'], '64')
            cp(kib[:, 64:128], kib[:, 0:64], ['kib'], ['kib'])
            tr(pT2[:, 0, :], kib[:], identb[:], ['kib', 'identb'], ['pT2'])
            cp(kiT_t[:], pT2[:, 0, :], ['pT2'], ['kiT_t'])
            dma(kiT_d[:, tok], kiT_t[:], ['kiT_t'], ['kiT_t_d'], 'kiT_t')
            ts(wi_all[:, i, :], proj[:, 2112:2120], float(512 ** -0.5), None, ALU.mult, None, ['proj'], ['wi_all'])
            cp(vaug[:, :, 0:64], proj[:, 1024:1536].rearrange("p (h d) -> p h d", h=8), ['proj'], ['vaug'], eng='pool')
            dma(va_d[tok, :], vaug[:].rearrange("p h d -> p (h d)"), ['vaug'], ['va_d'], 'vaug')
            rms_heads(proj[:, 2120:2504].rearrange("p (h d) -> p h d", h=1), 1, 384, gmqa,
                      tokb[:, 0:384].rearrange("p (h d) -> p h d", h=1), ['proj'], ['tokb'], 'gmqa')
            for k in range(3):
                tr(pT2[:, k, :], tokb[:, k * 128:(k + 1) * 128], identb[:], ['tokb', 'identb'], ['pT2'])
            cp(cqT[:], pT2[:, 0:3, :], ['pT2'], ['cqT'])
            for (c0, c1) in [(0, 512), (512, 768)]:
                for k in range(3):
                    mm(pm[:, c0:c1], cqT[:, k, :], wmqb[:, k, c0:c1], k == 0, k == 2, ['cqT', 'wmqb'], ['pm'])
            cp(qmf[:], pm[:, 0:768], ['pm'], ['qmf'], eng='act')
            q3 = qmf[:].rearrange("p (h d) -> p h d", h=8)
            n96 = sq[:, 0:768].rearrange("p (h d) -> p h d", h=8)
            rms_heads(q3, 8, 96, gmq, n96, ['qmf'], ['sq'], 'gmq')
            tb96 = tokb[:, 0:768].rearrange("p (h d) -> p h d", h=8)
            cp(tb96[:, :, 0:64], n96[:, :, 0:64], ['sq'], ['tokb'], eng='pool')
            rope(n96[:, :, 64:96], 8, 16, cs32, sn32, tb96[:, :, 64:96], ['sq'], ['tokb'], '32')
            for h in range(8):
                tr(pT2[0:96, h, :], tokb[:, h * 96:(h + 1) * 96], identb[:], ['tokb', 'identb'], ['pT2'])
            cp(qmT_t[0:96, :, :], pT2[0:96, :, :], ['pT2'], ['qmT_t'])
            dma(qmT_d[:, :, tok].rearrange("h d t -> d h t"), qmT_t[0:96, :, :], ['qmT_t'], ['qmT_t_d'], 'qmT_t')
            rms_heads(proj[:, 2504:2760].rearrange("p (h d) -> p h d", h=1), 1, 256, gmkva,
                      tokb[:, 0:256].rearrange("p (h d) -> p h d", h=1), ['proj'], ['tokb'], 'gmkva')
            for k in range(2):
                tr(pT2[:, k, :], tokb[:, k * 128:(k + 1) * 128], identb[:], ['tokb', 'identb'], ['pT2'])
            cp(ckvT[:], pT2[:, 0:2, :], ['pT2'], ['ckvT'])
            for (c0, c1) in [(0, 512), (512, 1024)]:
                for k in range(2):
                    mm(pm[:, c0:c1], ckvT[:, k, :], wmkvb[:, k, c0:c1], k == 0, k == 1, ['ckvT', 'wmkvb'], ['pm'])
            cp(kvf[:].rearrange("p h d -> p (h d)"), pm[:], ['pm'], ['kvf'], eng='act')
            cp(kmpre[:, :, 0:64], kvf[:, :, 0:64], ['kvf'], ['kmpre'], eng='pool')
            cp(kmpre[:, :, 64:96], bc1(proj[:, 2760:2792], [128, 8, 32]), ['proj'], ['kmpre'], eng='pool')
            cp(vmaug[:, :, 0:64], kvf[:, :, 64:128], ['kvf'], ['vmaug'], eng='pool')
            dma(vm_d[tok, :], vmaug[:].rearrange("p h d -> p (h d)"), ['vmaug'], ['vm_d'], 'vmaug')
            rms_heads(kmpre[:], 8, 96, gmk, n96, ['kmpre'], ['sq'], 'gmk')
            cp(tb96[:, :, 0:64], n96[:, :, 0:64], ['sq'], ['tokb'], eng='pool')
            rope(n96[:, :, 64:96], 8, 16, cs32, sn32, tb96[:, :, 64:96], ['sq'], ['tokb'], '32')
            for h in range(8):
                tr(pT2[0:96, h, :], tokb[:, h * 96:(h + 1) * 96], identb[:], ['tokb', 'identb'], ['pT2'])
            cp(kmT_t[0:96, :, :], pT2[0:96, :, :], ['pT2'], ['kmT_t'])
            dma(kmT_d[:, :, tok].rearrange("h d t -> d h t"), kmT_t[0:96, :, :], ['kmT_t'], ['kmT_t_d'], 'kmT_t')
        P.barrier()
    st01.close()

    def attention_phase(tag, dsa):
        with contextlib.ExitStack() as st:
            ones64 = sbt(st, "ones64" + tag, [128, 64])
            memset(ones64[:], 1.0, ['ones64'])
            if dsa:
                kT = sbt(st, "kaT", [128, 4, S], BF16)
                kiT = sbt(st, "kiT", [128, S], BF16)
                for c in range(4):
                    dma(kT[:, c, :], kaT_d[c], ['kaT_t_d'], ['kT%d' % c], 'kT%d' % c)
                dma(kiT[:], kiT_d, ['kiT_t_d'], ['kiT'], 'kiT')
                v_src = va_d
                qT_g = sbt(st, "qaTg", [128, 4, 512], BF16)
                qiTg = [sbt(st, "qiTg%d" % i, [128, 4, 512], BF16) for i in range(2)]
                score = sbt(st, "score", [128, S])
                junk = sbt(st, "junkc", [128, S], BF16)
                bias_g = [sbt(st, "bias_g%d" % i, [128, 4, S], BF16) for i in range(2)]
                dg = sbt(st, "dg", [128, 8, 128], BF16)
                rbuf = [sbt(st, "rbuf%d" % i, [128, 512], BF16) for i in range(2)]
                lo = sbt(st, "lo", [128, 1]); hi = sbt(st, "hi", [128, 1]); mid = sbt(st, "mid", [128, 1])
                wv = sbt(st, "wv", [128, NITER + 1]); cnt = sbt(st, "cnt", [128, 1]); stp = sbt(st, "stp", [128, 1])
                pd = [pst(st, "pd%d" % i, [128, 512]) for i in range(2)]
                psc = pst(st, "psc", [128, 512])
                scale = 64 ** -0.5
                Kd = 64
                out_d = atA_d
            else:
                kT = sbt(st, "kmT", [128, 8, S], BF16)
                for h in range(8):
                    dma(kT[0:96, h, :], kmT_d[h], ['kmT_t_d'], ['kT%d' % h], 'kT%d' % h)
                v_src = vm_d
                qT_g = sbt(st, "qmTg", [128, 8, 512], BF16)
                scale = 96 ** -0.5
                Kd = 96
                out_d = atM_d
            vv = sbt(st, "vv" + tag, [128, NT, 520], BF16)
            v_v = v_src.rearrange("(t p) c -> p t c", p=128)
            for q in range(4):
                dma(vv[:, q * 8:(q + 1) * 8, :], v_v[:, q * 8:(q + 1) * 8, :], ['va_d', 'vm_d'], ['vv%d' % q], 'vv%d' % q)
            vres = ['vv%d' % q for q in range(4)]
            ptb = [sbt(st, "ptb%d%s" % (i, tag), [128, 512], BF16) for i in range(2)]
            rz = sbt(st, "rz" + tag, [128, 512]); of = sbt(st, "of" + tag, [128, 512])
            yb = [sbt(st, "yb%d%s" % (i, tag), [128, 512], BF16) for i in range(2)]
            pS = [pst(st, "pS%d%s" % (i, tag), [128, 512]) for i in range(2)]
            pO = [pst(st, "pO%d%s" % (i, tag), [128, 512]) for i in range(2)]
            pB = pst(st, "pB" + tag, [128, 512])
            kres = ['kT%d' % c for c in range(8)]
            ctr = 0
            def idx_block(g, b):
                bg = bias_g[g % 2]
                bgn = 'bias_g%d' % (g % 2)
                jq = 4 * g + b
                Sp = (jq + 1) * 128
                shp = [128, 8, 128]
                tt(dg[:], bc1(identb[:], shp), bc2(wi_all[:, jq, :], shp), ALU.mult, ['identb', 'wi_all'], ['dg'])
                items_i = [(c, h) for c in range(g + 1) for h in range(8)]
                qn = 'qiTg%d' % (g % 2)
                qi_ = qiTg[g % 2]

                def idx_dots(k_):
                    c, h = items_i[k_]
                    wc = 512 if c < g else (b + 1) * 128
                    k0 = c * 512
                    hp = h % 2; hc = h // 2
                    prt = slice(hp * 64, hp * 64 + 64)
                    dd = k_ % 2
                    mm(pd[dd][:, 0:wc], qi_[prt, hc, b * 128:(b + 1) * 128], kiT[prt, k0:k0 + wc], True, True,
                       [qn, 'kiT'], ['pd%d' % dd])
                    act(rbuf[dd][:, 0:wc], pd[dd][:, 0:wc], AF.Relu, ['pd%d' % dd], ['rbuf%d' % dd])

                def idx_acc(k_):
                    c, h = items_i[k_]
                    wc = 512 if c < g else (b + 1) * 128
                    k0 = c * 512
                    dd = k_ % 2
                    mm(psc[:, 0:wc], dg[:, h, :], rbuf[dd][:, 0:wc], h == 0, h == 7, ['dg', 'rbuf%d' % dd], ['psc'])
                    if h == 7:
                        if c < g:
                            cp(score[:, k0:k0 + 512], psc[:], ['psc'], ['score'])
                        else:
                            if b > 0:
                                cp(score[:, k0:k0 + b * 128], psc[:, 0:b * 128], ['psc'], ['score'])
                            tt(score[:, jq * 128:(jq + 1) * 128], psc[:, b * 128:(b + 1) * 128], trib[:], ALU.add,
                               ['psc', 'trib'], ['score'])
                idx_dots(0)
                for k_ in range(len(items_i)):
                    if k_ + 1 < len(items_i):
                        idx_dots(k_ + 1)
                    idx_acc(k_)
                if jq >= 2:
                    red(hi[:], score[:, 0:Sp], ALU.max, ['score'], ['hi'])
                    red(lo[:], score[:, 0:jq * 128], ALU.min, ['score'], ['lo'])
                    tt(stp[:], hi[:], lo[:], ALU.subtract, ['hi', 'lo'], ['stp'])
                    for k in range(NITER + 1):
                        ts(wv[:, k:k + 1], stp[:], float(2.0 ** -(k + 1)), None, ALU.mult, None, ['stp'], ['wv'])
                    tt(mid[:], lo[:], wv[:, 0:1], ALU.add, ['lo', 'wv'], ['mid'])
                    for k in range(NITER):
                        ts(junk[:, 0:Sp], score[:, 0:Sp], mid[:, 0:1], 0.0, ALU.is_ge, ALU.add, ['score', 'mid'],
                           ['junk', 'cnt'], accum_out=cnt[:])
                        ts(stp[:], cnt[:], 255.5, wv[:, k:k + 1], ALU.is_ge, ALU.mult, ['cnt', 'wv'], ['stp'])
                        tt(lo[:], lo[:], stp[:], ALU.add, ['lo', 'stp'], ['lo'])
                        tt(mid[:], lo[:], wv[:, k + 1:k + 2], ALU.add, ['lo', 'wv'], ['mid'])
                else:
                    memset(lo[:], -10000.0, ['lo'])
                ts(bg[:, b, 0:Sp], score[:, 0:Sp], lo[:, 0:1], NEG, ALU.is_lt, ALU.mult, ['score', 'lo'], [bgn])

            def att_part(g, heads):
                gsl = slice(g * 512, (g + 1) * 512)
                nsc = 4 * g + 4
                items_a = [(h, sc) for h in heads for sc in range(nsc)]
                if dsa:
                    bg = bias_g[g % 2]
                    bgn = 'bias_g%d' % (g % 2)

                def att_S(k_):
                    h, sc = items_a[k_]
                    r_ = sc - 4 * g
                    qlo = max(r_, 0) * 128
                    ss = k_ % 2
                    ssl = slice(sc * 128, (sc + 1) * 128)
                    if dsa:
                        hp = h % 2; hc = h // 2
                        prt = slice(hp * 64, hp * 64 + 64)
                        mm(pS[ss][:, qlo:512], kT[prt, hc, ssl], qT_g[prt, hc, qlo:512], True, False,
                           ['kT%d' % hc, 'qT_g'], ['pS%d' % ss])
                        b0 = max(r_, 0)
                        for b in range(b0, 4):
                            mm(pS[ss][:, b * 128:(b + 1) * 128], bg[:, b, ssl], identb[:], False, b == 3,
                               [bgn, 'identb'], ['pS%d' % ss])
                    else:
                        last = r_ < 0
                        mm(pS[ss][:, qlo:512], kT[0:96, h, ssl], qT_g[0:96, h, qlo:512], True, last,
                           ['kT%d' % h, 'qT_g'], ['pS%d' % ss])
                        if r_ >= 0:
                            mm(pS[ss][:, qlo:qlo + 128], trib[:], identb[:], False, True, ['trib', 'identb'], ['pS%d' % ss])
                    act(ptb[ss][:, qlo:512], pS[ss][:, qlo:512], AF.Exp, ['pS%d' % ss], ['ptb%d' % ss], scale=float(scale))

                def att_PV(k_):
                    h, sc = items_a[k_]
                    r_ = sc - 4 * g
                    qlo = max(r_, 0) * 128
                    ss = k_ % 2
                    po = h % 2
                    pon = 'pO%d' % po
                    mm(pO[po][0:65, qlo:512], vv[:, sc, h * 65:(h + 1) * 65], ptb[ss][:, qlo:512], sc == 0, sc == nsc - 1,
                       ['ptb%d' % ss] + vres, [pon])
                    if sc == nsc - 1:
                        rcp(rz[64:65, :], pO[po][64:65, :], [pon], ['rz'])
                        mm(pB[0:64, :], ones64[64:65, 0:64], rz[64:65, :], True, True, ['ones64', 'rz'], ['pB'])
                        cp(of[0:64, :], pO[po][0:64, :], [pon], ['of'], eng='act')
                        ybn = 'yb%d' % po
                        tt(yb[po][0:64, :], of[0:64, :], pB[0:64, :], ALU.mult, ['of', 'pB'], [ybn])
                        dma(out_d[h, :, gsl], yb[po][0:64, :], [ybn], ['at_d' + tag], ybn + tag)
                att_S(0)
                for k_ in range(len(items_a)):
                    if k_ + 1 < len(items_a):
                        att_S(k_ + 1)
                    att_PV(k_)

            def load_q(g):
                gsl = slice(g * 512, (g + 1) * 512)
                if dsa:
                    dma(qT_g[:], qaT_d[:, :, gsl].rearrange("c p t -> p c t"), ['qaT_t_d'], ['qT_g'], 'qT_g')
                else:
                    dma(qT_g[0:96, :, :], qmT_d[:, :, gsl].rearrange("h d t -> d h t"), ['qmT_t_d'], ['qT_g'], 'qT_g')

            def load_qi(g):
                gsl = slice(g * 512, (g + 1) * 512)
                dma(qiTg[g % 2][:], qiT_d[:, :, gsl].rearrange("c p t -> p c t"), ['qiT_t_d'], ['qiTg%d' % (g % 2)], 'qiTg%d' % (g % 2))

            if dsa:
                load_qi(0)
                for b in range(4):
                    idx_block(0, b)
                for g in range(NG):
                    load_q(g)
                    if g + 1 < NG:
                        load_qi(g + 1)
                    for part in range(4):
                        if g + 1 < NG:
                            idx_block(g + 1, part)
                        att_part(g, [2 * part, 2 * part + 1])
            else:
                stgc = [sbt(st, "stgc%d" % i, [128, 4096]) for i in range(2)]
                cvb = [sbt(st, "cvb%d" % i, [128, 4, D], BF16) for i in range(3)]
                uv_v = uv_d.rearrange("(p r) d -> p r d", p=128)
                conv = [(tsrc, col, rc) for (tsrc, col) in [(pu_d, 0), (pv_d, D)] for rc in range(32)]

                def conv_iter(it):
                    tsrc, col, rc = conv[it]
                    t_v = tsrc.rearrange("(p r) d -> p r d", p=128)
                    sl = it % 2
                    cs_ = it % 3
                    sv = stgc[sl][:].rearrange("p (r d) -> p r d", r=4)
                    dma(sv, t_v[:, rc * 4:(rc + 1) * 4, :], [], ['stgc%d' % sl], 'stgc%d' % sl)
                    cp(cvb[cs_][:], sv, ['stgc%d' % sl], ['cvb%d' % cs_], eng=('pool', 'dve')[it % 2])
                    dma(uv_v[:, rc * 4:(rc + 1) * 4, col:col + D], cvb[cs_][:], ['cvb%d' % cs_], ['uv_d%d' % cs_], 'cvb%d' % cs_)
                for g in range(NG):
                    load_q(g)
                    for it in range(g * 8, g * 8 + 8):
                        conv_iter(it)
                    att_part(g, list(range(8)))
            P.barrier()

    if phases >= 2:
        attention_phase("A", True)
    if phases >= 3:
        attention_phase("M", False)

    if phases >= 4:
        with contextlib.ExitStack() as st:
            woab = sbt(st, "woab", [128, 4, D], BF16); womb = sbt(st, "womb", [128, 4, D], BF16)
            woutb = sbt(st, "woutb", [128, 8, D], BF16); wpqb = sbt(st, "wpqb", [128, 8, 2048], BF16)
            subkT = sbt(st, "subkT", [128, 16, 128], BF16)
            iota16 = sbt(st, "iota16", [128, 16])
            G1 = sbt(st, "G1p", [128, D]); A2 = sbt(st, "A2p", [128, D]); B2 = sbt(st, "B2p", [128, D]); G2 = sbt(st, "G2p", [128, D])
            dma(iota16[:], iota16_d, [], ['iota16'], 'iota16')
            for qi_, (tn_, nm_) in enumerate([(G1, 'G1'), (A2, 'A2'), (B2, 'B2'), (G2, 'G2')]):
                dma(tn_[:], mod_d[qi_], ['mod_d'], [nm_], 'ld' + nm_)
            pkb = [pst(st, "pk%d" % i, [128, 512]) for i in range(2)]
            pk = pkb[0]
            pT4 = pst(st, "pT4", [128, 8, 128], BF16)
            pacc = pst(st, "pacc", [128, 1024])
            with contextlib.ExitStack() as st2:
                stg = [sbt(st2, "stg%d" % i, [128, 4096]) for i in range(2)]
                skb = sbt(st2, "skb", [128, 16, 128], BF16)
                ci = 0
                for (wsrc, wdst, nk, ncol, nm) in [(woa_d, woab, 4, 1024, 'woab'), (wom_d, womb, 4, 1024, 'womb'),
                                                    (wout_d, woutb, 8, 1024, 'woutb'), (wpq_d, wpqb, 8, 2048, 'wpqb')]:
                    wv_ = wsrc.rearrange("(k p) n -> p k n", p=128)
                    cw = 4096 // nk
                    for c0 in range(0, ncol, cw):
                        sl = ci % 2
                        sv = stg[sl][:, 0:nk * cw].rearrange("p (k n) -> p k n", k=nk)
                        dma(sv, wv_[:, :, c0:c0 + cw], [], ['stg%d' % sl], 'stg%d' % sl)
                        cp(wdst[:, :, c0:c0 + cw], sv, ['stg%d' % sl], [nm], eng=('pool' if ci % 2 == 0 else 'act'))
                        ci += 1
                sl = ci % 2
                sv = stg[sl][:, 0:2048].rearrange("p (c d) -> p c d", c=16)
                dma(sv, subk_d.rearrange("c n d -> n c d"), [], ['stg%d' % sl], 'stg%d' % sl)
                cp(skb[:], sv, ['stg%d' % sl], ['skb'])
                for c in range(16):
                    tr(pT4[:, c % 8, :], skb[:, c, :], identb[:], ['skb', 'identb'], ['pT4'])
                    if c % 8 == 7:
                        cp(subkT[:, c - 7:c + 1, :], pT4[:], ['pT4'], ['subkT'])
                P.barrier()

            def sb4(name, shape, dt=F32):
                return sbt(st, name, shape, dt)
            aT_t = sb4("aT_t", [128, 4, 128], BF16); mT_t = sb4("mT_t", [128, 4, 128], BF16)
            gS4 = sb4("gS4", [128, 16, 128], BF16)
            t1 = sb4("t1", [128, 128]); t2 = sb4("t2", [128, 128])
            mixT = sb4("mixT", [128, 8, 128], BF16)
            xt2 = sb4("xt2", [128, D])
            tmpx = xt2
            x1 = [sb4("x1_%d" % i, [128, D]) for i in range(2)]
            ssq = sb4("ssq4", [128, 1]); rstd = sb4("rstd4", [128, 1])
            h2b = [sb4("h2b%d" % i, [128, D], BF16) for i in range(2)]; h2T = sb4("h2T", [128, 8, 128], BF16)
            prod = [sb4("prod%d" % i, [128, D], BF16) for i in range(2)]
            qpT = sb4("qpT", [128, 16, 128], BF16)
            s_all = sb4("s_all", [128, 16, 128]); s_wk = sb4("s_wk", [128, 128])
            tops = sb4("tops", [128, 16, 16]); topi = sb4("topi", [128, 16, 16], U32); topf = sb4("topf", [128, 16, 16])
            cand = s_all[:].rearrange("p (h two) n -> p h (two n)", two=2)
            cwk = sb4("cwk", [128, 256])
            best = sb4("best", [128, 8, 16]); bpos = sb4("bpos", [128, 8, 16], U32)
            ak = sb4("ak", [128, 8, 16], U32); bk = sb4("bk", [128, 8, 16], U32)
            akf = sb4("akf", [128, 8, 16]); bkf = sb4("bkf", [128, 8, 16])
            oh = s_all[:].rearrange("p c (a b) -> p (c a) b", b=16).rearrange("p (h k) a -> p h k a", h=8)
            i0 = sb4("i0", [128, 8, 16]); i1 = sb4("i1", [128, 8, 16])
            idf = sb4("idf", [128, 128])
            ids = [sb4("ids%d" % i, [128, 128], U32) for i in range(2)]
            gate = [sb4("gate%d" % i, [128, 8, 16]) for i in range(2)]
            gz = sb4("gz", [128, 8]); ngmax = sb4("ngmax", [128, 8])
            actv = [sb4("actv%d" % i, [128, 128]) for i in range(2)]
            coef = [sb4("coef%d" % i, [128, 128]) for i in range(2)]
            gtmp = [sb4("gtmp%d" % i, [128, 128]) for i in range(2)]
            NRING = 16
            uvb = [sb4("uvb%d" % i, [128, 2 * D], BF16) for i in range(NRING)]
            dgb = [sb4("dgb%d" % i, [128, 128], BF16) for i in range(4)]
            ob = xt2
            GELU_S = float(2.0 * np.sqrt(2.0 / np.pi))
            shp4 = [128, 8, 16, 16]

            def prologue(i):
                p = i % 2
                tok = slice(i * 128, (i + 1) * 128)
                X1 = 'x1_%d' % p
                PH = 'ph2_%d' % p
                dma(aT_t[:], atA_d[:, :, tok].rearrange("(c two) d t -> (two d) c t", two=2), ['at_dA'], ['aT_t'], 'aT_t')
                dma(mT_t[:], atM_d[:, :, tok].rearrange("(c two) d t -> (two d) c t", two=2), ['at_dM'], ['mT_t'], 'mT_t')
                dma(gS4[:], gS_d[:, :, tok].rearrange("c p t -> p c t"), ['gS_d'], ['gS4'], 'gS4')
                dma(xt2[:], x_d[tok, :], [], ['xt2'], 'xt2')
                yield

                def mix_mm(fc):
                    fsl = slice(fc * 128, (fc + 1) * 128)
                    pkx = pkb[fc % 2]
                    pn = 'pk%d' % (fc % 2)
                    for k in range(4):
                        mm(pkx[:, 0:128], woab[:, k, fsl], aT_t[:, k, :], k == 0, k == 3, ['woab', 'aT_t'], [pn])
                    for k in range(4):
                        mm(pkx[:, 128:256], womb[:, k, fsl], mT_t[:, k, :], k == 0, k == 3, ['womb', 'mT_t'], [pn])

                def mix_dve(fc):
                    pkx = pkb[fc % 2]
                    pn = 'pk%d' % (fc % 2)
                    tt(t1[:], pkx[:, 0:128], gS4[:, fc, :], ALU.mult, [pn, 'gS4'], ['t1'])
                    tt(t2[:], pkx[:, 128:256], gS4[:, 8 + fc, :], ALU.mult, [pn, 'gS4'], ['t2'])
                    tt(mixT[:, fc, :], t1[:], t2[:], ALU.add, ['t1', 't2'], ['mixT'])
                mix_mm(0); mix_mm(1)
                yield
                for s_ in range(1, 4):
                    mix_dve(2 * s_ - 2); mix_dve(2 * s_ - 1)
                    mix_mm(2 * s_); mix_mm(2 * s_ + 1)
                    yield
                mix_dve(6); mix_dve(7)
                PKB = ['pk0', 'pk1']
                for k in range(8):
                    mm(pkb[0][:], mixT[:, k, :], woutb[:, k, 0:512], k == 0, k == 7, ['mixT', 'woutb'], ['pk0'])
                for k in range(8):
                    mm(pkb[1][:], mixT[:, k, :], woutb[:, k, 512:1024], k == 0, k == 7, ['mixT', 'woutb'], ['pk1'])
                yield
                tt(x1[p][:, 0:512], pkb[0][:], G1[:, 0:512], ALU.mult, ['pk0', 'G1'], [X1])
                tt(x1[p][:, 512:1024], pkb[1][:], G1[:, 512:1024], ALU.mult, ['pk1', 'G1'], [X1])
                tt(x1[p][:], x1[p][:], xt2[:], ALU.add, [X1, 'xt2'], [X1])
                yield
                act(prod[0][:], x1[p][:], AF.Square, [X1], ['prod0', 'ssq4'], accum_out=ssq[:])
                act(ssq[:], ssq[:], AF.Sqrt, ['ssq4'], ['ssq4'], scale=float(1.0 / D), bias=float(EPS))
                yield
                rcp(rstd[:], ssq[:], ['ssq4'], ['rstd4'])
                stt(tmpx[:], x1[p][:], rstd[:, 0:1], A2[:], ALU.mult, ALU.mult, [X1, 'rstd4', 'A2'], ['xt2'])
                tt(h2b[p][:], tmpx[:], B2[:], ALU.add, ['xt2', 'B2'], [PH])
                yield
                for k in range(8):
                    tr(pT4[:, k, :], h2b[p][:, k * 128:(k + 1) * 128], identb[:], [PH, 'identb'], ['pT4'])
                yield
                cp(h2T[:], pT4[:], ['pT4'], ['h2T'], eng='act')

                def q_mm(c4):
                    for cc in range(4):
                        c = c4 * 4 + cc
                        for k in range(8):
                            mm(pkb[c4 % 2][:, cc * 128:(cc + 1) * 128], wpqb[:, k, c * 128:(c + 1) * 128], h2T[:, k, :],
                               k == 0, k == 7, ['wpqb', 'h2T'], ['pk%d' % (c4 % 2)])

                def q_ev(c4):
                    cp(qpT[:, c4 * 4:(c4 + 1) * 4, :].rearrange("p c t -> p (c t)"), pkb[c4 % 2][:], ['pk%d' % (c4 % 2)], ['qpT'], eng='act')

                def s_mm(c4):
                    for cc in range(4):
                        c = c4 * 4 + cc
                        mm(pkb[c4 % 2][:, cc * 128:(cc + 1) * 128], qpT[:, c, :], subkT[:, c, :], True, True, ['qpT', 'subkT'],
                           ['pk%d' % (c4 % 2)])

                def s_ev(c4):
                    cp(s_all[:, c4 * 4:(c4 + 1) * 4, :].rearrange("p c t -> p (c t)"), pkb[c4 % 2][:], ['pk%d' % (c4 % 2)], ['s_all'], eng='act')

                def topk1(c):
                    sv_ = s_all[:, c, :]
                    P.op('dve', lambda e, c=c, sv_=sv_: e.max(out=tops[:, c, 0:8], in_=sv_), ['s_all'], ['tops'])
                    P.op('dve', lambda e, c=c, sv_=sv_: e.max_index(out=topi[:, c, 0:8], in_max=tops[:, c, 0:8], in_values=sv_),
                         ['s_all', 'tops'], ['topi'])
                    P.op('dve', lambda e, c=c, sv_=sv_: e.match_replace(out=s_wk[:], in_to_replace=tops[:, c, 0:8], in_values=sv_,
                                                                    imm_value=-1e30), ['s_all', 'tops'], ['s_wk'])
                    P.op('dve', lambda e, c=c: e.max(out=tops[:, c, 8:16], in_=s_wk[:]), ['s_wk'], ['tops'])
                    P.op('dve', lambda e, c=c: e.max_index(out=topi[:, c, 8:16], in_max=tops[:, c, 8:16], in_values=s_wk[:]),
                         ['s_wk', 'tops'], ['topi'])
                q_mm(0)
                yield
                for c4 in range(1, 4):
                    q_ev(c4 - 1); q_mm(c4)
                    yield
                q_ev(3); s_mm(0)
                yield
                s_ev(0); s_mm(1)
                yield
                s_ev(1); s_mm(2)
                for c in range(0, 4):
                    topk1(c)
                yield
                s_ev(2); s_mm(3)
                for c in range(4, 8):
                    topk1(c)
                yield
                s_ev(3)
                for c in range(8, 12):
                    topk1(c)
                yield
                for c in range(12, 16):
                    topk1(c)
                cp(topf[:], topi[:], ['topi'], ['topf'])
                t4 = tops[:].rearrange("p (h two) k -> p h two k", two=2)
                c4v = cand.rearrange("p h (a b) -> p h a b", a=16)
                tt(c4v, t4[:, :, 0, :].unsqueeze(3).to_broadcast(shp4), t4[:, :, 1, :].unsqueeze(2).to_broadcast(shp4), ALU.add,
                   ['tops'], ['s_all'])
                yield
                for h in range(8):
                    cv = cand[:, h, :]
                    P.op('dve', lambda e, h=h, cv=cv: e.max(out=best[:, h, 0:8], in_=cv), ['s_all'], ['best'])
                    P.op('dve', lambda e, h=h, cv=cv: e.max_index(out=bpos[:, h, 0:8], in_max=best[:, h, 0:8], in_values=cv),
                         ['s_all', 'best'], ['bpos'])
                    P.op('dve', lambda e, h=h, cv=cv: e.match_replace(out=cwk[:], in_to_replace=best[:, h, 0:8], in_values=cv,
                                                                    imm_value=-1e30), ['s_all', 'best'], ['cwk'])
                    P.op('dve', lambda e, h=h: e.max(out=best[:, h, 8:16], in_=cwk[:]), ['cwk'], ['best'])
                    P.op('dve', lambda e, h=h: e.max_index(out=bpos[:, h, 8:16], in_max=best[:, h, 8:16], in_values=cwk[:]),
                         ['cwk', 'best'], ['bpos'])
                    if h == 3:
                        yield
                gt_ = gate[p]; GN = 'gate%d' % p
                ts(ngmax[:], best[:, :, 0], -1.0, None, ALU.mult, None, ['best'], ['ngmax'])
                tt(gt_[:], best[:], bc2(ngmax[:], [128, 8, 16]), ALU.add, ['best', 'ngmax'], [GN])
                act(gt_[:], gt_[:], AF.Exp, [GN], [GN])
                yield
                ts(ak[:], bpos[:], 4, None, ALU.logical_shift_right, None, ['bpos'], ['ak'])
                ts(bk[:], bpos[:], 15, None, ALU.bitwise_and, None, ['bpos'], ['bk'])
                cp(akf[:], ak[:], ['ak'], ['akf'])
                cp(bkf[:], bk[:], ['bk'], ['bkf'])
                tf4 = topf[:].rearrange("p (h two) k -> p h two k", two=2)
                io4 = iota16[:].unsqueeze(1).unsqueeze(1).to_broadcast(shp4)
                for (kf_, half, dsti, nm) in [(akf, 0, i0, 'i0'), (bkf, 1, i1, 'i1')]:
                    tt(oh, kf_[:].unsqueeze(3).to_broadcast(shp4), io4, ALU.is_equal, ['akf', 'bkf', 'iota16'], ['s_all'])
                    tt(oh, oh, tf4[:, :, half, :].unsqueeze(2).to_broadcast(shp4), ALU.mult, ['s_all', 'topf'], ['s_all'])
                    red(dsti[:], oh, ALU.add, ['s_all'], [nm])
                stt(idf[:].rearrange("p (h k) -> p h k", h=8), i0[:], 128.0, i1[:], ALU.mult, ALU.add, ['i0', 'i1'], ['idf'])
                cp(ids[p][:], idf[:], ['idf'], ['ids%d' % p])
                red(gz[:], gt_[:], ALU.add, [GN], ['gz'])
                rcp(gz[:], gz[:], ['gz'], ['gz'])
                tt(gt_[:], gt_[:], bc2(gz[:], [128, 8, 16]), ALU.mult, [GN, 'gz'], [GN])

            NTOT = NT * 32

            def pg_gather(G):
                i, gi = divmod(G, 32)
                p = i % 2
                for q_ in range(4):
                    sl_ = gi * 4 + q_
                    rb = (G * 4 + q_) % NRING
                    un = 'uvb%d' % rb
                    P.dma('pool', lambda e, sl_=sl_, rb=rb, p=p: e.indirect_dma_start(
                        out=uvb[rb][:], out_offset=None, in_=uv_d,
                        in_offset=bass.IndirectOffsetOnAxis(ap=ids[p][:, sl_:sl_ + 1], axis=0)), ['ids%d' % p], [un], un)

            def pg_dot(G):
                i, gi = divmod(G, 32)
                p = i % 2
                for q_ in range(4):
                    sl_ = gi * 4 + q_
                    rb = (G * 4 + q_) % NRING
                    pr = (G * 4 + q_) % 2
                    tt(prod[pr][:], uvb[rb][:, 0:D], h2b[p][:], ALU.mult, ['uvb%d' % rb, 'ph2_%d' % p], ['prod%d' % pr])
                    act(prod[pr][:], prod[pr][:], AF.Identity, ['prod%d' % pr], ['prod%d' % pr, 'actv%d_%d' % (p, gi)], accum_out=actv[p][:, sl_:sl_ + 1])

            def pg_coefA(G):
                i, gi = divmod(G, 32)
                p = i % 2
                s4 = slice(gi * 4, gi * 4 + 4)
                a_ = actv[p][:, s4]; t_ = gtmp[p][:, s4]
                AN = 'actv%d_%d' % (p, gi); TN = 'gtmp%d_%d' % (p, gi)
                stt(t_, a_, 0.044715, a_, ALU.mult, ALU.mult, [AN], [TN])
                stt(t_, t_, 1.0, a_, ALU.add, ALU.mult, [TN, AN], [TN])
                act(t_, t_, AF.Sigmoid, [TN], [TN], scale=GELU_S)

            def pg_coefB(G):
                i, gi = divmod(G, 32)
                p = i % 2
                s4 = slice(gi * 4, gi * 4 + 4)
                a_ = actv[p][:, s4]; t_ = gtmp[p][:, s4]
                AN = 'actv%d_%d' % (p, gi); TN = 'gtmp%d_%d' % (p, gi)
                gate_f = gate[p][:].rearrange("p h k -> p (h k)")
                tt(t_, t_, a_, ALU.mult, [TN, AN], [TN])
                tt(coef[p][:, s4], t_, gate_f[:, s4], ALU.mult, [TN, 'gate%d' % p], ['coef%d_%d' % (p, gi)])

            def pg_acc(G):
                i, gi = divmod(G, 32)
                p = i % 2
                for q_ in range(4):
                    sl_ = gi * 4 + q_
                    rb = (G * 4 + q_) % NRING
                    db = sl_ % 4
                    ts(dgb[db][:], identb[:], coef[p][:, sl_:sl_ + 1], None, ALU.mult, None, ['identb', 'coef%d_%d' % (p, gi)], ['dgb%d' % db])
                    for hf in range(2):
                        mm(pacc[:, hf * 512:(hf + 1) * 512], dgb[db][:], uvb[rb][:, D + hf * 512:D + (hf + 1) * 512],
                           sl_ == 0, sl_ == 127, ['dgb%d' % db, 'uvb%d' % rb], ['pacc'])

            def epilogue(i):
                p = i % 2
                tt(tmpx[:], pacc[:], G2[:], ALU.mult, ['pacc', 'G2'], ['xt2'])
                tt(ob[:], tmpx[:], x1[p][:], ALU.add, ['xt2', 'x1_%d' % p], ['xt2'])
                dma(out_d[i * 128:(i + 1) * 128, :], ob[:], ['xt2'], ['out_d'], 'ob')

            for _ in prologue(0):
                pass
            NLEAD = NRING // 4
            for G0 in range(NLEAD):
                pg_gather(G0)
            pg_dot(0)
            gen = None
            for G in range(NTOT + 1):
                i, gi = divmod(G, 32)
                if gi == 0 and G < NTOT:
                    gen = prologue(i + 1) if i + 1 < NT else None
                if G + 1 < NTOT:
                    pg_dot(G + 1)
                if G < NTOT:
                    pg_coefA(G)
                if G >= 1:
                    pg_coefB(G - 1)
                    pg_acc(G - 1)
                    if (G - 1) % 32 == 31:
                        epilogue((G - 1) // 32)
                if gen is not None:
                    if gi < 26:
                        next(gen, None)
                    elif gi == 26:
                        for _ in gen:
                            pass
                        gen = None
                if G >= 1 and G - 1 + NRING // 4 < NTOT:
                    pg_gather(G - 1 + NRING // 4)
            P.barrier()

    P.emit(nc)
    top.close()
    return nc


def make_inputs(inputs):
    f = lambda a: np.ascontiguousarray(np.asarray(a))
    common = {}
    for k in ["g_norm1", "g_norm2", "b_ada", "g_a_q", "g_a_k", "g_idx_k", "g_mq_a", "g_mkv_a", "g_m_q", "g_m_k"]:
        common[k] = f(np.asarray(inputs[k], np.float32).reshape(1, -1))
    for k in ["w_ada", "w_in", "w_mq_up", "w_mkv_up", "w_o_a", "w_o_m", "w_out", "w_peer_q", "peer_u", "peer_v"]:
        common[k] = f(np.asarray(inputs[k], np.float32)[0])
    common["peer_subkeys"] = f(np.asarray(inputs["peer_subkeys"], np.float32)[0].reshape(16, 128, 128))
    common["identb"] = np.eye(128, dtype=np.float32).astype(ml_dtypes.bfloat16)
    q = np.arange(128)[:, None]; s = np.arange(128)[None, :]
    common["trib"] = np.where(s <= q, 0.0, NEG).astype(np.float32).astype(ml_dtypes.bfloat16)
    inv64 = (10000.0 ** (-(np.arange(32, dtype=np.float32)) / np.float32(32))).astype(np.float32)
    inv32 = (10000.0 ** (-(np.arange(16, dtype=np.float32)) / np.float32(16))).astype(np.float32)
    common["inv64"] = f(np.broadcast_to(inv64[None, :], (128, 32)))
    common["inv32"] = f(np.broadcast_to(inv32[None, :], (128, 16)))
    common["iota16"] = f(np.broadcast_to(np.arange(16, dtype=np.float32)[None, :], (128, 16)))
    x = np.asarray(inputs["x"], np.float32)
    c = np.asarray(inputs["c"], np.float32)
    pos = np.asarray(inputs["positions"], np.int32)
    maps = []
    for b in range(x.shape[0]):
        m = dict(common)
        m["x"] = f(x[b])
        m["c_col"] = f(c[b].reshape(8, 128).T)
        m["pos_t"] = f(pos[b].reshape(NT, 128).T)
        maps.append(m)
    return maps


def kernel(**inputs):
    maps = make_inputs(inputs)
    nc = build_nc()
    res = run_bass_kernel_spmd(nc, maps, core_ids=list(range(8)))
    return np.stack([np.asarray(r["out"], np.float32) for r in res.results], axis=0)
```

```python
import contextlib
import numpy as np
import ml_dtypes
import concourse.bass as bass
import concourse.mybir as mybir
from concourse.bass_utils import run_bass_kernel_spmd

F32 = mybir.dt.float32
BF16 = mybir.dt.bfloat16
I32 = mybir.dt.int32
U32 = mybir.dt.uint32
AF = mybir.ActivationFunctionType
ALU = mybir.AluOpType
AX = mybir.AxisListType

S = 4096
D = 1024
NT = 32
NG = 8
EPS = 1e-6
NEG = -30000.0
NITER = 18
NSLOT = 4
TWO_PI = float(2 * np.pi)
NO_SELF_WAIT = False


class Prog:
    ENG = ('pe', 'act', 'dve', 'pool', 'sp')

    def __init__(self):
        self.ops = {e: [] for e in self.ENG}
        self.cnt = {e: 0 for e in self.ENG}
        self.known = {e: {} for e in self.ENG}
        self.res = {}
        self.dma_cnt = {}

    def _deps(self, eng, reads, writes):
        need = {}

        def add(sk, v):
            if sk == ('eng', 'pe') and eng == 'pe':
                return
            if NO_SELF_WAIT and sk == ('eng', eng):
                return
            if v > need.get(sk, 0):
                need[sk] = v
        for k in reads:
            st = self.res.get(k)
            if st and st[0] is not None:
                add(*st[0])
        for k in writes:
            st = self.res.get(k)
            if st:
                if st[0] is not None:
                    add(*st[0])
                for sk, v in st[1].items():
                    add(sk, v)
        waits = []
        kn = self.known[eng]
        for sk, v in need.items():
            if kn.get(sk, 0) >= v:
                continue
            kn[sk] = v
            waits.append((sk, v))
        return waits

    def _commit(self, tok, reads, writes):
        sk, v = tok
        for k in reads:
            st = self.res.setdefault(k, [None, {}])
            if v > st[1].get(sk, 0):
                st[1][sk] = v
        for k in writes:
            self.res[k] = [tok, {}]

    def op(self, eng, fn, r=(), w=()):
        waits = self._deps(eng, r, w)
        self.cnt[eng] += 1
        tok = (('eng', eng), self.cnt[eng])
        self.ops[eng].append((waits, fn, ('eng', eng), 1))
        self._commit(tok, r, w)

    def dma(self, eng, fn, r=(), w=(), sem=None):
        waits = self._deps(eng, r, w)
        c = self.dma_cnt.get(sem, 0) + 16
        self.dma_cnt[sem] = c
        tok = (('dma', sem), c)
        self.ops[eng].append((waits, fn, ('dma', sem), 16))
        self._commit(tok, r, w)

    def barrier(self):
        allk = [(('eng', e), self.cnt[e]) for e in self.ENG if self.cnt[e] > 0]
        allk += [(('dma', k), c) for k, c in self.dma_cnt.items()]
        for e in self.ENG:
            waits = []
            kn = self.known[e]
            for sk, v in allk:
                if sk == ('eng', e):
                    continue
                if kn.get(sk, 0) >= v:
                    continue
                kn[sk] = v
                waits.append((sk, v))
            if waits:
                self.ops[e].append((waits, None, None, 0))

    def emit(self, nc):
        self.barrier()
        with contextlib.ExitStack() as st:
            sems = {}
            for e in self.ENG:
                sems[('eng', e)] = st.enter_context(nc.semaphore("s_" + e))
            for i, k in enumerate(self.dma_cnt):
                sems[('dma', k)] = st.enter_context(nc.semaphore("d%d" % i))
            block = st.enter_context(nc.Block())

            def run(e, name):
                for (waits, fn, sk, inc) in self.ops[name]:
                    for (wk, v) in waits:
                        e.wait_ge(sems[wk], v)
                    if fn is not None:
                        fn(e).then_inc(sems[sk], inc)

            @block.tensor
            def _(e):
                run(e, 'pe')

            @block.scalar
            def _(e):
                run(e, 'act')

            @block.vector
            def _(e):
                run(e, 'dve')

            @block.gpsimd
            def _(e):
                run(e, 'pool')

            @block.sync
            def _(e):
                run(e, 'sp')


def build_nc(debug=False, phases=4, nt1=NT, skip=(), nslot=NSLOT, gcols=D):
    nc = bass.Bass("TRN2", target_bir_lowering=False)
    P = Prog()

    def din(name, shape, dt=F32):
        return nc.dram_tensor(name, list(shape), dt, kind="ExternalInput").ap()

    def dscr(name, shape, dt=BF16):
        return nc.dram_tensor(name, list(shape), dt, kind="ExternalOutput" if debug else "Internal").ap()

    x_d = din("x", [S, D])
    ccol_d = din("c_col", [128, 8])
    pos_d = din("pos_t", [128, NT], I32)
    g1_d = din("g_norm1", [1, D]); g2_d = din("g_norm2", [1, D])
    wada_d = din("w_ada", [D, 6 * D]); bada_d = din("b_ada", [1, 6 * D])
    win_d = din("w_in", [D, 4840])
    gaq_d = din("g_a_q", [1, 64]); gak_d = din("g_a_k", [1, 64]); gik_d = din("g_idx_k", [1, 64])
    gmqa_d = din("g_mq_a", [1, 384]); wmq_d = din("w_mq_up", [384, 768])
    gmkva_d = din("g_mkv_a", [1, 256]); wmkv_d = din("w_mkv_up", [256, 1024])
    gmq_d = din("g_m_q", [1, 96]); gmk_d = din("g_m_k", [1, 96])
    woa_d = din("w_o_a", [512, D]); wom_d = din("w_o_m", [512, D]); wout_d = din("w_out", [D, D])
    wpq_d = din("w_peer_q", [D, 2048]); subk_d = din("peer_subkeys", [16, 128, 128])
    pu_d = din("peer_u", [16384, D]); pv_d = din("peer_v", [16384, D])
    identb_d = din("identb", [128, 128], BF16)
    trib_d = din("trib", [128, 128], BF16)
    inv64_d = din("inv64", [128, 32]); inv32_d = din("inv32", [128, 16])
    iota16_d = din("iota16", [128, 16])
    out_d = nc.dram_tensor("out", [S, D], F32, kind="ExternalOutput").ap()

    qaT_d = dscr("qaT_s", [4, 128, S]); kaT_d = dscr("kaT_s", [4, 128, S]); qiT_d = dscr("qiT_s", [4, 128, S])
    kiT_d = dscr("kiT_s", [128, S])
    va_d = dscr("va_s", [S, 520]); vm_d = dscr("vm_s", [S, 520])
    qmT_d = dscr("qmT_s", [8, 96, S]); kmT_d = dscr("kmT_s", [8, 96, S])
    gS_d = dscr("gS_s", [16, 128, S])
    atA_d = dscr("atA_s", [8, 64, S]); atM_d = dscr("atM_s", [8, 64, S])
    mod_d = dscr("mod_s", [4, 128, D], F32)
    uv_d = nc.dram_tensor("uv_s", [16384, 2048], BF16, kind="Internal").ap()

    top = contextlib.ExitStack()

    uid = [0]

    def sbt(st, name, shape, dt=F32):
        uid[0] += 1
        return st.enter_context(nc.sbuf_tensor("sb%d_%s" % (uid[0], name), list(shape), dt))

    def pst(st, name, shape, dt=F32):
        uid[0] += 1
        return st.enter_context(nc.psum_tensor("ps%d_%s" % (uid[0], name), list(shape), dt))

    def tt(out, in0, in1, op, r, w, eng='dve'):
        P.op(eng, lambda e: e.tensor_tensor(out=out, in0=in0, in1=in1, op=op), r, w)

    def ts(out, in0, s1, s2, op0, op1, r, w, eng='dve', accum_out=None):
        if op1 is None:
            P.op(eng, lambda e: e.tensor_scalar(out=out, in0=in0, scalar1=s1, scalar2=None, op0=op0), r, w)
        elif accum_out is None:
            P.op(eng, lambda e: e.tensor_scalar(out=out, in0=in0, scalar1=s1, scalar2=s2, op0=op0, op1=op1), r, w)
        else:
            P.op(eng, lambda e: e.tensor_scalar(out=out, in0=in0, scalar1=s1, scalar2=s2, op0=op0, op1=op1, accum_out=accum_out), r, w)

    def stt(out, in0, scalar, in1, op0, op1, r, w, accum_out=None):
        if accum_out is None:
            P.op('dve', lambda e: e.scalar_tensor_tensor(out=out, in0=in0, scalar=scalar, in1=in1, op0=op0, op1=op1), r, w)
        else:
            P.op('dve', lambda e: e.scalar_tensor_tensor(out=out, in0=in0, scalar=scalar, in1=in1, op0=op0, op1=op1, accum_out=accum_out), r, w)

    def act(out, in_, func, r, w, scale=None, bias=None, accum_out=None):
        kw = {}
        if scale is not None:
            kw['scale'] = scale
        if bias is not None:
            kw['bias'] = bias
        if accum_out is not None:
            kw['accum_out'] = accum_out
        P.op('act', lambda e: e.activation(out=out, in_=in_, func=func, **kw), r, w)

    def cp(out, in_, r, w, eng='dve'):
        if eng == 'act':
            act(out, in_, AF.Copy, r, w)
        else:
            P.op(eng, lambda e: e.tensor_copy(out=out, in_=in_), r, w)

    def red(out, in_, op, r, w):
        P.op('dve', lambda e: e.tensor_reduce(out=out, in_=in_, axis=AX.X, op=op), r, w)

    def rcp(out, in_, r, w):
        P.op('dve', lambda e: e.reciprocal(out=out, in_=in_), r, w)

    def mm(out, lhsT, rhs, start, stop, r, w):
        P.op('pe', lambda e: e.matmul(out=out, lhsT=lhsT, rhs=rhs, start=start, stop=stop), r, w)

    def tr(out, in_, ident, r, w):
        P.op('pe', lambda e: e.transpose(out=out, in_=in_, identity=ident), r, w)

    def dma(out, in_, r, w, sem, eng='sp'):
        P.dma(eng, lambda e: e.dma_start(out=out, in_=in_), r, w, sem)

    def memset(ap, val, w, eng='dve'):
        P.op(eng, lambda e: e.memset(ap, val), (), w)

    def bc1(ap, shape):
        return ap.unsqueeze(1).to_broadcast(shape)

    def bc2(ap, shape):
        return ap.unsqueeze(2).to_broadcast(shape)

    identb = sbt(top, "identb", [128, 128], BF16)
    trib = sbt(top, "trib", [128, 128], BF16)
    wi_all = sbt(top, "wi_all", [128, NT, 8])
    st01 = contextlib.ExitStack()
    A1 = sbt(st01, "A1", [128, D]); B1 = sbt(st01, "B1", [128, D])
    dma(identb[:], identb_d, [], ['identb'], 'identb')
    dma(trib[:], trib_d, [], ['trib'], 'trib')

    with contextlib.ExitStack() as st:
        ccol = sbt(st, "ccol", [128, 8]); cact = sbt(st, "cact", [128, 8])
        cb = sbt(st, "cb", [128, 8, 128])
        g1b = sbt(st, "g1b", [128, D]); g2b = sbt(st, "g2b", [128, D])
        G1 = sbt(st, "G1", [128, D]); A2 = sbt(st, "A2", [128, D]); B2 = sbt(st, "B2", [128, D]); G2 = sbt(st, "G2", [128, D])
        wa = [sbt(st, "wa%d" % i, [128, 8, 512]) for i in range(2)]
        bb = [sbt(st, "bb%d" % i, [128, 512]) for i in range(2)]
        tmp0 = sbt(st, "tmp0", [128, 512])
        pm0 = [pst(st, "pm0_%d" % i, [128, 512]) for i in range(2)]
        dma(ccol[:], ccol_d, [], ['ccol'], 'ccol')
        dma(g1b[:], g1_d.to_broadcast([128, D]), [], ['g1b'], 'g1b')
        dma(g2b[:], g2_d.to_broadcast([128, D]), [], ['g2b'], 'g2b')
        act(cact[:], ccol[:], AF.Silu, ['ccol'], ['cact'])
        cp(cb[:], cact[:].unsqueeze(2).to_broadcast([128, 8, 128]), ['cact'], ['cb'])
        wada_v = wada_d.rearrange("(k p) n -> p k n", p=128)
        dests = [B1, A1, G1, B2, A2, G2]
        dnames = ['B1', 'A1', 'G1', 'B2', 'A2', 'G2']
        for n in range(12):
            sl = n % 2
            dma(wa[sl][:], wada_v[:, :, n * 512:(n + 1) * 512], [], ['wa%d' % sl], 'wa%d' % sl)
            dma(bb[sl][:], bada_d[:, n * 512:(n + 1) * 512].to_broadcast([128, 512]), [], ['bb%d' % sl], 'bb%d' % sl)
            for k in range(8):
                mm(pm0[sl][:], cb[:, k, :], wa[sl][:, k, :], k == 0, k == 7, ['cb', 'wa%d' % sl], ['pm0_%d' % sl])
            which = n // 2
            dst = dests[which][:, (n % 2) * 512:(n % 2 + 1) * 512]
            dn = dnames[which]
            if which in (1, 4):
                gsrc = (g1b if which == 1 else g2b)[:, (n % 2) * 512:(n % 2 + 1) * 512]
                tt(tmp0[:], pm0[sl][:], bb[sl][:], ALU.add, ['pm0_%d' % sl, 'bb%d' % sl], ['tmp0'])
                stt(dst, tmp0[:], 1.0, gsrc, ALU.add, ALU.mult, ['tmp0', 'g1b', 'g2b'], [dn])
            else:
                tt(dst, pm0[sl][:], bb[sl][:], ALU.add, ['pm0_%d' % sl, 'bb%d' % sl], [dn])
        for qi_, (tn_, nm_) in enumerate([(G1, 'G1'), (A2, 'A2'), (B2, 'B2'), (G2, 'G2')]):
            dma(mod_d[qi_], tn_[:], [nm_], ['mod_d'], 'mod' + nm_)
        P.barrier()

    if phases == 0:
        P.emit(nc)
        st01.close()
        top.close()
        return nc

    with contextlib.ExitStack() as st:
        winb = sbt(st, "winb", [128, 8, 4840], BF16)
        wmqb = sbt(st, "wmqb", [128, 3, 768], BF16)
        wmkvb = sbt(st, "wmkvb", [128, 2, 1024], BF16)
        gaq = sbt(st, "gaq", [128, 64]); gak = sbt(st, "gak", [128, 64]); gik = sbt(st, "gik", [128, 64])
        gmqa = sbt(st, "gmqa", [128, 384]); gmkva = sbt(st, "gmkva", [128, 256])
        gmq = sbt(st, "gmq", [128, 96]); gmk = sbt(st, "gmk", [128, 96])
        cos64 = sbt(st, "cos64", [128, NT, 32]); sin64 = sbt(st, "sin64", [128, NT, 32])
        cos32 = sbt(st, "cos32", [128, NT, 16]); sin32 = sbt(st, "sin32", [128, NT, 16])
        for tns, src, nm in [(gaq, gaq_d, 'gaq'), (gak, gak_d, 'gak'), (gik, gik_d, 'gik'), (gmqa, gmqa_d, 'gmqa'),
                             (gmkva, gmkva_d, 'gmkva'), (gmq, gmq_d, 'gmq'), (gmk, gmk_d, 'gmk')]:
            dma(tns[:], src.to_broadcast(list(tns[:].shape)), [], [nm], nm)
        with contextlib.ExitStack() as st2:
            stage = [sbt(st2, "stage%d" % i, [128, 4096]) for i in range(2)]
            win_v = win_d.rearrange("(k p) n -> p k n", p=128)
            ci = 0
            for c0 in range(0, 4840, 512):
                c1 = min(c0 + 512, 4840)
                wdt = c1 - c0
                sl = ci % 2
                sv = stage[sl][:, 0:8 * wdt].rearrange("p (k n) -> p k n", k=8)
                dma(sv, win_v[:, :, c0:c1], [], ['stage%d' % sl], 'stage%d' % sl)
                cp(winb[:, :, c0:c1], sv, ['stage%d' % sl], ['winb'], eng=('pool' if ci % 2 == 0 else 'act'))
                ci += 1
            sv = stage[ci % 2][:, 0:3 * 768].rearrange("p (k n) -> p k n", k=3)
            dma(sv, wmq_d.rearrange("(k p) n -> p k n", p=128), [], ['stage%d' % (ci % 2)], 'stage%d' % (ci % 2))
            cp(wmqb[:], sv, ['stage%d' % (ci % 2)], ['wmqb'], eng='pool')
            ci += 1
            sv = stage[ci % 2][:, 0:2 * 1024].rearrange("p (k n) -> p k n", k=2)
            dma(sv, wmkv_d.rearrange("(k p) n -> p k n", p=128), [], ['stage%d' % (ci % 2)], 'stage%d' % (ci % 2))
            cp(wmkvb[:], sv, ['stage%d' % (ci % 2)], ['wmkvb'], eng='pool')
            posi = sbt(st2, "posi", [128, NT], I32); posf = sbt(st2, "posf", [128, NT])
            inv64 = sbt(st2, "inv64", [128, 32]); inv32 = sbt(st2, "inv32", [128, 16])
            rt_a = sbt(st2, "rt_a", [128, NT, 32]); rt_k = sbt(st2, "rt_k", [128, NT, 32])
            rt_i = sbt(st2, "rt_i", [128, NT, 32], I32); rt_y = sbt(st2, "rt_y", [128, NT, 32])
            dma(posi[:], pos_d, [], ['posi'], 'posi')
            dma(inv64[:], inv64_d, [], ['inv64'], 'inv64')
            dma(inv32[:], inv32_d, [], ['inv32'], 'inv32')
            cp(posf[:], posi[:], ['posi'], ['posf'])
            for (inv, hf, cs, sn, nm) in [(inv64, 32, cos64, sin64, '64'), (inv32, 16, cos32, sin32, '32')]:
                a = rt_a[:, :, 0:hf]; kk = rt_k[:, :, 0:hf]; ii = rt_i[:, :, 0:hf]; y = rt_y[:, :, 0:hf]
                shp = [128, NT, hf]
                tt(a, bc2(posf[:], shp), bc1(inv[:], shp), ALU.mult, ['posf', 'inv' + nm], ['rt_a'])
                ts(kk, a, float(1.0 / TWO_PI), None, ALU.mult, None, ['rt_a'], ['rt_k'])
                cp(ii, kk, ['rt_k'], ['rt_i'])
                cp(kk, ii, ['rt_i'], ['rt_k'])
                stt(a, kk, -TWO_PI, a, ALU.mult, ALU.add, ['rt_k', 'rt_a'], ['rt_a'])
                ts(kk, a, float(np.pi / 2), float(np.pi), ALU.add, ALU.is_gt, ['rt_a'], ['rt_k'])
                stt(y, kk, -TWO_PI, a, ALU.mult, ALU.add, ['rt_k', 'rt_a'], ['rt_y'])
                ts(y, y, float(np.pi / 2), None, ALU.add, None, ['rt_y'], ['rt_y'])
                ts(y, y, float(np.pi), float(-np.pi), ALU.min, ALU.max, ['rt_y'], ['rt_y'])
                ts(a, a, float(np.pi), float(-np.pi), ALU.min, ALU.max, ['rt_a'], ['rt_a'])
                act(sn[:], a, AF.Sin, ['rt_a'], ['sin' + nm])
                act(cs[:], y, AF.Sin, ['rt_y'], ['cos' + nm])
            P.barrier()

        xt = [sbt(st, "xt%d" % i, [128, D]) for i in range(2)]
        junkb = sbt(st, "junkb", [128, D], BF16)
        ssq = sbt(st, "ssq", [128, 1]); rstd = sbt(st, "rstd", [128, 1])
        htmp = sbt(st, "htmp", [128, D]); hb = sbt(st, "hb", [128, D], BF16)
        hT = [sbt(st, "hT%d" % i, [128, 8, 128], BF16) for i in range(2)]
        proj = sbt(st, "proj", [128, 2792])
        sq = sbt(st, "sq", [128, 1024]); nrm = sbt(st, "nrm", [128, 1024])
        s8 = sbt(st, "s8", [128, 8]); r8 = sbt(st, "r8", [128, 8])
        rp = [sbt(st, "rp%d" % i, [128, 8, 32]) for i in range(4)]
        tokb = sbt(st, "tokb", [128, 768], BF16)
        kib = sbt(st, "kib", [128, 128], BF16)
        cqT = sbt(st, "cqT", [128, 3, 128], BF16); ckvT = sbt(st, "ckvT", [128, 2, 128], BF16)
        qmf = sbt(st, "qmf", [128, 768]); kvf = sbt(st, "kvf", [128, 8, 128]); kmpre = sbt(st, "kmpre", [128, 8, 96])
        vaug = sbt(st, "vaug", [128, 8, 65], BF16); vmaug = sbt(st, "vmaug", [128, 8, 65], BF16)
        qaT_t = sbt(st, "qaT_t", [128, 4, 128], BF16); kaT_t = sbt(st, "kaT_t", [128, 4, 128], BF16)
        qiT_t = sbt(st, "qiT_t", [128, 4, 128], BF16); kiT_t = sbt(st, "kiT_t", [128, 128], BF16)
        qmT_t = sbt(st, "qmT_t", [128, 8, 128], BF16); kmT_t = sbt(st, "kmT_t", [128, 8, 128], BF16)
        gS_t = sbt(st, "gS_t", [128, 16, 128], BF16)
        pT = pst(st, "pT", [128, 8, 128], BF16)
        pT2 = pst(st, "pT2", [128, 8, 128], BF16)
        pproj = [pst(st, "pproj%d" % i, [128, 512]) for i in range(2)]
        pgate = pst(st, "pgate", [128, 512])
        pm = pst(st, "pm", [128, 1024])
        memset(vaug[:], 1.0, ['vaug'], eng='pool')
        memset(vmaug[:], 1.0, ['vmaug'], eng='pool')

        def rms_heads(src3, H, Dh, gain, dst3, rs, ws, gname):
            shp = [128, H, Dh]
            sqv = sq[:, 0:H * Dh].rearrange("p (h d) -> p h d", h=H)
            tt(sqv, src3, src3, ALU.mult, rs, ['sq'])
            red(s8[:, 0:H], sqv, ALU.add, ['sq'], ['s8'])
            ts(s8[:, 0:H], s8[:, 0:H], float(1.0 / Dh), float(EPS), ALU.mult, ALU.add, ['s8'], ['s8'])
            act(s8[:, 0:H], s8[:, 0:H], AF.Sqrt, ['s8'], ['s8'])
            rcp(r8[:, 0:H], s8[:, 0:H], ['s8'], ['r8'])
            nv = nrm[:, 0:H * Dh].rearrange("p (h d) -> p h d", h=H)
            tt(nv, src3, bc2(r8[:, 0:H], shp), ALU.mult, rs + ['r8'], ['nrm'])
            tt(dst3, nv, bc1(gain[:], shp), ALU.mult, ['nrm', gname], ws)

        def rope(src3, H, hf, cosv, sinv, dst3, rs, ws, cname):
            shp = [128, H, hf]
            x1 = src3[:, :, 0:hf]; x2 = src3[:, :, hf:2 * hf]
            cb_ = bc1(cosv, shp); sb_ = bc1(sinv, shp)
            t = [rp[i][:, 0:H, 0:hf] for i in range(4)]
            tt(t[0], x1, cb_, ALU.mult, rs + ['cos' + cname], ['rp0'])
            tt(t[1], x2, sb_, ALU.mult, rs + ['sin' + cname], ['rp1'], eng='pool')
            tt(dst3[:, :, 0:hf], t[0], t[1], ALU.subtract, ['rp0', 'rp1'], ws)
            tt(t[2], x2, cb_, ALU.mult, rs + ['cos' + cname], ['rp2'])
            tt(t[3], x1, sb_, ALU.mult, rs + ['sin' + cname], ['rp3'], eng='pool')
            tt(dst3[:, :, hf:2 * hf], t[2], t[3], ALU.add, ['rp2', 'rp3'], ws)

        for i in range(nt1):
            hs = i % 2
            hTn = 'hT%d' % hs
            xs = i % 2
            xn = 'xt%d' % xs
            tok = slice(i * 128, (i + 1) * 128)
            dma(xt[xs][:], x_d[tok, :], [], [xn], xn)
            act(junkb[:], xt[xs][:], AF.Square, [xn], ['junkb', 'ssq'], accum_out=ssq[:])
            ts(ssq[:], ssq[:], float(1.0 / D), float(EPS), ALU.mult, ALU.add, ['ssq'], ['ssq'])
            act(ssq[:], ssq[:], AF.Sqrt, ['ssq'], ['ssq'])
            rcp(rstd[:], ssq[:], ['ssq'], ['rstd'])
            stt(htmp[:], xt[xs][:], rstd[:, 0:1], A1[:], ALU.mult, ALU.mult, [xn, 'rstd', 'A1'], ['htmp'])
            tt(hb[:], htmp[:], B1[:], ALU.add, ['htmp', 'B1'], ['hb'], eng='pool')
            for k in range(8):
                tr(pT[:, k, :], hb[:, k * 128:(k + 1) * 128], identb[:], ['hb', 'identb'], ['pT'])
            cp(hT[hs][:], pT[:], ['pT'], [hTn], eng='act')
            for cc in range(6):
                c0 = cc * 512
                c1 = min(c0 + 512, 2792)
                pp = cc % 2
                for k in range(8):
                    mm(pproj[pp][:, 0:c1 - c0], hT[hs][:, k, :], winb[:, k, c0:c1], k == 0, k == 7,
                       [hTn, 'winb'], ['pproj%d' % pp])
                cp(proj[:, c0:c1], pproj[pp][:, 0:c1 - c0], ['pproj%d' % pp], ['proj'], eng='act')
            for fc in range(16):
                c0 = 2792 + fc * 128
                for k in range(8):
                    mm(pgate[:, (fc % 4) * 128:(fc % 4 + 1) * 128], winb[:, k, c0:c0 + 128], hT[hs][:, k, :], k == 0, k == 7,
                       [hTn, 'winb'], ['pgate'])
                if fc % 4 == 3:
                    act(gS_t[:, fc - 3:fc + 1, :].rearrange("p c t -> p (c t)"), pgate[:], AF.Sigmoid, ['pgate'], ['gS_t'])
            dma(gS_d[:, :, tok].rearrange("c p t -> p c t"), gS_t[:], ['gS_t'], ['gS_d'], 'gS_t')
            cs64 = cos64[:, i, :]; sn64 = sin64[:, i, :]; cs32 = cos32[:, i, :]; sn32 = sin32[:, i, :]
            for (c0, gn, gt_, dstT, dn, dd_) in [(0, 'gaq', gaq, qaT_t, 'qaT_t', qaT_d), (512, 'gak', gak, kaT_t, 'kaT_t', kaT_d)]:
                src3 = proj[:, c0:c0 + 512].rearrange("p (h d) -> p h d", h=8)
                n3 = sq[:, 0:512].rearrange("p (h d) -> p h d", h=8)
                rms_heads(src3, 8, 64, gt_, n3, ['proj'], ['sq'], gn)
                rope(n3, 8, 32, cs64, sn64, tokb[:, 0:512].rearrange("p (h d) -> p h d", h=8), ['sq'], ['tokb'], '64')
                for k in range(4):
                    tr(pT2[:, k, :], tokb[:, k * 128:(k + 1) * 128], identb[:], ['tokb', 'identb'], ['pT2'])
                cp(dstT[:], pT2[:, 0:4, :], ['pT2'], [dn])
                dma(dd_[:, :, tok].rearrange("c p t -> p c t"), dstT[:], [dn], [dn + '_d'], dn)
            rope(proj[:, 1536:2048].rearrange("p (h d) -> p h d", h=8), 8, 32, cs64, sn64,
                 tokb[:, 0:512].rearrange("p (h d) -> p h d", h=8), ['proj'], ['tokb'], '64')
            for k in range(4):
                tr(pT2[:, k, :], tokb[:, k * 128:(k + 1) * 128], identb[:], ['tokb', 'identb'], ['pT2'])
            cp(qiT_t[:], pT2[:, 0:4, :], ['pT2'], ['qiT_t'])
            dma(qiT_d[:, :, tok].rearrange("c p t -> p c t"), qiT_t[:], ['qiT_t'], ['qiT_t_d'], 'qiT_t')
            n3 = sq[:, 0:64].rearrange("p (h d) -> p h d", h=1)
            rms_heads(proj[:, 2048:2112].rearrange("p (h d) -> p h d", h=1), 1, 64, gik, n3, ['proj'], ['sq'], 'gik')
            rope(n3, 1, 32, cs64, sn64, kib[:, 0:64].rearrange("p (h d) -> p h d", h=1), ['sq'], ['kib'], '64')
            cp(kib[:, 64:128], kib[:, 0:64], ['kib'], ['kib'])
            tr(pT2[:, 0, :], kib[:], identb[:], ['kib', 'identb'], ['pT2'])
            cp(kiT_t[:], pT2[:, 0, :], ['pT2'], ['kiT_t'])
            dma(kiT_d[:, tok], kiT_t[:], ['kiT_t'], ['kiT_t_d'], 'kiT_t')
            ts(wi_all[:, i, :], proj[:, 2112:2120], float(512 ** -0.5), None, ALU.mult, None, ['proj'], ['wi_all'])
            cp(vaug[:, :, 0:64], proj[:, 1024:1536].rearrange("p (h d) -> p h d", h=8), ['proj'], ['vaug'], eng='pool')
            dma(va_d[tok, :], vaug[:].rearrange("p h d -> p (h d)"), ['vaug'], ['va_d'], 'vaug')
            rms_heads(proj[:, 2120:2504].rearrange("p (h d) -> p h d", h=1), 1, 384, gmqa,
                      tokb[:, 0:384].rearrange("p (h d) -> p h d", h=1), ['proj'], ['tokb'], 'gmqa')
            for k in range(3):
                tr(pT2[:, k, :], tokb[:, k * 128:(k + 1) * 128], identb[:], ['tokb', 'identb'], ['pT2'])
            cp(cqT[:], pT2[:, 0:3, :], ['pT2'], ['cqT'])
            for (c0, c1) in [(0, 512), (512, 768)]:
                for k in range(3):
                    mm(pm[:, c0:c1], cqT[:, k, :], wmqb[:, k, c0:c1], k == 0, k == 2, ['cqT', 'wmqb'], ['pm'])
            cp(qmf[:], pm[:, 0:768], ['pm'], ['qmf'], eng='act')
            q3 = qmf[:].rearrange("p (h d) -> p h d", h=8)
            n96 = sq[:, 0:768].rearrange("p (h d) -> p h d", h=8)
            rms_heads(q3, 8, 96, gmq, n96, ['qmf'], ['sq'], 'gmq')
            tb96 = tokb[:, 0:768].rearrange("p (h d) -> p h d", h=8)
            cp(tb96[:, :, 0:64], n96[:, :, 0:64], ['sq'], ['tokb'], eng='pool')
            rope(n96[:, :, 64:96], 8, 16, cs32, sn32, tb96[:, :, 64:96], ['sq'], ['tokb'], '32')
            for h in range(8):
                tr(pT2[0:96, h, :], tokb[:, h * 96:(h + 1) * 96], identb[:], ['tokb', 'identb'], ['pT2'])
            cp(qmT_t[0:96, :, :], pT2[0:96, :, :], ['pT2'], ['qmT_t'])
            dma(qmT_d[:, :, tok].rearrange("h d t -> d h t"), qmT_t[0:96, :, :], ['qmT_t'], ['qmT_t_d'], 'qmT_t')
            rms_heads(proj[:, 2504:2760].rearrange("p (h d) -> p h d", h=1), 1, 256, gmkva,
                      tokb[:, 0:256].rearrange("p (h d) -> p h d", h=1), ['proj'], ['tokb'], 'gmkva')
            for k in range(2):
                tr(pT2[:, k, :], tokb[:, k * 128:(k + 1) * 128], identb[:], ['tokb', 'identb'], ['pT2'])
            cp(ckvT[:], pT2[:, 0:2, :], ['pT2'], ['ckvT'])
            for (c0, c1) in [(0, 512), (512, 1024)]:
                for k in range(2):
                    mm(pm[:, c0:c1], ckvT[:, k, :], wmkvb[:, k, c0:c1], k == 0, k == 1, ['ckvT', 'wmkvb'], ['pm'])
            cp(kvf[:].rearrange("p h d -> p (h d)"), pm[:], ['pm'], ['kvf'], eng='act')
            cp(kmpre[:, :, 0:64], kvf[:, :, 0:64], ['kvf'], ['kmpre'], eng='pool')
            cp(kmpre[:, :, 64:96], bc1(proj[:, 2760:2792], [128, 8, 32]), ['proj'], ['kmpre'], eng='pool')
            cp(vmaug[:, :, 0:64], kvf[:, :, 64:128], ['kvf'], ['vmaug'], eng='pool')
            dma(vm_d[tok, :], vmaug[:].rearrange("p h d -> p (h d)"), ['vmaug'], ['vm_d'], 'vmaug')
            rms_heads(kmpre[:], 8, 96, gmk, n96, ['kmpre'], ['sq'], 'gmk')
            cp(tb96[:, :, 0:64], n96[:, :, 0:64], ['sq'], ['tokb'], eng='pool')
            rope(n96[:, :, 64:96], 8, 16, cs32, sn32, tb96[:, :, 64:96], ['sq'], ['tokb'], '32')
            for h in range(8):
                tr(pT2[0:96, h, :], tokb[:, h * 96:(h + 1) * 96], identb[:], ['tokb', 'identb'], ['pT2'])
            cp(kmT_t[0:96, :, :], pT2[0:96, :, :], ['pT2'], ['kmT_t'])
            dma(kmT_d[:, :, tok].rearrange("h d t -> d h t"), kmT_t[0:96, :, :], ['kmT_t'], ['kmT_t_d'], 'kmT_t')
        P.barrier()
    st01.close()

    def attention_phase(tag, dsa):
        with contextlib.ExitStack() as st:
            ones64 = sbt(st, "ones64" + tag, [128, 64])
            memset(ones64[:], 1.0, ['ones64'])
            if dsa:
                kT = sbt(st, "kaT", [128, 4, S], BF16)
                kiT = sbt(st, "kiT", [128, S], BF16)
                for c in range(4):
                    dma(kT[:, c, :], kaT_d[c], ['kaT_t_d'], ['kT%d' % c], 'kT%d' % c)
                dma(kiT[:], kiT_d, ['kiT_t_d'], ['kiT'], 'kiT')
                v_src = va_d
                qT_g = sbt(st, "qaTg", [128, 4, 512], BF16)
                qiTg = [sbt(st, "qiTg%d" % i, [128, 4, 512], BF16) for i in range(2)]
                score = sbt(st, "score", [128, S])
                junk = sbt(st, "junkc", [128, S], BF16)
                bias_g = [sbt(st, "bias_g%d" % i, [128, 4, S], BF16) for i in range(2)]
                dg = sbt(st, "dg", [128, 8, 128], BF16)
                rbuf = [sbt(st, "rbuf%d" % i, [128, 512], BF16) for i in range(2)]
                lo = sbt(st, "lo", [128, 1]); hi = sbt(st, "hi", [128, 1]); mid = sbt(st, "mid", [128, 1])
                wv = sbt(st, "wv", [128, NITER + 1]); cnt = sbt(st, "cnt", [128, 1]); stp = sbt(st, "stp", [128, 1])
                pd = [pst(st, "pd%d" % i, [128, 512]) for i in range(2)]
                psc = pst(st, "psc", [128, 512])
                scale = 64 ** -0.5
                Kd = 64
                out_d = atA_d
            else:
                kT = sbt(st, "kmT", [128, 8, S], BF16)
                for h in range(8):
                    dma(kT[0:96, h, :], kmT_d[h], ['kmT_t_d'], ['kT%d' % h], 'kT%d' % h)
                v_src = vm_d
                qT_g = sbt(st, "qmTg", [128, 8, 512], BF16)
                scale = 96 ** -0.5
                Kd = 96
                out_d = atM_d
            vv = sbt(st, "vv" + tag, [128, NT, 520], BF16)
            v_v = v_src.rearrange("(t p) c -> p t c", p=128)
            for q in range(4):
                dma(vv[:, q * 8:(q + 1) * 8, :], v_v[:, q * 8:(q + 1) * 8, :], ['va_d', 'vm_d'], ['vv%d' % q], 'vv%d' % q)
            vres = ['vv%d' % q for q in range(4)]
            ptb = [sbt(st, "ptb%d%s" % (i, tag), [128, 512], BF16) for i in range(2)]
            rz = sbt(st, "rz" + tag, [128, 512]); of = sbt(st, "of" + tag, [128, 512])
            yb = [sbt(st, "yb%d%s" % (i, tag), [128, 512], BF16) for i in range(2)]
            pS = [pst(st, "pS%d%s" % (i, tag), [128, 512]) for i in range(2)]
            pO = [pst(st, "pO%d%s" % (i, tag), [128, 512]) for i in range(2)]
            pB = pst(st, "pB" + tag, [128, 512])
            kres = ['kT%d' % c for c in range(8)]
            ctr = 0
            def idx_block(g, b):
                bg = bias_g[g % 2]
                bgn = 'bias_g%d' % (g % 2)
                jq = 4 * g + b
                Sp = (jq + 1) * 128
                shp = [128, 8, 128]
                tt(dg[:], bc1(identb[:], shp), bc2(wi_all[:, jq, :], shp), ALU.mult, ['identb', 'wi_all'], ['dg'])
                items_i = [(c, h) for c in range(g + 1) for h in range(8)]
                qn = 'qiTg%d' % (g % 2)
                qi_ = qiTg[g % 2]

                def idx_dots(k_):
                    c, h = items_i[k_]
                    wc = 512 if c < g else (b + 1) * 128
                    k0 = c * 512
                    hp = h % 2; hc = h // 2
                    prt = slice(hp * 64, hp * 64 + 64)
                    dd = k_ % 2
                    mm(pd[dd][:, 0:wc], qi_[prt, hc, b * 128:(b + 1) * 128], kiT[prt, k0:k0 + wc], True, True,
                       [qn, 'kiT'], ['pd%d' % dd])
                    act(rbuf[dd][:, 0:wc], pd[dd][:, 0:wc], AF.Relu, ['pd%d' % dd], ['rbuf%d' % dd])

                def idx_acc(k_):
                    c, h = items_i[k_]
                    wc = 512 if c < g else (b + 1) * 128
                    k0 = c * 512
                    dd = k_ % 2
                    mm(psc[:, 0:wc], dg[:, h, :], rbuf[dd][:, 0:wc], h == 0, h == 7, ['dg', 'rbuf%d' % dd], ['psc'])
                    if h == 7:
                        if c < g:
                            cp(score[:, k0:k0 + 512], psc[:], ['psc'], ['score'])
                        else:
                            if b > 0:
                                cp(score[:, k0:k0 + b * 128], psc[:, 0:b * 128], ['psc'], ['score'])
                            tt(score[:, jq * 128:(jq + 1) * 128], psc[:, b * 128:(b + 1) * 128], trib[:], ALU.add,
                               ['psc', 'trib'], ['score'])
                idx_dots(0)
                for k_ in range(len(items_i)):
                    if k_ + 1 < len(items_i):
                        idx_dots(k_ + 1)
                    idx_acc(k_)
                if jq >= 2:
                    red(hi[:], score[:, 0:Sp], ALU.max, ['score'], ['hi'])
                    red(lo[:], score[:, 0:jq * 128], ALU.min, ['score'], ['lo'])
                    tt(stp[:], hi[:], lo[:], ALU.subtract, ['hi', 'lo'], ['stp'])
                    for k in range(NITER + 1):
                        ts(wv[:, k:k + 1], stp[:], float(2.0 ** -(k + 1)), None, ALU.mult, None, ['stp'], ['wv'])
                    tt(mid[:], lo[:], wv[:, 0:1], ALU.add, ['lo', 'wv'], ['mid'])
                    for k in range(NITER):
                        ts(junk[:, 0:Sp], score[:, 0:Sp], mid[:, 0:1], 0.0, ALU.is_ge, ALU.add, ['score', 'mid'],
                           ['junk', 'cnt'], accum_out=cnt[:])
                        ts(stp[:], cnt[:], 255.5, wv[:, k:k + 1], ALU.is_ge, ALU.mult, ['cnt', 'wv'], ['stp'])
                        tt(lo[:], lo[:], stp[:], ALU.add, ['lo', 'stp'], ['lo'])
                        tt(mid[:], lo[:], wv[:, k + 1:k + 2], ALU.add, ['lo', 'wv'], ['mid'])
                else:
                    memset(lo[:], -10000.0, ['lo'])
                ts(bg[:, b, 0:Sp], score[:, 0:Sp], lo[:, 0:1], NEG, ALU.is_lt, ALU.mult, ['score', 'lo'], [bgn])

            def att_part(g, heads):
                gsl = slice(g * 512, (g + 1) * 512)
                nsc = 4 * g + 4
                items_a = [(h, sc) for h in heads for sc in range(nsc)]
                if dsa:
                    bg = bias_g[g % 2]
                    bgn = 'bias_g%d' % (g % 2)

                def att_S(k_):
                    h, sc = items_a[k_]
                    r_ = sc - 4 * g
                    qlo = max(r_, 0) * 128
                    ss = k_ % 2
                    ssl = slice(sc * 128, (sc + 1) * 128)
                    if dsa:
                        hp = h % 2; hc = h // 2
                        prt = slice(hp * 64, hp * 64 + 64)
                        mm(pS[ss][:, qlo:512], kT[prt, hc, ssl], qT_g[prt, hc, qlo:512], True, False,
                           ['kT%d' % hc, 'qT_g'], ['pS%d' % ss])
                        b0 = max(r_, 0)
                        for b in range(b0, 4):
                            mm(pS[ss][:, b * 128:(b + 1) * 128], bg[:, b, ssl], identb[:], False, b == 3,
                               [bgn, 'identb'], ['pS%d' % ss])
                    else:
                        last = r_ < 0
                        mm(pS[ss][:, qlo:512], kT[0:96, h, ssl], qT_g[0:96, h, qlo:512], True, last,
                           ['kT%d' % h, 'qT_g'], ['pS%d' % ss])
                        if r_ >= 0:
                            mm(pS[ss][:, qlo:qlo + 128], trib[:], identb[:], False, True, ['trib', 'identb'], ['pS%d' % ss])
                    act(ptb[ss][:, qlo:512], pS[ss][:, qlo:512], AF.Exp, ['pS%d' % ss], ['ptb%d' % ss], scale=float(scale))

                def att_PV(k_):
                    h, sc = items_a[k_]
                    r_ = sc - 4 * g
                    qlo = max(r_, 0) * 128
                    ss = k_ % 2
                    po = h % 2
                    pon = 'pO%d' % po
                    mm(pO[po][0:65, qlo:512], vv[:, sc, h * 65:(h + 1) * 65], ptb[ss][:, qlo:512], sc == 0, sc == nsc - 1,
                       ['ptb%d' % ss] + vres, [pon])
                    if sc == nsc - 1:
                        rcp(rz[64:65, :], pO[po][64:65, :], [pon], ['rz'])
                        mm(pB[0:64, :], ones64[64:65, 0:64], rz[64:65, :], True, True, ['ones64', 'rz'], ['pB'])
                        cp(of[0:64, :], pO[po][0:64, :], [pon], ['of'], eng='act')
                        ybn = 'yb%d' % po
                        tt(yb[po][0:64, :], of[0:64, :], pB[0:64, :], ALU.mult, ['of', 'pB'], [ybn])
                        dma(out_d[h, :, gsl], yb[po][0:64, :], [ybn], ['at_d' + tag], ybn + tag)
                att_S(0)
                for k_ in range(len(items_a)):
                    if k_ + 1 < len(items_a):
                        att_S(k_ + 1)
                    att_PV(k_)

            def load_q(g):
                gsl = slice(g * 512, (g + 1) * 512)
                if dsa:
                    dma(qT_g[:], qaT_d[:, :, gsl].rearrange("c p t -> p c t"), ['qaT_t_d'], ['qT_g'], 'qT_g')
                else:
                    dma(qT_g[0:96, :, :], qmT_d[:, :, gsl].rearrange("h d t -> d h t"), ['qmT_t_d'], ['qT_g'], 'qT_g')

            def load_qi(g):
                gsl = slice(g * 512, (g + 1) * 512)
                dma(qiTg[g % 2][:], qiT_d[:, :, gsl].rearrange("c p t -> p c t"), ['qiT_t_d'], ['qiTg%d' % (g % 2)], 'qiTg%d' % (g % 2))

            if dsa:
                load_qi(0)
                for b in range(4):
                    idx_block(0, b)
                for g in range(NG):
                    load_q(g)
                    if g + 1 < NG:
                        load_qi(g + 1)
                    for part in range(4):
                        if g + 1 < NG:
                            idx_block(g + 1, part)
                        att_part(g, [2 * part, 2 * part + 1])
            else:
                stgc = [sbt(st, "stgc%d" % i, [128, 4096]) for i in range(2)]
                cvb = [sbt(st, "cvb%d" % i, [128, 4, D], BF16) for i in range(3)]
                uv_v = uv_d.rearrange("(p r) d -> p r d", p=128)
                conv = [(tsrc, col, rc) for (tsrc, col) in [(pu_d, 0), (pv_d, D)] for rc in range(32)]

                def conv_iter(it):
                    tsrc, col, rc = conv[it]
                    t_v = tsrc.rearrange("(p r) d -> p r d", p=128)
                    sl = it % 2
                    cs_ = it % 3
                    sv = stgc[sl][:].rearrange("p (r d) -> p r d", r=4)
                    dma(sv, t_v[:, rc * 4:(rc + 1) * 4, :], [], ['stgc%d' % sl], 'stgc%d' % sl)
                    cp(cvb[cs_][:], sv, ['stgc%d' % sl], ['cvb%d' % cs_], eng=('pool', 'dve')[it % 2])
                    dma(uv_v[:, rc * 4:(rc + 1) * 4, col:col + D], cvb[cs_][:], ['cvb%d' % cs_], ['uv_d%d' % cs_], 'cvb%d' % cs_)
                for g in range(NG):
                    load_q(g)
                    for it in range(g * 8, g * 8 + 8):
                        conv_iter(it)
                    att_part(g, list(range(8)))
            P.barrier()

    if phases >= 2:
        attention_phase("A", True)
    if phases >= 3:
        attention_phase("M", False)

    if phases >= 4:
        with contextlib.ExitStack() as st:
            woab = sbt(st, "woab", [128, 4, D], BF16); womb = sbt(st, "womb", [128, 4, D], BF16)
            woutb = sbt(st, "woutb", [128, 8, D], BF16); wpqb = sbt(st, "wpqb", [128, 8, 2048], BF16)
            subkT = sbt(st, "subkT", [128, 16, 128], BF16)
            iota16 = sbt(st, "iota16", [128, 16])
            G1 = sbt(st, "G1p", [128, D]); A2 = sbt(st, "A2p", [128, D]); B2 = sbt(st, "B2p", [128, D]); G2 = sbt(st, "G2p", [128, D])
            dma(iota16[:], iota16_d, [], ['iota16'], 'iota16')
            for qi_, (tn_, nm_) in enumerate([(G1, 'G1'), (A2, 'A2'), (B2, 'B2'), (G2, 'G2')]):
                dma(tn_[:], mod_d[qi_], ['mod_d'], [nm_], 'ld' + nm_)
            pkb = [pst(st, "pk%d" % i, [128, 512]) for i in range(2)]
            pk = pkb[0]
            pT4 = pst(st, "pT4", [128, 8, 128], BF16)
            pacc = pst(st, "pacc", [128, 1024])
            with contextlib.ExitStack() as st2:
                stg = [sbt(st2, "stg%d" % i, [128, 4096]) for i in range(2)]
                skb = sbt(st2, "skb", [128, 16, 128], BF16)
                ci = 0
                for (wsrc, wdst, nk, ncol, nm) in [(woa_d, woab, 4, 1024, 'woab'), (wom_d, womb, 4, 1024, 'womb'),
                                                    (wout_d, woutb, 8, 1024, 'woutb'), (wpq_d, wpqb, 8, 2048, 'wpqb')]:
                    wv_ = wsrc.rearrange("(k p) n -> p k n", p=128)
                    cw = 4096 // nk
                    for c0 in range(0, ncol, cw):
                        sl = ci % 2
                        sv = stg[sl][:, 0:nk * cw].rearrange("p (k n) -> p k n", k=nk)
                        dma(sv, wv_[:, :, c0:c0 + cw], [], ['stg%d' % sl], 'stg%d' % sl)
                        cp(wdst[:, :, c0:c0 + cw], sv, ['stg%d' % sl], [nm], eng=('pool' if ci % 2 == 0 else 'act'))
                        ci += 1
                sl = ci % 2
                sv = stg[sl][:, 0:2048].rearrange("p (c d) -> p c d", c=16)
                dma(sv, subk_d.rearrange("c n d -> n c d"), [], ['stg%d' % sl], 'stg%d' % sl)
                cp(skb[:], sv, ['stg%d' % sl], ['skb'])
                for c in range(16):
                    tr(pT4[:, c % 8, :], skb[:, c, :], identb[:], ['skb', 'identb'], ['pT4'])
                    if c % 8 == 7:
                        cp(subkT[:, c - 7:c + 1, :], pT4[:], ['pT4'], ['subkT'])
                P.barrier()

            def sb4(name, shape, dt=F32):
                return sbt(st, name, shape, dt)
            aT_t = sb4("aT_t", [128, 4, 128], BF16); mT_t = sb4("mT_t", [128, 4, 128], BF16)
            gS4 = sb4("gS4", [128, 16, 128], BF16)
            t1 = sb4("t1", [128, 128]); t2 = sb4("t2", [128, 128])
            mixT = sb4("mixT", [128, 8, 128], BF16)
            xt2 = sb4("xt2", [128, D])
            tmpx = xt2
            x1 = [sb4("x1_%d" % i, [128, D]) for i in range(2)]
            ssq = sb4("ssq4", [128, 1]); rstd = sb4("rstd4", [128, 1])
            h2b = [sb4("h2b%d" % i, [128, D], BF16) for i in range(2)]; h2T = sb4("h2T", [128, 8, 128], BF16)
            prod = [sb4("prod%d" % i, [128, D], BF16) for i in range(2)]
            qpT = sb4("qpT", [128, 16, 128], BF16)
            s_all = sb4("s_all", [128, 16, 128]); s_wk = sb4("s_wk", [128, 128])
            tops = sb4("tops", [128, 16, 16]); topi = sb4("topi", [128, 16, 16], U32); topf = sb4("topf", [128, 16, 16])
            cand = s_all[:].rearrange("p (h two) n -> p h (two n)", two=2)
            cwk = sb4("cwk", [128, 256])
            best = sb4("best", [128, 8, 16]); bpos = sb4("bpos", [128, 8, 16], U32)
            ak = sb4("ak", [128, 8, 16], U32); bk = sb4("bk", [128, 8, 16], U32)
            akf = sb4("akf", [128, 8, 16]); bkf = sb4("bkf", [128, 8, 16])
            oh = s_all[:].rearrange("p c (a b) -> p (c a) b", b=16).rearrange("p (h k) a -> p h k a", h=8)
            i0 = sb4("i0", [128, 8, 16]); i1 = sb4("i1", [128, 8, 16])
            idf = sb4("idf", [128, 128])
            ids = [sb4("ids%d" % i, [128, 128], U32) for i in range(2)]
            gate = [sb4("gate%d" % i, [128, 8, 16]) for i in range(2)]
            gz = sb4("gz", [128, 8]); ngmax = sb4("ngmax", [128, 8])
            actv = [sb4("actv%d" % i, [128, 128]) for i in range(2)]
            coef = [sb4("coef%d" % i, [128, 128]) for i in range(2)]
            gtmp = [sb4("gtmp%d" % i, [128, 128]) for i in range(2)]
            NRING = 16
            uvb = [sb4("uvb%d" % i, [128, 2 * D], BF16) for i in range(NRING)]
            dgb = [sb4("dgb%d" % i, [128, 128], BF16) for i in range(4)]
            ob = xt2
            GELU_S = float(2.0 * np.sqrt(2.0 / np.pi))
            shp4 = [128, 8, 16, 16]

            def prologue(i):
                p = i % 2
                tok = slice(i * 128, (i + 1) * 128)
                X1 = 'x1_%d' % p
                PH = 'ph2_%d' % p
                dma(aT_t[:], atA_d[:, :, tok].rearrange("(c two) d t -> (two d) c t", two=2), ['at_dA'], ['aT_t'], 'aT_t')
                dma(mT_t[:], atM_d[:, :, tok].rearrange("(c two) d t -> (two d) c t", two=2), ['at_dM'], ['mT_t'], 'mT_t')
                dma(gS4[:], gS_d[:, :, tok].rearrange("c p t -> p c t"), ['gS_d'], ['gS4'], 'gS4')
                dma(xt2[:], x_d[tok, :], [], ['xt2'], 'xt2')
                yield

                def mix_mm(fc):
                    fsl = slice(fc * 128, (fc + 1) * 128)
                    pkx = pkb[fc % 2]
                    pn = 'pk%d' % (fc % 2)
                    for k in range(4):
                        mm(pkx[:, 0:128], woab[:, k, fsl], aT_t[:, k, :], k == 0, k == 3, ['woab', 'aT_t'], [pn])
                    for k in range(4):
                        mm(pkx[:, 128:256], womb[:, k, fsl], mT_t[:, k, :], k == 0, k == 3, ['womb', 'mT_t'], [pn])

                def mix_dve(fc):
                    pkx = pkb[fc % 2]
                    pn = 'pk%d' % (fc % 2)
                    tt(t1[:], pkx[:, 0:128], gS4[:, fc, :], ALU.mult, [pn, 'gS4'], ['t1'])
                    tt(t2[:], pkx[:, 128:256], gS4[:, 8 + fc, :], ALU.mult, [pn, 'gS4'], ['t2'])
                    tt(mixT[:, fc, :], t1[:], t2[:], ALU.add, ['t1', 't2'], ['mixT'])
                mix_mm(0); mix_mm(1)
                yield
                for s_ in range(1, 4):
                    mix_dve(2 * s_ - 2); mix_dve(2 * s_ - 1)
                    mix_mm(2 * s_); mix_mm(2 * s_ + 1)
                    yield
                mix_dve(6); mix_dve(7)
                PKB = ['pk0', 'pk1']
                for k in range(8):
                    mm(pkb[0][:], mixT[:, k, :], woutb[:, k, 0:512], k == 0, k == 7, ['mixT', 'woutb'], ['pk0'])
                for k in range(8):
                    mm(pkb[1][:], mixT[:, k, :], woutb[:, k, 512:1024], k == 0, k == 7, ['mixT', 'woutb'], ['pk1'])
                yield
                tt(x1[p][:, 0:512], pkb[0][:], G1[:, 0:512], ALU.mult, ['pk0', 'G1'], [X1])
                tt(x1[p][:, 512:1024], pkb[1][:], G1[:, 512:1024], ALU.mult, ['pk1', 'G1'], [X1])
                tt(x1[p][:], x1[p][:], xt2[:], ALU.add, [X1, 'xt2'], [X1])
                yield
                act(prod[0][:], x1[p][:], AF.Square, [X1], ['prod0', 'ssq4'], accum_out=ssq[:])
                act(ssq[:], ssq[:], AF.Sqrt, ['ssq4'], ['ssq4'], scale=float(1.0 / D), bias=float(EPS))
                yield
                rcp(rstd[:], ssq[:], ['ssq4'], ['rstd4'])
                stt(tmpx[:], x1[p][:], rstd[:, 0:1], A2[:], ALU.mult, ALU.mult, [X1, 'rstd4', 'A2'], ['xt2'])
                tt(h2b[p][:], tmpx[:], B2[:], ALU.add, ['xt2', 'B2'], [PH])
                yield
                for k in range(8):
                    tr(pT4[:, k, :], h2b[p][:, k * 128:(k + 1) * 128], identb[:], [PH, 'identb'], ['pT4'])
                yield
                cp(h2T[:], pT4[:], ['pT4'], ['h2T'], eng='act')

                def q_mm(c4):
                    for cc in range(4):
                        c = c4 * 4 + cc
                        for k in range(8):
                            mm(pkb[c4 % 2][:, cc * 128:(cc + 1) * 128], wpqb[:, k, c * 128:(c + 1) * 128], h2T[:, k, :],
                               k == 0, k == 7, ['wpqb', 'h2T'], ['pk%d' % (c4 % 2)])

                def q_ev(c4):
                    cp(qpT[:, c4 * 4:(c4 + 1) * 4, :].rearrange("p c t -> p (c t)"), pkb[c4 % 2][:], ['pk%d' % (c4 % 2)], ['qpT'], eng='act')

                def s_mm(c4):
                    for cc in range(4):
                        c = c4 * 4 + cc
                        mm(pkb[c4 % 2][:, cc * 128:(cc + 1) * 128], qpT[:, c, :], subkT[:, c, :], True, True, ['qpT', 'subkT'],
                           ['pk%d' % (c4 % 2)])

                def s_ev(c4):
                    cp(s_all[:, c4 * 4:(c4 + 1) * 4, :].rearrange("p c t -> p (c t)"), pkb[c4 % 2][:], ['pk%d' % (c4 % 2)], ['s_all'], eng='act')

                def topk1(c):
                    sv_ = s_all[:, c, :]
                    P.op('dve', lambda e, c=c, sv_=sv_: e.max(out=tops[:, c, 0:8], in_=sv_), ['s_all'], ['tops'])
                    P.op('dve', lambda e, c=c, sv_=sv_: e.max_index(out=topi[:, c, 0:8], in_max=tops[:, c, 0:8], in_values=sv_),
                         ['s_all', 'tops'], ['topi'])
                    P.op('dve', lambda e, c=c, sv_=sv_: e.match_replace(out=s_wk[:], in_to_replace=tops[:, c, 0:8], in_values=sv_,
                                                                    imm_value=-1e30), ['s_all', 'tops'], ['s_wk'])
                    P.op('dve', lambda e, c=c: e.max(out=tops[:, c, 8:16], in_=s_wk[:]), ['s_wk'], ['tops'])
                    P.op('dve', lambda e, c=c: e.max_index(out=topi[:, c, 8:16], in_max=tops[:, c, 8:16], in_values=s_wk[:]),
                         ['s_wk', 'tops'], ['topi'])
                q_mm(0)
                yield
                for c4 in range(1, 4):
                    q_ev(c4 - 1); q_mm(c4)
                    yield
                q_ev(3); s_mm(0)
                yield
                s_ev(0); s_mm(1)
                yield
                s_ev(1); s_mm(2)
                for c in (0, 1, 2):
                    topk1(c)
                yield
                s_ev(2); s_mm(3)
                for c in (3, 4, 5):
                    topk1(c)
                yield
                s_ev(3)
                for c in (6, 7, 8):
                    topk1(c)
                yield
                for c in (9, 10, 11):
                    topk1(c)
                yield
                for c in (12, 13):
                    topk1(c)
                yield
                for c in (14, 15):
                    topk1(c)
                cp(topf[:], topi[:], ['topi'], ['topf'])
                t4 = tops[:].rearrange("p (h two) k -> p h two k", two=2)
                c4v = cand.rearrange("p h (a b) -> p h a b", a=16)
                tt(c4v, t4[:, :, 0, :].unsqueeze(3).to_broadcast(shp4), t4[:, :, 1, :].unsqueeze(2).to_broadcast(shp4), ALU.add,
                   ['tops'], ['s_all'])
                yield
                for h in range(8):
                    cv = cand[:, h, :]
                    P.op('dve', lambda e, h=h, cv=cv: e.max(out=best[:, h, 0:8], in_=cv), ['s_all'], ['best'])
                    P.op('dve', lambda e, h=h, cv=cv: e.max_index(out=bpos[:, h, 0:8], in_max=best[:, h, 0:8], in_values=cv),
                         ['s_all', 'best'], ['bpos'])
                    P.op('dve', lambda e, h=h, cv=cv: e.match_replace(out=cwk[:], in_to_replace=best[:, h, 0:8], in_values=cv,
                                                                    imm_value=-1e30), ['s_all', 'best'], ['cwk'])
                    P.op('dve', lambda e, h=h: e.max(out=best[:, h, 8:16], in_=cwk[:]), ['cwk'], ['best'])
                    P.op('dve', lambda e, h=h: e.max_index(out=bpos[:, h, 8:16], in_max=best[:, h, 8:16], in_values=cwk[:]),
                         ['cwk', 'best'], ['bpos'])
                    if h in (2, 5):
                        yield
                gt_ = gate[p]; GN = 'gate%d' % p
                ts(ngmax[:], best[:, :, 0], -1.0, None, ALU.mult, None, ['best'], ['ngmax'])
                tt(gt_[:], best[:], bc2(ngmax[:], [128, 8, 16]), ALU.add, ['best', 'ngmax'], [GN])
                act(gt_[:], gt_[:], AF.Exp, [GN], [GN])
                yield
                ts(ak[:], bpos[:], 4, None, ALU.logical_shift_right, None, ['bpos'], ['ak'])
                ts(bk[:], bpos[:], 15, None, ALU.bitwise_and, None, ['bpos'], ['bk'])
                cp(akf[:], ak[:], ['ak'], ['akf'])
                cp(bkf[:], bk[:], ['bk'], ['bkf'])
                tf4 = topf[:].rearrange("p (h two) k -> p h two k", two=2)
                io4 = iota16[:].unsqueeze(1).unsqueeze(1).to_broadcast(shp4)
                for (kf_, half, dsti, nm) in [(akf, 0, i0, 'i0'), (bkf, 1, i1, 'i1')]:
                    tt(oh, kf_[:].unsqueeze(3).to_broadcast(shp4), io4, ALU.is_equal, ['akf', 'bkf', 'iota16'], ['s_all'])
                    tt(oh, oh, tf4[:, :, half, :].unsqueeze(2).to_broadcast(shp4), ALU.mult, ['s_all', 'topf'], ['s_all'])
                    red(dsti[:], oh, ALU.add, ['s_all'], [nm])
                stt(idf[:].rearrange("p (h k) -> p h k", h=8), i0[:], 128.0, i1[:], ALU.mult, ALU.add, ['i0', 'i1'], ['idf'])
                cp(ids[p][:], idf[:], ['idf'], ['ids%d' % p])
                red(gz[:], gt_[:], ALU.add, [GN], ['gz'])
                rcp(gz[:], gz[:], ['gz'], ['gz'])
                tt(gt_[:], gt_[:], bc2(gz[:], [128, 8, 16]), ALU.mult, [GN, 'gz'], [GN])

            NTOT = NT * 32

            def pg_gather(G):
                i, gi = divmod(G, 32)
                p = i % 2
                for q_ in range(4):
                    sl_ = gi * 4 + q_
                    rb = (G * 4 + q_) % NRING
                    un = 'uvb%d' % rb
                    P.dma('pool', lambda e, sl_=sl_, rb=rb, p=p: e.indirect_dma_start(
                        out=uvb[rb][:], out_offset=None, in_=uv_d,
                        in_offset=bass.IndirectOffsetOnAxis(ap=ids[p][:, sl_:sl_ + 1], axis=0)), ['ids%d' % p], [un], un)

            def pg_dot(G):
                i, gi = divmod(G, 32)
                p = i % 2
                for q_ in range(4):
                    sl_ = gi * 4 + q_
                    rb = (G * 4 + q_) % NRING
                    pr = (G * 4 + q_) % 2
                    tt(prod[pr][:], uvb[rb][:, 0:D], h2b[p][:], ALU.mult, ['uvb%d' % rb, 'ph2_%d' % p], ['prod%d' % pr])
                    act(prod[pr][:], prod[pr][:], AF.Identity, ['prod%d' % pr], ['prod%d' % pr, 'actv%d_%d' % (p, gi)], accum_out=actv[p][:, sl_:sl_ + 1])

            def pg_coefA(G):
                i, gi = divmod(G, 32)
                p = i % 2
                s4 = slice(gi * 4, gi * 4 + 4)
                a_ = actv[p][:, s4]; t_ = gtmp[p][:, s4]
                AN = 'actv%d_%d' % (p, gi); TN = 'gtmp%d_%d' % (p, gi)
                stt(t_, a_, 0.044715, a_, ALU.mult, ALU.mult, [AN], [TN])
                stt(t_, t_, 1.0, a_, ALU.add, ALU.mult, [TN, AN], [TN])
                act(t_, t_, AF.Sigmoid, [TN], [TN], scale=GELU_S)

            def pg_coefB(G):
                i, gi = divmod(G, 32)
                p = i % 2
                s4 = slice(gi * 4, gi * 4 + 4)
                a_ = actv[p][:, s4]; t_ = gtmp[p][:, s4]
                AN = 'actv%d_%d' % (p, gi); TN = 'gtmp%d_%d' % (p, gi)
                gate_f = gate[p][:].rearrange("p h k -> p (h k)")
                tt(t_, t_, a_, ALU.mult, [TN, AN], [TN])
                tt(coef[p][:, s4], t_, gate_f[:, s4], ALU.mult, [TN, 'gate%d' % p], ['coef%d_%d' % (p, gi)])

            def pg_acc(G):
                i, gi = divmod(G, 32)
                p = i % 2
                for q_ in range(4):
                    sl_ = gi * 4 + q_
                    rb = (G * 4 + q_) % NRING
                    db = sl_ % 4
                    ts(dgb[db][:], identb[:], coef[p][:, sl_:sl_ + 1], None, ALU.mult, None, ['identb', 'coef%d_%d' % (p, gi)], ['dgb%d' % db])
                    for hf in range(2):
                        mm(pacc[:, hf * 512:(hf + 1) * 512], dgb[db][:], uvb[rb][:, D + hf * 512:D + (hf + 1) * 512],
                           sl_ == 0, sl_ == 127, ['dgb%d' % db, 'uvb%d' % rb], ['pacc'])

            def epilogue(i):
                p = i % 2
                tt(tmpx[:], pacc[:], G2[:], ALU.mult, ['pacc', 'G2'], ['xt2'])
                tt(ob[:], tmpx[:], x1[p][:], ALU.add, ['xt2', 'x1_%d' % p], ['xt2'])
                dma(out_d[i * 128:(i + 1) * 128, :], ob[:], ['xt2'], ['out_d'], 'ob')

            for _ in prologue(0):
                pass
            NLEAD = NRING // 4
            for G0 in range(NLEAD):
                pg_gather(G0)
            pg_dot(0)
            gen = None
            for G in range(NTOT + 1):
                i, gi = divmod(G, 32)
                if gi == 0 and G < NTOT:
                    gen = prologue(i + 1) if i + 1 < NT else None
                if G + 1 < NTOT:
                    pg_dot(G + 1)
                if G < NTOT:
                    pg_coefA(G)
                if G >= 1:
                    pg_coefB(G - 1)
                    pg_acc(G - 1)
                    if (G - 1) % 32 == 31:
                        epilogue((G - 1) // 32)
                if gen is not None:
                    if gi < 26:
                        next(gen, None)
                    elif gi == 26:
                        for _ in gen:
                            pass
                        gen = None
                if G >= 1 and G - 1 + NRING // 4 < NTOT:
                    pg_gather(G - 1 + NRING // 4)
            P.barrier()

    P.emit(nc)
    top.close()
    return nc


def make_inputs(inputs):
    f = lambda a: np.ascontiguousarray(np.asarray(a))
    common = {}
    for k in ["g_norm1", "g_norm2", "b_ada", "g_a_q", "g_a_k", "g_idx_k", "g_mq_a", "g_mkv_a", "g_m_q", "g_m_k"]:
        common[k] = f(np.asarray(inputs[k], np.float32).reshape(1, -1))
    for k in ["w_ada", "w_in", "w_mq_up", "w_mkv_up", "w_o_a", "w_o_m", "w_out", "w_peer_q", "peer_u", "peer_v"]:
        common[k] = f(np.asarray(inputs[k], np.float32)[0])
    common["peer_subkeys"] = f(np.asarray(inputs["peer_subkeys"], np.float32)[0].reshape(16, 128, 128))
    common["identb"] = np.eye(128, dtype=np.float32).astype(ml_dtypes.bfloat16)
    q = np.arange(128)[:, None]; s = np.arange(128)[None, :]
    common["trib"] = np.where(s <= q, 0.0, NEG).astype(np.float32).astype(ml_dtypes.bfloat16)
    inv64 = (10000.0 ** (-(np.arange(32, dtype=np.float32)) / np.float32(32))).astype(np.float32)
    inv32 = (10000.0 ** (-(np.arange(16, dtype=np.float32)) / np.float32(16))).astype(np.float32)
    common["inv64"] = f(np.broadcast_to(inv64[None, :], (128, 32)))
    common["inv32"] = f(np.broadcast_to(inv32[None, :], (128, 16)))
    common["iota16"] = f(np.broadcast_to(np.arange(16, dtype=np.float32)[None, :], (128, 16)))
    x = np.asarray(inputs["x"], np.float32)
    c = np.asarray(inputs["c"], np.float32)
    pos = np.asarray(inputs["positions"], np.int32)
    maps = []
    for b in range(x.shape[0]):
        m = dict(common)
        m["x"] = f(x[b])
        m["c_col"] = f(c[b].reshape(8, 128).T)
        m["pos_t"] = f(pos[b].reshape(NT, 128).T)
        maps.append(m)
    return maps


def kernel(**inputs):
    maps = make_inputs(inputs)
    nc = build_nc()
    res = run_bass_kernel_spmd(nc, maps, core_ids=list(range(8)))
    return np.stack([np.asarray(r["out"], np.float32) for r in res.results], axis=0)
```

```python
import contextlib
import numpy as np
import ml_dtypes
import concourse.bass as bass
import concourse.mybir as mybir
from concourse.bass_utils import run_bass_kernel_spmd

F32 = mybir.dt.float32
BF16 = mybir.dt.bfloat16
I32 = mybir.dt.int32
U32 = mybir.dt.uint32
AF = mybir.ActivationFunctionType
ALU = mybir.AluOpType
AX = mybir.AxisListType

S = 4096
D = 1024
NT = 32
NG = 8
EPS = 1e-6
NEG = -30000.0
NITER = 18
NSLOT = 4
TWO_PI = float(2 * np.pi)
NO_SELF_WAIT = False


class Prog:
    ENG = ('pe', 'act', 'dve', 'pool', 'sp')

    def __init__(self):
        self.ops = {e: [] for e in self.ENG}
        self.cnt = {e: 0 for e in self.ENG}
        self.known = {e: {} for e in self.ENG}
        self.res = {}
        self.dma_cnt = {}

    def _deps(self, eng, reads, writes):
        need = {}

        def add(sk, v):
            if sk == ('eng', 'pe') and eng == 'pe':
                return
            if NO_SELF_WAIT and sk == ('eng', eng):
                return
            if v > need.get(sk, 0):
                need[sk] = v
        for k in reads:
            st = self.res.get(k)
            if st and st[0] is not None:
                add(*st[0])
        for k in writes:
            st = self.res.get(k)
            if st:
                if st[0] is not None:
                    add(*st[0])
                for sk, v in st[1].items():
                    add(sk, v)
        waits = []
        kn = self.known[eng]
        for sk, v in need.items():
            if kn.get(sk, 0) >= v:
                continue
            kn[sk] = v
            waits.append((sk, v))
        return waits

    def _commit(self, tok, reads, writes):
        sk, v = tok
        for k in reads:
            st = self.res.setdefault(k, [None, {}])
            if v > st[1].get(sk, 0):
                st[1][sk] = v
        for k in writes:
            self.res[k] = [tok, {}]

    def op(self, eng, fn, r=(), w=()):
        waits = self._deps(eng, r, w)
        self.cnt[eng] += 1
        tok = (('eng', eng), self.cnt[eng])
        self.ops[eng].append((waits, fn, ('eng', eng), 1))
        self._commit(tok, r, w)

    def dma(self, eng, fn, r=(), w=(), sem=None):
        waits = self._deps(eng, r, w)
        c = self.dma_cnt.get(sem, 0) + 16
        self.dma_cnt[sem] = c
        tok = (('dma', sem), c)
        self.ops[eng].append((waits, fn, ('dma', sem), 16))
        self._commit(tok, r, w)

    def barrier(self):
        allk = [(('eng', e), self.cnt[e]) for e in self.ENG if self.cnt[e] > 0]
        allk += [(('dma', k), c) for k, c in self.dma_cnt.items()]
        for e in self.ENG:
            waits = []
            kn = self.known[e]
            for sk, v in allk:
                if sk == ('eng', e):
                    continue
                if kn.get(sk, 0) >= v:
                    continue
                kn[sk] = v
                waits.append((sk, v))
            if waits:
                self.ops[e].append((waits, None, None, 0))

    def emit(self, nc):
        self.barrier()
        with contextlib.ExitStack() as st:
            sems = {}
            for e in self.ENG:
                sems[('eng', e)] = st.enter_context(nc.semaphore("s_" + e))
            for i, k in enumerate(self.dma_cnt):
                sems[('dma', k)] = st.enter_context(nc.semaphore("d%d" % i))
            block = st.enter_context(nc.Block())

            def run(e, name):
                for (waits, fn, sk, inc) in self.ops[name]:
                    for (wk, v) in waits:
                        e.wait_ge(sems[wk], v)
                    if fn is not None:
                        fn(e).then_inc(sems[sk], inc)

            @block.tensor
            def _(e):
                run(e, 'pe')

            @block.scalar
            def _(e):
                run(e, 'act')

            @block.vector
            def _(e):
                run(e, 'dve')

            @block.gpsimd
            def _(e):
                run(e, 'pool')

            @block.sync
            def _(e):
                run(e, 'sp')


def build_nc(debug=False, phases=4, nt1=NT, skip=(), nslot=NSLOT, gcols=D):
    nc = bass.Bass("TRN2", target_bir_lowering=False)
    P = Prog()

    def din(name, shape, dt=F32):
        return nc.dram_tensor(name, list(shape), dt, kind="ExternalInput").ap()

    def dscr(name, shape, dt=BF16):
        return nc.dram_tensor(name, list(shape), dt, kind="ExternalOutput" if debug else "Internal").ap()

    x_d = din("x", [S, D])
    ccol_d = din("c_col", [128, 8])
    pos_d = din("pos_t", [128, NT], I32)
    g1_d = din("g_norm1", [1, D]); g2_d = din("g_norm2", [1, D])
    wada_d = din("w_ada", [D, 6 * D]); bada_d = din("b_ada", [1, 6 * D])
    win_d = din("w_in", [D, 4840])
    gaq_d = din("g_a_q", [1, 64]); gak_d = din("g_a_k", [1, 64]); gik_d = din("g_idx_k", [1, 64])
    gmqa_d = din("g_mq_a", [1, 384]); wmq_d = din("w_mq_up", [384, 768])
    gmkva_d = din("g_mkv_a", [1, 256]); wmkv_d = din("w_mkv_up", [256, 1024])
    gmq_d = din("g_m_q", [1, 96]); gmk_d = din("g_m_k", [1, 96])
    woa_d = din("w_o_a", [512, D]); wom_d = din("w_o_m", [512, D]); wout_d = din("w_out", [D, D])
    wpq_d = din("w_peer_q", [D, 2048]); subk_d = din("peer_subkeys", [16, 128, 128])
    pu_d = din("peer_u", [16384, D]); pv_d = din("peer_v", [16384, D])
    identb_d = din("identb", [128, 128], BF16)
    trib_d = din("trib", [128, 128], BF16)
    inv64_d = din("inv64", [128, 32]); inv32_d = din("inv32", [128, 16])
    iota16_d = din("iota16", [128, 16])
    out_d = nc.dram_tensor("out", [S, D], F32, kind="ExternalOutput").ap()

    qaT_d = dscr("qaT_s", [4, 128, S]); kaT_d = dscr("kaT_s", [4, 128, S]); qiT_d = dscr("qiT_s", [4, 128, S])
    kiT_d = dscr("kiT_s", [128, S])
    va_d = dscr("va_s", [S, 520]); vm_d = dscr("vm_s", [S, 520])
    qmT_d = dscr("qmT_s", [8, 96, S]); kmT_d = dscr("kmT_s", [8, 96, S])
    gS_d = dscr("gS_s", [16, 128, S])
    atA_d = dscr("atA_s", [8, 64, S]); atM_d = dscr("atM_s", [8, 64, S])
    mod_d = dscr("mod_s", [4, 128, D], F32)
    uv_d = nc.dram_tensor("uv_s", [16384, 2048], BF16, kind="Internal").ap()

    top = contextlib.ExitStack()

    uid = [0]

    def sbt(st, name, shape, dt=F32):
        uid[0] += 1
        return st.enter_context(nc.sbuf_tensor("sb%d_%s" % (uid[0], name), list(shape), dt))

    def pst(st, name, shape, dt=F32):
        uid[0] += 1
        return st.enter_context(nc.psum_tensor("ps%d_%s" % (uid[0], name), list(shape), dt))

    def tt(out, in0, in1, op, r, w, eng='dve'):
        P.op(eng, lambda e: e.tensor_tensor(out=out, in0=in0, in1=in1, op=op), r, w)

    def ts(out, in0, s1, s2, op0, op1, r, w, eng='dve', accum_out=None):
        if op1 is None:
            P.op(eng, lambda e: e.tensor_scalar(out=out, in0=in0, scalar1=s1, scalar2=None, op0=op0), r, w)
        elif accum_out is None:
            P.op(eng, lambda e: e.tensor_scalar(out=out, in0=in0, scalar1=s1, scalar2=s2, op0=op0, op1=op1), r, w)
        else:
            P.op(eng, lambda e: e.tensor_scalar(out=out, in0=in0, scalar1=s1, scalar2=s2, op0=op0, op1=op1, accum_out=accum_out), r, w)

    def stt(out, in0, scalar, in1, op0, op1, r, w, accum_out=None):
        if accum_out is None:
            P.op('dve', lambda e: e.scalar_tensor_tensor(out=out, in0=in0, scalar=scalar, in1=in1, op0=op0, op1=op1), r, w)
        else:
            P.op('dve', lambda e: e.scalar_tensor_tensor(out=out, in0=in0, scalar=scalar, in1=in1, op0=op0, op1=op1, accum_out=accum_out), r, w)

    def act(out, in_, func, r, w, scale=None, bias=None, accum_out=None):
        kw = {}
        if scale is not None:
            kw['scale'] = scale
        if bias is not None:
            kw['bias'] = bias
        if accum_out is not None:
            kw['accum_out'] = accum_out
        P.op('act', lambda e: e.activation(out=out, in_=in_, func=func, **kw), r, w)

    def cp(out, in_, r, w, eng='dve'):
        if eng == 'act':
            act(out, in_, AF.Copy, r, w)
        else:
            P.op(eng, lambda e: e.tensor_copy(out=out, in_=in_), r, w)

    def red(out, in_, op, r, w):
        P.op('dve', lambda e: e.tensor_reduce(out=out, in_=in_, axis=AX.X, op=op), r, w)

    def rcp(out, in_, r, w):
        P.op('dve', lambda e: e.reciprocal(out=out, in_=in_), r, w)

    def mm(out, lhsT, rhs, start, stop, r, w):
        P.op('pe', lambda e: e.matmul(out=out, lhsT=lhsT, rhs=rhs, start=start, stop=stop), r, w)

    def tr(out, in_, ident, r, w):
        P.op('pe', lambda e: e.transpose(out=out, in_=in_, identity=ident), r, w)

    def dma(out, in_, r, w, sem, eng='sp'):
        P.dma(eng, lambda e: e.dma_start(out=out, in_=in_), r, w, sem)

    def memset(ap, val, w, eng='dve'):
        P.op(eng, lambda e: e.memset(ap, val), (), w)

    def bc1(ap, shape):
        return ap.unsqueeze(1).to_broadcast(shape)

    def bc2(ap, shape):
        return ap.unsqueeze(2).to_broadcast(shape)

    identb = sbt(top, "identb", [128, 128], BF16)
    trib = sbt(top, "trib", [128, 128], BF16)
    wi_all = sbt(top, "wi_all", [128, NT, 8])
    st01 = contextlib.ExitStack()
    A1 = sbt(st01, "A1", [128, D]); B1 = sbt(st01, "B1", [128, D])
    dma(identb[:], identb_d, [], ['identb'], 'identb')
    dma(trib[:], trib_d, [], ['trib'], 'trib')

    with contextlib.ExitStack() as st:
        ccol = sbt(st, "ccol", [128, 8]); cact = sbt(st, "cact", [128, 8])
        cb = sbt(st, "cb", [128, 8, 128])
        g1b = sbt(st, "g1b", [128, D]); g2b = sbt(st, "g2b", [128, D])
        G1 = sbt(st, "G1", [128, D]); A2 = sbt(st, "A2", [128, D]); B2 = sbt(st, "B2", [128, D]); G2 = sbt(st, "G2", [128, D])
        wa = [sbt(st, "wa%d" % i, [128, 8, 512]) for i in range(2)]
        bb = [sbt(st, "bb%d" % i, [128, 512]) for i in range(2)]
        tmp0 = sbt(st, "tmp0", [128, 512])
        pm0 = [pst(st, "pm0_%d" % i, [128, 512]) for i in range(2)]
        dma(ccol[:], ccol_d, [], ['ccol'], 'ccol')
        dma(g1b[:], g1_d.to_broadcast([128, D]), [], ['g1b'], 'g1b')
        dma(g2b[:], g2_d.to_broadcast([128, D]), [], ['g2b'], 'g2b')
        act(cact[:], ccol[:], AF.Silu, ['ccol'], ['cact'])
        cp(cb[:], cact[:].unsqueeze(2).to_broadcast([128, 8, 128]), ['cact'], ['cb'])
        wada_v = wada_d.rearrange("(k p) n -> p k n", p=128)
        dests = [B1, A1, G1, B2, A2, G2]
        dnames = ['B1', 'A1', 'G1', 'B2', 'A2', 'G2']
        for n in range(12):
            sl = n % 2
            dma(wa[sl][:], wada_v[:, :, n * 512:(n + 1) * 512], [], ['wa%d' % sl], 'wa%d' % sl)
            dma(bb[sl][:], bada_d[:, n * 512:(n + 1) * 512].to_broadcast([128, 512]), [], ['bb%d' % sl], 'bb%d' % sl)
            for k in range(8):
                mm(pm0[sl][:], cb[:, k, :], wa[sl][:, k, :], k == 0, k == 7, ['cb', 'wa%d' % sl], ['pm0_%d' % sl])
            which = n // 2
            dst = dests[which][:, (n % 2) * 512:(n % 2 + 1) * 512]
            dn = dnames[which]
            if which in (1, 4):
                gsrc = (g1b if which == 1 else g2b)[:, (n % 2) * 512:(n % 2 + 1) * 512]
                tt(tmp0[:], pm0[sl][:], bb[sl][:], ALU.add, ['pm0_%d' % sl, 'bb%d' % sl], ['tmp0'])
                stt(dst, tmp0[:], 1.0, gsrc, ALU.add, ALU.mult, ['tmp0', 'g1b', 'g2b'], [dn])
            else:
                tt(dst, pm0[sl][:], bb[sl][:], ALU.add, ['pm0_%d' % sl, 'bb%d' % sl], [dn])
        for qi_, (tn_, nm_) in enumerate([(G1, 'G1'), (A2, 'A2'), (B2, 'B2'), (G2, 'G2')]):
            dma(mod_d[qi_], tn_[:], [nm_], ['mod_d'], 'mod' + nm_)
        P.barrier()

    if phases == 0:
        P.emit(nc)
        st01.close()
        top.close()
        return nc

    with contextlib.ExitStack() as st:
        winb = sbt(st, "winb", [128, 8, 4840], BF16)
        wmqb = sbt(st, "wmqb", [128, 3, 768], BF16)
        wmkvb = sbt(st, "wmkvb", [128, 2, 1024], BF16)
        gaq = sbt(st, "gaq", [128, 64]); gak = sbt(st, "gak", [128, 64]); gik = sbt(st, "gik", [128, 64])
        gmqa = sbt(st, "gmqa", [128, 384]); gmkva = sbt(st, "gmkva", [128, 256])
        gmq = sbt(st, "gmq", [128, 96]); gmk = sbt(st, "gmk", [128, 96])
        cos64 = sbt(st, "cos64", [128, NT, 32]); sin64 = sbt(st, "sin64", [128, NT, 32])
        cos32 = sbt(st, "cos32", [128, NT, 16]); sin32 = sbt(st, "sin32", [128, NT, 16])
        for tns, src, nm in [(gaq, gaq_d, 'gaq'), (gak, gak_d, 'gak'), (gik, gik_d, 'gik'), (gmqa, gmqa_d, 'gmqa'),
                             (gmkva, gmkva_d, 'gmkva'), (gmq, gmq_d, 'gmq'), (gmk, gmk_d, 'gmk')]:
            dma(tns[:], src.to_broadcast(list(tns[:].shape)), [], [nm], nm)
        with contextlib.ExitStack() as st2:
            stage = [sbt(st2, "stage%d" % i, [128, 4096]) for i in range(2)]
            win_v = win_d.rearrange("(k p) n -> p k n", p=128)
            ci = 0
            for c0 in range(0, 4840, 512):
                c1 = min(c0 + 512, 4840)
                wdt = c1 - c0
                sl = ci % 2
                sv = stage[sl][:, 0:8 * wdt].rearrange("p (k n) -> p k n", k=8)
                dma(sv, win_v[:, :, c0:c1], [], ['stage%d' % sl], 'stage%d' % sl)
                cp(winb[:, :, c0:c1], sv, ['stage%d' % sl], ['winb'], eng=('pool' if ci % 2 == 0 else 'act'))
                ci += 1
            sv = stage[ci % 2][:, 0:3 * 768].rearrange("p (k n) -> p k n", k=3)
            dma(sv, wmq_d.rearrange("(k p) n -> p k n", p=128), [], ['stage%d' % (ci % 2)], 'stage%d' % (ci % 2))
            cp(wmqb[:], sv, ['stage%d' % (ci % 2)], ['wmqb'], eng='pool')
            ci += 1
            sv = stage[ci % 2][:, 0:2 * 1024].rearrange("p (k n) -> p k n", k=2)
            dma(sv, wmkv_d.rearrange("(k p) n -> p k n", p=128), [], ['stage%d' % (ci % 2)], 'stage%d' % (ci % 2))
            cp(wmkvb[:], sv, ['stage%d' % (ci % 2)], ['wmkvb'], eng='pool')
            posi = sbt(st2, "posi", [128, NT], I32); posf = sbt(st2, "posf", [128, NT])
            inv64 = sbt(st2, "inv64", [128, 32]); inv32 = sbt(st2, "inv32", [128, 16])
            rt_a = sbt(st2, "rt_a", [128, NT, 32]); rt_k = sbt(st2, "rt_k", [128, NT, 32])
            rt_i = sbt(st2, "rt_i", [128, NT, 32], I32); rt_y = sbt(st2, "rt_y", [128, NT, 32])
            dma(posi[:], pos_d, [], ['posi'], 'posi')
            dma(inv64[:], inv64_d, [], ['inv64'], 'inv64')
            dma(inv32[:], inv32_d, [], ['inv32'], 'inv32')
            cp(posf[:], posi[:], ['posi'], ['posf'])
            for (inv, hf, cs, sn, nm) in [(inv64, 32, cos64, sin64, '64'), (inv32, 16, cos32, sin32, '32')]:
                a = rt_a[:, :, 0:hf]; kk = rt_k[:, :, 0:hf]; ii = rt_i[:, :, 0:hf]; y = rt_y[:, :, 0:hf]
                shp = [128, NT, hf]
                tt(a, bc2(posf[:], shp), bc1(inv[:], shp), ALU.mult, ['posf', 'inv' + nm], ['rt_a'])
                ts(kk, a, float(1.0 / TWO_PI), None, ALU.mult, None, ['rt_a'], ['rt_k'])
                cp(ii, kk, ['rt_k'], ['rt_i'])
                cp(kk, ii, ['rt_i'], ['rt_k'])
                stt(a, kk, -TWO_PI, a, ALU.mult, ALU.add, ['rt_k', 'rt_a'], ['rt_a'])
                ts(kk, a, float(np.pi / 2), float(np.pi), ALU.add, ALU.is_gt, ['rt_a'], ['rt_k'])
                stt(y, kk, -TWO_PI, a, ALU.mult, ALU.add, ['rt_k', 'rt_a'], ['rt_y'])
                ts(y, y, float(np.pi / 2), None, ALU.add, None, ['rt_y'], ['rt_y'])
                ts(y, y, float(np.pi), float(-np.pi), ALU.min, ALU.max, ['rt_y'], ['rt_y'])
                ts(a, a, float(np.pi), float(-np.pi), ALU.min, ALU.max, ['rt_a'], ['rt_a'])
                act(sn[:], a, AF.Sin, ['rt_a'], ['sin' + nm])
                act(cs[:], y, AF.Sin, ['rt_y'], ['cos' + nm])
            P.barrier()

        xt = [sbt(st, "xt%d" % i, [128, D]) for i in range(2)]
        junkb = sbt(st, "junkb", [128, D], BF16)
        ssq = sbt(st, "ssq", [128, 1]); rstd = sbt(st, "rstd", [128, 1])
        htmp = sbt(st, "htmp", [128, D]); hb = sbt(st, "hb", [128, D], BF16)
        hT = [sbt(st, "hT%d" % i, [128, 8, 128], BF16) for i in range(2)]
        proj = sbt(st, "proj", [128, 2792])
        sq = sbt(st, "sq", [128, 1024]); nrm = sbt(st, "nrm", [128, 1024])
        s8 = sbt(st, "s8", [128, 8]); r8 = sbt(st, "r8", [128, 8])
        rp = [sbt(st, "rp%d" % i, [128, 8, 32]) for i in range(4)]
        tokb = sbt(st, "tokb", [128, 768], BF16)
        kib = sbt(st, "kib", [128, 128], BF16)
        cqT = sbt(st, "cqT", [128, 3, 128], BF16); ckvT = sbt(st, "ckvT", [128, 2, 128], BF16)
        qmf = sbt(st, "qmf", [128, 768]); kvf = sbt(st, "kvf", [128, 8, 128]); kmpre = sbt(st, "kmpre", [128, 8, 96])
        vaug = sbt(st, "vaug", [128, 8, 65], BF16); vmaug = sbt(st, "vmaug", [128, 8, 65], BF16)
        qaT_t = sbt(st, "qaT_t", [128, 4, 128], BF16); kaT_t = sbt(st, "kaT_t", [128, 4, 128], BF16)
        qiT_t = sbt(st, "qiT_t", [128, 4, 128], BF16); kiT_t = sbt(st, "kiT_t", [128, 128], BF16)
        qmT_t = sbt(st, "qmT_t", [128, 8, 128], BF16); kmT_t = sbt(st, "kmT_t", [128, 8, 128], BF16)
        gS_t = sbt(st, "gS_t", [128, 16, 128], BF16)
        pT = pst(st, "pT", [128, 8, 128], BF16)
        pT2 = pst(st, "pT2", [128, 8, 128], BF16)
        pproj = [pst(st, "pproj%d" % i, [128, 512]) for i in range(2)]
        pgate = pst(st, "pgate", [128, 512])
        pm = pst(st, "pm", [128, 1024])
        memset(vaug[:], 1.0, ['vaug'], eng='pool')
        memset(vmaug[:], 1.0, ['vmaug'], eng='pool')

        def rms_heads(src3, H, Dh, gain, dst3, rs, ws, gname):
            shp = [128, H, Dh]
            sqv = sq[:, 0:H * Dh].rearrange("p (h d) -> p h d", h=H)
            tt(sqv, src3, src3, ALU.mult, rs, ['sq'])
            red(s8[:, 0:H], sqv, ALU.add, ['sq'], ['s8'])
            ts(s8[:, 0:H], s8[:, 0:H], float(1.0 / Dh), float(EPS), ALU.mult, ALU.add, ['s8'], ['s8'])
            act(s8[:, 0:H], s8[:, 0:H], AF.Sqrt, ['s8'], ['s8'])
            rcp(r8[:, 0:H], s8[:, 0:H], ['s8'], ['r8'])
            nv = nrm[:, 0:H * Dh].rearrange("p (h d) -> p h d", h=H)
            tt(nv, src3, bc2(r8[:, 0:H], shp), ALU.mult, rs + ['r8'], ['nrm'])
            tt(dst3, nv, bc1(gain[:], shp), ALU.mult, ['nrm', gname], ws)

        def rope(src3, H, hf, cosv, sinv, dst3, rs, ws, cname):
            shp = [128, H, hf]
            x1 = src3[:, :, 0:hf]; x2 = src3[:, :, hf:2 * hf]
            cb_ = bc1(cosv, shp); sb_ = bc1(sinv, shp)
            t = [rp[i][:, 0:H, 0:hf] for i in range(4)]
            tt(t[0], x1, cb_, ALU.mult, rs + ['cos' + cname], ['rp0'])
            tt(t[1], x2, sb_, ALU.mult, rs + ['sin' + cname], ['rp1'], eng='pool')
            tt(dst3[:, :, 0:hf], t[0], t[1], ALU.subtract, ['rp0', 'rp1'], ws)
            tt(t[2], x2, cb_, ALU.mult, rs + ['cos' + cname], ['rp2'])
            tt(t[3], x1, sb_, ALU.mult, rs + ['sin' + cname], ['rp3'], eng='pool')
            tt(dst3[:, :, hf:2 * hf], t[2], t[3], ALU.add, ['rp2', 'rp3'], ws)

        for i in range(nt1):
            hs = i % 2
            hTn = 'hT%d' % hs
            xs = i % 2
            xn = 'xt%d' % xs
            tok = slice(i * 128, (i + 1) * 128)
            dma(xt[xs][:], x_d[tok, :], [], [xn], xn)
            act(junkb[:], xt[xs][:], AF.Square, [xn], ['junkb', 'ssq'], accum_out=ssq[:])
            ts(ssq[:], ssq[:], float(1.0 / D), float(EPS), ALU.mult, ALU.add, ['ssq'], ['ssq'])
            act(ssq[:], ssq[:], AF.Sqrt, ['ssq'], ['ssq'])
            rcp(rstd[:], ssq[:], ['ssq'], ['rstd'])
            stt(htmp[:], xt[xs][:], rstd[:, 0:1], A1[:], ALU.mult, ALU.mult, [xn, 'rstd', 'A1'], ['htmp'])
            tt(hb[:], htmp[:], B1[:], ALU.add, ['htmp', 'B1'], ['hb'], eng='pool')
            for k in range(8):
                tr(pT[:, k, :], hb[:, k * 128:(k + 1) * 128], identb[:], ['hb', 'identb'], ['pT'])
            cp(hT[hs][:], pT[:], ['pT'], [hTn], eng='act')
            for cc in range(6):
                c0 = cc * 512
                c1 = min(c0 + 512, 2792)
                pp = cc % 2
                for k in range(8):
                    mm(pproj[pp][:, 0:c1 - c0], hT[hs][:, k, :], winb[:, k, c0:c1], k == 0, k == 7,
                       [hTn, 'winb'], ['pproj%d' % pp])
                cp(proj[:, c0:c1], pproj[pp][:, 0:c1 - c0], ['pproj%d' % pp], ['proj'], eng='act')
            for fc in range(16):
                c0 = 2792 + fc * 128
                for k in range(8):
                    mm(pgate[:, (fc % 4) * 128:(fc % 4 + 1) * 128], winb[:, k, c0:c0 + 128], hT[hs][:, k, :], k == 0, k == 7,
                       [hTn, 'winb'], ['pgate'])
                if fc % 4 == 3:
                    act(gS_t[:, fc - 3:fc + 1, :].rearrange("p c t -> p (c t)"), pgate[:], AF.Sigmoid, ['pgate'], ['gS_t'])
            dma(gS_d[:, :, tok].rearrange("c p t -> p c t"), gS_t[:], ['gS_t'], ['gS_d'], 'gS_t')
            cs64 = cos64[:, i, :]; sn64 = sin64[:, i, :]; cs32 = cos32[:, i, :]; sn32 = sin32[:, i, :]
            for (c0, gn, gt_, dstT, dn, dd_) in [(0, 'gaq', gaq, qaT_t, 'qaT_t', qaT_d), (512, 'gak', gak, kaT_t, 'kaT_t', kaT_d)]:
                src3 = proj[:, c0:c0 + 512].rearrange("p (h d) -> p h d", h=8)
                n3 = sq[:, 0:512].rearrange("p (h d) -> p h d", h=8)
                rms_heads(src3, 8, 64, gt_, n3, ['proj'], ['sq'], gn)
                rope(n3, 8, 32, cs64, sn64, tokb[:, 0:512].rearrange("p (h d) -> p h d", h=8), ['sq'], ['tokb'], '64')
                for k in range(4):
                    tr(pT2[:, k, :], tokb[:, k * 128:(k + 1) * 128], identb[:], ['tokb', 'identb'], ['pT2'])
                cp(dstT[:], pT2[:, 0:4, :], ['pT2'], [dn])
                dma(dd_[:, :, tok].rearrange("c p t -> p c t"), dstT[:], [dn], [dn + '_d'], dn)
            rope(proj[:, 1536:2048].rearrange("p (h d) -> p h d", h=8), 8, 32, cs64, sn64,
                 tokb[:, 0:512].rearrange("p (h d) -> p h d", h=8), ['proj'], ['tokb'], '64')
            for k in range(4):
                tr(pT2[:, k, :], tokb[:, k * 128:(k + 1) * 128], identb[:], ['tokb', 'identb'], ['pT2'])
            cp(qiT_t[:], pT2[:, 0:4, :], ['pT2'], ['qiT_t'])
            dma(qiT_d[:, :, tok].rearrange("c p t -> p c t"), qiT_t[:], ['qiT_t'], ['qiT_t_d'], 'qiT_t')
            n3 = sq[:, 0:64].rearrange("p (h d) -> p h d", h=1)
            rms_heads(proj[:, 2048:2112].rearrange("p (h d) -> p h d", h=1), 1, 64, gik, n3, ['proj'], ['sq'], 'gik')
            rope(n3, 1, 32, cs64, sn64, kib[:, 0:64].rearrange("p (h d) -> p h d", h=1), ['sq'], ['kib'], '64')
            cp(kib[:, 64:128], kib[:, 0:64], ['kib'], ['kib'])
            tr(pT2[:, 0, :], kib[:], identb[:], ['kib', 'identb'], ['pT2'])
            cp(kiT_t[:], pT2[:, 0, :], ['pT2'], ['kiT_t'])
            dma(kiT_d[:, tok], kiT_t[:], ['kiT_t'], ['kiT_t_d'], 'kiT_t')
            ts(wi_all[:, i, :], proj[:, 2112:2120], float(512 ** -0.5), None, ALU.mult, None, ['proj'], ['wi_all'])
            cp(vaug[:, :, 0:64], proj[:, 1024:1536].rearrange("p (h d) -> p h d", h=8), ['proj'], ['vaug'], eng='pool')
            dma(va_d[tok, :], vaug[:].rearrange("p h d -> p (h d)"), ['vaug'], ['va_d'], 'vaug')
            rms_heads(proj[:, 2120:2504].rearrange("p (h d) -> p h d", h=1), 1, 384, gmqa,
                      tokb[:, 0:384].rearrange("p (h d) -> p h d", h=1), ['proj'], ['tokb'], 'gmqa')
            for k in range(3):
                tr(pT2[:, k, :], tokb[:, k * 128:(k + 1) * 128], identb[:], ['tokb', 'identb'], ['pT2'])
            cp(cqT[:], pT2[:, 0:3, :], ['pT2'], ['cqT'])
            for (c0, c1) in [(0, 512), (512, 768)]:
                for k in range(3):
                    mm(pm[:, c0:c1], cqT[:, k, :], wmqb[:, k, c0:c1], k == 0, k == 2, ['cqT', 'wmqb'], ['pm'])
            cp(qmf[:], pm[:, 0:768], ['pm'], ['qmf'], eng='act')
            q3 = qmf[:].rearrange("p (h d) -> p h d", h=8)
            n96 = sq[:, 0:768].rearrange("p (h d) -> p h d", h=8)
            rms_heads(q3, 8, 96, gmq, n96, ['qmf'], ['sq'], 'gmq')
            tb96 = tokb[:, 0:768].rearrange("p (h d) -> p h d", h=8)
            cp(tb96[:, :, 0:64], n96[:, :, 0:64], ['sq'], ['tokb'], eng='pool')
            rope(n96[:, :, 64:96], 8, 16, cs32, sn32, tb96[:, :, 64:96], ['sq'], ['tokb'], '32')
            for h in range(8):
                tr(pT2[0:96, h, :], tokb[:, h * 96:(h + 1) * 96], identb[:], ['tokb', 'identb'], ['pT2'])
            cp(qmT_t[0:96, :, :], pT2[0:96, :, :], ['pT2'], ['qmT_t'])
            dma(qmT_d[:, :, tok].rearrange("h d t -> d h t"), qmT_t[0:96, :, :], ['qmT_t'], ['qmT_t_d'], 'qmT_t')
            rms_heads(proj[:, 2504:2760].rearrange("p (h d) -> p h d", h=1), 1, 256, gmkva,
                      tokb[:, 0:256].rearrange("p (h d) -> p h d", h=1), ['proj'], ['tokb'], 'gmkva')
            for k in range(2):
                tr(pT2[:, k, :], tokb[:, k * 128:(k + 1) * 128], identb[:], ['tokb', 'identb'], ['pT2'])
            cp(ckvT[:], pT2[:, 0:2, :], ['pT2'], ['ckvT'])
            for (c0, c1) in [(0, 512), (512, 1024)]:
                for k in range(2):
                    mm(pm[:, c0:c1], ckvT[:, k, :], wmkvb[:, k, c0:c1], k == 0, k == 1, ['ckvT', 'wmkvb'], ['pm'])
            cp(kvf[:].rearrange("p h d -> p (h d)"), pm[:], ['pm'], ['kvf'], eng='act')
            cp(kmpre[:, :, 0:64], kvf[:, :, 0:64], ['kvf'], ['kmpre'], eng='pool')
            cp(kmpre[:, :, 64:96], bc1(proj[:, 2760:2792], [128, 8, 32]), ['proj'], ['kmpre'], eng='pool')
            cp(vmaug[:, :, 0:64], kvf[:, :, 64:128], ['kvf'], ['vmaug'], eng='pool')
            dma(vm_d[tok, :], vmaug[:].rearrange("p h d -> p (h d)"), ['vmaug'], ['vm_d'], 'vmaug')
            rms_heads(kmpre[:], 8, 96, gmk, n96, ['kmpre'], ['sq'], 'gmk')
            cp(tb96[:, :, 0:64], n96[:, :, 0:64], ['sq'], ['tokb'], eng='pool')
            rope(n96[:, :, 64:96], 8, 16, cs32, sn32, tb96[:, :, 64:96], ['sq'], ['tokb'], '32')
            for h in range(8):
                tr(pT2[0:96, h, :], tokb[:, h * 96:(h + 1) * 96], identb[:], ['tokb', 'identb'], ['pT2'])
            cp(kmT_t[0:96, :, :], pT2[0:96, :, :], ['pT2'], ['kmT_t'])
            dma(kmT_d[:, :, tok].rearrange("h d t -> d h t"), kmT_t[0:96, :, :], ['kmT_t'], ['kmT_t_d'], 'kmT_t')
        P.barrier()
    st01.close()

    def attention_phase(tag, dsa):
        with contextlib.ExitStack() as st:
            ones64 = sbt(st, "ones64" + tag, [128, 64])
            memset(ones64[:], 1.0, ['ones64'])
            if dsa:
                kT = sbt(st, "kaT", [128, 4, S], BF16)
                kiT = sbt(st, "kiT", [128, S], BF16)
                for c in range(4):
                    dma(kT[:, c, :], kaT_d[c], ['kaT_t_d'], ['kT%d' % c], 'kT%d' % c)
                dma(kiT[:], kiT_d, ['kiT_t_d'], ['kiT'], 'kiT')
                v_src = va_d
                qT_g = sbt(st, "qaTg", [128, 4, 512], BF16)
                qiTg = [sbt(st, "qiTg%d" % i, [128, 4, 512], BF16) for i in range(2)]
                score = sbt(st, "score", [128, S])
                junk = sbt(st, "junkc", [128, S], BF16)
                bias_g = [sbt(st, "bias_g%d" % i, [128, 4, S], BF16) for i in range(2)]
                dg = sbt(st, "dg", [128, 8, 128], BF16)
                rbuf = [sbt(st, "rbuf%d" % i, [128, 512], BF16) for i in range(2)]
                lo = sbt(st, "lo", [128, 1]); hi = sbt(st, "hi", [128, 1]); mid = sbt(st, "mid", [128, 1])
                wv = sbt(st, "wv", [128, NITER + 1]); cnt = sbt(st, "cnt", [128, 1]); stp = sbt(st, "stp", [128, 1])
                pw2 = sbt(st, "pw2", [128, NITER + 1])
                for k in range(NITER + 1):
                    memset(pw2[:, k:k + 1], float(2.0 ** -(k + 1)), ['pw2'])
                pd = [pst(st, "pd%d" % i, [128, 512]) for i in range(2)]
                psc = pst(st, "psc", [128, 512])
                scale = 64 ** -0.5
                Kd = 64
                out_d = atA_d
            else:
                kT = sbt(st, "kmT", [128, 8, S], BF16)
                for h in range(8):
                    dma(kT[0:96, h, :], kmT_d[h], ['kmT_t_d'], ['kT%d' % h], 'kT%d' % h)
                v_src = vm_d
                qT_g = sbt(st, "qmTg", [128, 8, 512], BF16)
                scale = 96 ** -0.5
                Kd = 96
                out_d = atM_d
            vv = sbt(st, "vv" + tag, [128, NT, 520], BF16)
            v_v = v_src.rearrange("(t p) c -> p t c", p=128)
            for q in range(4):
                dma(vv[:, q * 8:(q + 1) * 8, :], v_v[:, q * 8:(q + 1) * 8, :], ['va_d', 'vm_d'], ['vv%d' % q], 'vv%d' % q)
            vres = ['vv%d' % q for q in range(4)]
            ptb = [sbt(st, "ptb%d%s" % (i, tag), [128, 512], BF16) for i in range(2)]
            rz = sbt(st, "rz" + tag, [128, 512]); of = sbt(st, "of" + tag, [128, 512])
            yb = [sbt(st, "yb%d%s" % (i, tag), [128, 512], BF16) for i in range(2)]
            pS = [pst(st, "pS%d%s" % (i, tag), [128, 512]) for i in range(2)]
            pO = [pst(st, "pO%d%s" % (i, tag), [128, 512]) for i in range(2)]
            pB = pst(st, "pB" + tag, [128, 512])
            kres = ['kT%d' % c for c in range(8)]
            ctr = 0
            def idx_block(g, b):
                bg = bias_g[g % 2]
                bgn = 'bias_g%d' % (g % 2)
                jq = 4 * g + b
                Sp = (jq + 1) * 128
                shp = [128, 8, 128]
                tt(dg[:], bc1(identb[:], shp), bc2(wi_all[:, jq, :], shp), ALU.mult, ['identb', 'wi_all'], ['dg'])
                items_i = [(c, h) for c in range(g + 1) for h in range(8)]
                qn = 'qiTg%d' % (g % 2)
                qi_ = qiTg[g % 2]

                def idx_dots(k_):
                    c, h = items_i[k_]
                    wc = 512 if c < g else (b + 1) * 128
                    k0 = c * 512
                    hp = h % 2; hc = h // 2
                    prt = slice(hp * 64, hp * 64 + 64)
                    dd = k_ % 2
                    mm(pd[dd][:, 0:wc], qi_[prt, hc, b * 128:(b + 1) * 128], kiT[prt, k0:k0 + wc], True, True,
                       [qn, 'kiT'], ['pd%d' % dd])
                    act(rbuf[dd][:, 0:wc], pd[dd][:, 0:wc], AF.Relu, ['pd%d' % dd], ['rbuf%d' % dd])

                def idx_acc(k_):
                    c, h = items_i[k_]
                    wc = 512 if c < g else (b + 1) * 128
                    k0 = c * 512
                    dd = k_ % 2
                    mm(psc[:, 0:wc], dg[:, h, :], rbuf[dd][:, 0:wc], h == 0, h == 7, ['dg', 'rbuf%d' % dd], ['psc'])
                    if h == 7:
                        if c < g:
                            cp(score[:, k0:k0 + 512], psc[:], ['psc'], ['score'])
                        else:
                            if b > 0:
                                cp(score[:, k0:k0 + b * 128], psc[:, 0:b * 128], ['psc'], ['score'])
                            tt(score[:, jq * 128:(jq + 1) * 128], psc[:, b * 128:(b + 1) * 128], trib[:], ALU.add,
                               ['psc', 'trib'], ['score'])
                idx_dots(0)
                for k_ in range(len(items_i)):
                    if k_ + 1 < len(items_i):
                        idx_dots(k_ + 1)
                    idx_acc(k_)
                if jq >= 2:
                    red(hi[:], score[:, 0:Sp], ALU.max, ['score'], ['hi'])
                    red(lo[:], score[:, 0:jq * 128], ALU.min, ['score'], ['lo'])
                    tt(stp[:], hi[:], lo[:], ALU.subtract, ['hi', 'lo'], ['stp'])
                    ts(wv[:], pw2[:], stp[:, 0:1], None, ALU.mult, None, ['stp', 'pw2'], ['wv'])
                    tt(mid[:], lo[:], wv[:, 0:1], ALU.add, ['lo', 'wv'], ['mid'])
                    for k in range(NITER):
                        ts(junk[:, 0:Sp], score[:, 0:Sp], mid[:, 0:1], 0.0, ALU.is_ge, ALU.add, ['score', 'mid'],
                           ['junk', 'cnt'], accum_out=cnt[:])
                        ts(stp[:], cnt[:], 255.5, wv[:, k:k + 1], ALU.is_ge, ALU.mult, ['cnt', 'wv'], ['stp'])
                        tt(lo[:], lo[:], stp[:], ALU.add, ['lo', 'stp'], ['lo'])
                        tt(mid[:], lo[:], wv[:, k + 1:k + 2], ALU.add, ['lo', 'wv'], ['mid'])
                else:
                    memset(lo[:], -10000.0, ['lo'])
                ts(bg[:, b, 0:Sp], score[:, 0:Sp], lo[:, 0:1], NEG, ALU.is_lt, ALU.mult, ['score', 'lo'], [bgn])

            def att_part(g, heads):
                gsl = slice(g * 512, (g + 1) * 512)
                nsc = 4 * g + 4
                items_a = [(h, sc) for h in heads for sc in range(nsc)]
                if dsa:
                    bg = bias_g[g % 2]
                    bgn = 'bias_g%d' % (g % 2)

                def att_S(k_):
                    h, sc = items_a[k_]
                    r_ = sc - 4 * g
                    qlo = max(r_, 0) * 128
                    ss = k_ % 2
                    ssl = slice(sc * 128, (sc + 1) * 128)
                    if dsa:
                        hp = h % 2; hc = h // 2
                        prt = slice(hp * 64, hp * 64 + 64)
                        mm(pS[ss][:, qlo:512], kT[prt, hc, ssl], qT_g[prt, hc, qlo:512], True, False,
                           ['kT%d' % hc, 'qT_g'], ['pS%d' % ss])
                        b0 = max(r_, 0)
                        for b in range(b0, 4):
                            mm(pS[ss][:, b * 128:(b + 1) * 128], bg[:, b, ssl], identb[:], False, b == 3,
                               [bgn, 'identb'], ['pS%d' % ss])
                    else:
                        last = r_ < 0
                        mm(pS[ss][:, qlo:512], kT[0:96, h, ssl], qT_g[0:96, h, qlo:512], True, last,
                           ['kT%d' % h, 'qT_g'], ['pS%d' % ss])
                        if r_ >= 0:
                            mm(pS[ss][:, qlo:qlo + 128], trib[:], identb[:], False, True, ['trib', 'identb'], ['pS%d' % ss])
                    act(ptb[ss][:, qlo:512], pS[ss][:, qlo:512], AF.Exp, ['pS%d' % ss], ['ptb%d' % ss], scale=float(scale))

                def att_PV(k_):
                    h, sc = items_a[k_]
                    r_ = sc - 4 * g
                    qlo = max(r_, 0) * 128
                    ss = k_ % 2
                    po = h % 2
                    pon = 'pO%d' % po
                    mm(pO[po][0:65, qlo:512], vv[:, sc, h * 65:(h + 1) * 65], ptb[ss][:, qlo:512], sc == 0, sc == nsc - 1,
                       ['ptb%d' % ss] + vres, [pon])
                    if sc == nsc - 1:
                        rcp(rz[64:65, :], pO[po][64:65, :], [pon], ['rz'])
                        mm(pB[0:64, :], ones64[64:65, 0:64], rz[64:65, :], True, True, ['ones64', 'rz'], ['pB'])
                        cp(of[0:64, :], pO[po][0:64, :], [pon], ['of'], eng='act')
                        ybn = 'yb%d' % po
                        tt(yb[po][0:64, :], of[0:64, :], pB[0:64, :], ALU.mult, ['of', 'pB'], [ybn])
                        dma(out_d[h, :, gsl], yb[po][0:64, :], [ybn], ['at_d' + tag], ybn + tag)
                att_S(0)
                for k_ in range(len(items_a)):
                    if k_ + 1 < len(items_a):
                        att_S(k_ + 1)
                    att_PV(k_)

            def load_q(g):
                gsl = slice(g * 512, (g + 1) * 512)
                if dsa:
                    dma(qT_g[:], qaT_d[:, :, gsl].rearrange("c p t -> p c t"), ['qaT_t_d'], ['qT_g'], 'qT_g')
                else:
                    dma(qT_g[0:96, :, :], qmT_d[:, :, gsl].rearrange("h d t -> d h t"), ['qmT_t_d'], ['qT_g'], 'qT_g')

            def load_qi(g):
                gsl = slice(g * 512, (g + 1) * 512)
                dma(qiTg[g % 2][:], qiT_d[:, :, gsl].rearrange("c p t -> p c t"), ['qiT_t_d'], ['qiTg%d' % (g % 2)], 'qiTg%d' % (g % 2))

            if dsa:
                load_qi(0)
                for b in range(4):
                    idx_block(0, b)
                for g in range(NG):
                    load_q(g)
                    if g + 1 < NG:
                        load_qi(g + 1)
                    for part in range(4):
                        if g + 1 < NG:
                            idx_block(g + 1, part)
                        att_part(g, [2 * part, 2 * part + 1])
            else:
                stgc = [sbt(st, "stgc%d" % i, [128, 4096]) for i in range(2)]
                cvb = [sbt(st, "cvb%d" % i, [128, 4, D], BF16) for i in range(3)]
                uv_v = uv_d.rearrange("(p r) d -> p r d", p=128)
                conv = [(tsrc, col, rc) for (tsrc, col) in [(pu_d, 0), (pv_d, D)] for rc in range(32)]

                def conv_iter(it):
                    tsrc, col, rc = conv[it]
                    t_v = tsrc.rearrange("(p r) d -> p r d", p=128)
                    sl = it % 2
                    cs_ = it % 3
                    sv = stgc[sl][:].rearrange("p (r d) -> p r d", r=4)
                    dma(sv, t_v[:, rc * 4:(rc + 1) * 4, :], [], ['stgc%d' % sl], 'stgc%d' % sl)
                    cp(cvb[cs_][:], sv, ['stgc%d' % sl], ['cvb%d' % cs_], eng=('pool', 'dve')[it % 2])
                    dma(uv_v[:, rc * 4:(rc + 1) * 4, col:col + D], cvb[cs_][:], ['cvb%d' % cs_], ['uv_d%d' % cs_], 'cvb%d' % cs_)
                for g in range(NG):
                    load_q(g)
                    for it in range(g * 8, g * 8 + 8):
                        conv_iter(it)
                    att_part(g, list(range(8)))
            P.barrier()

    if phases >= 2:
        attention_phase("A", True)
    if phases >= 3:
        attention_phase("M", False)

    if phases >= 4:
        with contextlib.ExitStack() as st:
            woab = sbt(st, "woab", [128, 4, D], BF16); womb = sbt(st, "womb", [128, 4, D], BF16)
            woutb = sbt(st, "woutb", [128, 8, D], BF16); wpqb = sbt(st, "wpqb", [128, 8, 2048], BF16)
            subkT = sbt(st, "subkT", [128, 16, 128], BF16)
            iota16 = sbt(st, "iota16", [128, 16])
            G1 = sbt(st, "G1p", [128, D]); A2 = sbt(st, "A2p", [128, D]); B2 = sbt(st, "B2p", [128, D]); G2 = sbt(st, "G2p", [128, D])
            dma(iota16[:], iota16_d, [], ['iota16'], 'iota16')
            for qi_, (tn_, nm_) in enumerate([(G1, 'G1'), (A2, 'A2'), (B2, 'B2'), (G2, 'G2')]):
                dma(tn_[:], mod_d[qi_], ['mod_d'], [nm_], 'ld' + nm_)
            pkb = [pst(st, "pk%d" % i, [128, 512]) for i in range(2)]
            pk = pkb[0]
            pT4 = pst(st, "pT4", [128, 8, 128], BF16)
            paccs = [pst(st, "pacc%d" % i, [128, 1024]) for i in range(2)]
            with contextlib.ExitStack() as st2:
                stg = [sbt(st2, "stg%d" % i, [128, 4096]) for i in range(2)]
                skb = sbt(st2, "skb", [128, 16, 128], BF16)
                ci = 0
                for (wsrc, wdst, nk, ncol, nm) in [(woa_d, woab, 4, 1024, 'woab'), (wom_d, womb, 4, 1024, 'womb'),
                                                    (wout_d, woutb, 8, 1024, 'woutb'), (wpq_d, wpqb, 8, 2048, 'wpqb')]:
                    wv_ = wsrc.rearrange("(k p) n -> p k n", p=128)
                    cw = 4096 // nk
                    for c0 in range(0, ncol, cw):
                        sl = ci % 2
                        sv = stg[sl][:, 0:nk * cw].rearrange("p (k n) -> p k n", k=nk)
                        dma(sv, wv_[:, :, c0:c0 + cw], [], ['stg%d' % sl], 'stg%d' % sl)
                        cp(wdst[:, :, c0:c0 + cw], sv, ['stg%d' % sl], [nm], eng=('pool' if ci % 2 == 0 else 'act'))
                        ci += 1
                sl = ci % 2
                sv = stg[sl][:, 0:2048].rearrange("p (c d) -> p c d", c=16)
                dma(sv, subk_d.rearrange("c n d -> n c d"), [], ['stg%d' % sl], 'stg%d' % sl)
                cp(skb[:], sv, ['stg%d' % sl], ['skb'])
                for c in range(16):
                    tr(pT4[:, c % 8, :], skb[:, c, :], identb[:], ['skb', 'identb'], ['pT4'])
                    if c % 8 == 7:
                        cp(subkT[:, c - 7:c + 1, :], pT4[:], ['pT4'], ['subkT'])
                P.barrier()

            def sb4(name, shape, dt=F32):
                return sbt(st, name, shape, dt)
            aT_t = sb4("aT_t", [128, 4, 128], BF16); mT_t = sb4("mT_t", [128, 4, 128], BF16)
            gS4 = sb4("gS4", [128, 16, 128], BF16)
            t1 = sb4("t1", [128, 128]); t2 = sb4("t2", [128, 128])
            mixT = sb4("mixT", [128, 8, 128], BF16)
            xt2 = sb4("xt2", [128, D])
            tmpx = xt2
            x1 = [sb4("x1_%d" % i, [128, D]) for i in range(2)]
            ssq = sb4("ssq4", [128, 1]); rstd = sb4("rstd4", [128, 1])
            h2b = [sb4("h2b%d" % i, [128, D], BF16) for i in range(2)]; h2T = sb4("h2T", [128, 8, 128], BF16)
            prod = [sb4("prod%d" % i, [128, D], BF16) for i in range(2)]
            qpT = sb4("qpT", [128, 16, 128], BF16)
            s_all = sb4("s_all", [128, 16, 128]); s_wk = sb4("s_wk", [128, 128])
            tops = sb4("tops", [128, 16, 16]); topi = sb4("topi", [128, 16, 16], U32); topf = sb4("topf", [128, 16, 16])
            cand = s_all[:].rearrange("p (h two) n -> p h (two n)", two=2)
            cwk = sb4("cwk", [128, 256])
            best = sb4("best", [128, 8, 16]); bpos = sb4("bpos", [128, 8, 16], U32)
            ak = sb4("ak", [128, 8, 16], U32); bk = sb4("bk", [128, 8, 16], U32)
            akf = sb4("akf", [128, 8, 16]); bkf = sb4("bkf", [128, 8, 16])
            oh = s_all[:].rearrange("p c (a b) -> p (c a) b", b=16).rearrange("p (h k) a -> p h k a", h=8)
            i0 = sb4("i0", [128, 8, 16]); i1 = sb4("i1", [128, 8, 16])
            idf = sb4("idf", [128, 128])
            ids = [sb4("ids%d" % i, [128, 128], U32) for i in range(2)]
            gate = [sb4("gate%d" % i, [128, 8, 16]) for i in range(2)]
            gz = sb4("gz", [128, 8]); ngmax = sb4("ngmax", [128, 8])
            actv = [sb4("actv%d" % i, [128, 128]) for i in range(2)]
            coef = [sb4("coef%d" % i, [128, 128]) for i in range(2)]
            gtmp = [sb4("gtmp%d" % i, [128, 128]) for i in range(2)]
            NRING = 16
            uvb = [sb4("uvb%d" % i, [128, 2 * D], BF16) for i in range(NRING)]
            dgb = [sb4("dgb%d" % i, [128, 128], BF16) for i in range(4)]
            ob = xt2
            GELU_S = float(2.0 * np.sqrt(2.0 / np.pi))
            shp4 = [128, 8, 16, 16]

            def prologue(i):
                p = i % 2
                tok = slice(i * 128, (i + 1) * 128)
                X1 = 'x1_%d' % p
                PH = 'ph2_%d' % p
                dma(aT_t[:], atA_d[:, :, tok].rearrange("(c two) d t -> (two d) c t", two=2), ['at_dA'], ['aT_t'], 'aT_t')
                dma(mT_t[:], atM_d[:, :, tok].rearrange("(c two) d t -> (two d) c t", two=2), ['at_dM'], ['mT_t'], 'mT_t')
                dma(gS4[:], gS_d[:, :, tok].rearrange("c p t -> p c t"), ['gS_d'], ['gS4'], 'gS4')
                dma(xt2[:], x_d[tok, :], [], ['xt2'], 'xt2')
                yield

                def mix_mm(fc):
                    fsl = slice(fc * 128, (fc + 1) * 128)
                    pkx = pkb[fc % 2]
                    pn = 'pk%d' % (fc % 2)
                    for k in range(4):
                        mm(pkx[:, 0:128], woab[:, k, fsl], aT_t[:, k, :], k == 0, k == 3, ['woab', 'aT_t'], [pn])
                    for k in range(4):
                        mm(pkx[:, 128:256], womb[:, k, fsl], mT_t[:, k, :], k == 0, k == 3, ['womb', 'mT_t'], [pn])

                def mix_dve(fc):
                    pkx = pkb[fc % 2]
                    pn = 'pk%d' % (fc % 2)
                    tt(t1[:], pkx[:, 0:128], gS4[:, fc, :], ALU.mult, [pn, 'gS4'], ['t1'])
                    tt(t2[:], pkx[:, 128:256], gS4[:, 8 + fc, :], ALU.mult, [pn, 'gS4'], ['t2'])
                    tt(mixT[:, fc, :], t1[:], t2[:], ALU.add, ['t1', 't2'], ['mixT'])
                mix_mm(0); mix_mm(1)
                yield
                for s_ in range(1, 4):
                    mix_dve(2 * s_ - 2); mix_dve(2 * s_ - 1)
                    mix_mm(2 * s_); mix_mm(2 * s_ + 1)
                    yield
                mix_dve(6); mix_dve(7)
                PKB = ['pk0', 'pk1']
                for k in range(8):
                    mm(pkb[0][:], mixT[:, k, :], woutb[:, k, 0:512], k == 0, k == 7, ['mixT', 'woutb'], ['pk0'])
                for k in range(8):
                    mm(pkb[1][:], mixT[:, k, :], woutb[:, k, 512:1024], k == 0, k == 7, ['mixT', 'woutb'], ['pk1'])
                yield
                tt(x1[p][:, 0:512], pkb[0][:], G1[:, 0:512], ALU.mult, ['pk0', 'G1'], [X1])
                tt(x1[p][:, 512:1024], pkb[1][:], G1[:, 512:1024], ALU.mult, ['pk1', 'G1'], [X1])
                tt(x1[p][:], x1[p][:], xt2[:], ALU.add, [X1, 'xt2'], [X1])
                yield
                act(prod[0][:], x1[p][:], AF.Square, [X1], ['prod0', 'ssq4'], accum_out=ssq[:])
                act(ssq[:], ssq[:], AF.Sqrt, ['ssq4'], ['ssq4'], scale=float(1.0 / D), bias=float(EPS))
                yield
                rcp(rstd[:], ssq[:], ['ssq4'], ['rstd4'])
                stt(tmpx[:], x1[p][:], rstd[:, 0:1], A2[:], ALU.mult, ALU.mult, [X1, 'rstd4', 'A2'], ['xt2'])
                tt(h2b[p][:], tmpx[:], B2[:], ALU.add, ['xt2', 'B2'], [PH])
                yield
                for k in range(8):
                    tr(pT4[:, k, :], h2b[p][:, k * 128:(k + 1) * 128], identb[:], [PH, 'identb'], ['pT4'])
                yield
                cp(h2T[:], pT4[:], ['pT4'], ['h2T'], eng='act')

                def q_mm(c4):
                    for cc in range(4):
                        c = c4 * 4 + cc
                        for k in range(8):
                            mm(pkb[c4 % 2][:, cc * 128:(cc + 1) * 128], wpqb[:, k, c * 128:(c + 1) * 128], h2T[:, k, :],
                               k == 0, k == 7, ['wpqb', 'h2T'], ['pk%d' % (c4 % 2)])

                def q_ev(c4):
                    cp(qpT[:, c4 * 4:(c4 + 1) * 4, :].rearrange("p c t -> p (c t)"), pkb[c4 % 2][:], ['pk%d' % (c4 % 2)], ['qpT'], eng='act')

                def s_mm(c4):
                    for cc in range(4):
                        c = c4 * 4 + cc
                        mm(pkb[c4 % 2][:, cc * 128:(cc + 1) * 128], qpT[:, c, :], subkT[:, c, :], True, True, ['qpT', 'subkT'],
                           ['pk%d' % (c4 % 2)])

                def s_ev(c4):
                    cp(s_all[:, c4 * 4:(c4 + 1) * 4, :].rearrange("p c t -> p (c t)"), pkb[c4 % 2][:], ['pk%d' % (c4 % 2)], ['s_all'], eng='act')

                def topk1(c):
                    sv_ = s_all[:, c, :]
                    P.op('dve', lambda e, c=c, sv_=sv_: e.max(out=tops[:, c, 0:8], in_=sv_), ['s_all'], ['tops'])
                    P.op('dve', lambda e, c=c, sv_=sv_: e.max_index(out=topi[:, c, 0:8], in_max=tops[:, c, 0:8], in_values=sv_),
                         ['s_all', 'tops'], ['topi'])
                    P.op('dve', lambda e, c=c, sv_=sv_: e.match_replace(out=s_wk[:], in_to_replace=tops[:, c, 0:8], in_values=sv_,
                                                                    imm_value=-1e30), ['s_all', 'tops'], ['s_wk'])
                    P.op('dve', lambda e, c=c: e.max(out=tops[:, c, 8:16], in_=s_wk[:]), ['s_wk'], ['tops'])
                    P.op('dve', lambda e, c=c: e.max_index(out=topi[:, c, 8:16], in_max=tops[:, c, 8:16], in_values=s_wk[:]),
                         ['s_wk', 'tops'], ['topi'])
                q_mm(0)
                yield
                for c4 in range(1, 4):
                    q_ev(c4 - 1); q_mm(c4)
                    yield
                q_ev(3); s_mm(0)
                yield
                s_ev(0); s_mm(1)
                yield
                s_ev(1); s_mm(2)
                for c in (0, 1, 2):
                    topk1(c)
                yield
                s_ev(2); s_mm(3)
                for c in (3, 4, 5):
                    topk1(c)
                yield
                s_ev(3)
                for c in (6, 7, 8):
                    topk1(c)
                yield
                for c in (9, 10, 11):
                    topk1(c)
                yield
                for c in (12, 13):
                    topk1(c)
                yield
                for c in (14, 15):
                    topk1(c)
                cp(topf[:], topi[:], ['topi'], ['topf'])
                t4 = tops[:].rearrange("p (h two) k -> p h two k", two=2)
                c4v = cand.rearrange("p h (a b) -> p h a b", a=16)
                tt(c4v, t4[:, :, 0, :].unsqueeze(3).to_broadcast(shp4), t4[:, :, 1, :].unsqueeze(2).to_broadcast(shp4), ALU.add,
                   ['tops'], ['s_all'])
                yield
                for h in range(8):
                    cv = cand[:, h, :]
                    P.op('dve', lambda e, h=h, cv=cv: e.max(out=best[:, h, 0:8], in_=cv), ['s_all'], ['best'])
                    P.op('dve', lambda e, h=h, cv=cv: e.max_index(out=bpos[:, h, 0:8], in_max=best[:, h, 0:8], in_values=cv),
                         ['s_all', 'best'], ['bpos'])
                    P.op('dve', lambda e, h=h, cv=cv: e.match_replace(out=cwk[:], in_to_replace=best[:, h, 0:8], in_values=cv,
                                                                    imm_value=-1e30), ['s_all', 'best'], ['cwk'])
                    P.op('dve', lambda e, h=h: e.max(out=best[:, h, 8:16], in_=cwk[:]), ['cwk'], ['best'])
                    P.op('dve', lambda e, h=h: e.max_index(out=bpos[:, h, 8:16], in_max=best[:, h, 8:16], in_values=cwk[:]),
                         ['cwk', 'best'], ['bpos'])
                    if h in (2, 5):
                        yield
                gt_ = gate[p]; GN = 'gate%d' % p
                ts(ngmax[:], best[:, :, 0], -1.0, None, ALU.mult, None, ['best'], ['ngmax'])
                tt(gt_[:], best[:], bc2(ngmax[:], [128, 8, 16]), ALU.add, ['best', 'ngmax'], [GN])
                act(gt_[:], gt_[:], AF.Exp, [GN], [GN])
                yield
                ts(ak[:], bpos[:], 4, None, ALU.logical_shift_right, None, ['bpos'], ['ak'])
                ts(bk[:], bpos[:], 15, None, ALU.bitwise_and, None, ['bpos'], ['bk'])
                cp(akf[:], ak[:], ['ak'], ['akf'])
                cp(bkf[:], bk[:], ['bk'], ['bkf'])
                tf4 = topf[:].rearrange("p (h two) k -> p h two k", two=2)
                io4 = iota16[:].unsqueeze(1).unsqueeze(1).to_broadcast(shp4)
                for (kf_, half, dsti, nm) in [(akf, 0, i0, 'i0'), (bkf, 1, i1, 'i1')]:
                    tt(oh, kf_[:].unsqueeze(3).to_broadcast(shp4), io4, ALU.is_equal, ['akf', 'bkf', 'iota16'], ['s_all'])
                    tt(oh, oh, tf4[:, :, half, :].unsqueeze(2).to_broadcast(shp4), ALU.mult, ['s_all', 'topf'], ['s_all'])
                    red(dsti[:], oh, ALU.add, ['s_all'], [nm])
                stt(idf[:].rearrange("p (h k) -> p h k", h=8), i0[:], 128.0, i1[:], ALU.mult, ALU.add, ['i0', 'i1'], ['idf'])
                cp(ids[p][:], idf[:], ['idf'], ['ids%d' % p])
                red(gz[:], gt_[:], ALU.add, [GN], ['gz'])
                rcp(gz[:], gz[:], ['gz'], ['gz'])
                tt(gt_[:], gt_[:], bc2(gz[:], [128, 8, 16]), ALU.mult, [GN, 'gz'], [GN])

            NTOT = NT * 32

            def pg_gather(G):
                i, gi = divmod(G, 32)
                p = i % 2
                for q_ in range(4):
                    sl_ = gi * 4 + q_
                    rb = (G * 4 + q_) % NRING
                    un = 'uvb%d' % rb
                    P.dma('pool', lambda e, sl_=sl_, rb=rb, p=p: e.indirect_dma_start(
                        out=uvb[rb][:], out_offset=None, in_=uv_d,
                        in_offset=bass.IndirectOffsetOnAxis(ap=ids[p][:, sl_:sl_ + 1], axis=0)), ['ids%d' % p], [un], un)

            def pg_dot(G):
                i, gi = divmod(G, 32)
                p = i % 2
                for q_ in range(4):
                    sl_ = gi * 4 + q_
                    rb = (G * 4 + q_) % NRING
                    pr = (G * 4 + q_) % 2
                    tt(prod[pr][:], uvb[rb][:, 0:D], h2b[p][:], ALU.mult, ['uvb%d' % rb, 'ph2_%d' % p], ['prod%d' % pr])
                    act(prod[pr][:], prod[pr][:], AF.Identity, ['prod%d' % pr], ['prod%d' % pr, 'actv%d_%d' % (p, gi)], accum_out=actv[p][:, sl_:sl_ + 1])

            def pg_coefA(G):
                i, gi = divmod(G, 32)
                p = i % 2
                s4 = slice(gi * 4, gi * 4 + 4)
                a_ = actv[p][:, s4]; t_ = gtmp[p][:, s4]
                AN = 'actv%d_%d' % (p, gi); TN = 'gtmp%d_%d' % (p, gi)
                stt(t_, a_, 0.044715, a_, ALU.mult, ALU.mult, [AN], [TN])
                stt(t_, t_, 1.0, a_, ALU.add, ALU.mult, [TN, AN], [TN])
                act(t_, t_, AF.Sigmoid, [TN], [TN], scale=GELU_S)

            def pg_coefB(G):
                i, gi = divmod(G, 32)
                p = i % 2
                s4 = slice(gi * 4, gi * 4 + 4)
                a_ = actv[p][:, s4]; t_ = gtmp[p][:, s4]
                AN = 'actv%d_%d' % (p, gi); TN = 'gtmp%d_%d' % (p, gi)
                gate_f = gate[p][:].rearrange("p h k -> p (h k)")
                tt(t_, t_, a_, ALU.mult, [TN, AN], [TN])
                tt(coef[p][:, s4], t_, gate_f[:, s4], ALU.mult, [TN, 'gate%d' % p], ['coef%d_%d' % (p, gi)])

            def pg_acc(G):
                i, gi = divmod(G, 32)
                p = i % 2
                for q_ in range(4):
                    sl_ = gi * 4 + q_
                    rb = (G * 4 + q_) % NRING
                    db = sl_ % 4
                    ts(dgb[db][:], identb[:], coef[p][:, sl_:sl_ + 1], None, ALU.mult, None, ['identb', 'coef%d_%d' % (p, gi)], ['dgb%d' % db])
                    for hf in range(2):
                        mm(paccs[p][:, hf * 512:(hf + 1) * 512], dgb[db][:], uvb[rb][:, D + hf * 512:D + (hf + 1) * 512],
                           sl_ == 0, sl_ == 127, ['dgb%d' % db, 'uvb%d' % rb], ['pacc%d' % p])

            def epilogue(i):
                p = i % 2
                PA = 'pacc%d' % p; X1 = 'x1_%d' % p
                tt(paccs[p][:], paccs[p][:], G2[:], ALU.mult, [PA, 'G2'], [PA])
                tt(x1[p][:], paccs[p][:], x1[p][:], ALU.add, [PA, X1], [X1])
                dma(out_d[i * 128:(i + 1) * 128, :], x1[p][:], [X1], ['out_d'], 'ob%d' % p)

            for _ in prologue(0):
                pass
            NLEAD = NRING // 4
            for G0 in range(NLEAD):
                pg_gather(G0)
            pg_dot(0)
            gen = None
            for G in range(NTOT + 1):
                i, gi = divmod(G, 32)
                if gi == 0 and G < NTOT:
                    gen = prologue(i + 1) if i + 1 < NT else None
                if G + 1 < NTOT:
                    pg_dot(G + 1)
                if G < NTOT:
                    pg_coefA(G)
                if G >= 1:
                    pg_coefB(G - 1)
                    pg_acc(G - 1)
                if G >= 32 and gi == 3:
                    epilogue(i - 1)
                if G == NTOT:
                    epilogue(NT - 1)
                if gen is not None:
                    if gi < 26:
                        next(gen, None)
                    elif gi == 26:
                        for _ in gen:
                            pass
                        gen = None
                if G >= 1 and G - 1 + NRING // 4 < NTOT:
                    pg_gather(G - 1 + NRING // 4)
            P.barrier()

    P.emit(nc)
    top.close()
    return nc


def make_inputs(inputs):
    f = lambda a: np.ascontiguousarray(np.asarray(a))
    common = {}
    for k in ["g_norm1", "g_norm2", "b_ada", "g_a_q", "g_a_k", "g_idx_k", "g_mq_a", "g_mkv_a", "g_m_q", "g_m_k"]:
        common[k] = f(np.asarray(inputs[k], np.float32).reshape(1, -1))
    for k in ["w_ada", "w_in", "w_mq_up", "w_mkv_up", "w_o_a", "w_o_m", "w_out", "w_peer_q", "peer_u", "peer_v"]:
        common[k] = f(np.asarray(inputs[k], np.float32)[0])
    common["peer_subkeys"] = f(np.asarray(inputs["peer_subkeys"], np.float32)[0].reshape(16, 128, 128))
    common["identb"] = np.eye(128, dtype=np.float32).astype(ml_dtypes.bfloat16)
    q = np.arange(128)[:, None]; s = np.arange(128)[None, :]
    common["trib"] = np.where(s <= q, 0.0, NEG).astype(np.float32).astype(ml_dtypes.bfloat16)
    inv64 = (10000.0 ** (-(np.arange(32, dtype=np.float32)) / np.float32(32))).astype(np.float32)
    inv32 = (10000.0 ** (-(np.arange(16, dtype=np.float32)) / np.float32(16))).astype(np.float32)
    common["inv64"] = f(np.broadcast_to(inv64[None, :], (128, 32)))
    common["inv32"] = f(np.broadcast_to(inv32[None, :], (128, 16)))
    common["iota16"] = f(np.broadcast_to(np.arange(16, dtype=np.float32)[None, :], (128, 16)))
    x = np.asarray(inputs["x"], np.float32)
    c = np.asarray(inputs["c"], np.float32)
    pos = np.asarray(inputs["positions"], np.int32)
    maps = []
    for b in range(x.shape[0]):
        m = dict(common)
        m["x"] = f(x[b])
        m["c_col"] = f(c[b].reshape(8, 128).T)
        m["pos_t"] = f(pos[b].reshape(NT, 128).T)
        maps.append(m)
    return maps


def kernel(**inputs):
    maps = make_inputs(inputs)
    nc = build_nc()
    res = run_bass_kernel_spmd(nc, maps, core_ids=list(range(8)))
    return np.stack([np.asarray(r["out"], np.float32) for r in res.results], axis=0)
```
